# Optimizing a Trainium2 kernel written in Bass

```python
import math
import jax
import jax.numpy as jnp
from jax import lax
import numpy as np

D_MODEL = 1024
BATCH = 8
SEQ = 2048
DEPTH = 2

GRID_W = 64
CTX_LEN = 256
HEAD_DIM = 64
ROPE_BASE = 10000.0
ROPE_QUARTER = HEAD_DIM // 4
NORM_EPS = 1e-6
NEG_INF = -1e30
F32 = jnp.float32

S5_WIDTH = D_MODEL // 2
S5_GROUP = 16
S5_GROUPS = S5_WIDTH // S5_GROUP
S5_STATE = 64
WA_Q_HEADS = (D_MODEL // 2) // HEAD_DIM
WA_KV_HEADS = 2
WA_GROUP = WA_Q_HEADS // WA_KV_HEADS
WINDOW = 128
WIN_BLOCK = 128
WA_Q_WIDTH = WA_Q_HEADS * HEAD_DIM
WA_KV_WIDTH = WA_KV_HEADS * HEAD_DIM
EVEN_IN_WIDTH = S5_WIDTH + WA_Q_WIDTH + 2 * WA_KV_WIDTH
EVEN_MIX_WIDTH = S5_WIDTH + WA_Q_WIDTH
DA_HEADS = D_MODEL // (2 * HEAD_DIM)
DA_BLOCK = 128
DA_WIDTH = DA_HEADS * 2 * HEAD_DIM
N_EXPERTS = 16
D_EXPERT = 2 * D_MODEL
CAPACITY_FACTOR = 2

N_EVEN = (DEPTH + 1) // 2
N_ODD = DEPTH // 2

kernel_name = 'hybrid_s5_swa_diffattn_ecmoe_diffusion'


def rmsnorm(t, g):
    tf = t.astype(F32)
    y = tf * lax.rsqrt(jnp.mean(tf * tf, axis=-1, keepdims=True) + NORM_EPS)
    return (y * g.astype(F32)).astype(t.dtype)


def modulate(h, shift, scale):
    return h * (1 + scale) + shift


def axial_rope_tables(n_tokens, dtype):
    rows = n_tokens // GRID_W
    row = jnp.repeat(jnp.arange(rows), GRID_W).astype(F32)
    col = jnp.tile(jnp.arange(GRID_W), rows).astype(F32)
    inv = ROPE_BASE ** (-jnp.arange(ROPE_QUARTER, dtype=F32) / ROPE_QUARTER)
    ang_r = row[:, None] * inv[None, :]
    ang_c = col[:, None] * inv[None, :]
    ang = jnp.concatenate([ang_r, ang_r, ang_c, ang_c], axis=-1)
    return jnp.cos(ang).astype(dtype), jnp.sin(ang).astype(dtype)


def apply_rope(t, cos, sin):
    r = ROPE_QUARTER
    rot = jnp.concatenate([-t[..., r:2 * r], t[..., :r], -t[..., 3 * r:], t[..., 2 * r:3 * r]], axis=-1)
    shape = (1, cos.shape[0]) + (1,) * (t.ndim - 3) + (HEAD_DIM,)
    return t * cos.reshape(shape) + rot * sin.reshape(shape)


def s5_discretise(lam_re, lam_im, log_step, b_re, b_im):
    dt = jnp.exp(log_step)[:, None]
    mag = jnp.exp(lam_re * dt)
    a_re = mag * jnp.cos(lam_im * dt)
    a_im = mag * jnp.sin(lam_im * dt)
    den = lam_re * lam_re + lam_im * lam_im
    f_re = ((a_re - 1.0) * lam_re + a_im * lam_im) / den
    f_im = (a_im * lam_re - (a_re - 1.0) * lam_im) / den
    bb_re = f_re[..., None] * b_re - f_im[..., None] * b_im
    bb_im = f_re[..., None] * b_im + f_im[..., None] * b_re
    return a_re, a_im, bb_re, bb_im


def _cscan_op(e1, e2):
    a1r, a1i, b1r, b1i = e1
    a2r, a2i, b2r, b2i = e2
    return (a2r * a1r - a2i * a1i, a2r * a1i + a2i * a1r,
            a2r * b1r - a2i * b1i + b2r, a2r * b1i + a2i * b1r + b2i)


def complex_diag_scan(a_re, a_im, bu_re, bu_im, reverse):
    shape = (bu_re.shape[0], 1) + a_re.shape
    ar = jnp.broadcast_to(a_re[None, None], shape)
    ai = jnp.broadcast_to(a_im[None, None], shape)
    _, _, h_re, h_im = lax.associative_scan(_cscan_op, (ar, ai, bu_re, bu_im), reverse=reverse, axis=0)
    return h_re, h_im


def s5_readout(c_re, c_im, h_re, h_im):
    return jnp.einsum('gcp,lbgp->blgc', c_re, h_re) - jnp.einsum('gcp,lbgp->blgc', c_im, h_im)


def s5_output(y, u, dtype, d_skip, glu_w, glu_b):
    bsz, n = y.shape[:2]
    y = y.reshape(bsz, n, S5_WIDTH) + d_skip.astype(F32) * u.reshape(bsz, n, S5_WIDTH)
    z = jax.nn.gelu(y.astype(dtype))
    return z * jax.nn.sigmoid(z @ glu_w + glu_b)


def s5_mixer(u_lat, u_ctx, lam_re, lam_im, log_step, b_re, b_im, c_re, c_im, d_skip, glu_w, glu_b, need_ctx):
    bsz, n_lat, _ = u_lat.shape
    n_ctx = u_ctx.shape[1]
    ul = u_lat.astype(F32).reshape(bsz, n_lat, S5_GROUPS, S5_GROUP)
    uc = u_ctx.astype(F32).reshape(bsz, n_ctx, S5_GROUPS, S5_GROUP)
    y_lat = 0.0
    y_ctx = 0.0
    for k, rev in ((0, False), (1, True)):
        a_re, a_im, bb_re, bb_im = s5_discretise(lam_re[k].astype(F32), lam_im[k].astype(F32),
                                                 log_step[k].astype(F32), b_re[k].astype(F32), b_im[k].astype(F32))
        cr, ci = c_re[k].astype(F32), c_im[k].astype(F32)
        hc_re, hc_im = complex_diag_scan(a_re, a_im, jnp.einsum('blgh,gph->lbgp', uc, bb_re),
                                         jnp.einsum('blgh,gph->lbgp', uc, bb_im), rev)
        end = 0 if rev else -1
        h0_re, h0_im = hc_re[end], hc_im[end]
        bl_re = jnp.einsum('blgh,gph->lbgp', ul, bb_re)
        bl_im = jnp.einsum('blgh,gph->lbgp', ul, bb_im)
        first = -1 if rev else 0
        bl_re = bl_re.at[first].add(a_re * h0_re - a_im * h0_im)
        bl_im = bl_im.at[first].add(a_re * h0_im + a_im * h0_re)
        hl_re, hl_im = complex_diag_scan(a_re, a_im, bl_re, bl_im, rev)
        y_lat = y_lat + s5_readout(cr, ci, hl_re, hl_im)
        if need_ctx:
            y_ctx = y_ctx + s5_readout(cr, ci, hc_re, hc_im)
    out_lat = s5_output(y_lat, ul, u_lat.dtype, d_skip, glu_w, glu_b)
    out_ctx = s5_output(y_ctx, uc, u_ctx.dtype, d_skip, glu_w, glu_b) if need_ctx else None
    return out_lat, out_ctx


def window_attn_latent(q, k, v, kc, vc, sink):
    bsz, n = q.shape[:2]
    n_ctx = kc.shape[1]
    nb = n // WIN_BLOCK
    scale = HEAD_DIM ** -0.5
    qb = q.reshape(bsz, nb, WIN_BLOCK, WA_KV_HEADS, WA_GROUP, HEAD_DIM)

    def band(t):
        tp = jnp.pad(t, ((0, 0), (WIN_BLOCK, WIN_BLOCK), (0, 0), (0, 0)))
        tp = tp.reshape(bsz, nb + 2, WIN_BLOCK, WA_KV_HEADS, HEAD_DIM)
        return jnp.concatenate([tp[:, :-2], tp[:, 1:-1], tp[:, 2:]], axis=2)

    kw, vw = band(k), band(v)
    s_win = jnp.einsum('bnqkgd,bnskd->bnkgqs', qb, kw).astype(F32) * scale
    s_ctx = jnp.einsum('bnqkgd,bckd->bnkgqc', qb, kc).astype(F32) * scale
    qpos = jnp.arange(nb)[:, None, None] * WIN_BLOCK + jnp.arange(WIN_BLOCK)[None, :, None]
    kpos = (jnp.arange(nb)[:, None, None] - 1) * WIN_BLOCK + jnp.arange(3 * WIN_BLOCK)[None, None, :]
    valid = (jnp.abs(kpos - qpos) <= WINDOW) & (kpos >= 0) & (kpos < n)
    s_win = jnp.where(valid[None, :, None, None], s_win, NEG_INF)
    s_sink = jnp.broadcast_to(sink.astype(F32).reshape(1, 1, WA_KV_HEADS, WA_GROUP, 1, 1), s_ctx.shape[:-1] + (1,))
    p = jax.nn.softmax(jnp.concatenate([s_ctx, s_win, s_sink], axis=-1), axis=-1).astype(v.dtype)
    o = (jnp.einsum('bnkgqc,bckd->bnqkgd', p[..., :n_ctx], vc)
         + jnp.einsum('bnkgqs,bnskd->bnqkgd', p[..., n_ctx:n_ctx + 3 * WIN_BLOCK], vw))
    return o.reshape(bsz, n, WA_Q_WIDTH)


def window_attn_context(qc, kc, vc, sink):
    bsz, n_ctx = qc.shape[:2]
    scale = HEAD_DIM ** -0.5
    qg = qc.reshape(bsz, n_ctx, WA_KV_HEADS, WA_GROUP, HEAD_DIM)
    s = jnp.einsum('bqkgd,bckd->bkgqc', qg, kc).astype(F32) * scale
    s_sink = jnp.broadcast_to(sink.astype(F32).reshape(1, WA_KV_HEADS, WA_GROUP, 1, 1), s.shape[:-1] + (1,))
    p = jax.nn.softmax(jnp.concatenate([s, s_sink], axis=-1), axis=-1)[..., :n_ctx].astype(vc.dtype)
    o = jnp.einsum('bkgqc,bckd->bqkgd', p, vc)
    return o.reshape(bsz, n_ctx, WA_Q_WIDTH)


def even_mixer(hx, hc, cos, sin, w_in, w_out, lam_re, lam_im, log_step, b_re, b_im, c_re, c_im,
               d_skip, glu_w, glu_b, sink, need_ctx):
    def split(p):
        bsz, n = p.shape[:2]
        o = S5_WIDTH
        u = p[..., :o]
        q = p[..., o:o + WA_Q_WIDTH].reshape(bsz, n, WA_Q_HEADS, HEAD_DIM)
        o += WA_Q_WIDTH
        k = p[..., o:o + WA_KV_WIDTH].reshape(bsz, n, WA_KV_HEADS, HEAD_DIM)
        o += WA_KV_WIDTH
        v = p[..., o:].reshape(bsz, n, WA_KV_HEADS, HEAD_DIM)
        return u, q, k, v

    ux, qx, kx, vx = split(hx @ w_in)
    uc, qc, kc, vc = split(hc @ w_in)
    qx = apply_rope(qx, cos, sin)
    kx = apply_rope(kx, cos, sin)
    sx, sc = s5_mixer(ux, uc, lam_re, lam_im, log_step, b_re, b_im, c_re, c_im, d_skip, glu_w, glu_b, need_ctx)
    ax = window_attn_latent(qx, kx, vx, kc, vc, sink)
    ox = jnp.concatenate([sx, ax], axis=-1) @ w_out
    oc = None
    if need_ctx:
        ac = window_attn_context(qc, kc, vc, sink)
        oc = jnp.concatenate([sc, ac], axis=-1) @ w_out
    return ox, oc


def diff_attn_block(q, k_all, v_all, lam):
    s = jnp.einsum('bqhjd,bshjd->bhjqs', q, k_all).astype(F32) * (HEAD_DIM ** -0.5)
    p = jax.nn.softmax(s, axis=-1)
    a = (p[:, :, 0] - lam * p[:, :, 1]).astype(v_all.dtype)
    return jnp.einsum('bhqs,bshe->bqhe', a, v_all)


def diff_attn_latent(q, k, v, kc, vc, lam):
    bsz, n = q.shape[:2]
    nb = n // DA_BLOCK
    k_all = jnp.concatenate([kc, k], axis=1)
    v_all = jnp.concatenate([vc, v], axis=1)
    qb = q.reshape(bsz, nb, DA_BLOCK, DA_HEADS, 2, HEAD_DIM).transpose(1, 0, 2, 3, 4, 5)
    o = lax.map(lambda qblk: diff_attn_block(qblk, k_all, v_all, lam), qb)
    return o.transpose(1, 0, 2, 3, 4).reshape(bsz, n, DA_HEADS, 2 * HEAD_DIM)


def odd_mixer(hx, hc, cos, sin, w_in, w_out, lq1, lk1, lq2, lk2, subln_g, lam_init, need_ctx):
    def split(p):
        bsz, n = p.shape[:2]
        q = p[..., :DA_WIDTH].reshape(bsz, n, DA_HEADS, 2, HEAD_DIM)
        k = p[..., DA_WIDTH:2 * DA_WIDTH].reshape(bsz, n, DA_HEADS, 2, HEAD_DIM)
        v = p[..., 2 * DA_WIDTH:].reshape(bsz, n, DA_HEADS, 2 * HEAD_DIM)
        return q, k, v

    qx, kx, vx = split(hx @ w_in)
    qc, kc, vc = split(hc @ w_in)
    qx = apply_rope(qx, cos, sin)
    kx = apply_rope(kx, cos, sin)
    lam = (jnp.exp(jnp.sum(lq1.astype(F32) * lk1.astype(F32)))
           - jnp.exp(jnp.sum(lq2.astype(F32) * lk2.astype(F32))) + lam_init)

    def post(o):
        bsz, n = o.shape[:2]
        return (rmsnorm(o, subln_g) * (1.0 - lam_init)).reshape(bsz, n, DA_WIDTH) @ w_out

    ox = post(diff_attn_latent(qx, kx, vx, kc, vc, lam))
    oc = post(diff_attn_block(qc, kc, vc, lam)) if need_ctx else None
    return ox, oc


def ec_moe(h, w_router, w_gate, w_up, w_down):
    bsz, n, d = h.shape
    cap = CAPACITY_FACTOR * n // N_EXPERTS
    aff = jax.nn.softmax(jnp.einsum('bld,de->ble', h, w_router).astype(F32), axis=-1)
    gates, idx = lax.top_k(aff.transpose(0, 2, 1), cap)
    xg = jax.vmap(lambda hb, ib: hb[ib])(h, idx)
    hid = jax.nn.silu(jnp.einsum('becd,edf->becf', xg, w_gate)) * jnp.einsum('becd,edf->becf', xg, w_up)
    y = jnp.einsum('becf,efd->becd', hid, w_down) * gates[..., None].astype(h.dtype)
    return jax.vmap(lambda yb, ib: jnp.zeros((n, d), h.dtype).at[ib.reshape(-1)].add(yb.reshape(-1, d)))(y, idx)


def setup_inputs(seed: int = 0) -> dict:
    key = jax.random.key(seed)
    ks = iter(jax.random.split(key, 48))

    def nrm(shape, s):
        return jax.random.normal(next(ks), shape, F32) * s

    D = D_MODEL
    G, P, GH = S5_GROUPS, S5_STATE, S5_GROUP
    n_idx = jnp.arange(P, dtype=F32)
    return {
        'x': nrm((BATCH, SEQ, D), 1.0),
        'c': nrm((BATCH, D), 1.0),
        'ctx': nrm((BATCH, CTX_LEN, D), 1.0),
        'c_ctx': nrm((D,), 1.0),
        'ada_w': nrm((DEPTH, D, 6 * D), 0.3 * D ** -0.5),
        'ada_b': nrm((DEPTH, 6 * D), 0.02),
        'norm_mix_g': 1.0 + nrm((DEPTH, D), 0.05),
        'norm_ffn_g': 1.0 + nrm((DEPTH, D), 0.05),
        'ev_w_in': nrm((N_EVEN, D, EVEN_IN_WIDTH), D ** -0.5),
        'ev_w_out': nrm((N_EVEN, EVEN_MIX_WIDTH, D), EVEN_MIX_WIDTH ** -0.5),
        's5_lam_re': -0.5 + nrm((N_EVEN, 2, G, P), 0.01),
        's5_lam_im': math.pi * n_idx + nrm((N_EVEN, 2, G, P), 0.01),
        's5_log_step': jax.random.uniform(next(ks), (N_EVEN, 2, G), F32, math.log(1e-3), math.log(1e-1)),
        's5_b_re': nrm((N_EVEN, 2, G, P, GH), (2 * GH) ** -0.5),
        's5_b_im': nrm((N_EVEN, 2, G, P, GH), (2 * GH) ** -0.5),
        's5_c_re': nrm((N_EVEN, 2, G, GH, P), (2 * P) ** -0.5),
        's5_c_im': nrm((N_EVEN, 2, G, GH, P), (2 * P) ** -0.5),
        's5_d': nrm((N_EVEN, S5_WIDTH), 1.0),
        's5_glu_w': nrm((N_EVEN, S5_WIDTH, S5_WIDTH), S5_WIDTH ** -0.5),
        's5_glu_b': nrm((N_EVEN, S5_WIDTH), 0.02),
        'wa_sink': nrm((N_EVEN, WA_Q_HEADS), 0.5),
        'od_w_in': nrm((N_ODD, D, 3 * DA_WIDTH), D ** -0.5),
        'od_w_out': nrm((N_ODD, DA_WIDTH, D), DA_WIDTH ** -0.5),
        'da_lq1': nrm((N_ODD, HEAD_DIM), 0.1),
        'da_lk1': nrm((N_ODD, HEAD_DIM), 0.1),
        'da_lq2': nrm((N_ODD, HEAD_DIM), 0.1),
        'da_lk2': nrm((N_ODD, HEAD_DIM), 0.1),
        'da_subln_g': 1.0 + nrm((N_ODD, 2 * HEAD_DIM), 0.05),
        'moe_router': nrm((DEPTH, D, N_EXPERTS), D ** -0.5),
        'moe_w_gate': nrm((DEPTH, N_EXPERTS, D, D_EXPERT), D ** -0.5),
        'moe_w_up': nrm((DEPTH, N_EXPERTS, D, D_EXPERT), D ** -0.5),
        'moe_w_down': nrm((DEPTH, N_EXPERTS, D_EXPERT, D), D_EXPERT ** -0.5),
        'final_g': 1.0 + nrm((D,), 0.05),
    }


def reference(x, c, ctx, c_ctx, ada_w, ada_b, norm_mix_g, norm_ffn_g, ev_w_in, ev_w_out,
              s5_lam_re, s5_lam_im, s5_log_step, s5_b_re, s5_b_im, s5_c_re, s5_c_im, s5_d,
              s5_glu_w, s5_glu_b, wa_sink, od_w_in, od_w_out, da_lq1, da_lk1, da_lq2, da_lk2,
              da_subln_g, moe_router, moe_w_gate, moe_w_up, moe_w_down, final_g):
    n_lat = x.shape[1]
    cos, sin = axial_rope_tables(n_lat, x.dtype)
    xs, cs = x, ctx
    for l in range(DEPTH):
        need_ctx = l < DEPTH - 1
        ml = jnp.split((jax.nn.silu(c) @ ada_w[l] + ada_b[l])[:, None, :], 6, axis=-1)
        mc = jnp.split((jax.nn.silu(c_ctx) @ ada_w[l] + ada_b[l])[None, None, :], 6, axis=-1)
        hx = modulate(rmsnorm(xs, norm_mix_g[l]), ml[0], ml[1])
        hc = modulate(rmsnorm(cs, norm_mix_g[l]), mc[0], mc[1])
        i = l // 2
        if l % 2 == 0:
            ox, oc = even_mixer(hx, hc, cos, sin, ev_w_in[i], ev_w_out[i], s5_lam_re[i], s5_lam_im[i],
                                s5_log_step[i], s5_b_re[i], s5_b_im[i], s5_c_re[i], s5_c_im[i], s5_d[i],
                                s5_glu_w[i], s5_glu_b[i], wa_sink[i], need_ctx)
        else:
            lam_init = 0.8 - 0.6 * math.exp(-0.3 * l)
            ox, oc = odd_mixer(hx, hc, cos, sin, od_w_in[i], od_w_out[i], da_lq1[i], da_lk1[i],
                               da_lq2[i], da_lk2[i], da_subln_g[i], lam_init, need_ctx)
        xs = xs + ml[2] * ox
        hx = modulate(rmsnorm(xs, norm_ffn_g[l]), ml[3], ml[4])
        xs = xs + ml[5] * ec_moe(hx, moe_router[l], moe_w_gate[l], moe_w_up[l], moe_w_down[l])
        if need_ctx:
            cs = cs + mc[2] * oc
            hc = modulate(rmsnorm(cs, norm_ffn_g[l]), mc[3], mc[4])
            cs = cs + mc[5] * ec_moe(hc, moe_router[l], moe_w_gate[l], moe_w_up[l], moe_w_down[l])
    return rmsnorm(xs, final_g)
```

```python
import contextlib
import math
import numpy as np
import concourse.bass as bass
import concourse.mybir as mybir
from concourse.bass_utils import run_bass_kernel_spmd
from concourse.alu_op_type import AluOpType as ALU

dt = mybir.dt
F32, BF16, U32, I32 = dt.float32, dt.bfloat16, dt.uint32, dt.int32
AF = mybir.ActivationFunctionType
AX = mybir.AxisListType

D = 1024
NL = 2048
NC_ = 256
NT = NL + NC_
NCH = NT // 128
NE = 16
DF = 2048
NDS = 40


class Prog:
    def __init__(self, nc, es):
        self.nc = nc
        self.es = es
        self.eng = {'pe': nc.tensor, 'act': nc.scalar, 'dve': nc.vector, 'pool': nc.gpsimd, 'sp': nc.sync}
        self.sem = {k: es.enter_context(nc.semaphore('s_' + k)) for k in self.eng}
        self.cnt = {k: 0 for k in self.eng}
        self.waited = {k: {} for k in self.eng}
        self.dsem = [es.enter_context(nc.semaphore('d%d' % i)) for i in range(NDS)]
        self.dcnt = [0] * NDS
        self.dnext = 0
        self.dlast = [None] * NDS
        self.lastw = {}
        self.readers = {}
        self.nops = 0

    def _deps(self, reads, writes):
        toks = []
        for k in reads:
            if k in self.lastw:
                toks.append(self.lastw[k])
        for k in writes:
            if k in self.lastw:
                toks.append(self.lastw[k])
            toks.extend(self.readers.get(k, ()))
        return toks

    def _commit(self, tok, reads, writes):
        for k in reads:
            self.readers.setdefault(k, []).append(tok)
        for k in writes:
            self.lastw[k] = tok
            self.readers[k] = []

    def _wait(self, e, toks):
        best = {}
        for t in toks:
            if t is None:
                continue
            if e == 'pe' and t[0] == 'pe':
                continue
            if t[0] not in best or best[t[0]][2] < t[2]:
                best[t[0]] = t
        for key, t in best.items():
            if self.waited[e].get(key, 0) >= t[2]:
                continue
            self.eng[e].wait_ge(t[1], t[2])
            self.waited[e][key] = t[2]

    def op(self, e, fn, reads=(), writes=(), signal=True):
        self.nops += 1
        self._wait(e, self._deps(reads, writes))
        inst = fn(self.eng[e])
        if signal:
            self.cnt[e] += 1
            inst.then_inc(self.sem[e], 1)
            tok = (e, self.sem[e], self.cnt[e])
        else:
            tok = (e, self.sem[e], self.cnt[e] + 1)
        self._commit(tok, reads, writes)

    def dma(self, out, in_, reads=(), writes=(), q='sp', fn=None):
        self.nops += 1
        i = self.dnext
        self.dnext = (i + 1) % NDS
        self._wait(q, self._deps(reads, writes) + [self.dlast[i]])
        self.dcnt[i] += 16
        if fn is None:
            inst = self.eng[q].dma_start(out=out, in_=in_)
        else:
            inst = fn(self.eng[q])
        inst.then_inc(self.dsem[i], 16)
        tok = ('d%d' % i, self.dsem[i], self.dcnt[i])
        self.dlast[i] = tok
        self._commit(tok, reads, writes)

    def inherit(self, newkey, oldkeys):
        toks = list(self.readers.get(newkey, []))
        if newkey in self.lastw:
            toks.append(self.lastw[newkey])
        for k in oldkeys:
            if k in self.lastw:
                toks.append(self.lastw[k])
            toks.extend(self.readers.get(k, ()))
        self.lastw.pop(newkey, None)
        self.readers[newkey] = toks

    def finish(self, keys):
        toks = [self.lastw[k] for k in keys if k in self.lastw]
        self._wait('sp', toks)


_DSZ = {F32: 4, BF16: 2, U32: 4, I32: 4}


class Arena:
    def __init__(self, nc, es, P, nbytes):
        self.t = es.enter_context(nc.sbuf_tensor("arena", [128, nbytes // 4], F32))
        self.P = P
        self.cap = nbytes
        self.top = 0
        self.live = []
        self.freed = []

    def alloc(self, key, shape, d=F32):
        elems = 1
        for x in shape[1:]:
            elems *= x
        nb = (elems * _DSZ[d] + 63) // 64 * 64
        off = self.top
        self.top += nb
        assert self.top <= self.cap, "arena overflow %s: %d > %d" % (key, self.top, self.cap)
        olds = [k for (k, o, n) in self.freed if o < off + nb and off < o + n]
        self.P.inherit(key, olds)
        self.live.append((key, off, nb))
        ap = self.t[0:shape[0], off // 4:(off + nb) // 4]
        if d != F32:
            ap = ap.bitcast(d)
        ap = ap[:, 0:elems]
        if len(shape) > 2:
            names = ["a%d" % i for i in range(len(shape) - 1)]
            pat = "p (" + " ".join(names) + ") -> p " + " ".join(names)
            ap = ap.rearrange(pat, **{n: v for n, v in zip(names, shape[1:])})
        return ap

    def mark(self):
        return (self.top, len(self.live))

    def release(self, m):
        self.freed.extend(self.live[m[1]:])
        del self.live[m[1]:]
        self.top = m[0]


def build(stage=99, dbg=False):
    nc = bass.Bass("TRN2", target_bir_lowering=False)
    es = contextlib.ExitStack()
    with es:
        _build(nc, es, stage, dbg)
    return nc


def _build(nc, es, stage, dbg):
    def din(name, shape, d=F32):
        return nc.dram_tensor(name, list(shape), d, kind="ExternalInput").ap()

    def dscr(name, shape, d=F32):
        return nc.dram_tensor(name, list(shape), d, kind="Internal").ap()

    xin = din("xin", [NT, D])
    cc = din("cc", [2, D])
    ada_w = din("ada_w", [2, D, 6 * D])
    ada_b = din("ada_b", [2, 6 * D])
    norm_mix_g = din("norm_mix_g", [2, D])
    norm_ffn_g = din("norm_ffn_g", [2, D])
    final_g = din("final_g", [1, D])
    c_ident = din("c_ident", [128, 128])
    c_cos = din("c_cos", [128, NL])
    c_sin = din("c_sin", [128, NL])
    c_mask = din("c_mask", [128, 2, 128])
    ev_w_in = din("ev_w_in", [D, 1280])
    ev_w_out = din("ev_w_out", [D, D])
    wa_sink = din("wa_sink", [1, 8])
    s5_lam_re = din("s5_lam_re", [2, 32, 64])
    s5_lam_im = din("s5_lam_im", [2, 32, 64])
    s5_log_step = din("s5_log_step", [2, 32])
    s5_b_re = din("s5_b_re", [2, 32, 64, 16])
    s5_b_im = din("s5_b_im", [2, 32, 64, 16])
    s5_c_re = din("s5_c_re", [2, 32, 16, 64])
    s5_c_im = din("s5_c_im", [2, 32, 16, 64])
    s5_d = din("s5_d", [512])
    s5_glu_w = din("s5_glu_w", [512, 512])
    s5_glu_b = din("s5_glu_b", [512])
    c_gp = din("c_gp", [128, 8, 240])
    c_ep = din("c_ep", [128, 8, 240])
    c_sh = din("c_sh", [16, 8, 128])
    modrow_d = dscr("modrow_d", [2, 2, 6 * D])
    moe_router = din("moe_router", [2, D, NE])
    moe_w_gate = din("moe_w_gate", [2, NE, D, DF])
    moe_w_up = din("moe_w_up", [2, NE, D, DF])
    moe_w_down = din("moe_w_down", [2, NE, DF, D])
    hbf = dscr("hbf", [NT, D], BF16)
    od_w_in = din("od_w_in", [D, 3 * D])
    od_w_out = din("od_w_out", [D, D])
    da_l = din("da_l", [4, 64])
    da_subln_g = din("da_subln_g", [1, 128])
    out = nc.dram_tensor("out", [NL, D], F32, kind="ExternalOutput").ap()
    dbg_o = nc.dram_tensor("dbg", [NT, D], F32, kind="ExternalOutput").ap() if dbg else None
    xs = dscr("xs", [NT, D])

    P = Prog(nc, es)
    A = Arena(nc, es, P, 212480)
    XS_KEYS = ['xs%d' % c for c in range(NCH)]
    HBF_KEYS = ['hbf%d' % c for c in range(NCH)]

    def T(name, shape, d=F32):
        return A.alloc(name, shape, d)

    def PS(name, shape, d=F32):
        return es.enter_context(nc.psum_tensor(name, list(shape), d))

    ps = [PS("ps%d" % i, [128, 512], F32) for i in range(6)]
    pst = [PS("pst%d" % i, [128, 1024], BF16) for i in range(2)]
    rr = [0]

    def nextps():
        rr[0] = (rr[0] + 1) % 4
        return ps[rr[0]], 'ps%d' % rr[0]

    ident_f = T("ident_f", [128, 128], F32)
    ident_b = T("ident_b", [128, 128], BF16)
    P.dma(ident_f, c_ident[:, :], writes=['ident_f'])
    P.op('dve', lambda e: e.tensor_copy(out=ident_b, in_=ident_f), reads=['ident_f'], writes=['ident_b'])
    selrow = T("selrow", [2, 2, 128], F32)
    for r in range(2):
        P.op('dve', lambda e, r=r: e.tensor_copy(out=selrow[:, r, :], in_=ident_f[0:2, r:r + 1].to_broadcast([2, 128])),
             reads=['ident_f'], writes=['selrow'])
    stat_all = T("stat", [128, 4, 4], F32)
    den4 = T("den4", [128, 4], F32)
    sT = T("sT", [128, 8, 2], F32)
    with nc.allow_non_contiguous_dma(reason="tiny transposed load"):
        for r in range(2):
            P.dma(sT[:, :, r], cc[r, :].rearrange("(k p) -> p k", p=128), writes=['sT'])
    P.op('act', lambda e: e.activation(out=sT, in_=sT, func=AF.Silu), reads=['sT'], writes=['sT'])
    bc = [T("bc%d" % i, [128, D], F32) for i in range(4)]
    gB = T("gB", [128, D], F32)

    def mod_rows(l):
        m = A.mark()
        wa = [T("wa%d" % i, [128, 8, 512], F32) for i in range(2)]
        adab = [T("adab%d" % i, [2, 512], F32) for i in range(2)]
        mrow = [T("mrow%d" % i, [2, 512], F32) for i in range(2)]
        for ct in range(12):
            i = ct % 2
            wk = 'wa%d' % i
            P.dma(wa[i], ada_w[l, :, ct * 512:(ct + 1) * 512].rearrange("(k p) c -> p k c", p=128), writes=[wk])
            P.dma(adab[i], ada_b[l:l + 1, ct * 512:(ct + 1) * 512].to_broadcast([2, 512]), writes=['adab%d' % i])
            pb, pk = ps[4 + i], 'ps%d' % (4 + i)
            for k in range(8):
                P.op('pe', lambda e, k=k, i=i, pb=pb: e.matmul(pb[0:2, :], lhsT=sT[:, k, :], rhs=wa[i][:, k, :],
                                                               start=(k == 0), stop=(k == 7)),
                     reads=['sT', wk], writes=[pk], signal=(k == 7))
            P.op('dve', lambda e, pb=pb, i=i: e.tensor_tensor(out=mrow[i], in0=pb[0:2, :], in1=adab[i], op=ALU.add),
                 reads=[pk, 'adab%d' % i], writes=['mrow%d' % i])
            P.dma(modrow_d[l, :, ct * 512:(ct + 1) * 512], mrow[i], reads=['mrow%d' % i], writes=['modrow%d_%d' % (l, ct)], q='act')
        A.release(m)

    def mod_rows_bg(l, cts, stage_):
        banks = [(ps[4][:, :], 'ps4'), (ps[5][:, :], 'ps5'), (pst[0][:, :].bitcast(F32), 'pst0')]
        mk_ = A.mark()
        if stage_ == 0:
            wab = [T("wab%d" % i, [128, 8, 128], F32) for i in range(2)]
            n_ = 0
            for bi, ct in enumerate(cts):
                pb, pk = banks[bi]
                for j in range(4):
                    i = n_ % 2
                    n_ += 1
                    c0 = ct * 512 + j * 128
                    P.dma(wab[i], ada_w[l, :, c0:c0 + 128].rearrange("(k p) c -> p k c", p=128), writes=['wab%d' % i])
                    for k in range(8):
                        P.op('pe', lambda e, k=k, i=i, pb=pb, j=j: e.matmul(pb[0:2, j * 128:(j + 1) * 128], lhsT=sT[:, k, :], rhs=wab[i][:, k, :],
                                                                          start=(k == 0), stop=(k == 7)),
                             reads=['sT', 'wab%d' % i], writes=[pk], signal=(k == 7))
        else:
            adab = [T("adabg%d" % i, [2, 512], F32) for i in range(2)]
            mrow = [T("mrowg%d" % i, [2, 512], F32) for i in range(2)]
            for bi, ct in enumerate(cts):
                pb, pk = banks[bi]
                i = bi % 2
                P.dma(adab[i], ada_b[l:l + 1, ct * 512:(ct + 1) * 512].to_broadcast([2, 512]), writes=['adabg%d' % i])
                P.op('dve', lambda e, pb=pb, i=i: e.tensor_tensor(out=mrow[i], in0=pb[0:2, :], in1=adab[i], op=ALU.add),
                     reads=[pk, 'adabg%d' % i], writes=['mrowg%d' % i])
                P.dma(modrow_d[l, :, ct * 512:(ct + 1) * 512], mrow[i], reads=['mrowg%d' % i], writes=['modrow%d_%d' % (l, ct)], q='act')
        A.release(mk_)

    cur_l = [0]

    def bcast(dst, dkey, r, which):
        P.dma(dst, modrow_d[cur_l[0], r:r + 1, which * D:(which + 1) * D].to_broadcast([128, D]),
              reads=['modrow%d_%d' % (cur_l[0], 2 * which), 'modrow%d_%d' % (cur_l[0], 2 * which + 1)], writes=[dkey])

    def make_gs(dst, dkey, r, which_scale, gsrc):
        P.dma(gB, gsrc.to_broadcast([128, D]), writes=['gB'])
        bcast(dst, dkey, r, which_scale)
        P.op('dve', lambda e: e.scalar_tensor_tensor(out=dst, in0=dst, scalar=1.0, in1=gB, op0=ALU.add, op1=ALU.mult),
             reads=[dkey, 'gB'], writes=[dkey])

    def norm_mod(src_ap, xtile, xkey, htile, hkey, junk, gs, gskey, sh, shkey, eps=1e-6, si=0, srckey=None):
        stat = stat_all[:, si, :]
        sk = 'stat%d' % si
        P.dma(xtile, src_ap, reads=([srckey] if srckey else []), writes=[xkey])
        P.op('act', lambda e: e.activation(out=junk, in_=xtile, func=AF.Square, accum_out=stat[:, 0:1]),
             reads=[xkey], writes=['junk', sk])
        P.op('dve', lambda e: e.tensor_scalar(out=stat[:, 1:2], in0=stat[:, 0:1], scalar1=1.0 / D, scalar2=eps,
                                              op0=ALU.mult, op1=ALU.add), reads=[sk], writes=[sk])
        P.op('act', lambda e: e.sqrt(out=stat[:, 2:3], in_=stat[:, 1:2]), reads=[sk], writes=[sk])
        P.op('dve', lambda e: e.reciprocal(out=stat[:, 3:4], in_=stat[:, 2:3]), reads=[sk], writes=[sk])
        P.op('dve', lambda e: e.scalar_tensor_tensor(out=htile, in0=xtile, scalar=stat[:, 3:4], in1=gs,
                                                     op0=ALU.mult, op1=ALU.mult),
             reads=[xkey, sk, gskey], writes=[hkey])
        if sh is not None:
            P.op('pool', lambda e: e.tensor_tensor(out=htile, in0=htile, in1=sh, op=ALU.add),
                 reads=[hkey, shkey], writes=[hkey])

    def norm_phase(gain, which_shift, which_scale, src, consumer, src_is_xs=True):
        m = A.mark()
        xt = [T("xt%d" % i, [128, D], F32) for i in range(4)]
        ht = [T("ht%d" % i, [128, D], F32) for i in range(4)]
        junk = T("junk", [128, D], BF16)
        make_gs(bc[0], 'bc0', 0, which_scale, gain)
        bcast(bc[1], 'bc1', 0, which_shift)
        make_gs(bc[2], 'bc2', 1, which_scale, gain)
        bcast(bc[3], 'bc3', 1, which_shift)
        for c in range(NCH):
            i = c % 4
            lat = c >= 2
            norm_mod(src[c * 128:(c + 1) * 128, :], xt[i], 'xt%d' % i, ht[i], 'ht%d' % i, junk,
                     bc[0] if lat else bc[2], 'bc0' if lat else 'bc2', bc[1] if lat else bc[3], 'bc1' if lat else 'bc3', si=i,
                     srckey=('xs%d' % c) if src_is_xs else None)
            consumer(c, ht[i], 'ht%d' % i)
        A.release(m)

    def to_hT(hT, hb):
        def f(c, htile, hkey):
            i = c % 2
            P.op('act', lambda e: e.copy(out=hb[i], in_=htile), reads=[hkey], writes=['hb%d' % i])
            for k in range(8):
                P.op('pe', lambda e, k=k: e.transpose(out=pst[i][:, k * 128:(k + 1) * 128], in_=hb[i][:, k * 128:(k + 1) * 128],
                                                      identity=ident_b),
                     reads=['hb%d' % i, 'ident_b'], writes=['pst%d' % i], signal=(k == 7))
            P.op('dve', lambda e: e.tensor_copy(out=hT[:, :, c * 128:(c + 1) * 128],
                                                in_=pst[i][:, :].rearrange("p (k t) -> p k t", k=8)),
                 reads=['pst%d' % i], writes=['hT'])
            if dbg and stage == 1:
                P.dma(dbg_o[c * 128:(c + 1) * 128, :], htile, reads=[hkey], writes=['dbg_o'])
        return f

    mod_rows(0)
    m_mixer = A.mark()
    hT = T("hT", [128, 8, NT], BF16)
    uT = T("uT", [128, 4, NT], BF16)
    m1 = A.mark()
    hb = [T("hb%d" % i, [128, D], BF16) for i in range(2)]
    norm_phase(norm_mix_g[0:1, :], 0, 1, xin, to_hT(hT, hb), src_is_xs=False)
    A.release(m1)
    if stage == 1:
        P.finish(['dbg_o'])
        return

    m_att = A.mark()
    w_in_b = T("w_in_b", [128, 8, 1280], BF16)
    wr_b = T("wr_b", [128, 8, 640], BF16)
    P.dma(w_in_b, ev_w_in.rearrange("(k p) c -> p k c", p=128), writes=['w_in_b'], q='pool')
    for k in range(8):
        srcv = w_in_b[:, k, 512:1152].rearrange("p (m b j) -> p m b j", b=2, j=16)
        dstv = wr_b[:, k, :].rearrange("p (m b j) -> p m b j", b=2, j=16)
        P.op('act', lambda e, srcv=srcv, dstv=dstv: e.mul(out=dstv[:, :, 0, :], in_=srcv[:, :, 1, :], mul=-1.0),
             reads=['w_in_b'], writes=['wr_b'])
        P.op('dve', lambda e, srcv=srcv, dstv=dstv: e.tensor_copy(out=dstv[:, :, 1, :], in_=srcv[:, :, 0, :]),
             reads=['w_in_b'], writes=['wr_b'])
    cosT = T("cosT", [128, NL], F32)
    sinT = T("sinT", [128, NL], F32)
    P.dma(cosT, c_cos[:, :], writes=['cosT'])
    P.dma(sinT, c_sin[:, :], writes=['sinT'])
    maskb = T("maskb", [128, 2, 4, 128], BF16)
    esink = T("esink", [128, 8], F32)
    m2 = A.mark()
    maskf = T("maskf", [128, 2, 128], F32)
    P.dma(maskf, c_mask[:, :, :], writes=['maskf'])
    for g in range(4):
        P.op('dve', lambda e, g=g: e.tensor_copy(out=maskb[:, :, g, :], in_=maskf), reads=['maskf'], writes=['maskb'])
    A.release(m2)
    P.dma(esink, wa_sink[0:1, :].to_broadcast([128, 8]), writes=['esink'])
    P.op('act', lambda e: e.activation(out=esink, in_=esink, func=AF.Exp), reads=['esink'], writes=['esink'])

    qT = T("qT", [128, 2, NCH, 4, 128], BF16)
    P.op('pool', lambda e: e.memset(qT, 0.0), writes=['qT'])
    wq_p = T("wq_p", [128, 8, 4, 128], BF16)
    wqr_p = T("wqr_p", [128, 8, 4, 128], BF16)
    for k in range(8):
        P.op('act', lambda e, k=k: e.copy(out=wq_p[:, k, :, :].rearrange("p g (a j) -> p g a j", a=2),
                                          in_=w_in_b[:, k, 512:1024].rearrange("p (a g j) -> p g a j", a=2, g=4)),
             reads=['w_in_b'], writes=['wq_p'])
        P.op('dve', lambda e, k=k: e.tensor_copy(out=wqr_p[:, k, :, :].rearrange("p g (a j) -> p g a j", a=2),
                                                 in_=wr_b[:, k, 0:512].rearrange("p (a g j) -> p g a j", a=2, g=4)),
             reads=['wr_b'], writes=['wqr_p'])
    kT = T("kT", [128, NT], BF16)
    Vx = T("Vx", [128, NCH, 2, 65], BF16)
    rtmp = [T("rtmp%d" % i, [128, 512], F32) for i in range(2)]
    P.op('pool', lambda e: e.memset(Vx, 1.0), writes=['Vx'])
    ttiles = [(0, 256)] + [(256 + i * 512, 512) for i in range(4)]

    for cch in range(4):
        for (t0, tn) in ttiles:
            pb, pk = nextps()
            for k in range(8):
                P.op('pe', lambda e, k=k, pb=pb, t0=t0, tn=tn: e.matmul(pb[:, 0:tn], lhsT=w_in_b[:, k, cch * 128:(cch + 1) * 128],
                                                                     rhs=hT[:, k, t0:t0 + tn], start=(k == 0), stop=(k == 7)),
                     reads=['w_in_b', 'hT'], writes=[pk], signal=(k == 7))
            P.op('act', lambda e, pb=pb, t0=t0, tn=tn: e.copy(out=uT[:, cch, t0:t0 + tn], in_=pb[:, 0:tn]),
                 reads=[pk], writes=['uT'])

    def proj_rope(parts, dkey, lhs_plain, lhs_rot, view=lambda ap: ap):
        for (t0, tn) in ttiles:
            pb, pk = nextps()
            for k in range(8):
                P.op('pe', lambda e, k=k, pb=pb, t0=t0, tn=tn: e.matmul(pb[:, 0:tn], lhsT=lhs_plain(k), rhs=hT[:, k, t0:t0 + tn],
                                                                     start=(k == 0), stop=(k == 7)),
                     reads=['w_in_b', 'wq_p', 'hT'], writes=[pk], signal=(k == 7))
            if t0 == 0:
                for (p0, p1, dst_of) in parts:
                    P.op('act', lambda e, pb=pb, tn=tn, t0=t0, p0=p0, p1=p1, dst_of=dst_of: e.copy(out=dst_of(t0, tn), in_=view(pb[p0:p1, 0:tn])),
                         reads=[pk], writes=[dkey])
                continue
            pb2, pk2 = nextps()
            for k in range(8):
                P.op('pe', lambda e, k=k, pb2=pb2, t0=t0, tn=tn: e.matmul(pb2[:, 0:tn], lhsT=lhs_rot(k), rhs=hT[:, k, t0:t0 + tn],
                                                                       start=(k == 0), stop=(k == 7)),
                     reads=['wr_b', 'wqr_p', 'hT'], writes=[pk2], signal=(k == 7))
            l0 = t0 - 256
            P.op('dve', lambda e, pb=pb, l0=l0: e.tensor_tensor(out=rtmp[0], in0=pb[:, :], in1=cosT[:, l0:l0 + 512], op=ALU.mult),
                 reads=[pk, 'cosT'], writes=['rtmp0'])
            P.op('dve', lambda e, pb2=pb2, l0=l0: e.tensor_tensor(out=rtmp[1], in0=pb2[:, :], in1=sinT[:, l0:l0 + 512], op=ALU.mult),
                 reads=[pk2, 'sinT'], writes=['rtmp1'])
            for (p0, p1, dst_of) in parts:
                P.op('pool', lambda e, t0=t0, p0=p0, p1=p1, dst_of=dst_of: e.tensor_tensor(out=dst_of(t0, 512), in0=view(rtmp[0][p0:p1, :]),
                                                                                        in1=view(rtmp[1][p0:p1, :]), op=ALU.add),
                     reads=['rtmp0', 'rtmp1'], writes=[dkey])

    for g in range(4):
        proj_rope([(0, 64, lambda t0, tn, g=g: qT[0:64, 0, t0 // 128:(t0 + tn) // 128, g, :]),
                   (64, 128, lambda t0, tn, g=g: qT[64:128, 1, t0 // 128:(t0 + tn) // 128, g, :])], 'qT',
                  lambda k, g=g: wq_p[:, k, g, :], lambda k, g=g: wqr_p[:, k, g, :],
                  view=lambda ap: ap.rearrange("p (c t) -> p c t", t=128))
    proj_rope([(0, 128, lambda t0, tn: kT[:, t0:t0 + tn])], 'kT', lambda k: w_in_b[:, k, 1024:1152], lambda k: wr_b[:, k, 512:640])
    for c in range(NCH):
        pb, pk = nextps()
        for k in range(8):
            P.op('pe', lambda e, k=k, pb=pb, c=c: e.matmul(pb[:, 0:128], lhsT=hT[:, k, c * 128:(c + 1) * 128], rhs=w_in_b[:, k, 1152:1280],
                                                        start=(k == 0), stop=(k == 7)),
                 reads=['w_in_b', 'hT'], writes=[pk], signal=(k == 7))
        P.op('act', lambda e, pb=pb, c=c: e.copy(out=Vx[:, c, :, 0:64], in_=pb[:, 0:128].rearrange("p (h j) -> p h j", h=2)),
             reads=[pk], writes=['Vx'])

    m3 = A.mark()
    Ebuf = [T("Ebuf%d" % i, [128, 512], BF16) for i in range(5)]
    aw = [T("aw%d" % i, [128, 512], BF16) for i in range(2)]
    dbgt = T("dbgt", [128, 512], F32) if dbg else None
    mixT = hT

    for qc in range(NCH):
        awt = aw[qc % 2]
        awk = 'aw%d' % (qc % 2)
        kbs = [(0, None), (1, None)]
        if qc >= 2:
            n = qc - 2
            if n - 1 >= 0:
                kbs.append((qc - 1, 0))
            kbs.append((qc, None))
            if n + 1 <= 15:
                kbs.append((qc + 1, 1))
        for kh in range(2):
            p0 = 64 * kh
            for bi, (kb, mk) in enumerate(kbs):
                pb, pk = nextps()
                P.op('pe', lambda e, pb=pb, kb=kb: e.matmul(pb[:, :], lhsT=kT[:, kb * 128:(kb + 1) * 128],
                                                         rhs=qT[:, kh, qc, :, :].rearrange("p g q -> p (g q)"), start=True, stop=True),
                     reads=['kT', 'qT'], writes=[pk])
                P.op('act', lambda e, pb=pb, bi=bi: e.activation(out=Ebuf[bi], in_=pb[:, :], func=AF.Exp, scale=0.125),
                     reads=[pk], writes=['Ebuf%d' % bi])
                if mk is not None:
                    P.op('dve', lambda e, bi=bi, mk=mk: e.tensor_tensor(out=Ebuf[bi], in0=Ebuf[bi],
                                                                       in1=maskb[:, mk, :, :].rearrange("p g q -> p (g q)"), op=ALU.mult),
                         reads=['Ebuf%d' % bi, 'maskb'], writes=['Ebuf%d' % bi])
            po, pok = ps[4 + kh], 'ps%d' % (4 + kh)
            for g in range(4):
                for bi, (kb, mk) in enumerate(kbs):
                    P.op('pe', lambda e, g=g, bi=bi, kb=kb: e.matmul(po[:, g * 65:(g + 1) * 65], lhsT=Ebuf[bi][:, g * 128:(g + 1) * 128],
                                                                  rhs=Vx[:, kb, kh, :], start=(bi == 0), stop=(bi == len(kbs) - 1)),
                         reads=['Ebuf%d' % bi, 'Vx'], writes=[pok], signal=(g == 3 and bi == len(kbs) - 1))
            pov = po[:, 0:260].rearrange("p (g j) -> p g j", g=4)
            P.op('dve', lambda e, pov=pov: e.tensor_tensor(out=den4, in0=pov[:, :, 64], in1=esink[:, 4 * kh:4 * kh + 4], op=ALU.add),
                 reads=[pok, 'esink'], writes=['den4'])
            P.op('dve', lambda e: e.reciprocal(out=den4, in_=den4), reads=['den4'], writes=['den4'])
            P.op('dve', lambda e, pov=pov: e.tensor_tensor(out=awt[:, kh * 256:(kh + 1) * 256].rearrange("p (g j) -> p g j", g=4), in0=pov[:, :, 0:64],
                                                          in1=den4.unsqueeze(2).to_broadcast([128, 4, 64]), op=ALU.mult),
                 reads=[pok, 'den4'], writes=[awk])
        if dbg and stage == 2:
            P.op('act', lambda e: e.copy(out=dbgt, in_=awt), reads=[awk], writes=['dbgt'])
            P.dma(dbg_o[qc * 128:(qc + 1) * 128, 0:512], dbgt, reads=['dbgt'], writes=['dbg_o'])
        i = qc % 2
        for j in range(4):
            P.op('pe', lambda e, j=j: e.transpose(out=pst[i][:, j * 128:(j + 1) * 128], in_=awt[:, j * 128:(j + 1) * 128], identity=ident_b),
                 reads=[awk, 'ident_b'], writes=['pst%d' % i], signal=(j == 3))
        P.op('dve', lambda e: e.tensor_copy(out=mixT[:, 4:8, qc * 128:(qc + 1) * 128],
                                            in_=pst[i][:, 0:512].rearrange("p (k t) -> p k t", k=4)),
             reads=['pst%d' % i], writes=['hT'])
    A.release(m_att)
    if stage == 2:
        P.finish(['dbg_o'])
        return

    TWO_PI = 2.0 * math.pi
    NCK = NT // 8
    zT = T("zT", [128, 4, NT], BF16)
    GP = T("GP", [128, 8, 240], BF16)
    EP = T("EP", [128, 8, 240], BF16)
    Shm = T("Shm", [16, 8, 128], BF16)
    P.dma(GP, c_gp[:, :, :], writes=['GP'], q='pool')
    P.dma(EP, c_ep[:, :, :], writes=['EP'], q='pool')
    P.dma(Shm, c_sh[:, :, :], writes=['Shm'], q='pool')
    NSEG = 4
    SEGL = NCK // NSEG
    mio = T("mio", [128, SEGL, 16], F32)
    m_io = A.mark()
    mio_i = T("mio_i", [128, SEGL, 16], I32)
    P.op('pool', lambda e: e.iota(mio_i, pattern=[[1, SEGL], [0, 16]], base=1, channel_multiplier=0), writes=['mio_i'])
    P.op('dve', lambda e: e.tensor_copy(out=mio, in_=mio_i), reads=['mio_i'], writes=['mio'])
    A.release(m_io)
    dcol = T("dcol", [128, 4], F32)
    gbcol = T("gbcol", [128, 4], F32)
    with nc.allow_non_contiguous_dma(reason="tiny transposed loads"):
        P.dma(dcol, s5_d.rearrange("(c p) -> p c", p=128), writes=['dcol'])
        P.dma(gbcol, s5_glu_b.rearrange("(c p) -> p c", p=128), writes=['gbcol'])

    def ew(eng, fn, reads, writes):
        P.op(eng, fn, reads=reads, writes=writes)

    def s5_pass(cch):
        g0 = 8 * cch
        mp = A.mark()
        ISm = T("ISm", [128, 16, 128], BF16)
        ISs = T("ISs", [128, 16, 128], BF16)
        SOm = T("SOm", [128, 16, 128], BF16)
        TKm = T("TKm", [128, 16, 128], BF16)
        AR8 = T("AR8", [128, NSEG, 2, 16], F32)
        AI8 = T("AI8", [128, NSEG, 2, 16], F32)
        PRt = T("PRt", [128, SEGL, 16], F32)
        PIt = T("PIt", [128, SEGL, 16], F32)
        ms = A.mark()
        LAMR = T("LAMR", [128, 16], F32)
        LAMI = T("LAMI", [128, 16], F32)
        DT = T("DT", [128, 16], F32)
        BR = T("BR", [128, 16, 16], F32)
        BI = T("BI", [128, 16, 16], F32)
        CR = T("CR", [128, 16, 16], F32)
        CI = T("CI", [128, 16, 16], F32)
        CRt = T("CRt", [128, 2, 64], F32)
        CIt = T("CIt", [128, 2, 64], F32)
        with nc.allow_non_contiguous_dma(reason="small parameter loads"):
            for k in range(2):
                for hf in range(2):
                    P.dma(LAMR[64 * hf:64 * hf + 64, 8 * k:8 * k + 8], s5_lam_re[k, g0:g0 + 8, :].rearrange("g p -> p g"), writes=['LAMR'])
                    P.dma(LAMI[64 * hf:64 * hf + 64, 8 * k:8 * k + 8], s5_lam_im[k, g0:g0 + 8, :].rearrange("g p -> p g"), writes=['LAMI'])
                    P.dma(BR[64 * hf:64 * hf + 64, 8 * k:8 * k + 8, :], s5_b_re[k, g0:g0 + 8, :, :].rearrange("g p h -> p g h"), writes=['BR'])
                    P.dma(BI[64 * hf:64 * hf + 64, 8 * k:8 * k + 8, :], s5_b_im[k, g0:g0 + 8, :, :].rearrange("g p h -> p g h"), writes=['BI'])
                P.dma(DT[:, 8 * k:8 * k + 8], s5_log_step[k:k + 1, g0:g0 + 8].to_broadcast([128, 8]), writes=['DT'])
        for k in range(2):
            for (src, tt_, tk_, dst, dk_) in ((s5_c_re, CRt, 'CRt', CR, 'CR'), (s5_c_im, CIt, 'CIt', CI, 'CI')):
                for dup in range(2):
                    P.dma(tt_[:, dup, :], src[k, g0:g0 + 8, :, :].rearrange("g c p -> (g c) p"), writes=[tk_])
                pb, pk = nextps()
                P.op('pe', lambda e, pb=pb, tt_=tt_: e.transpose(out=pb[:, 0:128], in_=tt_.rearrange("r d p -> r (d p)"), identity=ident_f),
                     reads=[tk_, 'ident_f'], writes=[pk])
                P.op('act', lambda e, pb=pb, dst=dst, k=k: e.copy(out=dst[:, 8 * k:8 * k + 8, :], in_=pb[:, 0:128].rearrange("p (g c) -> p g c", g=8)),
                     reads=[pk], writes=[dk_])
        MAG = T("MAG", [128, 16], F32)
        ANG = T("ANG", [128, 16], F32)
        NR = T("NR", [128, 16], F32)
        RR = T("RR", [128, 16], F32)
        SN = T("SN", [128, 16], F32)
        CS = T("CS", [128, 16], F32)
        t1 = T("t1", [128, 16], F32)
        t2 = T("t2", [128, 16], F32)
        FR = T("FR", [128, 16], F32)
        FI = T("FI", [128, 16], F32)
        APR = T("APR", [128, 9, 16], F32)
        API = T("API", [128, 9, 16], F32)
        ew('act', lambda e: e.activation(out=DT, in_=DT, func=AF.Exp), ['DT'], ['DT'])
        ew('dve', lambda e: e.tensor_tensor(out=MAG, in0=LAMR, in1=DT, op=ALU.mult), ['LAMR', 'DT'], ['MAG'])
        ew('act', lambda e: e.activation(out=MAG, in_=MAG, func=AF.Exp), ['MAG'], ['MAG'])
        ew('dve', lambda e: e.tensor_tensor(out=ANG, in0=LAMI, in1=DT, op=ALU.mult), ['LAMI', 'DT'], ['ANG'])
        ew('dve', lambda e: e.tensor_scalar(out=NR, in0=ANG, scalar1=1.0 / TWO_PI, scalar2=12582912.0, op0=ALU.mult, op1=ALU.add), ['ANG'], ['NR'])
        ew('dve', lambda e: e.tensor_scalar(out=NR, in0=NR, scalar1=-12582912.0, scalar2=None, op0=ALU.add), ['NR'], ['NR'])
        ew('dve', lambda e: e.scalar_tensor_tensor(out=RR, in0=NR, scalar=-6.28125, in1=ANG, op0=ALU.mult, op1=ALU.add), ['NR', 'ANG'], ['RR'])
        ew('dve', lambda e: e.scalar_tensor_tensor(out=RR, in0=NR, scalar=-(TWO_PI - 6.28125), in1=RR, op0=ALU.mult, op1=ALU.add), ['NR', 'RR'], ['RR'])
        ew('dve', lambda e: e.tensor_scalar(out=RR, in0=RR, scalar1=math.pi, scalar2=-math.pi, op0=ALU.min, op1=ALU.max), ['RR'], ['RR'])
        ew('act', lambda e: e.activation(out=SN, in_=RR, func=AF.Sin), ['RR'], ['SN'])
        ew('dve', lambda e: e.tensor_scalar(out=t1, in0=RR, scalar1=-1.0, scalar2=None, op0=ALU.mult), ['RR'], ['t1'])
        ew('dve', lambda e: e.tensor_tensor(out=t1, in0=t1, in1=RR, op=ALU.max), ['t1', 'RR'], ['t1'])
        ew('dve', lambda e: e.tensor_scalar(out=t1, in0=t1, scalar1=-1.0, scalar2=math.pi / 2, op0=ALU.mult, op1=ALU.add), ['t1'], ['t1'])
        ew('act', lambda e: e.activation(out=CS, in_=t1, func=AF.Sin), ['t1'], ['CS'])
        ew('dve', lambda e: e.memset(APR[:, 0, :], 1.0), [], ['APR'])
        ew('dve', lambda e: e.memset(API[:, 0, :], 0.0), [], ['API'])
        ew('dve', lambda e: e.tensor_tensor(out=APR[:, 1, :], in0=MAG, in1=CS, op=ALU.mult), ['MAG', 'CS'], ['APR'])
        ew('dve', lambda e: e.tensor_tensor(out=API[:, 1, :], in0=MAG, in1=SN, op=ALU.mult), ['MAG', 'SN'], ['API'])
        for tau in range(1, 8):
            ew('dve', lambda e, tau=tau: e.tensor_tensor(out=t1, in0=APR[:, tau, :], in1=APR[:, 1, :], op=ALU.mult), ['APR'], ['t1'])
            ew('dve', lambda e, tau=tau: e.tensor_tensor(out=t2, in0=API[:, tau, :], in1=API[:, 1, :], op=ALU.mult), ['API'], ['t2'])
            ew('dve', lambda e, tau=tau: e.tensor_tensor(out=APR[:, tau + 1, :], in0=t1, in1=t2, op=ALU.subtract), ['t1', 't2'], ['APR'])
            ew('dve', lambda e, tau=tau: e.tensor_tensor(out=t1, in0=APR[:, tau, :], in1=API[:, 1, :], op=ALU.mult), ['APR', 'API'], ['t1'])
            ew('dve', lambda e, tau=tau: e.tensor_tensor(out=t2, in0=API[:, tau, :], in1=APR[:, 1, :], op=ALU.mult), ['APR', 'API'], ['t2'])
            ew('dve', lambda e, tau=tau: e.tensor_tensor(out=API[:, tau + 1, :], in0=t1, in1=t2, op=ALU.add), ['t1', 't2'], ['API'])
        for sg_ in range(NSEG):
            ew('dve', lambda e, sg_=sg_: e.tensor_copy(out=AR8[:, sg_, 0, :], in_=APR[:, 8, :]), ['APR'], ['AR8'])
            ew('dve', lambda e, sg_=sg_: e.tensor_copy(out=AR8[:, sg_, 1, :], in_=APR[:, 8, :]), ['APR'], ['AR8'])
            ew('dve', lambda e, sg_=sg_: e.tensor_copy(out=AI8[:, sg_, 0, :], in_=API[:, 8, :]), ['API'], ['AI8'])
            ew('dve', lambda e, sg_=sg_: e.tensor_scalar(out=AI8[:, sg_, 1, :], in0=API[:, 8, :], scalar1=-1.0, scalar2=None, op0=ALU.mult), ['API'], ['AI8'])
        TA = T("TA", [128, SEGL, 16], F32)
        TN = T("TN", [128, SEGL, 16], F32)
        TM = T("TM", [128, SEGL, 16], F32)
        TS = T("TS", [128, SEGL, 16], F32)
        bM = lambda x: x.unsqueeze(1).to_broadcast([128, SEGL, 16])
        ew('dve', lambda e: e.tensor_tensor(out=t1, in0=LAMR, in1=DT, op=ALU.mult), ['LAMR', 'DT'], ['t1'])
        ew('dve', lambda e: e.tensor_tensor(out=TM, in0=mio, in1=bM(t1), op=ALU.mult), ['mio', 't1'], ['TM'])
        ew('act', lambda e: e.activation(out=TM, in_=TM, func=AF.Exp, scale=8.0), ['TM'], ['TM'])
        ew('dve', lambda e: e.tensor_tensor(out=TA, in0=mio, in1=bM(ANG), op=ALU.mult), ['mio', 'ANG'], ['TA'])
        ew('dve', lambda e: e.tensor_scalar(out=TA, in0=TA, scalar1=8.0, scalar2=None, op0=ALU.mult), ['TA'], ['TA'])
        ew('dve', lambda e: e.tensor_scalar(out=TN, in0=TA, scalar1=1.0 / TWO_PI, scalar2=12582912.0, op0=ALU.mult, op1=ALU.add), ['TA'], ['TN'])
        ew('dve', lambda e: e.tensor_scalar(out=TN, in0=TN, scalar1=-12582912.0, scalar2=None, op0=ALU.add), ['TN'], ['TN'])
        ew('dve', lambda e: e.scalar_tensor_tensor(out=TA, in0=TN, scalar=-6.28125, in1=TA, op0=ALU.mult, op1=ALU.add), ['TN', 'TA'], ['TA'])
        ew('dve', lambda e: e.scalar_tensor_tensor(out=TA, in0=TN, scalar=-(TWO_PI - 6.28125), in1=TA, op0=ALU.mult, op1=ALU.add), ['TN', 'TA'], ['TA'])
        ew('dve', lambda e: e.tensor_scalar(out=TA, in0=TA, scalar1=math.pi, scalar2=-math.pi, op0=ALU.min, op1=ALU.max), ['TA'], ['TA'])
        ew('act', lambda e: e.activation(out=TS, in_=TA, func=AF.Sin), ['TA'], ['TS'])
        ew('dve', lambda e: e.tensor_scalar(out=TN, in0=TA, scalar1=-1.0, scalar2=None, op0=ALU.mult), ['TA'], ['TN'])
        ew('dve', lambda e: e.tensor_tensor(out=TN, in0=TN, in1=TA, op=ALU.max), ['TN', 'TA'], ['TN'])
        ew('dve', lambda e: e.tensor_scalar(out=TN, in0=TN, scalar1=-1.0, scalar2=math.pi / 2, op0=ALU.mult, op1=ALU.add), ['TN'], ['TN'])
        ew('act', lambda e: e.activation(out=TN, in_=TN, func=AF.Sin), ['TN'], ['TN'])
        ew('dve', lambda e: e.tensor_tensor(out=PRt, in0=TM, in1=TN, op=ALU.mult), ['TM', 'TN'], ['PRt'])
        ew('dve', lambda e: e.tensor_tensor(out=PIt, in0=TM, in1=TS, op=ALU.mult), ['TM', 'TS'], ['PIt'])
        ew('dve', lambda e: e.tensor_tensor(out=t1, in0=LAMR, in1=LAMR, op=ALU.mult), ['LAMR'], ['t1'])
        ew('dve', lambda e: e.tensor_tensor(out=t2, in0=LAMI, in1=LAMI, op=ALU.mult), ['LAMI'], ['t2'])
        ew('dve', lambda e: e.tensor_tensor(out=t1, in0=t1, in1=t2, op=ALU.add), ['t1', 't2'], ['t1'])
        ew('dve', lambda e: e.reciprocal(out=NR, in_=t1), ['t1'], ['NR'])
        ew('dve', lambda e: e.tensor_scalar(out=RR, in0=APR[:, 1, :], scalar1=-1.0, scalar2=None, op0=ALU.add), ['APR'], ['RR'])
        ew('dve', lambda e: e.tensor_tensor(out=t1, in0=RR, in1=LAMR, op=ALU.mult), ['RR', 'LAMR'], ['t1'])
        ew('dve', lambda e: e.tensor_tensor(out=t2, in0=API[:, 1, :], in1=LAMI, op=ALU.mult), ['API', 'LAMI'], ['t2'])
        ew('dve', lambda e: e.tensor_tensor(out=t1, in0=t1, in1=t2, op=ALU.add), ['t1', 't2'], ['t1'])
        ew('dve', lambda e: e.tensor_tensor(out=FR, in0=t1, in1=NR, op=ALU.mult), ['t1', 'NR'], ['FR'])
        ew('dve', lambda e: e.tensor_tensor(out=t1, in0=API[:, 1, :], in1=LAMR, op=ALU.mult), ['API', 'LAMR'], ['t1'])
        ew('dve', lambda e: e.tensor_tensor(out=t2, in0=RR, in1=LAMI, op=ALU.mult), ['RR', 'LAMI'], ['t2'])
        ew('dve', lambda e: e.tensor_tensor(out=t1, in0=t1, in1=t2, op=ALU.subtract), ['t1', 't2'], ['t1'])
        ew('dve', lambda e: e.tensor_tensor(out=FI, in0=t1, in1=NR, op=ALU.mult), ['t1', 'NR'], ['FI'])
        B1 = T("B1", [128, 16, 16], F32)
        B2 = T("B2", [128, 16, 16], F32)
        w1 = T("w1", [128, 16, 16], F32)
        w2 = T("w2", [128, 16, 16], F32)
        bF = lambda x: x.unsqueeze(2).to_broadcast([128, 16, 16])
        ew('dve', lambda e: e.tensor_tensor(out=w1, in0=BR, in1=bF(FR), op=ALU.mult), ['BR', 'FR'], ['w1'])
        ew('dve', lambda e: e.tensor_tensor(out=w2, in0=BI, in1=bF(FI), op=ALU.mult), ['BI', 'FI'], ['w2'])
        ew('dve', lambda e: e.tensor_tensor(out=w1, in0=w1, in1=w2, op=ALU.subtract), ['w1', 'w2'], ['w1'])
        ew('dve', lambda e: e.tensor_tensor(out=w2, in0=BI, in1=bF(FR), op=ALU.mult), ['BI', 'FR', 'w1'], ['w2'])
        ew('dve', lambda e: e.tensor_tensor(out=BI, in0=BR, in1=bF(FI), op=ALU.mult), ['BR', 'FI', 'w2'], ['BI'])
        ew('dve', lambda e: e.tensor_tensor(out=w2, in0=w2, in1=BI, op=ALU.add), ['w2', 'BI'], ['w2'])
        ew('dve', lambda e: e.tensor_copy(out=B1[0:64], in_=w1[0:64]), ['w1'], ['B1'])
        ew('dve', lambda e: e.tensor_copy(out=B1[64:128], in_=w2[64:128]), ['w2'], ['B1'])
        ew('dve', lambda e: e.tensor_scalar(out=B2[0:64], in0=w2[0:64], scalar1=-1.0, scalar2=None, op0=ALU.mult), ['w2'], ['B2'])
        ew('dve', lambda e: e.tensor_copy(out=B2[64:128], in_=w1[64:128]), ['w1'], ['B2'])
        C1 = T("C1", [128, 16, 16], F32)
        C2 = T("C2", [128, 16, 16], F32)
        ew('dve', lambda e: e.tensor_copy(out=C1[0:64], in_=CR[0:64]), ['CR'], ['C1'])
        ew('dve', lambda e: e.tensor_scalar(out=C1[64:128], in0=CI[64:128], scalar1=-1.0, scalar2=None, op0=ALU.mult), ['CI'], ['C1'])
        ew('dve', lambda e: e.tensor_scalar(out=C2[0:64], in0=CI[0:64], scalar1=-1.0, scalar2=None, op0=ALU.mult), ['CI'], ['C2'])
        ew('dve', lambda e: e.tensor_scalar(out=C2[64:128], in0=CR[64:128], scalar1=-1.0, scalar2=None, op0=ALU.mult), ['CR'], ['C2'])
        Zt = T("Zt", [128, 16, 8, 16], BF16)
        Zst = T("Zst", [128, 16, 8, 16], BF16)
        SOx = T("SOx", [128, 16, 9, 16], F32)
        for sidx in range(8):
            tau = 7 - sidx
            par = lambda tau=tau: APR[:, tau, :].unsqueeze(2).to_broadcast([128, 16, 16])
            pai = lambda tau=tau: API[:, tau, :].unsqueeze(2).to_broadcast([128, 16, 16])
            ew('dve', lambda e, par=par: e.tensor_tensor(out=w1, in0=B1, in1=par(), op=ALU.mult), ['B1', 'APR'], ['w1'])
            ew('pool', lambda e, pai=pai: e.tensor_tensor(out=w2, in0=B2, in1=pai(), op=ALU.mult), ['B2', 'API'], ['w2'])
            ew('dve', lambda e, sidx=sidx: e.tensor_tensor(out=Zt[:, :, sidx, :], in0=w1, in1=w2, op=ALU.add), ['w1', 'w2'], ['Zt'])
            ew('dve', lambda e, par=par: e.tensor_tensor(out=w1, in0=B2, in1=par(), op=ALU.mult), ['B2', 'APR'], ['w1'])
            ew('pool', lambda e, pai=pai: e.tensor_tensor(out=w2, in0=B1, in1=pai(), op=ALU.mult), ['B1', 'API'], ['w2'])
            ew('dve', lambda e, sidx=sidx: e.tensor_tensor(out=Zst[:, :, sidx, :], in0=w1, in1=w2, op=ALU.subtract), ['w1', 'w2'], ['Zst'])
        for tau in range(9):
            par = lambda tau=tau: APR[:, tau, :].unsqueeze(2).to_broadcast([128, 16, 16])
            pai = lambda tau=tau: API[:, tau, :].unsqueeze(2).to_broadcast([128, 16, 16])
            ew('dve', lambda e, par=par: e.tensor_tensor(out=w1, in0=C1, in1=par(), op=ALU.mult), ['C1', 'APR'], ['w1'])
            ew('pool', lambda e, pai=pai: e.tensor_tensor(out=w2, in0=C2, in1=pai(), op=ALU.mult), ['C2', 'API'], ['w2'])
            ew('dve', lambda e, tau=tau: e.tensor_tensor(out=SOx[:, :, tau, :], in0=w1, in1=w2, op=ALU.add), ['w1', 'w2'], ['SOx'])
        ew('act', lambda e: e.copy(out=SOm.rearrange("p g (t c) -> p g t c", t=8), in_=SOx[:, :, 1:9, :]), ['SOx'], ['SOm'])
        for gl2 in range(0, 16, 8):
            for (src, sk, dst, dk) in ((Zt, 'Zt', ISm, 'ISm'), (Zst, 'Zst', ISs, 'ISs')):
                i = (gl2 // 8) % 2
                for j in range(8):
                    P.op('pe', lambda e, j=j, src=src, i=i: e.transpose(out=pst[i][:, j * 128:(j + 1) * 128],
                                                                       in_=src[:, gl2 + j, :, :].rearrange("p s h -> p (s h)"), identity=ident_b),
                         reads=[sk, 'ident_b'], writes=['pst%d' % i], signal=(j == 7))
                P.op('dve', lambda e, dst=dst, i=i: e.tensor_copy(out=dst[:, gl2:gl2 + 8, :], in_=pst[i][:, :].rearrange("p (g m) -> p g m", g=8)),
                     reads=['pst%d' % i], writes=[dk])
        KTp = T("KTp", [16, 16, 256], BF16)
        ew('dve', lambda e: e.memset(KTp, 0.0), [], ['KTp'])
        for q4 in range(4):
            pb, pk = nextps()
            for j in range(4):
                gd = q4 * 4 + j
                P.op('pe', lambda e, pb=pb, j=j, gd=gd: e.matmul(pb[0:16, j * 128:(j + 1) * 128], lhsT=B1[:, gd, :],
                                                                rhs=SOx[:, gd, 0:8, :].rearrange("p t c -> p (t c)"), start=True, stop=True),
                     reads=['B1', 'SOx'], writes=[pk], signal=(j == 3))
            P.op('act', lambda e, pb=pb, q4=q4: e.copy(out=KTp[:, q4 * 4:q4 * 4 + 4, 128:256], in_=pb[0:16, :].rearrange("p (g m) -> p g m", g=4)),
                 reads=[pk], writes=['KTp'])
        for q4 in range(4):
            pb, pk = nextps()
            for j in range(4):
                gd = q4 * 4 + j
                for sidx in range(8):
                    P.op('pe', lambda e, pb=pb, j=j, gd=gd, sidx=sidx: e.matmul(pb[:, j * 128:(j + 1) * 128], lhsT=Shm[:, sidx, :],
                                                                               rhs=KTp[:, gd, 128 - 16 * sidx:256 - 16 * sidx],
                                                                               start=(sidx == 0), stop=(sidx == 7)),
                         reads=['Shm', 'KTp'], writes=[pk], signal=(j == 3 and sidx == 7))
            P.op('act', lambda e, pb=pb, q4=q4: e.copy(out=TKm[:, q4 * 4:q4 * 4 + 4, :], in_=pb[:, :].rearrange("p (g m) -> p g m", g=4)),
                 reads=[pk], writes=['TKm'])
        A.release(ms)

        VZ = T("VZ", [128, NCK, 2, 16], F32)
        U = T("U", [128, 16, NCK], BF16)
        m_loop = A.mark()
        lt1 = T("lt1", [128, NSEG, 2, 16], F32)
        lt2 = T("lt2", [128, NSEG, 2, 16], F32)
        ct1 = T("ct1", [128, SEGL, 2, 16], F32)
        ct2 = T("ct2", [128, SEGL, 2, 16], F32)
        for d_ in range(2):
            for gl in range(8):
                gd = 8 * d_ + gl
                pb, pk = nextps()
                for part in range(2):
                    for sp in range(8):
                        win = GP[:, gl, 112 - 16 * sp:240 - 16 * sp]
                        if d_ == 0:
                            c0, c1, rhs = ((0, 32, uT[:, cch, sp:256:8]), (32, 288, uT[:, cch, 256 + sp:NT:8]))[part]
                        else:
                            to = 7 - sp
                            c0, c1, rhs = ((0, 32, uT[:, cch, 248 + to:(to - 8 if to - 8 >= 0 else None):-8]),
                                           (32, 288, uT[:, cch, 2296 + to:248 + to:-8]))[part]
                        P.op('pe', lambda e, pb=pb, c0=c0, c1=c1, rhs=rhs, win=win, sp=sp: e.matmul(pb[:, c0:c1], lhsT=win, rhs=rhs,
                                                                                                 start=(sp == 0), stop=(sp == 7)),
                             reads=['GP', 'uT'], writes=[pk], signal=(sp == 7 and part == 1))
                P.op('act', lambda e, pb=pb, gd=gd: e.copy(out=U[:, gd, :], in_=pb[:, 0:NCK]), reads=[pk], writes=['U'])
        for gd in range(16):
            for (mat, mk, half) in ((ISm, 'ISm', 0), (ISs, 'ISs', 1)):
                pb, pk = nextps()
                P.op('pe', lambda e, pb=pb, mat=mat, gd=gd: e.matmul(pb[:, 0:NCK], lhsT=mat[:, gd, :], rhs=U[:, gd, :], start=True, stop=True),
                     reads=[mk, 'U'], writes=[pk])
                P.op('act' if half == 0 else 'dve',
                     (lambda e, pb=pb, gd=gd, half=half: e.copy(out=VZ[:, :, half, gd], in_=pb[:, 0:NCK])) if half == 0 else
                     (lambda e, pb=pb, gd=gd, half=half: e.tensor_copy(out=VZ[:, :, half, gd], in_=pb[:, 0:NCK])),
                     reads=[pk], writes=['VZ'])
        m_bg = A.mark()
        mod_rows_bg(1, [3 * cch, 3 * cch + 1, 3 * cch + 2], 0)
        VZs = VZ.rearrange("p (s m) x g -> p s m x g", s=NSEG)
        for m_ in range(1, SEGL):
            ew('dve', lambda e, m_=m_: e.tensor_tensor(out=lt1, in0=VZs[:, :, m_ - 1, :, :], in1=AR8, op=ALU.mult), ['VZ', 'AR8'], ['lt1'])
            ew('dve', lambda e, m_=m_: e.tensor_tensor(out=lt2, in0=VZs[:, :, m_ - 1, ::-1, :], in1=AI8, op=ALU.mult), ['VZ', 'AI8'], ['lt2'])
            ew('dve', lambda e: e.tensor_tensor(out=lt1, in0=lt1, in1=lt2, op=ALU.add), ['lt1', 'lt2'], ['lt1'])
            ew('dve', lambda e, m_=m_: e.tensor_tensor(out=VZs[:, :, m_, :, :], in0=VZs[:, :, m_, :, :], in1=lt1, op=ALU.add), ['VZ', 'lt1'], ['VZ'])
        for sg_ in range(1, NSEG):
            cprev = VZ[:, sg_ * SEGL - 1, :, :]
            cb = cprev.unsqueeze(1).to_broadcast([128, SEGL, 2, 16])
            cbs = VZ[:, sg_ * SEGL - 1, ::-1, :].unsqueeze(1).to_broadcast([128, SEGL, 2, 16])
            seg = VZ[:, sg_ * SEGL:(sg_ + 1) * SEGL, :, :]
            prb = PRt.unsqueeze(2).to_broadcast([128, SEGL, 2, 16])
            pib = PIt.unsqueeze(2).to_broadcast([128, SEGL, 2, 16])
            ew('dve', lambda e, cb=cb, prb=prb: e.tensor_tensor(out=ct1, in0=prb, in1=cb, op=ALU.mult), ['PRt', 'VZ'], ['ct1'])
            ew('pool', lambda e, cbs=cbs, pib=pib: e.tensor_tensor(out=ct2, in0=pib, in1=cbs, op=ALU.mult), ['PIt', 'VZ'], ['ct2'])
            ew('dve', lambda e: e.tensor_tensor(out=ct1[:, :, 0, :], in0=ct1[:, :, 0, :], in1=ct2[:, :, 0, :], op=ALU.add), ['ct1', 'ct2'], ['ct1'])
            ew('dve', lambda e: e.tensor_tensor(out=ct1[:, :, 1, :], in0=ct1[:, :, 1, :], in1=ct2[:, :, 1, :], op=ALU.subtract), ['ct1', 'ct2'], ['ct1'])
            ew('dve', lambda e, seg=seg: e.tensor_tensor(out=seg, in0=seg, in1=ct1, op=ALU.add), ['VZ', 'ct1'], ['VZ'])
        mod_rows_bg(1, [3 * cch, 3 * cch + 1, 3 * cch + 2], 1)
        A.release(m_loop)
        Xb = T("Xb", [128, 16, NCK], BF16)
        Yb = T("Yb", [128, 16, NCK], BF16)
        ew('pool', lambda e: e.memset(Xb[:, :, 0:1], 0.0), [], ['Xb'])
        ew('act', lambda e: e.copy(out=Xb[:, :, 1:NCK], in_=VZ[:, 0:NCK - 1, 0, :].rearrange("p n g -> p g n")), ['VZ'], ['Xb'])
        for gd in range(16):
            pb, pk = nextps()
            P.op('pe', lambda e, pb=pb, gd=gd: e.matmul(pb[:, 0:NCK], lhsT=TKm[:, gd, :], rhs=U[:, gd, :], start=True, stop=False),
                 reads=['TKm', 'U'], writes=[pk], signal=False)
            P.op('pe', lambda e, pb=pb, gd=gd: e.matmul(pb[:, 0:NCK], lhsT=SOm[:, gd, :], rhs=Xb[:, gd, :], start=False, stop=True),
                 reads=['SOm', 'Xb'], writes=[pk])
            P.op('act', lambda e, pb=pb, gd=gd: e.copy(out=Yb[:, gd, :], in_=pb[:, 0:NCK]), reads=[pk], writes=['Yb'])
        pre = [T("pre%d" % i, [128, NCK], F32) for i in range(2)]
        pr2 = [T("pr2%d" % i, [128, NCK], F32) for i in range(2)]
        for i_ in range(8):
            pb, pk = nextps()
            b2 = i_ % 2
            for part in range(2):
                for gl in range(8):
                    c0, c1, rhs = ((0, 32, Yb[:, gl, 0:32]), (32, 288, Yb[:, gl, 32:288]))[part]
                    P.op('pe', lambda e, pb=pb, c0=c0, c1=c1, rhs=rhs, gl=gl: e.matmul(pb[:, c0:c1], lhsT=EP[:, i_, 112 - 16 * gl:240 - 16 * gl], rhs=rhs,
                                                                                   start=(gl == 0), stop=False),
                         reads=['EP', 'Yb'], writes=[pk], signal=False)
                for gl in range(8):
                    c0, c1, rhs = ((0, 32, Yb[:, 8 + gl, 31::-1]), (32, 288, Yb[:, 8 + gl, 287:31:-1]))[part]
                    P.op('pe', lambda e, pb=pb, c0=c0, c1=c1, rhs=rhs, gl=gl: e.matmul(pb[:, c0:c1], lhsT=EP[:, 7 - i_, 112 - 16 * gl:240 - 16 * gl], rhs=rhs,
                                                                                   start=False, stop=(gl == 7)),
                         reads=['EP', 'Yb'], writes=[pk], signal=(gl == 7 and part == 1))
            ew('dve', lambda e, pb=pb, b2=b2: e.scalar_tensor_tensor(out=pre[b2], in0=uT[:, cch, i_:NT:8], scalar=dcol[:, cch:cch + 1], in1=pb[:, 0:NCK],
                                                                    op0=ALU.mult, op1=ALU.add), [pk, 'uT', 'dcol'], ['pre%d' % b2])
            ew('act', lambda e, b2=b2: e.activation(out=pr2[b2], in_=pre[b2], func=AF.Square), ['pre%d' % b2], ['pr2%d' % b2])
            ew('dve', lambda e, b2=b2: e.tensor_scalar(out=pr2[b2], in0=pr2[b2], scalar1=0.044715, scalar2=1.0, op0=ALU.mult, op1=ALU.add), ['pr2%d' % b2], ['pr2%d' % b2])
            ew('dve', lambda e, b2=b2: e.tensor_tensor(out=pr2[b2], in0=pr2[b2], in1=pre[b2], op=ALU.mult), ['pr2%d' % b2, 'pre%d' % b2], ['pr2%d' % b2])
            ew('act', lambda e, b2=b2: e.activation(out=pr2[b2], in_=pr2[b2], func=AF.Sigmoid, scale=1.5957691216057308), ['pr2%d' % b2], ['pr2%d' % b2])
            ew('dve', lambda e, b2=b2: e.tensor_tensor(out=zT[:, cch, i_:NT:8], in0=pr2[b2], in1=pre[b2], op=ALU.mult), ['pr2%d' % b2, 'pre%d' % b2], ['zT'])
        A.release(mp)

    for cch in range(4):
        s5_pass(cch)

    m_glu = A.mark()
    gw = T("gw", [128, 4, 512], BF16)
    P.dma(gw, s5_glu_w.rearrange("(k p) c -> p k c", p=128), writes=['gw'], q='pool')
    sg = [T("sg%d" % i, [128, 512], BF16) for i in range(2)]
    for co in range(4):
        for ti, (t0, tn) in enumerate(ttiles):
            pb, pk = nextps()
            for k in range(4):
                P.op('pe', lambda e, k=k, pb=pb, t0=t0, tn=tn: e.matmul(pb[:, 0:tn], lhsT=gw[:, k, co * 128:(co + 1) * 128], rhs=zT[:, k, t0:t0 + tn],
                                                                     start=(k == 0), stop=(k == 3)),
                     reads=['gw', 'zT'], writes=[pk], signal=(k == 3))
            i = ti % 2
            ew('act', lambda e, pb=pb, tn=tn, i=i: e.activation(out=sg[i][:, 0:tn], in_=pb[:, 0:tn], func=AF.Sigmoid, bias=gbcol[:, co:co + 1]),
               [pk, 'gbcol'], ['sg%d' % i])
            ew('dve', lambda e, t0=t0, tn=tn, i=i: e.tensor_tensor(out=mixT[:, co, t0:t0 + tn], in0=sg[i][:, 0:tn], in1=zT[:, co, t0:t0 + tn], op=ALU.mult),
               ['sg%d' % i, 'zT'], ['hT'])
    A.release(m_glu)
    if stage == 3:
        dt_ = T("dt_", [128, 512], F32)
        for c in range(NCH):
            for k in range(4):
                P.op('pe', lambda e, k=k, c=c: e.transpose(out=pst[0][:, k * 128:(k + 1) * 128], in_=mixT[:, k, c * 128:(c + 1) * 128], identity=ident_b),
                     reads=['hT', 'ident_b'], writes=['pst0'], signal=(k == 3))
            ew('act', lambda e: e.copy(out=dt_, in_=pst[0][:, 0:512]), ['pst0'], ['dt_'])
            P.dma(dbg_o[c * 128:(c + 1) * 128, 0:512], dt_, reads=['dt_'], writes=['dbg_o'])
        P.finish(['dbg_o'])
        return

    def outproj_phase(l, w_out_dram, x_src, mixT_, mkey, off, chunks):
        m = A.mark()
        w_out_b = T("w_out_b", [128, 8, D], BF16)
        P.dma(w_out_b, w_out_dram.rearrange("(k p) c -> p k c", p=128), writes=['w_out_b'], q='pool')
        bcast(bc[0], 'bc0', 0, 2)
        bcast(bc[2], 'bc2', 1, 2)
        xo = [T("xo%d" % i, [128, D], F32) for i in range(2)]
        xn = [T("xn%d" % i, [128, D], F32) for i in range(2)]
        for c in chunks:
            i = c % 2
            g2, g2k = (bc[0], 'bc0') if c >= 2 else (bc[2], 'bc2')
            P.dma(xo[i], x_src[c * 128:(c + 1) * 128, :], reads=(['xs%d' % c] if l == 1 else []), writes=['xo%d' % i])
            for hh in range(2):
                pb, pk = nextps()
                for k in range(8):
                    P.op('pe', lambda e, k=k, pb=pb, hh=hh: e.matmul(pb[:, :], lhsT=mixT_[:, k, c * 128 - off:(c + 1) * 128 - off], rhs=w_out_b[:, k, hh * 512:(hh + 1) * 512],
                                                                  start=(k == 0), stop=(k == 7)),
                         reads=[mkey, 'w_out_b'], writes=[pk], signal=(k == 7))
                P.op('dve', lambda e, pb=pb, hh=hh: e.tensor_tensor(out=xn[i][:, hh * 512:(hh + 1) * 512], in0=pb[:, :], in1=g2[:, hh * 512:(hh + 1) * 512], op=ALU.mult),
                     reads=[pk, g2k], writes=['xn%d' % i])
            P.op('pool', lambda e: e.tensor_tensor(out=xn[i], in0=xn[i], in1=xo[i], op=ALU.add), reads=['xn%d' % i, 'xo%d' % i], writes=['xn%d' % i])
            P.dma(xs[c * 128:(c + 1) * 128, :], xn[i], reads=['xn%d' % i], writes=['xs%d' % c], q='pool')
            if dbg and stage == 4:
                P.dma(dbg_o[c * 128:(c + 1) * 128, :], xn[i], reads=['xn%d' % i], writes=['dbg_o'])
        A.release(m)

    outproj_phase(0, ev_w_out, xin, mixT, 'hT', 0, list(range(NCH)))
    A.release(m_mixer)
    if stage == 4:
        P.finish(['dbg_o'] + XS_KEYS)
        return

    def moe_phase(l, with_ctx):
        mm = A.mark()
        nslot = 288 if with_ctx else 256
        scs = [(0, 128, 0), (1, 128, 128)] + ([(2, 32, 256)] if with_ctx else [])
        affT = T("affT", [16, NT], F32)
        wr_f = T("wr_f", [128, 8, NE], F32)
        P.dma(wr_f, moe_router[l].rearrange("(k p) e -> p k e", p=128), writes=['wr_f'])
        m_rt = A.mark()
        NRB = 4
        hTf2 = [T("hTf%d" % i, [128, 8, 128], F32) for i in range(NRB)]
        hb2 = [T("hbb%d" % i, [128, D], BF16) for i in range(NRB)]
        aff2 = [T("aff%d" % i, [128, NE], F32) for i in range(NRB)]
        sm2 = [T("sm%d" % i, [128, 4], F32) for i in range(NRB)]
        pstf_m = [pst[i][:, :].bitcast(F32) for i in range(2)]

        def cons(c, htile, hkey):
            if c < 2 and not with_ctx:
                return
            i = c % NRB
            hTf, hk = hTf2[i], 'hTf%d' % i
            aff, ak = aff2[i], 'aff%d' % i
            sm, sk = sm2[i], 'sm%d' % i
            P.op('act', lambda e: e.copy(out=hb2[i], in_=htile), reads=[hkey], writes=['hbb%d' % i])
            P.dma(hbf[c * 128:(c + 1) * 128, :], hb2[i], reads=['hbb%d' % i], writes=['hbf%d' % c], q='act')
            for half in range(2):
                if c % 2 == 0:
                    pb, pk = ps[4 + half][:, :], 'ps%d' % (4 + half)
                else:
                    pb, pk = pstf_m[half], 'pst%d' % half
                for k in range(4):
                    kk = half * 4 + k
                    P.op('pe', lambda e, pb=pb, k=k, kk=kk: e.transpose(out=pb[:, k * 128:(k + 1) * 128], in_=htile[:, kk * 128:(kk + 1) * 128], identity=ident_f),
                         reads=[hkey, 'ident_f'], writes=[pk], signal=(k == 3))
                P.op('act' if half == 0 else 'dve',
                     (lambda e, pb=pb, half=half: e.copy(out=hTf[:, half * 4:half * 4 + 4, :], in_=pb[:, :].rearrange("p (k t) -> p k t", k=4))) if half == 0 else
                     (lambda e, pb=pb, half=half: e.tensor_copy(out=hTf[:, half * 4:half * 4 + 4, :], in_=pb[:, :].rearrange("p (k t) -> p k t", k=4))),
                     reads=[pk], writes=[hk])
            pb, pk = nextps()
            for k in range(8):
                P.op('pe', lambda e, pb=pb, k=k: e.matmul(pb[:, 0:NE], lhsT=hTf[:, k, :], rhs=wr_f[:, k, :], start=(k == 0), stop=(k == 7)),
                     reads=[hk, 'wr_f'], writes=[pk], signal=(k == 7))
            P.op('dve', lambda e, pb=pb: e.reduce_max(out=sm[:, 0:1], in_=pb[:, 0:NE], axis=AX.X), reads=[pk], writes=[sk])
            P.op('dve', lambda e: e.tensor_scalar(out=sm[:, 1:2], in0=sm[:, 0:1], scalar1=-1.0, scalar2=None, op0=ALU.mult), reads=[sk], writes=[sk])
            P.op('act', lambda e, pb=pb: e.activation(out=aff, in_=pb[:, 0:NE], func=AF.Exp, bias=sm[:, 1:2], accum_out=sm[:, 2:3]),
                 reads=[pk, sk], writes=[ak, sk])
            P.op('dve', lambda e: e.reciprocal(out=sm[:, 3:4], in_=sm[:, 2:3]), reads=[sk], writes=[sk])
            P.op('dve', lambda e: e.tensor_scalar(out=aff, in0=aff, scalar1=sm[:, 3:4], scalar2=None, op0=ALU.mult), reads=[ak, sk], writes=[ak])
            pb2, pk2 = nextps()
            P.op('pe', lambda e, pb2=pb2: e.transpose(out=pb2[0:NE, 0:128], in_=aff, identity=ident_f), reads=[ak, 'ident_f'], writes=[pk2])
            P.op('act', lambda e, pb2=pb2: e.copy(out=affT[:, c * 128:(c + 1) * 128], in_=pb2[0:NE, 0:128]), reads=[pk2], writes=['affT'])

        norm_phase(norm_ffn_g[l:l + 1, :], 3, 4, xs, cons)
        A.release(m_rt)

        NB = 4
        Wg = [T("Wg%d" % i, [128, 8, 512], BF16) for i in range(NB)]
        Wu = [T("Wu%d" % i, [128, 8, 512], BF16) for i in range(NB)]
        Wd = [T("Wd%d" % i, [128, 4, D], BF16) for i in range(NB)]

        def load_w(e_, ft, b):
            P.dma(Wg[b], moe_w_gate[l, e_, :, ft * 512:(ft + 1) * 512].rearrange("(k p) f -> p k f", p=128), writes=['Wg%d' % b], q='pool')
            P.dma(Wu[b], moe_w_up[l, e_, :, ft * 512:(ft + 1) * 512].rearrange("(k p) f -> p k f", p=128), writes=['Wu%d' % b], q='pool')
            P.dma(Wd[b], moe_w_down[l, e_, ft * 512:(ft + 1) * 512, :].rearrange("(k p) d -> p k d", p=128), writes=['Wd%d' % b], q='pool')

        tiles = [(e_, ft) for e_ in range(NE) for ft in range(4)]
        for ti in range(NB - 1):
            load_w(tiles[ti][0], tiles[ti][1], ti % NB)

        vals = T("vals", [16, 288], F32)
        idxu = T("idxu", [16, 288], U32)
        idxf = T("idxf", [16, 288], F32)
        mt = A.mark()
        wk = T("wk", [16, NL], F32)
        for (t0, tn, o0, nr) in ([(256, NL, 0, 32)] + ([(0, 256, 256, 4)] if with_ctx else [])):
            cur = affT[:, t0:t0 + tn]
            curk = 'affT'
            for r in range(nr):
                vs = vals[:, o0 + 8 * r:o0 + 8 * r + 8]
                P.op('dve', lambda e, vs=vs, cur=cur: e.max(out=vs, in_=cur), reads=[curk], writes=['vals'])
                P.op('dve', lambda e, vs=vs, cur=cur, r=r, o0=o0: e.max_index(out=idxu[:, o0 + 8 * r:o0 + 8 * r + 8], in_max=vs, in_values=cur),
                     reads=[curk, 'vals'], writes=['idxu'])
                if r < nr - 1:
                    P.op('dve', lambda e, vs=vs, cur=cur, tn=tn: e.match_replace(out=wk[:, 0:tn], in_to_replace=vs, in_values=cur, imm_value=-1.0),
                         reads=[curk, 'vals', 'wk'], writes=['wk'])
                    cur = wk[:, 0:tn]
                    curk = 'wk'
        A.release(mt)
        P.op('dve', lambda e: e.tensor_copy(out=idxf[:, 0:nslot], in_=idxu[:, 0:nslot]), reads=['idxu'], writes=['idxf'])
        P.op('dve', lambda e: e.tensor_scalar(out=idxf[:, 0:256], in0=idxf[:, 0:256], scalar1=256.0, scalar2=None, op0=ALU.add), reads=['idxf'], writes=['idxf'])
        idxT = T("idxT", [128, 3, NE], U32)
        gate = T("gate", [128, 3, NE], F32)
        for (sc, rows, so) in scs:
            for (src, sk, dst, dk) in ((idxf, 'idxf', idxT, 'idxT'), (vals, 'vals', gate, 'gate')):
                pb, pk = nextps()
                P.op('pe', lambda e, pb=pb, src=src, rows=rows, so=so: e.transpose(out=pb[0:rows, 0:NE], in_=src[:, so:so + rows], identity=ident_f[0:NE, 0:NE]),
                     reads=[sk, 'ident_f'], writes=[pk])
                P.op('dve', lambda e, pb=pb, dst=dst, rows=rows, sc=sc: e.tensor_copy(out=dst[0:rows, sc, :], in_=pb[0:rows, 0:NE]), reads=[pk], writes=[dk])

        bcast(bc[0], 'bc0', 0, 5)
        if with_ctx:
            bcast(bc[2], 'bc2', 1, 5)
        xg = [T("xg%d" % i, [128, D], BF16) for i in range(3)]
        xgT = T("xgT", [128, 8, 288], BF16)
        hid = [T("hid%d" % i, [128, 384], BF16) for i in range(4)]
        for i in range(4):
            P.op('pool', lambda e, i=i: e.memset(hid[i], 0.0), writes=['hid%d' % i])
        sgt = [T("sgt%d" % i, [128, 288], F32) for i in range(2)]
        ysb = T("ysb", [128, 3, D], F32)
        ysc = [T("ysc%d" % i, [128, D], F32) for i in range(3)]

        pend_scatter = []
        yrr = [0]
        for ti, (e_, ft) in enumerate(tiles):
            b = ti % NB
            if ft == 0:
                for (sc, rows, so) in scs:
                    P.dma(None, None, reads=HBF_KEYS + ['idxT'], writes=['xg%d' % sc], q='pool',
                          fn=lambda e, sc=sc, rows=rows, e_=e_: e.indirect_dma_start(out=xg[sc][0:rows, :], out_offset=None, in_=hbf,
                                                                                   in_offset=bass.IndirectOffsetOnAxis(idxT[0:rows, sc, e_:e_ + 1], 0)))
            if ti + NB - 1 < len(tiles):
                load_w(tiles[ti + NB - 1][0], tiles[ti + NB - 1][1], (ti + NB - 1) % NB)
            for f_ in pend_scatter:
                f_()
            del pend_scatter[:]
            if ft == 0:
                for (sc, rows, so) in scs:
                    i = sc % 2
                    for k in range(8):
                        P.op('pe', lambda e, k=k, sc=sc, rows=rows, i=i: e.transpose(out=pst[i][:, k * 128:k * 128 + rows], in_=xg[sc][0:rows, k * 128:(k + 1) * 128],
                                                                                 identity=ident_b[0:rows, 0:rows]),
                             reads=['xg%d' % sc, 'ident_b'], writes=['pst%d' % i], signal=(k == 7))
                    P.op('dve', lambda e, sc=sc, rows=rows, so=so, i=i: e.tensor_copy(out=xgT[:, :, so:so + rows],
                                                                                   in_=pst[i][:, :].rearrange("p (k t) -> p k t", k=8)[:, :, 0:rows]),
                         reads=['pst%d' % i], writes=['xgT'])
            for fc in range(4):
                pg, pgk = ps[fc % 2], 'ps%d' % (fc % 2)
                pu, puk = ps[2 + fc % 2], 'ps%d' % (2 + fc % 2)
                for k in range(8):
                    P.op('pe', lambda e, k=k, pg=pg, fc=fc, b=b: e.matmul(pg[:, 0:nslot], lhsT=Wg[b][:, k, fc * 128:(fc + 1) * 128], rhs=xgT[:, k, 0:nslot],
                                                                      start=(k == 0), stop=(k == 7)),
                         reads=['Wg%d' % b, 'xgT'], writes=[pgk], signal=(k == 7))
                for k in range(8):
                    P.op('pe', lambda e, k=k, pu=pu, fc=fc, b=b: e.matmul(pu[:, 0:nslot], lhsT=Wu[b][:, k, fc * 128:(fc + 1) * 128], rhs=xgT[:, k, 0:nslot],
                                                                      start=(k == 0), stop=(k == 7)),
                         reads=['Wu%d' % b, 'xgT'], writes=[puk], signal=(k == 7))
                j = fc % 2
                P.op('act', lambda e, pg=pg, j=j: e.activation(out=sgt[j][:, 0:nslot], in_=pg[:, 0:nslot], func=AF.Silu), reads=[pgk], writes=['sgt%d' % j])
                P.op('dve', lambda e, pu=pu, j=j, fc=fc: e.tensor_tensor(out=hid[fc][:, 0:nslot], in0=sgt[j][:, 0:nslot], in1=pu[:, 0:nslot], op=ALU.mult),
                     reads=['sgt%d' % j, puk], writes=['hid%d' % fc])
            for (sc, rows, so) in scs:
                for dh in range(2):
                    yrr[0] = 1 - yrr[0]
                    py, pyk = ps[4 + yrr[0]], 'ps%d' % (4 + yrr[0])
                    for fc in range(4):
                        P.op('pe', lambda e, py=py, fc=fc, rows=rows, so=so, dh=dh, b=b: e.matmul(py[:, :], lhsT=hid[fc][:, so:so + 128],
                                                                                             rhs=Wd[b][:, fc, dh * 512:(dh + 1) * 512],
                                                                                             start=(fc == 0), stop=(fc == 3)),
                             reads=['hid%d' % fc, 'Wd%d' % b], writes=[pyk], signal=(fc == 3))
                    if ft == 0:
                        P.op('act', lambda e, py=py, rows=rows, sc=sc, dh=dh: e.copy(out=ysb[0:rows, sc, dh * 512:(dh + 1) * 512], in_=py[0:rows, :]),
                             reads=[pyk], writes=['ysb'])
                    else:
                        P.op('dve', lambda e, py=py, rows=rows, sc=sc, dh=dh: e.tensor_tensor(out=ysb[0:rows, sc, dh * 512:(dh + 1) * 512],
                                                                                           in0=ysb[0:rows, sc, dh * 512:(dh + 1) * 512], in1=py[0:rows, :], op=ALU.add),
                             reads=[pyk, 'ysb'], writes=['ysb'])
            if ft == 3:
                for (sc, rows, so) in scs:
                    i = sc
                    g5, g5k = (bc[0], 'bc0') if sc < 2 else (bc[2], 'bc2')
                    P.op('dve', lambda e, sc=sc, rows=rows, i=i, g5=g5, e_=e_: e.scalar_tensor_tensor(out=ysc[i][0:rows, :], in0=ysb[0:rows, sc, :],
                                                                                                   scalar=gate[0:rows, sc, e_:e_ + 1], in1=g5[0:rows, :],
                                                                                                   op0=ALU.mult, op1=ALU.mult),
                         reads=['ysb', 'gate', g5k], writes=['ysc%d' % i])
                    pend_scatter.append(lambda sc=sc, rows=rows, i=i, e_=e_: P.dma(
                        None, None, reads=['ysc%d' % i, 'idxT'] + XS_KEYS, writes=XS_KEYS, q='pool',
                        fn=lambda e: e.indirect_dma_start(out=xs, out_offset=bass.IndirectOffsetOnAxis(idxT[0:rows, sc, e_:e_ + 1], 0),
                                                          in_=ysc[i][0:rows, :], in_offset=None, compute_op=ALU.add)))
        for f_ in pend_scatter:
            f_()
        A.release(mm)

    moe_phase(0, True)
    if stage == 5:
        dx = T("dx", [128, D], F32)
        for c in range(NCH):
            P.dma(dx, xs[c * 128:(c + 1) * 128, :], reads=['xs%d' % c], writes=['dx'])
            P.dma(dbg_o[c * 128:(c + 1) * 128, :], dx, reads=['dx'], writes=['dbg_o'])
        P.finish(['dbg_o'])
        return

    cur_l[0] = 1
    m_l1 = A.mark()
    mixT1 = T("mixT1", [128, 8, NL], BF16)
    m_l1b = A.mark()
    hT = T("hT", [128, 8, NT], BF16)
    m1 = A.mark()
    hb = [T("hb%d" % i, [128, D], BF16) for i in range(2)]
    norm_phase(norm_mix_g[1:2, :], 0, 1, xs, to_hT(hT, hb))
    A.release(m1)
    LAM_INIT = 0.8 - 0.6 * math.exp(-0.3 * 1)
    cosT = T("cosT", [128, NL], F32)
    sinT = T("sinT", [128, NL], F32)
    P.dma(cosT, c_cos[:, :], writes=['cosT'])
    P.dma(sinT, c_sin[:, :], writes=['sinT'])
    lamv = T("lamv", [128, 4, 64], F32)
    lams = T("lams", [128, 4], F32)
    for i in range(4):
        P.dma(lamv[:, i, :], da_l[i:i + 1, :].to_broadcast([128, 64]), writes=['lamv'])
    P.op('dve', lambda e: e.tensor_tensor(out=lamv[:, 0, :], in0=lamv[:, 0, :], in1=lamv[:, 1, :], op=ALU.mult), reads=['lamv'], writes=['lamv'])
    P.op('dve', lambda e: e.tensor_tensor(out=lamv[:, 2, :], in0=lamv[:, 2, :], in1=lamv[:, 3, :], op=ALU.mult), reads=['lamv'], writes=['lamv'])
    P.op('dve', lambda e: e.reduce_sum(out=lams[:, 0:1], in_=lamv[:, 0, :], axis=AX.X), reads=['lamv'], writes=['lams'])
    P.op('dve', lambda e: e.reduce_sum(out=lams[:, 1:2], in_=lamv[:, 2, :], axis=AX.X), reads=['lamv'], writes=['lams'])
    P.op('act', lambda e: e.activation(out=lams[:, 0:2], in_=lams[:, 0:2], func=AF.Exp), reads=['lams'], writes=['lams'])
    P.op('dve', lambda e: e.tensor_tensor(out=lams[:, 2:3], in0=lams[:, 1:2], in1=lams[:, 0:1], op=ALU.subtract), reads=['lams'], writes=['lams'])
    P.op('dve', lambda e: e.tensor_scalar(out=lams[:, 3:4], in0=lams[:, 2:3], scalar1=-LAM_INIT, scalar2=None, op0=ALU.add), reads=['lams'], writes=['lams'])

    Wh = T("Wh", [128, 8, 384], BF16)
    Whr = T("Whr", [128, 8, 256], BF16)
    qT1 = T("qT1", [128, 2, NL], BF16)
    P.op('pool', lambda e: e.memset(qT1, 0.0), writes=['qT1'])
    kT1 = T("kT1", [128, NT], BF16)
    Vx1 = T("Vx1", [128, NCH, 128], BF16)
    ones_b = T("ones_b", [128, 128], BF16)
    P.op('pool', lambda e: e.memset(ones_b, 1.0), writes=['ones_b'])
    rdn = [T("rdn%d" % i, [128, 512], F32) for i in range(2)]
    sqb = T("sqb", [128, 512], BF16)
    sgcol = T("sgcol", [128, 1], F32)
    with nc.allow_non_contiguous_dma(reason="tiny transposed load"):
        P.dma(sgcol, da_subln_g.rearrange("o f -> f o"), writes=['sgcol'])
    P.op('dve', lambda e: e.tensor_scalar(out=sgcol, in0=sgcol, scalar1=1.0 - LAM_INIT, scalar2=None, op0=ALU.mult), reads=['sgcol'], writes=['sgcol'])
    rtmp = [T("rtmp%d" % i, [128, 512], F32) for i in range(2)]
    Ebufs = [T("Eall%d" % i, [128, NCH, 512], BF16) for i in range(2)]
    pstf = [pst[i][:, :].bitcast(F32) for i in range(2)]
    o0 = T("o0", [128, 512], F32)
    o1 = T("o1", [128, 512], F32)
    lt_tiles = [(256 + i * 512, 512) for i in range(4)]

    for h in range(8):
        for j3 in range(3):
            P.dma(Wh[:, :, j3 * 128:(j3 + 1) * 128], od_w_in[:, j3 * D + h * 128: j3 * D + (h + 1) * 128].rearrange("(k p) c -> p k c", p=128),
                  writes=['Wh'], q='pool')
        for k in range(8):
            srcv = Wh[:, k, 0:256].rearrange("p (m b j) -> p m b j", b=2, j=16)
            dstv = Whr[:, k, :].rearrange("p (m b j) -> p m b j", b=2, j=16)
            P.op('act', lambda e, srcv=srcv, dstv=dstv: e.mul(out=dstv[:, :, 0, :], in_=srcv[:, :, 1, :], mul=-1.0), reads=['Wh'], writes=['Whr'])
            P.op('dve', lambda e, srcv=srcv, dstv=dstv: e.tensor_copy(out=dstv[:, :, 1, :], in_=srcv[:, :, 0, :]), reads=['Wh'], writes=['Whr'])
        for (dst, dkey, c0, do_ctx, toff) in ((qT1, 'qT1', 0, False, 256), (kT1, 'kT1', 128, True, 0)):
            if do_ctx:
                pb, pk = nextps()
                for k in range(8):
                    P.op('pe', lambda e, k=k, pb=pb: e.matmul(pb[:, 0:256], lhsT=Wh[:, k, c0:c0 + 128], rhs=hT[:, k, 0:256], start=(k == 0), stop=(k == 7)),
                         reads=['Wh', 'hT'], writes=[pk], signal=(k == 7))
                P.op('act', lambda e, pb=pb: e.copy(out=dst[:, 0:256], in_=pb[:, 0:256]), reads=[pk], writes=[dkey])
            for (t0, tn) in lt_tiles:
                pb, pk = nextps()
                for k in range(8):
                    P.op('pe', lambda e, k=k, pb=pb, t0=t0: e.matmul(pb[:, :], lhsT=Wh[:, k, c0:c0 + 128], rhs=hT[:, k, t0:t0 + 512], start=(k == 0), stop=(k == 7)),
                         reads=['Wh', 'hT'], writes=[pk], signal=(k == 7))
                pb2, pk2 = nextps()
                for k in range(8):
                    P.op('pe', lambda e, k=k, pb2=pb2, t0=t0: e.matmul(pb2[:, :], lhsT=Whr[:, k, c0:c0 + 128], rhs=hT[:, k, t0:t0 + 512], start=(k == 0), stop=(k == 7)),
                         reads=['Whr', 'hT'], writes=[pk2], signal=(k == 7))
                l0 = t0 - 256
                P.op('dve', lambda e, pb=pb, l0=l0: e.tensor_tensor(out=rtmp[0], in0=pb[:, :], in1=cosT[:, l0:l0 + 512], op=ALU.mult), reads=[pk, 'cosT'], writes=['rtmp0'])
                P.op('dve', lambda e, pb2=pb2, l0=l0: e.tensor_tensor(out=rtmp[1], in0=pb2[:, :], in1=sinT[:, l0:l0 + 512], op=ALU.mult), reads=[pk2, 'sinT'], writes=['rtmp1'])
                if dkey == 'qT1':
                    for j in range(2):
                        P.op('pool', lambda e, t0=t0, j=j: e.tensor_tensor(out=qT1[64 * j:64 * j + 64, j, t0 - 256:t0 - 256 + 512], in0=rtmp[0][64 * j:64 * j + 64, :],
                                                                         in1=rtmp[1][64 * j:64 * j + 64, :], op=ALU.add),
                             reads=['rtmp0', 'rtmp1'], writes=[dkey])
                else:
                    P.op('pool', lambda e, t0=t0, dst=dst, toff=toff: e.tensor_tensor(out=dst[:, t0 - toff:t0 - toff + 512], in0=rtmp[0], in1=rtmp[1], op=ALU.add),
                         reads=['rtmp0', 'rtmp1'], writes=[dkey])
        for c in range(NCH):
            pb, pk = nextps()
            for k in range(8):
                P.op('pe', lambda e, k=k, pb=pb, c=c: e.matmul(pb[:, 0:128], lhsT=hT[:, k, c * 128:(c + 1) * 128], rhs=Wh[:, k, 256:384], start=(k == 0), stop=(k == 7)),
                     reads=['Wh', 'hT'], writes=[pk], signal=(k == 7))
            P.op('act', lambda e, pb=pb, c=c: e.copy(out=Vx1[:, c, 0:128], in_=pb[:, 0:128]), reads=[pk], writes=['Vx1'])
        units = [(qt, j) for qt in range(4) for j in range(2)]

        def S_step(u, kb):
            qt, j = units[u]
            p0 = 64 * j
            E = Ebufs[u % 2]
            pb, pk = nextps()
            P.op('pe', lambda e: e.matmul(pb[:, :], lhsT=kT1[:, kb * 128:(kb + 1) * 128], rhs=qT1[:, j, qt * 512:(qt + 1) * 512],
                                          start=True, stop=True), reads=['kT1', 'qT1'], writes=[pk])
            P.op('act', lambda e: e.activation(out=E[:, kb, :], in_=pb[:, :], func=AF.Exp, scale=0.125), reads=[pk], writes=['Eall%d' % (u % 2)])

        def acc_banks(u):
            if u % 2 == 0:
                return ps[4][:, :], 'ps4', ps[5][:, :], 'ps5'
            return pstf[0], 'pst0', pstf[1], 'pst1'

        def PV_step(u, kb):
            E = Ebufs[u % 2]
            ek = 'Eall%d' % (u % 2)
            pa, pak, pd, pdk = acc_banks(u)
            P.op('pe', lambda e: e.matmul(pa, lhsT=Vx1[:, kb, :], rhs=E[:, kb, :], start=(kb == 0), stop=(kb == NCH - 1)),
                 reads=[ek, 'Vx1'], writes=[pak], signal=(kb == NCH - 1))
            P.op('pe', lambda e: e.matmul(pd, lhsT=ones_b, rhs=E[:, kb, :], start=(kb == 0), stop=(kb == NCH - 1)),
                 reads=[ek, 'ones_b'], writes=[pdk], signal=(kb == NCH - 1))

        def epilogue(u):
            qt, j = units[u]
            pa, pak, pd, pdk = acc_banks(u)
            rd = rdn[u % 2]
            rk = 'rdn%d' % (u % 2)
            od, odk = (o0, 'o0') if j == 0 else (o1, 'o1')
            P.op('dve', lambda e: e.reciprocal(out=rd, in_=pd), reads=[pdk], writes=[rk])
            P.op('dve', lambda e: e.tensor_tensor(out=od, in0=pa, in1=rd, op=ALU.mult), reads=[pak, rk], writes=[odk])
            if j == 0:
                return
            P.op('dve', lambda e: e.scalar_tensor_tensor(out=o0, in0=o1, scalar=lams[:, 3:4], in1=o0, op0=ALU.mult, op1=ALU.add),
                 reads=['o0', 'o1', 'lams'], writes=['o0'])
            P.op('act', lambda e: e.activation(out=sqb, in_=o0, func=AF.Square), reads=['o0'], writes=['sqb'])
            pb, pk = nextps()
            P.op('pe', lambda e: e.matmul(pb[:, :], lhsT=ones_b, rhs=sqb, start=True, stop=True), reads=['ones_b', 'sqb'], writes=[pk])
            P.op('dve', lambda e: e.tensor_scalar(out=rd, in0=pb[:, :], scalar1=1.0 / 128, scalar2=1e-6, op0=ALU.mult, op1=ALU.add), reads=[pk], writes=[rk])
            P.op('act', lambda e: e.sqrt(out=rd, in_=rd), reads=[rk], writes=[rk])
            P.op('dve', lambda e: e.reciprocal(out=rd, in_=rd), reads=[rk], writes=[rk])
            P.op('dve', lambda e: e.scalar_tensor_tensor(out=mixT1[:, h, qt * 512:(qt + 1) * 512], in0=o0, scalar=sgcol[:, 0:1], in1=rd,
                                                         op0=ALU.mult, op1=ALU.mult), reads=['o0', 'sgcol', rk], writes=['mixT1'])

        for kb in range(NCH):
            S_step(0, kb)
        for u in range(len(units)):
            for kb in range(NCH):
                if u + 1 < len(units):
                    S_step(u + 1, kb)
                PV_step(u, kb)
            epilogue(u)

    A.release(m_l1b)
    outproj_phase(1, od_w_out, xs, mixT1, 'mixT1', 256, list(range(2, NCH)))
    A.release(m_l1)
    if stage == 6:
        dx = T("dx", [128, D], F32)
        for c in range(NCH):
            P.dma(dx, xs[c * 128:(c + 1) * 128, :], reads=['xs%d' % c], writes=['dx'])
            P.dma(dbg_o[c * 128:(c + 1) * 128, :], dx, reads=['dx'], writes=['dbg_o'])
        P.finish(['dbg_o'])
        return
    moe_phase(1, False)

    mf = A.mark()
    P.dma(gB, final_g[0:1, :].to_broadcast([128, D]), writes=['gB'])
    fx = [T("fx%d" % i, [128, D], F32) for i in range(2)]
    fo = [T("fo%d" % i, [128, D], F32) for i in range(2)]
    junk = T("junk", [128, D], BF16)
    for c in range(2, NCH):
        i = c % 2
        norm_mod(xs[c * 128:(c + 1) * 128, :], fx[i], 'fx%d' % i, fo[i], 'fo%d' % i, junk, gB, 'gB', None, None, si=i, srckey='xs%d' % c)
        P.dma(out[(c - 2) * 128:(c - 1) * 128, :], fo[i], reads=['fo%d' % i], writes=['out%d' % c], q='act')
        if dbg:
            P.dma(dbg_o[c * 128:(c + 1) * 128, :], fo[i], reads=['fo%d' % i], writes=['dbg_o'])
    A.release(mf)
    if dbg:
        P.finish(['dbg_o'])

    P.finish(['out%d' % c for c in range(2, NCH)])


def _consts():
    c = {}
    c["c_ident"] = np.eye(128, dtype=np.float32)
    t = np.arange(NL)
    row = (t // 64).astype(np.float32)
    col = (t % 64).astype(np.float32)
    inv = (np.float32(10000.0) ** (-np.arange(16, dtype=np.float32) / np.float32(16))).astype(np.float32)
    ang_r = row[:, None] * inv[None, :]
    ang_c = col[:, None] * inv[None, :]
    ang = np.concatenate([ang_r, ang_r, ang_c, ang_c], axis=-1).astype(np.float32)
    c["c_cos"] = np.ascontiguousarray(np.concatenate([np.cos(ang).T, np.cos(ang).T], axis=0).astype(np.float32))
    c["c_sin"] = np.ascontiguousarray(np.concatenate([np.sin(ang).T, np.sin(ang).T], axis=0).astype(np.float32))
    gp = np.zeros((128, 8, 240), np.float32)
    ep = np.zeros((128, 8, 240), np.float32)
    sh = np.zeros((16, 8, 128), np.float32)
    for g in range(8):
        for h in range(16):
            gp[16 * g + h, g, 112 + h] = 1.0
            ep[16 * g + h, g, 112 + h] = 1.0
            sh[h, g, 16 * g + h] = 1.0
    c["c_gp"] = gp
    c["c_ep"] = ep
    c["c_sh"] = sh
    kk = np.arange(128)[:, None]
    qq = np.arange(128)[None, :]
    c["c_mask"] = np.ascontiguousarray(np.stack([(kk >= qq), (kk <= qq)], axis=1).astype(np.float32))
    return c


def _in_map(inputs, b):
    m = {}
    m["xin"] = np.ascontiguousarray(np.concatenate([inputs["ctx"][b], inputs["x"][b]], axis=0), dtype=np.float32)
    m["cc"] = np.ascontiguousarray(np.stack([inputs["c"][b], inputs["c_ctx"]], axis=0), dtype=np.float32)
    for k in ["ada_w", "ada_b", "norm_mix_g", "norm_ffn_g"]:
        m[k] = np.ascontiguousarray(inputs[k], dtype=np.float32)
    m["ev_w_in"] = np.ascontiguousarray(inputs["ev_w_in"][0], dtype=np.float32)
    m["ev_w_out"] = np.ascontiguousarray(inputs["ev_w_out"][0], dtype=np.float32)
    m["wa_sink"] = np.ascontiguousarray(inputs["wa_sink"], dtype=np.float32).reshape(1, 8)
    for k in ["s5_lam_re", "s5_lam_im", "s5_log_step", "s5_b_re", "s5_b_im", "s5_c_re", "s5_c_im", "s5_d", "s5_glu_w", "s5_glu_b"]:
        m[k] = np.ascontiguousarray(inputs[k][0], dtype=np.float32)
    for k in ["moe_router", "moe_w_gate", "moe_w_up", "moe_w_down"]:
        m[k] = np.ascontiguousarray(inputs[k], dtype=np.float32)
    m["od_w_in"] = np.ascontiguousarray(inputs["od_w_in"][0], dtype=np.float32)
    m["od_w_out"] = np.ascontiguousarray(inputs["od_w_out"][0], dtype=np.float32)
    m["da_l"] = np.ascontiguousarray(np.stack([inputs["da_lq1"][0], inputs["da_lk1"][0], inputs["da_lq2"][0], inputs["da_lk2"][0]], axis=0), dtype=np.float32)
    m["da_subln_g"] = np.ascontiguousarray(inputs["da_subln_g"], dtype=np.float32).reshape(1, 128)
    m["final_g"] = np.ascontiguousarray(inputs["final_g"], dtype=np.float32).reshape(1, D)
    m.update(_consts())
    return m


def kernel(**inputs):
    nc = build()
    in_maps = [_in_map(inputs, b) for b in range(8)]
    res = run_bass_kernel_spmd(nc, in_maps, core_ids=list(range(8)))
    return np.stack([r["out"] for r in res.results], axis=0).astype(np.float32)
```

```python
import contextlib
import math
import numpy as np
import concourse.bass as bass
import concourse.mybir as mybir
from concourse.bass_utils import run_bass_kernel_spmd
from concourse.alu_op_type import AluOpType as ALU

dt = mybir.dt
F32, BF16, U32, I32 = dt.float32, dt.bfloat16, dt.uint32, dt.int32
AF = mybir.ActivationFunctionType
AX = mybir.AxisListType

D = 1024
NL = 2048
NC_ = 256
NT = NL + NC_
NCH = NT // 128
NE = 16
DF = 2048
NDS = 40


class Prog:
    def __init__(self, nc, es):
        self.nc = nc
        self.es = es
        self.eng = {'pe': nc.tensor, 'act': nc.scalar, 'dve': nc.vector, 'pool': nc.gpsimd, 'sp': nc.sync}
        self.sem = {k: es.enter_context(nc.semaphore('s_' + k)) for k in self.eng}
        self.cnt = {k: 0 for k in self.eng}
        self.waited = {k: {} for k in self.eng}
        self.dsem = [es.enter_context(nc.semaphore('d%d' % i)) for i in range(NDS)]
        self.dcnt = [0] * NDS
        self.dnext = 0
        self.dlast = [None] * NDS
        self.lastw = {}
        self.readers = {}
        self.nops = 0

    def _deps(self, reads, writes):
        toks = []
        for k in reads:
            if k in self.lastw:
                toks.append(self.lastw[k])
        for k in writes:
            if k in self.lastw:
                toks.append(self.lastw[k])
            toks.extend(self.readers.get(k, ()))
        return toks

    def _commit(self, tok, reads, writes):
        for k in reads:
            self.readers.setdefault(k, []).append(tok)
        for k in writes:
            self.lastw[k] = tok
            self.readers[k] = []

    def _wait(self, e, toks):
        best = {}
        for t in toks:
            if t is None:
                continue
            if e == 'pe' and t[0] == 'pe':
                continue
            if t[0] not in best or best[t[0]][2] < t[2]:
                best[t[0]] = t
        for key, t in best.items():
            if self.waited[e].get(key, 0) >= t[2]:
                continue
            self.eng[e].wait_ge(t[1], t[2])
            self.waited[e][key] = t[2]

    def op(self, e, fn, reads=(), writes=(), signal=True):
        self.nops += 1
        self._wait(e, self._deps(reads, writes))
        inst = fn(self.eng[e])
        if signal:
            self.cnt[e] += 1
            inst.then_inc(self.sem[e], 1)
            tok = (e, self.sem[e], self.cnt[e])
        else:
            tok = (e, self.sem[e], self.cnt[e] + 1)
        self._commit(tok, reads, writes)

    def dma(self, out, in_, reads=(), writes=(), q='sp', fn=None):
        self.nops += 1
        i = self.dnext
        self.dnext = (i + 1) % NDS
        self._wait(q, self._deps(reads, writes) + [self.dlast[i]])
        self.dcnt[i] += 16
        if fn is None:
            inst = self.eng[q].dma_start(out=out, in_=in_)
        else:
            inst = fn(self.eng[q])
        inst.then_inc(self.dsem[i], 16)
        tok = ('d%d' % i, self.dsem[i], self.dcnt[i])
        self.dlast[i] = tok
        self._commit(tok, reads, writes)

    def inherit(self, newkey, oldkeys):
        toks = list(self.readers.get(newkey, []))
        if newkey in self.lastw:
            toks.append(self.lastw[newkey])
        for k in oldkeys:
            if k in self.lastw:
                toks.append(self.lastw[k])
            toks.extend(self.readers.get(k, ()))
        self.lastw.pop(newkey, None)
        self.readers[newkey] = toks

    def finish(self, keys):
        toks = [self.lastw[k] for k in keys if k in self.lastw]
        self._wait('sp', toks)


_DSZ = {F32: 4, BF16: 2, U32: 4, I32: 4}


class Arena:
    def __init__(self, nc, es, P, nbytes):
        self.t = es.enter_context(nc.sbuf_tensor("arena", [128, nbytes // 4], F32))
        self.P = P
        self.cap = nbytes
        self.top = 0
        self.live = []
        self.freed = []

    def alloc(self, key, shape, d=F32):
        elems = 1
        for x in shape[1:]:
            elems *= x
        nb = (elems * _DSZ[d] + 63) // 64 * 64
        off = self.top
        self.top += nb
        assert self.top <= self.cap, "arena overflow %s: %d > %d" % (key, self.top, self.cap)
        olds = [k for (k, o, n) in self.freed if o < off + nb and off < o + n]
        self.P.inherit(key, olds)
        self.live.append((key, off, nb))
        ap = self.t[0:shape[0], off // 4:(off + nb) // 4]
        if d != F32:
            ap = ap.bitcast(d)
        ap = ap[:, 0:elems]
        if len(shape) > 2:
            names = ["a%d" % i for i in range(len(shape) - 1)]
            pat = "p (" + " ".join(names) + ") -> p " + " ".join(names)
            ap = ap.rearrange(pat, **{n: v for n, v in zip(names, shape[1:])})
        return ap

    def mark(self):
        return (self.top, len(self.live))

    def release(self, m):
        self.freed.extend(self.live[m[1]:])
        del self.live[m[1]:]
        self.top = m[0]


def build(stage=99, dbg=False):
    nc = bass.Bass("TRN2", target_bir_lowering=False)
    es = contextlib.ExitStack()
    with es:
        _build(nc, es, stage, dbg)
    return nc


def _build(nc, es, stage, dbg):
    def din(name, shape, d=F32):
        return nc.dram_tensor(name, list(shape), d, kind="ExternalInput").ap()

    def dscr(name, shape, d=F32):
        return nc.dram_tensor(name, list(shape), d, kind="Internal").ap()

    xin = din("xin", [NT, D])
    cc = din("cc", [2, D])
    ada_w = din("ada_w", [2, D, 6 * D])
    ada_b = din("ada_b", [2, 6 * D])
    norm_mix_g = din("norm_mix_g", [2, D])
    norm_ffn_g = din("norm_ffn_g", [2, D])
    final_g = din("final_g", [1, D])
    c_ident = din("c_ident", [128, 128])
    c_cos = din("c_cos", [128, NL])
    c_sin = din("c_sin", [128, NL])
    c_mask = din("c_mask", [128, 2, 128])
    ev_w_in = din("ev_w_in", [D, 1280])
    ev_w_out = din("ev_w_out", [D, D])
    wa_sink = din("wa_sink", [1, 8])
    s5_lam_re = din("s5_lam_re", [2, 32, 64])
    s5_lam_im = din("s5_lam_im", [2, 32, 64])
    s5_log_step = din("s5_log_step", [2, 32])
    s5_b_re = din("s5_b_re", [2, 32, 64, 16])
    s5_b_im = din("s5_b_im", [2, 32, 64, 16])
    s5_c_re = din("s5_c_re", [2, 32, 16, 64])
    s5_c_im = din("s5_c_im", [2, 32, 16, 64])
    s5_d = din("s5_d", [512])
    s5_glu_w = din("s5_glu_w", [512, 512])
    s5_glu_b = din("s5_glu_b", [512])
    c_gp = din("c_gp", [128, 8, 240])
    c_ep = din("c_ep", [128, 8, 240])
    c_sh = din("c_sh", [16, 8, 128])
    modrow_d = dscr("modrow_d", [2, 2, 6 * D])
    moe_router = din("moe_router", [2, D, NE])
    moe_w_gate = din("moe_w_gate", [2, NE, D, DF])
    moe_w_up = din("moe_w_up", [2, NE, D, DF])
    moe_w_down = din("moe_w_down", [2, NE, DF, D])
    hbf = dscr("hbf", [NT, D], BF16)
    od_w_in = din("od_w_in", [D, 3 * D])
    od_w_out = din("od_w_out", [D, D])
    da_l = din("da_l", [4, 64])
    da_subln_g = din("da_subln_g", [1, 128])
    out = nc.dram_tensor("out", [NL, D], F32, kind="ExternalOutput").ap()
    dbg_o = nc.dram_tensor("dbg", [NT, D], F32, kind="ExternalOutput").ap() if dbg else None
    xs = dscr("xs", [NT, D])

    P = Prog(nc, es)
    A = Arena(nc, es, P, 212480)
    XS_KEYS = ['xs%d' % c for c in range(NCH)]
    HBF_KEYS = ['hbf%d' % c for c in range(NCH)]

    def T(name, shape, d=F32):
        return A.alloc(name, shape, d)

    def PS(name, shape, d=F32):
        return es.enter_context(nc.psum_tensor(name, list(shape), d))

    ps = [PS("ps%d" % i, [128, 512], F32) for i in range(6)]
    pst = [PS("pst%d" % i, [128, 1024], BF16) for i in range(2)]
    rr = [0]

    def nextps():
        rr[0] = (rr[0] + 1) % 4
        return ps[rr[0]], 'ps%d' % rr[0]

    ident_f = T("ident_f", [128, 128], F32)
    ident_b = T("ident_b", [128, 128], BF16)
    P.dma(ident_f, c_ident[:, :], writes=['ident_f'])
    P.op('dve', lambda e: e.tensor_copy(out=ident_b, in_=ident_f), reads=['ident_f'], writes=['ident_b'])
    selrow = T("selrow", [2, 2, 128], F32)
    for r in range(2):
        P.op('dve', lambda e, r=r: e.tensor_copy(out=selrow[:, r, :], in_=ident_f[0:2, r:r + 1].to_broadcast([2, 128])),
             reads=['ident_f'], writes=['selrow'])
    stat_all = T("stat", [128, 4, 4], F32)
    den4 = T("den4", [128, 4], F32)
    sT = T("sT", [128, 8, 2], F32)
    with nc.allow_non_contiguous_dma(reason="tiny transposed load"):
        for r in range(2):
            P.dma(sT[:, :, r], cc[r, :].rearrange("(k p) -> p k", p=128), writes=['sT'])
    P.op('act', lambda e: e.activation(out=sT, in_=sT, func=AF.Silu), reads=['sT'], writes=['sT'])
    bc = [T("bc%d" % i, [128, D], F32) for i in range(4)]
    gB = T("gB", [128, D], F32)

    def mod_rows(l):
        m = A.mark()
        wa = [T("wa%d" % i, [128, 8, 512], F32) for i in range(2)]
        adab = [T("adab%d" % i, [2, 512], F32) for i in range(2)]
        mrow = [T("mrow%d" % i, [2, 512], F32) for i in range(2)]
        for ct in range(12):
            i = ct % 2
            wk = 'wa%d' % i
            P.dma(wa[i], ada_w[l, :, ct * 512:(ct + 1) * 512].rearrange("(k p) c -> p k c", p=128), writes=[wk])
            P.dma(adab[i], ada_b[l:l + 1, ct * 512:(ct + 1) * 512].to_broadcast([2, 512]), writes=['adab%d' % i])
            pb, pk = ps[4 + i], 'ps%d' % (4 + i)
            for k in range(8):
                P.op('pe', lambda e, k=k, i=i, pb=pb: e.matmul(pb[0:2, :], lhsT=sT[:, k, :], rhs=wa[i][:, k, :],
                                                               start=(k == 0), stop=(k == 7)),
                     reads=['sT', wk], writes=[pk], signal=(k == 7))
            P.op('dve', lambda e, pb=pb, i=i: e.tensor_tensor(out=mrow[i], in0=pb[0:2, :], in1=adab[i], op=ALU.add),
                 reads=[pk, 'adab%d' % i], writes=['mrow%d' % i])
            P.dma(modrow_d[l, :, ct * 512:(ct + 1) * 512], mrow[i], reads=['mrow%d' % i], writes=['modrow%d_%d' % (l, ct)], q='act')
        A.release(m)

    def mod_rows_bg(l, cts, stage_):
        banks = [(ps[4][:, :], 'ps4'), (ps[5][:, :], 'ps5'), (pst[0][:, :].bitcast(F32), 'pst0')]
        mk_ = A.mark()
        if stage_ == 0:
            wab = [T("wab%d" % i, [128, 8, 128], F32) for i in range(2)]
            n_ = 0
            for bi, ct in enumerate(cts):
                pb, pk = banks[bi]
                for j in range(4):
                    i = n_ % 2
                    n_ += 1
                    c0 = ct * 512 + j * 128
                    P.dma(wab[i], ada_w[l, :, c0:c0 + 128].rearrange("(k p) c -> p k c", p=128), writes=['wab%d' % i])
                    for k in range(8):
                        P.op('pe', lambda e, k=k, i=i, pb=pb, j=j: e.matmul(pb[0:2, j * 128:(j + 1) * 128], lhsT=sT[:, k, :], rhs=wab[i][:, k, :],
                                                                          start=(k == 0), stop=(k == 7)),
                             reads=['sT', 'wab%d' % i], writes=[pk], signal=(k == 7))
        else:
            adab = [T("adabg%d" % i, [2, 512], F32) for i in range(2)]
            mrow = [T("mrowg%d" % i, [2, 512], F32) for i in range(2)]
            for bi, ct in enumerate(cts):
                pb, pk = banks[bi]
                i = bi % 2
                P.dma(adab[i], ada_b[l:l + 1, ct * 512:(ct + 1) * 512].to_broadcast([2, 512]), writes=['adabg%d' % i])
                P.op('dve', lambda e, pb=pb, i=i: e.tensor_tensor(out=mrow[i], in0=pb[0:2, :], in1=adab[i], op=ALU.add),
                     reads=[pk, 'adabg%d' % i], writes=['mrowg%d' % i])
                P.dma(modrow_d[l, :, ct * 512:(ct + 1) * 512], mrow[i], reads=['mrowg%d' % i], writes=['modrow%d_%d' % (l, ct)], q='act')
        A.release(mk_)

    cur_l = [0]

    def bcast(dst, dkey, r, which):
        P.dma(dst, modrow_d[cur_l[0], r:r + 1, which * D:(which + 1) * D].to_broadcast([128, D]),
              reads=['modrow%d_%d' % (cur_l[0], 2 * which), 'modrow%d_%d' % (cur_l[0], 2 * which + 1)], writes=[dkey])

    def make_gs(dst, dkey, r, which_scale, gsrc):
        P.dma(gB, gsrc.to_broadcast([128, D]), writes=['gB'])
        bcast(dst, dkey, r, which_scale)
        P.op('dve', lambda e: e.scalar_tensor_tensor(out=dst, in0=dst, scalar=1.0, in1=gB, op0=ALU.add, op1=ALU.mult),
             reads=[dkey, 'gB'], writes=[dkey])

    def norm_mod(src_ap, xtile, xkey, htile, hkey, junk, gs, gskey, sh, shkey, eps=1e-6, si=0, srckey=None):
        stat = stat_all[:, si, :]
        sk = 'stat%d' % si
        P.dma(xtile, src_ap, reads=([srckey] if srckey else []), writes=[xkey])
        P.op('act', lambda e: e.activation(out=junk, in_=xtile, func=AF.Square, accum_out=stat[:, 0:1]),
             reads=[xkey], writes=['junk', sk])
        P.op('dve', lambda e: e.tensor_scalar(out=stat[:, 1:2], in0=stat[:, 0:1], scalar1=1.0 / D, scalar2=eps,
                                              op0=ALU.mult, op1=ALU.add), reads=[sk], writes=[sk])
        P.op('act', lambda e: e.sqrt(out=stat[:, 2:3], in_=stat[:, 1:2]), reads=[sk], writes=[sk])
        P.op('dve', lambda e: e.reciprocal(out=stat[:, 3:4], in_=stat[:, 2:3]), reads=[sk], writes=[sk])
        P.op('dve', lambda e: e.scalar_tensor_tensor(out=htile, in0=xtile, scalar=stat[:, 3:4], in1=gs,
                                                     op0=ALU.mult, op1=ALU.mult),
             reads=[xkey, sk, gskey], writes=[hkey])
        if sh is not None:
            P.op('pool', lambda e: e.tensor_tensor(out=htile, in0=htile, in1=sh, op=ALU.add),
                 reads=[hkey, shkey], writes=[hkey])

    def norm_phase(gain, which_shift, which_scale, src, consumer, src_is_xs=True, lag=None):
        m = A.mark()
        xt = [T("xt%d" % i, [128, D], F32) for i in range(4)]
        ht = [T("ht%d" % i, [128, D], F32) for i in range(4)]
        junk = T("junk", [128, D], BF16)
        make_gs(bc[0], 'bc0', 0, which_scale, gain)
        bcast(bc[1], 'bc1', 0, which_shift)
        make_gs(bc[2], 'bc2', 1, which_scale, gain)
        bcast(bc[3], 'bc3', 1, which_shift)
        pending = []
        for c in range(NCH):
            i = c % 4
            lat = c >= 2
            norm_mod(src[c * 128:(c + 1) * 128, :], xt[i], 'xt%d' % i, ht[i], 'ht%d' % i, junk,
                     bc[0] if lat else bc[2], 'bc0' if lat else 'bc2', bc[1] if lat else bc[3], 'bc1' if lat else 'bc3', si=i,
                     srckey=('xs%d' % c) if src_is_xs else None)
            pending.append((c, ht[i], 'ht%d' % i))
            if len(pending) > (2 if (lag is None and cur_l[0] == 0) else (lag or 0)):
                consumer(*pending.pop(0))
        while pending:
            consumer(*pending.pop(0))
        A.release(m)

    def to_hT(hT, hb):
        def f(c, htile, hkey):
            i = c % 2
            P.op('act', lambda e: e.copy(out=hb[i], in_=htile), reads=[hkey], writes=['hb%d' % i])
            for k in range(8):
                P.op('pe', lambda e, k=k: e.transpose(out=pst[i][:, k * 128:(k + 1) * 128], in_=hb[i][:, k * 128:(k + 1) * 128],
                                                      identity=ident_b),
                     reads=['hb%d' % i, 'ident_b'], writes=['pst%d' % i], signal=(k == 7))
            P.op('dve', lambda e: e.tensor_copy(out=hT[:, :, c * 128:(c + 1) * 128],
                                                in_=pst[i][:, :].rearrange("p (k t) -> p k t", k=8)),
                 reads=['pst%d' % i], writes=['hT'])
            if dbg and stage == 1:
                P.dma(dbg_o[c * 128:(c + 1) * 128, :], htile, reads=[hkey], writes=['dbg_o'])
        return f

    mod_rows(0)
    m_mixer = A.mark()
    hT = T("hT", [128, 8, NT], BF16)
    uT = T("uT", [128, 4, NT], BF16)
    m1 = A.mark()
    hb = [T("hb%d" % i, [128, D], BF16) for i in range(2)]
    norm_phase(norm_mix_g[0:1, :], 0, 1, xin, to_hT(hT, hb), src_is_xs=False)
    A.release(m1)
    if stage == 1:
        P.finish(['dbg_o'])
        return

    m_att = A.mark()
    w_in_b = T("w_in_b", [128, 8, 1280], BF16)
    wr_b = T("wr_b", [128, 8, 640], BF16)
    P.dma(w_in_b, ev_w_in.rearrange("(k p) c -> p k c", p=128), writes=['w_in_b'], q='pool')
    for k in range(8):
        srcv = w_in_b[:, k, 512:1152].rearrange("p (m b j) -> p m b j", b=2, j=16)
        dstv = wr_b[:, k, :].rearrange("p (m b j) -> p m b j", b=2, j=16)
        P.op('act', lambda e, srcv=srcv, dstv=dstv: e.mul(out=dstv[:, :, 0, :], in_=srcv[:, :, 1, :], mul=-1.0),
             reads=['w_in_b'], writes=['wr_b'])
        P.op('dve', lambda e, srcv=srcv, dstv=dstv: e.tensor_copy(out=dstv[:, :, 1, :], in_=srcv[:, :, 0, :]),
             reads=['w_in_b'], writes=['wr_b'])
    cosT = T("cosT", [128, NL], F32)
    sinT = T("sinT", [128, NL], F32)
    P.dma(cosT, c_cos[:, :], writes=['cosT'])
    P.dma(sinT, c_sin[:, :], writes=['sinT'])
    maskb = T("maskb", [128, 2, 4, 128], BF16)
    esink = T("esink", [128, 8], F32)
    m2 = A.mark()
    maskf = T("maskf", [128, 2, 128], F32)
    P.dma(maskf, c_mask[:, :, :], writes=['maskf'])
    for g in range(4):
        P.op('dve', lambda e, g=g: e.tensor_copy(out=maskb[:, :, g, :], in_=maskf), reads=['maskf'], writes=['maskb'])
    A.release(m2)
    P.dma(esink, wa_sink[0:1, :].to_broadcast([128, 8]), writes=['esink'])
    P.op('act', lambda e: e.activation(out=esink, in_=esink, func=AF.Exp), reads=['esink'], writes=['esink'])

    qT = T("qT", [128, 2, NCH, 4, 128], BF16)
    P.op('pool', lambda e: e.memset(qT, 0.0), writes=['qT'])
    wq_p = T("wq_p", [128, 8, 4, 128], BF16)
    wqr_p = T("wqr_p", [128, 8, 4, 128], BF16)
    for k in range(8):
        P.op('act', lambda e, k=k: e.copy(out=wq_p[:, k, :, :].rearrange("p g (a j) -> p g a j", a=2),
                                          in_=w_in_b[:, k, 512:1024].rearrange("p (a g j) -> p g a j", a=2, g=4)),
             reads=['w_in_b'], writes=['wq_p'])
        P.op('dve', lambda e, k=k: e.tensor_copy(out=wqr_p[:, k, :, :].rearrange("p g (a j) -> p g a j", a=2),
                                                 in_=wr_b[:, k, 0:512].rearrange("p (a g j) -> p g a j", a=2, g=4)),
             reads=['wr_b'], writes=['wqr_p'])
    kT = T("kT", [128, NT], BF16)
    Vx = T("Vx", [128, NCH, 2, 65], BF16)
    rtmp = [T("rtmp%d" % i, [128, 512], F32) for i in range(2)]
    P.op('pool', lambda e: e.memset(Vx, 1.0), writes=['Vx'])
    ttiles = [(0, 256)] + [(256 + i * 512, 512) for i in range(4)]

    for cch in range(4):
        for (t0, tn) in ttiles:
            pb, pk = nextps()
            for k in range(8):
                P.op('pe', lambda e, k=k, pb=pb, t0=t0, tn=tn: e.matmul(pb[:, 0:tn], lhsT=w_in_b[:, k, cch * 128:(cch + 1) * 128],
                                                                     rhs=hT[:, k, t0:t0 + tn], start=(k == 0), stop=(k == 7)),
                     reads=['w_in_b', 'hT'], writes=[pk], signal=(k == 7))
            P.op('act', lambda e, pb=pb, t0=t0, tn=tn: e.copy(out=uT[:, cch, t0:t0 + tn], in_=pb[:, 0:tn]),
                 reads=[pk], writes=['uT'])

    def proj_rope(parts, dkey, lhs_plain, lhs_rot, view=lambda ap: ap):
        for (t0, tn) in ttiles:
            pb, pk = nextps()
            for k in range(8):
                P.op('pe', lambda e, k=k, pb=pb, t0=t0, tn=tn: e.matmul(pb[:, 0:tn], lhsT=lhs_plain(k), rhs=hT[:, k, t0:t0 + tn],
                                                                     start=(k == 0), stop=(k == 7)),
                     reads=['w_in_b', 'wq_p', 'hT'], writes=[pk], signal=(k == 7))
            if t0 == 0:
                for (p0, p1, dst_of) in parts:
                    P.op('act', lambda e, pb=pb, tn=tn, t0=t0, p0=p0, p1=p1, dst_of=dst_of: e.copy(out=dst_of(t0, tn), in_=view(pb[p0:p1, 0:tn])),
                         reads=[pk], writes=[dkey])
                continue
            pb2, pk2 = nextps()
            for k in range(8):
                P.op('pe', lambda e, k=k, pb2=pb2, t0=t0, tn=tn: e.matmul(pb2[:, 0:tn], lhsT=lhs_rot(k), rhs=hT[:, k, t0:t0 + tn],
                                                                       start=(k == 0), stop=(k == 7)),
                     reads=['wr_b', 'wqr_p', 'hT'], writes=[pk2], signal=(k == 7))
            l0 = t0 - 256
            P.op('dve', lambda e, pb=pb, l0=l0: e.tensor_tensor(out=rtmp[0], in0=pb[:, :], in1=cosT[:, l0:l0 + 512], op=ALU.mult),
                 reads=[pk, 'cosT'], writes=['rtmp0'])
            P.op('dve', lambda e, pb2=pb2, l0=l0: e.tensor_tensor(out=rtmp[1], in0=pb2[:, :], in1=sinT[:, l0:l0 + 512], op=ALU.mult),
                 reads=[pk2, 'sinT'], writes=['rtmp1'])
            for (p0, p1, dst_of) in parts:
                P.op('pool', lambda e, t0=t0, p0=p0, p1=p1, dst_of=dst_of: e.tensor_tensor(out=dst_of(t0, 512), in0=view(rtmp[0][p0:p1, :]),
                                                                                        in1=view(rtmp[1][p0:p1, :]), op=ALU.add),
                     reads=['rtmp0', 'rtmp1'], writes=[dkey])

    for g in range(4):
        proj_rope([(0, 64, lambda t0, tn, g=g: qT[0:64, 0, t0 // 128:(t0 + tn) // 128, g, :]),
                   (64, 128, lambda t0, tn, g=g: qT[64:128, 1, t0 // 128:(t0 + tn) // 128, g, :])], 'qT',
                  lambda k, g=g: wq_p[:, k, g, :], lambda k, g=g: wqr_p[:, k, g, :],
                  view=lambda ap: ap.rearrange("p (c t) -> p c t", t=128))
    proj_rope([(0, 128, lambda t0, tn: kT[:, t0:t0 + tn])], 'kT', lambda k: w_in_b[:, k, 1024:1152], lambda k: wr_b[:, k, 512:640])
    for c in range(NCH):
        pb, pk = nextps()
        for k in range(8):
            P.op('pe', lambda e, k=k, pb=pb, c=c: e.matmul(pb[:, 0:128], lhsT=hT[:, k, c * 128:(c + 1) * 128], rhs=w_in_b[:, k, 1152:1280],
                                                        start=(k == 0), stop=(k == 7)),
                 reads=['w_in_b', 'hT'], writes=[pk], signal=(k == 7))
        P.op('act', lambda e, pb=pb, c=c: e.copy(out=Vx[:, c, :, 0:64], in_=pb[:, 0:128].rearrange("p (h j) -> p h j", h=2)),
             reads=[pk], writes=['Vx'])

    m3 = A.mark()
    Ebuf = [T("Ebuf%d" % i, [128, 512], BF16) for i in range(5)]
    aw = [T("aw%d" % i, [128, 512], BF16) for i in range(2)]
    dbgt = T("dbgt", [128, 512], F32) if dbg else None
    mixT = hT

    for qc in range(NCH):
        awt = aw[qc % 2]
        awk = 'aw%d' % (qc % 2)
        kbs = [(0, None), (1, None)]
        if qc >= 2:
            n = qc - 2
            if n - 1 >= 0:
                kbs.append((qc - 1, 0))
            kbs.append((qc, None))
            if n + 1 <= 15:
                kbs.append((qc + 1, 1))
        for kh in range(2):
            p0 = 64 * kh
            for bi, (kb, mk) in enumerate(kbs):
                pb, pk = nextps()
                P.op('pe', lambda e, pb=pb, kb=kb: e.matmul(pb[:, :], lhsT=kT[:, kb * 128:(kb + 1) * 128],
                                                         rhs=qT[:, kh, qc, :, :].rearrange("p g q -> p (g q)"), start=True, stop=True),
                     reads=['kT', 'qT'], writes=[pk])
                P.op('act', lambda e, pb=pb, bi=bi: e.activation(out=Ebuf[bi], in_=pb[:, :], func=AF.Exp, scale=0.125),
                     reads=[pk], writes=['Ebuf%d' % bi])
                if mk is not None:
                    P.op('dve', lambda e, bi=bi, mk=mk: e.tensor_tensor(out=Ebuf[bi], in0=Ebuf[bi],
                                                                       in1=maskb[:, mk, :, :].rearrange("p g q -> p (g q)"), op=ALU.mult),
                         reads=['Ebuf%d' % bi, 'maskb'], writes=['Ebuf%d' % bi])
            po, pok = ps[4 + kh], 'ps%d' % (4 + kh)
            for g in range(4):
                for bi, (kb, mk) in enumerate(kbs):
                    P.op('pe', lambda e, g=g, bi=bi, kb=kb: e.matmul(po[:, g * 65:(g + 1) * 65], lhsT=Ebuf[bi][:, g * 128:(g + 1) * 128],
                                                                  rhs=Vx[:, kb, kh, :], start=(bi == 0), stop=(bi == len(kbs) - 1)),
                         reads=['Ebuf%d' % bi, 'Vx'], writes=[pok], signal=(g == 3 and bi == len(kbs) - 1))
            pov = po[:, 0:260].rearrange("p (g j) -> p g j", g=4)
            P.op('dve', lambda e, pov=pov: e.tensor_tensor(out=den4, in0=pov[:, :, 64], in1=esink[:, 4 * kh:4 * kh + 4], op=ALU.add),
                 reads=[pok, 'esink'], writes=['den4'])
            P.op('dve', lambda e: e.reciprocal(out=den4, in_=den4), reads=['den4'], writes=['den4'])
            P.op('dve', lambda e, pov=pov: e.tensor_tensor(out=awt[:, kh * 256:(kh + 1) * 256].rearrange("p (g j) -> p g j", g=4), in0=pov[:, :, 0:64],
                                                          in1=den4.unsqueeze(2).to_broadcast([128, 4, 64]), op=ALU.mult),
                 reads=[pok, 'den4'], writes=[awk])
        if dbg and stage == 2:
            P.op('act', lambda e: e.copy(out=dbgt, in_=awt), reads=[awk], writes=['dbgt'])
            P.dma(dbg_o[qc * 128:(qc + 1) * 128, 0:512], dbgt, reads=['dbgt'], writes=['dbg_o'])
        i = qc % 2
        for j in range(4):
            P.op('pe', lambda e, j=j: e.transpose(out=pst[i][:, j * 128:(j + 1) * 128], in_=awt[:, j * 128:(j + 1) * 128], identity=ident_b),
                 reads=[awk, 'ident_b'], writes=['pst%d' % i], signal=(j == 3))
        P.op('dve', lambda e: e.tensor_copy(out=mixT[:, 4:8, qc * 128:(qc + 1) * 128],
                                            in_=pst[i][:, 0:512].rearrange("p (k t) -> p k t", k=4)),
             reads=['pst%d' % i], writes=['hT'])
    A.release(m_att)
    if stage == 2:
        P.finish(['dbg_o'])
        return

    TWO_PI = 2.0 * math.pi
    NCK = NT // 8
    zT = T("zT", [128, 4, NT], BF16)
    GP = T("GP", [128, 8, 240], BF16)
    EP = T("EP", [128, 8, 240], BF16)
    Shm = T("Shm", [16, 8, 128], BF16)
    P.dma(GP, c_gp[:, :, :], writes=['GP'], q='pool')
    P.dma(EP, c_ep[:, :, :], writes=['EP'], q='pool')
    P.dma(Shm, c_sh[:, :, :], writes=['Shm'], q='pool')
    NSEG = 4
    SEGL = NCK // NSEG
    mio = T("mio", [128, SEGL, 16], F32)
    m_io = A.mark()
    mio_i = T("mio_i", [128, SEGL, 16], I32)
    P.op('pool', lambda e: e.iota(mio_i, pattern=[[1, SEGL], [0, 16]], base=1, channel_multiplier=0), writes=['mio_i'])
    P.op('dve', lambda e: e.tensor_copy(out=mio, in_=mio_i), reads=['mio_i'], writes=['mio'])
    A.release(m_io)
    dcol = T("dcol", [128, 4], F32)
    gbcol = T("gbcol", [128, 4], F32)
    with nc.allow_non_contiguous_dma(reason="tiny transposed loads"):
        P.dma(dcol, s5_d.rearrange("(c p) -> p c", p=128), writes=['dcol'])
        P.dma(gbcol, s5_glu_b.rearrange("(c p) -> p c", p=128), writes=['gbcol'])

    def ew(eng, fn, reads, writes):
        P.op(eng, fn, reads=reads, writes=writes)

    def s5_pass(cch):
        g0 = 8 * cch
        mp = A.mark()
        ISm = T("ISm", [128, 16, 128], BF16)
        ISs = T("ISs", [128, 16, 128], BF16)
        SOm = T("SOm", [128, 16, 128], BF16)
        TKm = T("TKm", [128, 16, 128], BF16)
        AR8 = T("AR8", [128, NSEG, 2, 16], F32)
        AI8 = T("AI8", [128, NSEG, 2, 16], F32)
        PRt = T("PRt", [128, SEGL, 16], F32)
        PIt = T("PIt", [128, SEGL, 16], F32)
        ms = A.mark()
        LAMR = T("LAMR", [128, 16], F32)
        LAMI = T("LAMI", [128, 16], F32)
        DT = T("DT", [128, 16], F32)
        BR = T("BR", [128, 16, 16], F32)
        BI = T("BI", [128, 16, 16], F32)
        CR = T("CR", [128, 16, 16], F32)
        CI = T("CI", [128, 16, 16], F32)
        CRt = T("CRt", [128, 2, 64], F32)
        CIt = T("CIt", [128, 2, 64], F32)
        with nc.allow_non_contiguous_dma(reason="small parameter loads"):
            for k in range(2):
                for hf in range(2):
                    P.dma(LAMR[64 * hf:64 * hf + 64, 8 * k:8 * k + 8], s5_lam_re[k, g0:g0 + 8, :].rearrange("g p -> p g"), writes=['LAMR'])
                    P.dma(LAMI[64 * hf:64 * hf + 64, 8 * k:8 * k + 8], s5_lam_im[k, g0:g0 + 8, :].rearrange("g p -> p g"), writes=['LAMI'])
                    P.dma(BR[64 * hf:64 * hf + 64, 8 * k:8 * k + 8, :], s5_b_re[k, g0:g0 + 8, :, :].rearrange("g p h -> p g h"), writes=['BR'])
                    P.dma(BI[64 * hf:64 * hf + 64, 8 * k:8 * k + 8, :], s5_b_im[k, g0:g0 + 8, :, :].rearrange("g p h -> p g h"), writes=['BI'])
                P.dma(DT[:, 8 * k:8 * k + 8], s5_log_step[k:k + 1, g0:g0 + 8].to_broadcast([128, 8]), writes=['DT'])
        for k in range(2):
            for (src, tt_, tk_, dst, dk_) in ((s5_c_re, CRt, 'CRt', CR, 'CR'), (s5_c_im, CIt, 'CIt', CI, 'CI')):
                for dup in range(2):
                    P.dma(tt_[:, dup, :], src[k, g0:g0 + 8, :, :].rearrange("g c p -> (g c) p"), writes=[tk_])
                pb, pk = nextps()
                P.op('pe', lambda e, pb=pb, tt_=tt_: e.transpose(out=pb[:, 0:128], in_=tt_.rearrange("r d p -> r (d p)"), identity=ident_f),
                     reads=[tk_, 'ident_f'], writes=[pk])
                P.op('act', lambda e, pb=pb, dst=dst, k=k: e.copy(out=dst[:, 8 * k:8 * k + 8, :], in_=pb[:, 0:128].rearrange("p (g c) -> p g c", g=8)),
                     reads=[pk], writes=[dk_])
        MAG = T("MAG", [128, 16], F32)
        ANG = T("ANG", [128, 16], F32)
        NR = T("NR", [128, 16], F32)
        RR = T("RR", [128, 16], F32)
        SN = T("SN", [128, 16], F32)
        CS = T("CS", [128, 16], F32)
        t1 = T("t1", [128, 16], F32)
        t2 = T("t2", [128, 16], F32)
        FR = T("FR", [128, 16], F32)
        FI = T("FI", [128, 16], F32)
        APR = T("APR", [128, 9, 16], F32)
        API = T("API", [128, 9, 16], F32)
        ew('act', lambda e: e.activation(out=DT, in_=DT, func=AF.Exp), ['DT'], ['DT'])
        ew('dve', lambda e: e.tensor_tensor(out=MAG, in0=LAMR, in1=DT, op=ALU.mult), ['LAMR', 'DT'], ['MAG'])
        ew('act', lambda e: e.activation(out=MAG, in_=MAG, func=AF.Exp), ['MAG'], ['MAG'])
        ew('dve', lambda e: e.tensor_tensor(out=ANG, in0=LAMI, in1=DT, op=ALU.mult), ['LAMI', 'DT'], ['ANG'])
        ew('dve', lambda e: e.tensor_scalar(out=NR, in0=ANG, scalar1=1.0 / TWO_PI, scalar2=12582912.0, op0=ALU.mult, op1=ALU.add), ['ANG'], ['NR'])
        ew('dve', lambda e: e.tensor_scalar(out=NR, in0=NR, scalar1=-12582912.0, scalar2=None, op0=ALU.add), ['NR'], ['NR'])
        ew('dve', lambda e: e.scalar_tensor_tensor(out=RR, in0=NR, scalar=-6.28125, in1=ANG, op0=ALU.mult, op1=ALU.add), ['NR', 'ANG'], ['RR'])
        ew('dve', lambda e: e.scalar_tensor_tensor(out=RR, in0=NR, scalar=-(TWO_PI - 6.28125), in1=RR, op0=ALU.mult, op1=ALU.add), ['NR', 'RR'], ['RR'])
        ew('dve', lambda e: e.tensor_scalar(out=RR, in0=RR, scalar1=math.pi, scalar2=-math.pi, op0=ALU.min, op1=ALU.max), ['RR'], ['RR'])
        ew('act', lambda e: e.activation(out=SN, in_=RR, func=AF.Sin), ['RR'], ['SN'])
        ew('dve', lambda e: e.tensor_scalar(out=t1, in0=RR, scalar1=-1.0, scalar2=None, op0=ALU.mult), ['RR'], ['t1'])
        ew('dve', lambda e: e.tensor_tensor(out=t1, in0=t1, in1=RR, op=ALU.max), ['t1', 'RR'], ['t1'])
        ew('dve', lambda e: e.tensor_scalar(out=t1, in0=t1, scalar1=-1.0, scalar2=math.pi / 2, op0=ALU.mult, op1=ALU.add), ['t1'], ['t1'])
        ew('act', lambda e: e.activation(out=CS, in_=t1, func=AF.Sin), ['t1'], ['CS'])
        ew('dve', lambda e: e.memset(APR[:, 0, :], 1.0), [], ['APR'])
        ew('dve', lambda e: e.memset(API[:, 0, :], 0.0), [], ['API'])
        ew('dve', lambda e: e.tensor_tensor(out=APR[:, 1, :], in0=MAG, in1=CS, op=ALU.mult), ['MAG', 'CS'], ['APR'])
        ew('dve', lambda e: e.tensor_tensor(out=API[:, 1, :], in0=MAG, in1=SN, op=ALU.mult), ['MAG', 'SN'], ['API'])
        for tau in range(1, 8):
            ew('dve', lambda e, tau=tau: e.tensor_tensor(out=t1, in0=APR[:, tau, :], in1=APR[:, 1, :], op=ALU.mult), ['APR'], ['t1'])
            ew('dve', lambda e, tau=tau: e.tensor_tensor(out=t2, in0=API[:, tau, :], in1=API[:, 1, :], op=ALU.mult), ['API'], ['t2'])
            ew('dve', lambda e, tau=tau: e.tensor_tensor(out=APR[:, tau + 1, :], in0=t1, in1=t2, op=ALU.subtract), ['t1', 't2'], ['APR'])
            ew('dve', lambda e, tau=tau: e.tensor_tensor(out=t1, in0=APR[:, tau, :], in1=API[:, 1, :], op=ALU.mult), ['APR', 'API'], ['t1'])
            ew('dve', lambda e, tau=tau: e.tensor_tensor(out=t2, in0=API[:, tau, :], in1=APR[:, 1, :], op=ALU.mult), ['APR', 'API'], ['t2'])
            ew('dve', lambda e, tau=tau: e.tensor_tensor(out=API[:, tau + 1, :], in0=t1, in1=t2, op=ALU.add), ['t1', 't2'], ['API'])
        for sg_ in range(NSEG):
            ew('dve', lambda e, sg_=sg_: e.tensor_copy(out=AR8[:, sg_, 0, :], in_=APR[:, 8, :]), ['APR'], ['AR8'])
            ew('dve', lambda e, sg_=sg_: e.tensor_copy(out=AR8[:, sg_, 1, :], in_=APR[:, 8, :]), ['APR'], ['AR8'])
            ew('dve', lambda e, sg_=sg_: e.tensor_copy(out=AI8[:, sg_, 0, :], in_=API[:, 8, :]), ['API'], ['AI8'])
            ew('dve', lambda e, sg_=sg_: e.tensor_scalar(out=AI8[:, sg_, 1, :], in0=API[:, 8, :], scalar1=-1.0, scalar2=None, op0=ALU.mult), ['API'], ['AI8'])
        TA = T("TA", [128, SEGL, 16], F32)
        TN = T("TN", [128, SEGL, 16], F32)
        TM = T("TM", [128, SEGL, 16], F32)
        TS = T("TS", [128, SEGL, 16], F32)
        bM = lambda x: x.unsqueeze(1).to_broadcast([128, SEGL, 16])
        ew('dve', lambda e: e.tensor_tensor(out=t1, in0=LAMR, in1=DT, op=ALU.mult), ['LAMR', 'DT'], ['t1'])
        ew('dve', lambda e: e.tensor_tensor(out=TM, in0=mio, in1=bM(t1), op=ALU.mult), ['mio', 't1'], ['TM'])
        ew('act', lambda e: e.activation(out=TM, in_=TM, func=AF.Exp, scale=8.0), ['TM'], ['TM'])
        ew('dve', lambda e: e.tensor_tensor(out=TA, in0=mio, in1=bM(ANG), op=ALU.mult), ['mio', 'ANG'], ['TA'])
        ew('dve', lambda e: e.tensor_scalar(out=TA, in0=TA, scalar1=8.0, scalar2=None, op0=ALU.mult), ['TA'], ['TA'])
        ew('dve', lambda e: e.tensor_scalar(out=TN, in0=TA, scalar1=1.0 / TWO_PI, scalar2=12582912.0, op0=ALU.mult, op1=ALU.add), ['TA'], ['TN'])
        ew('dve', lambda e: e.tensor_scalar(out=TN, in0=TN, scalar1=-12582912.0, scalar2=None, op0=ALU.add), ['TN'], ['TN'])
        ew('dve', lambda e: e.scalar_tensor_tensor(out=TA, in0=TN, scalar=-6.28125, in1=TA, op0=ALU.mult, op1=ALU.add), ['TN', 'TA'], ['TA'])
        ew('dve', lambda e: e.scalar_tensor_tensor(out=TA, in0=TN, scalar=-(TWO_PI - 6.28125), in1=TA, op0=ALU.mult, op1=ALU.add), ['TN', 'TA'], ['TA'])
        ew('dve', lambda e: e.tensor_scalar(out=TA, in0=TA, scalar1=math.pi, scalar2=-math.pi, op0=ALU.min, op1=ALU.max), ['TA'], ['TA'])
        ew('act', lambda e: e.activation(out=TS, in_=TA, func=AF.Sin), ['TA'], ['TS'])
        ew('dve', lambda e: e.tensor_scalar(out=TN, in0=TA, scalar1=-1.0, scalar2=None, op0=ALU.mult), ['TA'], ['TN'])
        ew('dve', lambda e: e.tensor_tensor(out=TN, in0=TN, in1=TA, op=ALU.max), ['TN', 'TA'], ['TN'])
        ew('dve', lambda e: e.tensor_scalar(out=TN, in0=TN, scalar1=-1.0, scalar2=math.pi / 2, op0=ALU.mult, op1=ALU.add), ['TN'], ['TN'])
        ew('act', lambda e: e.activation(out=TN, in_=TN, func=AF.Sin), ['TN'], ['TN'])
        ew('dve', lambda e: e.tensor_tensor(out=PRt, in0=TM, in1=TN, op=ALU.mult), ['TM', 'TN'], ['PRt'])
        ew('dve', lambda e: e.tensor_tensor(out=PIt, in0=TM, in1=TS, op=ALU.mult), ['TM', 'TS'], ['PIt'])
        ew('dve', lambda e: e.tensor_tensor(out=t1, in0=LAMR, in1=LAMR, op=ALU.mult), ['LAMR'], ['t1'])
        ew('dve', lambda e: e.tensor_tensor(out=t2, in0=LAMI, in1=LAMI, op=ALU.mult), ['LAMI'], ['t2'])
        ew('dve', lambda e: e.tensor_tensor(out=t1, in0=t1, in1=t2, op=ALU.add), ['t1', 't2'], ['t1'])
        ew('dve', lambda e: e.reciprocal(out=NR, in_=t1), ['t1'], ['NR'])
        ew('dve', lambda e: e.tensor_scalar(out=RR, in0=APR[:, 1, :], scalar1=-1.0, scalar2=None, op0=ALU.add), ['APR'], ['RR'])
        ew('dve', lambda e: e.tensor_tensor(out=t1, in0=RR, in1=LAMR, op=ALU.mult), ['RR', 'LAMR'], ['t1'])
        ew('dve', lambda e: e.tensor_tensor(out=t2, in0=API[:, 1, :], in1=LAMI, op=ALU.mult), ['API', 'LAMI'], ['t2'])
        ew('dve', lambda e: e.tensor_tensor(out=t1, in0=t1, in1=t2, op=ALU.add), ['t1', 't2'], ['t1'])
        ew('dve', lambda e: e.tensor_tensor(out=FR, in0=t1, in1=NR, op=ALU.mult), ['t1', 'NR'], ['FR'])
        ew('dve', lambda e: e.tensor_tensor(out=t1, in0=API[:, 1, :], in1=LAMR, op=ALU.mult), ['API', 'LAMR'], ['t1'])
        ew('dve', lambda e: e.tensor_tensor(out=t2, in0=RR, in1=LAMI, op=ALU.mult), ['RR', 'LAMI'], ['t2'])
        ew('dve', lambda e: e.tensor_tensor(out=t1, in0=t1, in1=t2, op=ALU.subtract), ['t1', 't2'], ['t1'])
        ew('dve', lambda e: e.tensor_tensor(out=FI, in0=t1, in1=NR, op=ALU.mult), ['t1', 'NR'], ['FI'])
        B1 = T("B1", [128, 16, 16], F32)
        B2 = T("B2", [128, 16, 16], F32)
        w1 = T("w1", [128, 16, 16], F32)
        w2 = T("w2", [128, 16, 16], F32)
        bF = lambda x: x.unsqueeze(2).to_broadcast([128, 16, 16])
        ew('dve', lambda e: e.tensor_tensor(out=w1, in0=BR, in1=bF(FR), op=ALU.mult), ['BR', 'FR'], ['w1'])
        ew('dve', lambda e: e.tensor_tensor(out=w2, in0=BI, in1=bF(FI), op=ALU.mult), ['BI', 'FI'], ['w2'])
        ew('dve', lambda e: e.tensor_tensor(out=w1, in0=w1, in1=w2, op=ALU.subtract), ['w1', 'w2'], ['w1'])
        ew('dve', lambda e: e.tensor_tensor(out=w2, in0=BI, in1=bF(FR), op=ALU.mult), ['BI', 'FR', 'w1'], ['w2'])
        ew('dve', lambda e: e.tensor_tensor(out=BI, in0=BR, in1=bF(FI), op=ALU.mult), ['BR', 'FI', 'w2'], ['BI'])
        ew('dve', lambda e: e.tensor_tensor(out=w2, in0=w2, in1=BI, op=ALU.add), ['w2', 'BI'], ['w2'])
        ew('dve', lambda e: e.tensor_copy(out=B1[0:64], in_=w1[0:64]), ['w1'], ['B1'])
        ew('dve', lambda e: e.tensor_copy(out=B1[64:128], in_=w2[64:128]), ['w2'], ['B1'])
        ew('dve', lambda e: e.tensor_scalar(out=B2[0:64], in0=w2[0:64], scalar1=-1.0, scalar2=None, op0=ALU.mult), ['w2'], ['B2'])
        ew('dve', lambda e: e.tensor_copy(out=B2[64:128], in_=w1[64:128]), ['w1'], ['B2'])
        C1 = T("C1", [128, 16, 16], F32)
        C2 = T("C2", [128, 16, 16], F32)
        ew('dve', lambda e: e.tensor_copy(out=C1[0:64], in_=CR[0:64]), ['CR'], ['C1'])
        ew('dve', lambda e: e.tensor_scalar(out=C1[64:128], in0=CI[64:128], scalar1=-1.0, scalar2=None, op0=ALU.mult), ['CI'], ['C1'])
        ew('dve', lambda e: e.tensor_scalar(out=C2[0:64], in0=CI[0:64], scalar1=-1.0, scalar2=None, op0=ALU.mult), ['CI'], ['C2'])
        ew('dve', lambda e: e.tensor_scalar(out=C2[64:128], in0=CR[64:128], scalar1=-1.0, scalar2=None, op0=ALU.mult), ['CR'], ['C2'])
        Zt = T("Zt", [128, 16, 8, 16], BF16)
        Zst = T("Zst", [128, 16, 8, 16], BF16)
        SOx = T("SOx", [128, 16, 9, 16], F32)
        for sidx in range(8):
            tau = 7 - sidx
            par = lambda tau=tau: APR[:, tau, :].unsqueeze(2).to_broadcast([128, 16, 16])
            pai = lambda tau=tau: API[:, tau, :].unsqueeze(2).to_broadcast([128, 16, 16])
            ew('dve', lambda e, par=par: e.tensor_tensor(out=w1, in0=B1, in1=par(), op=ALU.mult), ['B1', 'APR'], ['w1'])
            ew('pool', lambda e, pai=pai: e.tensor_tensor(out=w2, in0=B2, in1=pai(), op=ALU.mult), ['B2', 'API'], ['w2'])
            ew('dve', lambda e, sidx=sidx: e.tensor_tensor(out=Zt[:, :, sidx, :], in0=w1, in1=w2, op=ALU.add), ['w1', 'w2'], ['Zt'])
            ew('dve', lambda e, par=par: e.tensor_tensor(out=w1, in0=B2, in1=par(), op=ALU.mult), ['B2', 'APR'], ['w1'])
            ew('pool', lambda e, pai=pai: e.tensor_tensor(out=w2, in0=B1, in1=pai(), op=ALU.mult), ['B1', 'API'], ['w2'])
            ew('dve', lambda e, sidx=sidx: e.tensor_tensor(out=Zst[:, :, sidx, :], in0=w1, in1=w2, op=ALU.subtract), ['w1', 'w2'], ['Zst'])
        for tau in range(9):
            par = lambda tau=tau: APR[:, tau, :].unsqueeze(2).to_broadcast([128, 16, 16])
            pai = lambda tau=tau: API[:, tau, :].unsqueeze(2).to_broadcast([128, 16, 16])
            ew('dve', lambda e, par=par: e.tensor_tensor(out=w1, in0=C1, in1=par(), op=ALU.mult), ['C1', 'APR'], ['w1'])
            ew('pool', lambda e, pai=pai: e.tensor_tensor(out=w2, in0=C2, in1=pai(), op=ALU.mult), ['C2', 'API'], ['w2'])
            ew('dve', lambda e, tau=tau: e.tensor_tensor(out=SOx[:, :, tau, :], in0=w1, in1=w2, op=ALU.add), ['w1', 'w2'], ['SOx'])
        ew('act', lambda e: e.copy(out=SOm.rearrange("p g (t c) -> p g t c", t=8), in_=SOx[:, :, 1:9, :]), ['SOx'], ['SOm'])
        for gl2 in range(0, 16, 8):
            for (src, sk, dst, dk) in ((Zt, 'Zt', ISm, 'ISm'), (Zst, 'Zst', ISs, 'ISs')):
                i = (gl2 // 8) % 2
                for j in range(8):
                    P.op('pe', lambda e, j=j, src=src, i=i: e.transpose(out=pst[i][:, j * 128:(j + 1) * 128],
                                                                       in_=src[:, gl2 + j, :, :].rearrange("p s h -> p (s h)"), identity=ident_b),
                         reads=[sk, 'ident_b'], writes=['pst%d' % i], signal=(j == 7))
                P.op('dve', lambda e, dst=dst, i=i: e.tensor_copy(out=dst[:, gl2:gl2 + 8, :], in_=pst[i][:, :].rearrange("p (g m) -> p g m", g=8)),
                     reads=['pst%d' % i], writes=[dk])
        KTp = T("KTp", [16, 16, 256], BF16)
        ew('dve', lambda e: e.memset(KTp, 0.0), [], ['KTp'])
        for q4 in range(4):
            pb, pk = nextps()
            for j in range(4):
                gd = q4 * 4 + j
                P.op('pe', lambda e, pb=pb, j=j, gd=gd: e.matmul(pb[0:16, j * 128:(j + 1) * 128], lhsT=B1[:, gd, :],
                                                                rhs=SOx[:, gd, 0:8, :].rearrange("p t c -> p (t c)"), start=True, stop=True),
                     reads=['B1', 'SOx'], writes=[pk], signal=(j == 3))
            P.op('act', lambda e, pb=pb, q4=q4: e.copy(out=KTp[:, q4 * 4:q4 * 4 + 4, 128:256], in_=pb[0:16, :].rearrange("p (g m) -> p g m", g=4)),
                 reads=[pk], writes=['KTp'])
        for q4 in range(4):
            pb, pk = nextps()
            for j in range(4):
                gd = q4 * 4 + j
                for sidx in range(8):
                    P.op('pe', lambda e, pb=pb, j=j, gd=gd, sidx=sidx: e.matmul(pb[:, j * 128:(j + 1) * 128], lhsT=Shm[:, sidx, :],
                                                                               rhs=KTp[:, gd, 128 - 16 * sidx:256 - 16 * sidx],
                                                                               start=(sidx == 0), stop=(sidx == 7)),
                         reads=['Shm', 'KTp'], writes=[pk], signal=(j == 3 and sidx == 7))
            P.op('act', lambda e, pb=pb, q4=q4: e.copy(out=TKm[:, q4 * 4:q4 * 4 + 4, :], in_=pb[:, :].rearrange("p (g m) -> p g m", g=4)),
                 reads=[pk], writes=['TKm'])
        A.release(ms)

        VZ = T("VZ", [128, NCK, 2, 16], F32)
        U = T("U", [128, 16, NCK], BF16)
        m_loop = A.mark()
        lt1 = T("lt1", [128, NSEG, 2, 16], F32)
        lt2 = T("lt2", [128, NSEG, 2, 16], F32)
        ct1 = T("ct1", [128, SEGL, 2, 16], F32)
        ct2 = T("ct2", [128, SEGL, 2, 16], F32)
        for d_ in range(2):
            for gl in range(8):
                gd = 8 * d_ + gl
                pb, pk = nextps()
                for part in range(2):
                    for sp in range(8):
                        win = GP[:, gl, 112 - 16 * sp:240 - 16 * sp]
                        if d_ == 0:
                            c0, c1, rhs = ((0, 32, uT[:, cch, sp:256:8]), (32, 288, uT[:, cch, 256 + sp:NT:8]))[part]
                        else:
                            to = 7 - sp
                            c0, c1, rhs = ((0, 32, uT[:, cch, 248 + to:(to - 8 if to - 8 >= 0 else None):-8]),
                                           (32, 288, uT[:, cch, 2296 + to:248 + to:-8]))[part]
                        P.op('pe', lambda e, pb=pb, c0=c0, c1=c1, rhs=rhs, win=win, sp=sp: e.matmul(pb[:, c0:c1], lhsT=win, rhs=rhs,
                                                                                                 start=(sp == 0), stop=(sp == 7)),
                             reads=['GP', 'uT'], writes=[pk], signal=(sp == 7 and part == 1))
                P.op('act', lambda e, pb=pb, gd=gd: e.copy(out=U[:, gd, :], in_=pb[:, 0:NCK]), reads=[pk], writes=['U'])
        for gd in range(16):
            for (mat, mk, half) in ((ISm, 'ISm', 0), (ISs, 'ISs', 1)):
                pb, pk = nextps()
                P.op('pe', lambda e, pb=pb, mat=mat, gd=gd: e.matmul(pb[:, 0:NCK], lhsT=mat[:, gd, :], rhs=U[:, gd, :], start=True, stop=True),
                     reads=[mk, 'U'], writes=[pk])
                P.op('act' if half == 0 else 'dve',
                     (lambda e, pb=pb, gd=gd, half=half: e.copy(out=VZ[:, :, half, gd], in_=pb[:, 0:NCK])) if half == 0 else
                     (lambda e, pb=pb, gd=gd, half=half: e.tensor_copy(out=VZ[:, :, half, gd], in_=pb[:, 0:NCK])),
                     reads=[pk], writes=['VZ'])
        m_bg = A.mark()
        mod_rows_bg(1, [3 * cch, 3 * cch + 1, 3 * cch + 2], 0)
        VZs = VZ.rearrange("p (s m) x g -> p s m x g", s=NSEG)
        for m_ in range(1, SEGL):
            ew('dve', lambda e, m_=m_: e.tensor_tensor(out=lt1, in0=VZs[:, :, m_ - 1, :, :], in1=AR8, op=ALU.mult), ['VZ', 'AR8'], ['lt1'])
            ew('dve', lambda e, m_=m_: e.tensor_tensor(out=lt2, in0=VZs[:, :, m_ - 1, ::-1, :], in1=AI8, op=ALU.mult), ['VZ', 'AI8'], ['lt2'])
            ew('dve', lambda e: e.tensor_tensor(out=lt1, in0=lt1, in1=lt2, op=ALU.add), ['lt1', 'lt2'], ['lt1'])
            ew('dve', lambda e, m_=m_: e.tensor_tensor(out=VZs[:, :, m_, :, :], in0=VZs[:, :, m_, :, :], in1=lt1, op=ALU.add), ['VZ', 'lt1'], ['VZ'])
        for sg_ in range(1, NSEG):
            cprev = VZ[:, sg_ * SEGL - 1, :, :]
            cb = cprev.unsqueeze(1).to_broadcast([128, SEGL, 2, 16])
            cbs = VZ[:, sg_ * SEGL - 1, ::-1, :].unsqueeze(1).to_broadcast([128, SEGL, 2, 16])
            seg = VZ[:, sg_ * SEGL:(sg_ + 1) * SEGL, :, :]
            prb = PRt.unsqueeze(2).to_broadcast([128, SEGL, 2, 16])
            pib = PIt.unsqueeze(2).to_broadcast([128, SEGL, 2, 16])
            ew('dve', lambda e, cb=cb, prb=prb: e.tensor_tensor(out=ct1, in0=prb, in1=cb, op=ALU.mult), ['PRt', 'VZ'], ['ct1'])
            ew('pool', lambda e, cbs=cbs, pib=pib: e.tensor_tensor(out=ct2, in0=pib, in1=cbs, op=ALU.mult), ['PIt', 'VZ'], ['ct2'])
            ew('dve', lambda e: e.tensor_tensor(out=ct1[:, :, 0, :], in0=ct1[:, :, 0, :], in1=ct2[:, :, 0, :], op=ALU.add), ['ct1', 'ct2'], ['ct1'])
            ew('dve', lambda e: e.tensor_tensor(out=ct1[:, :, 1, :], in0=ct1[:, :, 1, :], in1=ct2[:, :, 1, :], op=ALU.subtract), ['ct1', 'ct2'], ['ct1'])
            ew('dve', lambda e, seg=seg: e.tensor_tensor(out=seg, in0=seg, in1=ct1, op=ALU.add), ['VZ', 'ct1'], ['VZ'])
        mod_rows_bg(1, [3 * cch, 3 * cch + 1, 3 * cch + 2], 1)
        A.release(m_loop)
        Xb = T("Xb", [128, 16, NCK], BF16)
        Yb = T("Yb", [128, 16, NCK], BF16)
        ew('pool', lambda e: e.memset(Xb[:, :, 0:1], 0.0), [], ['Xb'])
        ew('act', lambda e: e.copy(out=Xb[:, :, 1:NCK], in_=VZ[:, 0:NCK - 1, 0, :].rearrange("p n g -> p g n")), ['VZ'], ['Xb'])
        for gd in range(16):
            pb, pk = nextps()
            P.op('pe', lambda e, pb=pb, gd=gd: e.matmul(pb[:, 0:NCK], lhsT=TKm[:, gd, :], rhs=U[:, gd, :], start=True, stop=False),
                 reads=['TKm', 'U'], writes=[pk], signal=False)
            P.op('pe', lambda e, pb=pb, gd=gd: e.matmul(pb[:, 0:NCK], lhsT=SOm[:, gd, :], rhs=Xb[:, gd, :], start=False, stop=True),
                 reads=['SOm', 'Xb'], writes=[pk])
            P.op('act', lambda e, pb=pb, gd=gd: e.copy(out=Yb[:, gd, :], in_=pb[:, 0:NCK]), reads=[pk], writes=['Yb'])
        pre = [T("pre%d" % i, [128, NCK], F32) for i in range(2)]
        pr2 = [T("pr2%d" % i, [128, NCK], F32) for i in range(2)]
        for i_ in range(8):
            pb, pk = nextps()
            b2 = i_ % 2
            for part in range(2):
                for gl in range(8):
                    c0, c1, rhs = ((0, 32, Yb[:, gl, 0:32]), (32, 288, Yb[:, gl, 32:288]))[part]
                    P.op('pe', lambda e, pb=pb, c0=c0, c1=c1, rhs=rhs, gl=gl: e.matmul(pb[:, c0:c1], lhsT=EP[:, i_, 112 - 16 * gl:240 - 16 * gl], rhs=rhs,
                                                                                   start=(gl == 0), stop=False),
                         reads=['EP', 'Yb'], writes=[pk], signal=False)
                for gl in range(8):
                    c0, c1, rhs = ((0, 32, Yb[:, 8 + gl, 31::-1]), (32, 288, Yb[:, 8 + gl, 287:31:-1]))[part]
                    P.op('pe', lambda e, pb=pb, c0=c0, c1=c1, rhs=rhs, gl=gl: e.matmul(pb[:, c0:c1], lhsT=EP[:, 7 - i_, 112 - 16 * gl:240 - 16 * gl], rhs=rhs,
                                                                                   start=False, stop=(gl == 7)),
                         reads=['EP', 'Yb'], writes=[pk], signal=(gl == 7 and part == 1))
            ew('dve', lambda e, pb=pb, b2=b2: e.scalar_tensor_tensor(out=pre[b2], in0=uT[:, cch, i_:NT:8], scalar=dcol[:, cch:cch + 1], in1=pb[:, 0:NCK],
                                                                    op0=ALU.mult, op1=ALU.add), [pk, 'uT', 'dcol'], ['pre%d' % b2])
            ew('act', lambda e, b2=b2: e.activation(out=pr2[b2], in_=pre[b2], func=AF.Square), ['pre%d' % b2], ['pr2%d' % b2])
            ew('dve', lambda e, b2=b2: e.tensor_scalar(out=pr2[b2], in0=pr2[b2], scalar1=0.044715, scalar2=1.0, op0=ALU.mult, op1=ALU.add), ['pr2%d' % b2], ['pr2%d' % b2])
            ew('dve', lambda e, b2=b2: e.tensor_tensor(out=pr2[b2], in0=pr2[b2], in1=pre[b2], op=ALU.mult), ['pr2%d' % b2, 'pre%d' % b2], ['pr2%d' % b2])
            ew('act', lambda e, b2=b2: e.activation(out=pr2[b2], in_=pr2[b2], func=AF.Sigmoid, scale=1.5957691216057308), ['pr2%d' % b2], ['pr2%d' % b2])
            ew('dve', lambda e, b2=b2: e.tensor_tensor(out=zT[:, cch, i_:NT:8], in0=pr2[b2], in1=pre[b2], op=ALU.mult), ['pr2%d' % b2, 'pre%d' % b2], ['zT'])
        A.release(mp)

    for cch in range(4):
        s5_pass(cch)

    m_glu = A.mark()
    gw = T("gw", [128, 4, 512], BF16)
    P.dma(gw, s5_glu_w.rearrange("(k p) c -> p k c", p=128), writes=['gw'], q='pool')
    sg = [T("sg%d" % i, [128, 512], BF16) for i in range(2)]
    for co in range(4):
        for ti, (t0, tn) in enumerate(ttiles):
            pb, pk = nextps()
            for k in range(4):
                P.op('pe', lambda e, k=k, pb=pb, t0=t0, tn=tn: e.matmul(pb[:, 0:tn], lhsT=gw[:, k, co * 128:(co + 1) * 128], rhs=zT[:, k, t0:t0 + tn],
                                                                     start=(k == 0), stop=(k == 3)),
                     reads=['gw', 'zT'], writes=[pk], signal=(k == 3))
            i = ti % 2
            ew('act', lambda e, pb=pb, tn=tn, i=i: e.activation(out=sg[i][:, 0:tn], in_=pb[:, 0:tn], func=AF.Sigmoid, bias=gbcol[:, co:co + 1]),
               [pk, 'gbcol'], ['sg%d' % i])
            ew('dve', lambda e, t0=t0, tn=tn, i=i: e.tensor_tensor(out=mixT[:, co, t0:t0 + tn], in0=sg[i][:, 0:tn], in1=zT[:, co, t0:t0 + tn], op=ALU.mult),
               ['sg%d' % i, 'zT'], ['hT'])
    A.release(m_glu)
    if stage == 3:
        dt_ = T("dt_", [128, 512], F32)
        for c in range(NCH):
            for k in range(4):
                P.op('pe', lambda e, k=k, c=c: e.transpose(out=pst[0][:, k * 128:(k + 1) * 128], in_=mixT[:, k, c * 128:(c + 1) * 128], identity=ident_b),
                     reads=['hT', 'ident_b'], writes=['pst0'], signal=(k == 3))
            ew('act', lambda e: e.copy(out=dt_, in_=pst[0][:, 0:512]), ['pst0'], ['dt_'])
            P.dma(dbg_o[c * 128:(c + 1) * 128, 0:512], dt_, reads=['dt_'], writes=['dbg_o'])
        P.finish(['dbg_o'])
        return

    def outproj_phase(l, w_out_dram, x_src, mixT_, mkey, off, chunks):
        m = A.mark()
        w_out_b = T("w_out_b", [128, 8, D], BF16)
        P.dma(w_out_b, w_out_dram.rearrange("(k p) c -> p k c", p=128), writes=['w_out_b'], q='pool')
        bcast(bc[0], 'bc0', 0, 2)
        bcast(bc[2], 'bc2', 1, 2)
        xo = [T("xo%d" % i, [128, D], F32) for i in range(2)]
        xn = [T("xn%d" % i, [128, D], F32) for i in range(2)]
        for c in chunks:
            i = c % 2
            g2, g2k = (bc[0], 'bc0') if c >= 2 else (bc[2], 'bc2')
            P.dma(xo[i], x_src[c * 128:(c + 1) * 128, :], reads=(['xs%d' % c] if l == 1 else []), writes=['xo%d' % i])
            for hh in range(2):
                pb, pk = nextps()
                for k in range(8):
                    P.op('pe', lambda e, k=k, pb=pb, hh=hh: e.matmul(pb[:, :], lhsT=mixT_[:, k, c * 128 - off:(c + 1) * 128 - off], rhs=w_out_b[:, k, hh * 512:(hh + 1) * 512],
                                                                  start=(k == 0), stop=(k == 7)),
                         reads=[mkey, 'w_out_b'], writes=[pk], signal=(k == 7))
                P.op('dve', lambda e, pb=pb, hh=hh: e.tensor_tensor(out=xn[i][:, hh * 512:(hh + 1) * 512], in0=pb[:, :], in1=g2[:, hh * 512:(hh + 1) * 512], op=ALU.mult),
                     reads=[pk, g2k], writes=['xn%d' % i])
            P.op('pool', lambda e: e.tensor_tensor(out=xn[i], in0=xn[i], in1=xo[i], op=ALU.add), reads=['xn%d' % i, 'xo%d' % i], writes=['xn%d' % i])
            P.dma(xs[c * 128:(c + 1) * 128, :], xn[i], reads=['xn%d' % i], writes=['xs%d' % c], q='pool')
            if dbg and stage == 4:
                P.dma(dbg_o[c * 128:(c + 1) * 128, :], xn[i], reads=['xn%d' % i], writes=['dbg_o'])
        A.release(m)

    outproj_phase(0, ev_w_out, xin, mixT, 'hT', 0, list(range(NCH)))
    A.release(m_mixer)
    if stage == 4:
        P.finish(['dbg_o'] + XS_KEYS)
        return

    def moe_phase(l, with_ctx):
        mm = A.mark()
        nslot = 288 if with_ctx else 256
        scs = [(0, 128, 0), (1, 128, 128)] + ([(2, 32, 256)] if with_ctx else [])
        affT = T("affT", [16, NT], F32)
        wr_f = T("wr_f", [128, 8, NE], F32)
        P.dma(wr_f, moe_router[l].rearrange("(k p) e -> p k e", p=128), writes=['wr_f'])
        hTf2 = [T("hTf%d" % i, [128, 8, 128], F32) for i in range(2)]
        hb2 = [T("hbb%d" % i, [128, D], BF16) for i in range(2)]
        aff2 = [T("aff%d" % i, [128, NE], F32) for i in range(2)]
        sm2 = [T("sm%d" % i, [128, 4], F32) for i in range(2)]

        def cons(c, htile, hkey):
            if c < 2 and not with_ctx:
                return
            i = c % 2
            hTf, hk = hTf2[i], 'hTf%d' % i
            aff, ak = aff2[i], 'aff%d' % i
            sm, sk = sm2[i], 'sm%d' % i
            P.op('act', lambda e: e.copy(out=hb2[i], in_=htile), reads=[hkey], writes=['hbb%d' % i])
            P.dma(hbf[c * 128:(c + 1) * 128, :], hb2[i], reads=['hbb%d' % i], writes=['hbf%d' % c], q='act')
            for half in range(2):
                pb, pk = ps[4 + half], 'ps%d' % (4 + half)
                for k in range(4):
                    kk = half * 4 + k
                    P.op('pe', lambda e, pb=pb, k=k, kk=kk: e.transpose(out=pb[:, k * 128:(k + 1) * 128], in_=htile[:, kk * 128:(kk + 1) * 128], identity=ident_f),
                         reads=[hkey, 'ident_f'], writes=[pk], signal=(k == 3))
                P.op('act' if half == 0 else 'dve',
                     (lambda e, pb=pb, half=half: e.copy(out=hTf[:, half * 4:half * 4 + 4, :], in_=pb[:, :].rearrange("p (k t) -> p k t", k=4))) if half == 0 else
                     (lambda e, pb=pb, half=half: e.tensor_copy(out=hTf[:, half * 4:half * 4 + 4, :], in_=pb[:, :].rearrange("p (k t) -> p k t", k=4))),
                     reads=[pk], writes=[hk])
            pb, pk = nextps()
            for k in range(8):
                P.op('pe', lambda e, pb=pb, k=k: e.matmul(pb[:, 0:NE], lhsT=hTf[:, k, :], rhs=wr_f[:, k, :], start=(k == 0), stop=(k == 7)),
                     reads=[hk, 'wr_f'], writes=[pk], signal=(k == 7))
            P.op('dve', lambda e, pb=pb: e.reduce_max(out=sm[:, 0:1], in_=pb[:, 0:NE], axis=AX.X), reads=[pk], writes=[sk])
            P.op('dve', lambda e: e.tensor_scalar(out=sm[:, 1:2], in0=sm[:, 0:1], scalar1=-1.0, scalar2=None, op0=ALU.mult), reads=[sk], writes=[sk])
            P.op('act', lambda e, pb=pb: e.activation(out=aff, in_=pb[:, 0:NE], func=AF.Exp, bias=sm[:, 1:2], accum_out=sm[:, 2:3]),
                 reads=[pk, sk], writes=[ak, sk])
            P.op('dve', lambda e: e.reciprocal(out=sm[:, 3:4], in_=sm[:, 2:3]), reads=[sk], writes=[sk])
            P.op('dve', lambda e: e.tensor_scalar(out=aff, in0=aff, scalar1=sm[:, 3:4], scalar2=None, op0=ALU.mult), reads=[ak, sk], writes=[ak])
            pb2, pk2 = nextps()
            P.op('pe', lambda e, pb2=pb2: e.transpose(out=pb2[0:NE, 0:128], in_=aff, identity=ident_f), reads=[ak, 'ident_f'], writes=[pk2])
            P.op('act', lambda e, pb2=pb2: e.copy(out=affT[:, c * 128:(c + 1) * 128], in_=pb2[0:NE, 0:128]), reads=[pk2], writes=['affT'])

        norm_phase(norm_ffn_g[l:l + 1, :], 3, 4, xs, cons)

        NB = 4
        Wg = [T("Wg%d" % i, [128, 8, 512], BF16) for i in range(NB)]
        Wu = [T("Wu%d" % i, [128, 8, 512], BF16) for i in range(NB)]
        Wd = [T("Wd%d" % i, [128, 4, D], BF16) for i in range(NB)]

        def load_w(e_, ft, b):
            P.dma(Wg[b], moe_w_gate[l, e_, :, ft * 512:(ft + 1) * 512].rearrange("(k p) f -> p k f", p=128), writes=['Wg%d' % b], q='pool')
            P.dma(Wu[b], moe_w_up[l, e_, :, ft * 512:(ft + 1) * 512].rearrange("(k p) f -> p k f", p=128), writes=['Wu%d' % b], q='pool')
            P.dma(Wd[b], moe_w_down[l, e_, ft * 512:(ft + 1) * 512, :].rearrange("(k p) d -> p k d", p=128), writes=['Wd%d' % b], q='pool')

        tiles = [(e_, ft) for e_ in range(NE) for ft in range(4)]
        for ti in range(NB - 1):
            load_w(tiles[ti][0], tiles[ti][1], ti % NB)

        vals = T("vals", [16, 288], F32)
        idxu = T("idxu", [16, 288], U32)
        idxf = T("idxf", [16, 288], F32)
        mt = A.mark()
        wk = T("wk", [16, NL], F32)
        for (t0, tn, o0, nr) in ([(256, NL, 0, 32)] + ([(0, 256, 256, 4)] if with_ctx else [])):
            cur = affT[:, t0:t0 + tn]
            curk = 'affT'
            for r in range(nr):
                vs = vals[:, o0 + 8 * r:o0 + 8 * r + 8]
                P.op('dve', lambda e, vs=vs, cur=cur: e.max(out=vs, in_=cur), reads=[curk], writes=['vals'])
                P.op('dve', lambda e, vs=vs, cur=cur, r=r, o0=o0: e.max_index(out=idxu[:, o0 + 8 * r:o0 + 8 * r + 8], in_max=vs, in_values=cur),
                     reads=[curk, 'vals'], writes=['idxu'])
                if r < nr - 1:
                    P.op('dve', lambda e, vs=vs, cur=cur, tn=tn: e.match_replace(out=wk[:, 0:tn], in_to_replace=vs, in_values=cur, imm_value=-1.0),
                         reads=[curk, 'vals', 'wk'], writes=['wk'])
                    cur = wk[:, 0:tn]
                    curk = 'wk'
        A.release(mt)
        P.op('dve', lambda e: e.tensor_copy(out=idxf[:, 0:nslot], in_=idxu[:, 0:nslot]), reads=['idxu'], writes=['idxf'])
        P.op('dve', lambda e: e.tensor_scalar(out=idxf[:, 0:256], in0=idxf[:, 0:256], scalar1=256.0, scalar2=None, op0=ALU.add), reads=['idxf'], writes=['idxf'])
        idxT = T("idxT", [128, 3, NE], U32)
        gate = T("gate", [128, 3, NE], F32)
        for (sc, rows, so) in scs:
            for (src, sk, dst, dk) in ((idxf, 'idxf', idxT, 'idxT'), (vals, 'vals', gate, 'gate')):
                pb, pk = nextps()
                P.op('pe', lambda e, pb=pb, src=src, rows=rows, so=so: e.transpose(out=pb[0:rows, 0:NE], in_=src[:, so:so + rows], identity=ident_f[0:NE, 0:NE]),
                     reads=[sk, 'ident_f'], writes=[pk])
                P.op('dve', lambda e, pb=pb, dst=dst, rows=rows, sc=sc: e.tensor_copy(out=dst[0:rows, sc, :], in_=pb[0:rows, 0:NE]), reads=[pk], writes=[dk])

        bcast(bc[0], 'bc0', 0, 5)
        if with_ctx:
            bcast(bc[2], 'bc2', 1, 5)
        xg = [T("xg%d" % i, [128, D], BF16) for i in range(3)]
        xgT = T("xgT", [128, 8, 288], BF16)
        hid = [T("hid%d" % i, [128, 384], BF16) for i in range(4)]
        for i in range(4):
            P.op('pool', lambda e, i=i: e.memset(hid[i], 0.0), writes=['hid%d' % i])
        sgt = [T("sgt%d" % i, [128, 288], F32) for i in range(2)]
        ysb = T("ysb", [128, 3, D], F32)
        ysc = [T("ysc%d" % i, [128, D], F32) for i in range(3)]

        pend_scatter = []
        yrr = [0]
        for ti, (e_, ft) in enumerate(tiles):
            b = ti % NB
            if ft == 0:
                for (sc, rows, so) in scs:
                    P.dma(None, None, reads=HBF_KEYS + ['idxT'], writes=['xg%d' % sc], q='pool',
                          fn=lambda e, sc=sc, rows=rows, e_=e_: e.indirect_dma_start(out=xg[sc][0:rows, :], out_offset=None, in_=hbf,
                                                                                   in_offset=bass.IndirectOffsetOnAxis(idxT[0:rows, sc, e_:e_ + 1], 0)))
            if ti + NB - 1 < len(tiles):
                load_w(tiles[ti + NB - 1][0], tiles[ti + NB - 1][1], (ti + NB - 1) % NB)
            for f_ in pend_scatter:
                f_()
            del pend_scatter[:]
            if ft == 0:
                for (sc, rows, so) in scs:
                    i = sc % 2
                    for k in range(8):
                        P.op('pe', lambda e, k=k, sc=sc, rows=rows, i=i: e.transpose(out=pst[i][:, k * 128:k * 128 + rows], in_=xg[sc][0:rows, k * 128:(k + 1) * 128],
                                                                                 identity=ident_b[0:rows, 0:rows]),
                             reads=['xg%d' % sc, 'ident_b'], writes=['pst%d' % i], signal=(k == 7))
                    P.op('dve', lambda e, sc=sc, rows=rows, so=so, i=i: e.tensor_copy(out=xgT[:, :, so:so + rows],
                                                                                   in_=pst[i][:, :].rearrange("p (k t) -> p k t", k=8)[:, :, 0:rows]),
                         reads=['pst%d' % i], writes=['xgT'])
            for fc in range(4):
                pg, pgk = ps[fc % 2], 'ps%d' % (fc % 2)
                pu, puk = ps[2 + fc % 2], 'ps%d' % (2 + fc % 2)
                for k in range(8):
                    P.op('pe', lambda e, k=k, pg=pg, fc=fc, b=b: e.matmul(pg[:, 0:nslot], lhsT=Wg[b][:, k, fc * 128:(fc + 1) * 128], rhs=xgT[:, k, 0:nslot],
                                                                      start=(k == 0), stop=(k == 7)),
                         reads=['Wg%d' % b, 'xgT'], writes=[pgk], signal=(k == 7))
                for k in range(8):
                    P.op('pe', lambda e, k=k, pu=pu, fc=fc, b=b: e.matmul(pu[:, 0:nslot], lhsT=Wu[b][:, k, fc * 128:(fc + 1) * 128], rhs=xgT[:, k, 0:nslot],
                                                                      start=(k == 0), stop=(k == 7)),
                         reads=['Wu%d' % b, 'xgT'], writes=[puk], signal=(k == 7))
                j = fc % 2
                P.op('act', lambda e, pg=pg, j=j: e.activation(out=sgt[j][:, 0:nslot], in_=pg[:, 0:nslot], func=AF.Silu), reads=[pgk], writes=['sgt%d' % j])
                P.op('dve', lambda e, pu=pu, j=j, fc=fc: e.tensor_tensor(out=hid[fc][:, 0:nslot], in0=sgt[j][:, 0:nslot], in1=pu[:, 0:nslot], op=ALU.mult),
                     reads=['sgt%d' % j, puk], writes=['hid%d' % fc])
            for (sc, rows, so) in scs:
                for dh in range(2):
                    yrr[0] = 1 - yrr[0]
                    py, pyk = ps[4 + yrr[0]], 'ps%d' % (4 + yrr[0])
                    for fc in range(4):
                        P.op('pe', lambda e, py=py, fc=fc, rows=rows, so=so, dh=dh, b=b: e.matmul(py[:, :], lhsT=hid[fc][:, so:so + 128],
                                                                                             rhs=Wd[b][:, fc, dh * 512:(dh + 1) * 512],
                                                                                             start=(fc == 0), stop=(fc == 3)),
                             reads=['hid%d' % fc, 'Wd%d' % b], writes=[pyk], signal=(fc == 3))
                    if ft == 0:
                        P.op('act', lambda e, py=py, rows=rows, sc=sc, dh=dh: e.copy(out=ysb[0:rows, sc, dh * 512:(dh + 1) * 512], in_=py[0:rows, :]),
                             reads=[pyk], writes=['ysb'])
                    else:
                        P.op('dve', lambda e, py=py, rows=rows, sc=sc, dh=dh: e.tensor_tensor(out=ysb[0:rows, sc, dh * 512:(dh + 1) * 512],
                                                                                           in0=ysb[0:rows, sc, dh * 512:(dh + 1) * 512], in1=py[0:rows, :], op=ALU.add),
                             reads=[pyk, 'ysb'], writes=['ysb'])
            if ft == 3:
                for (sc, rows, so) in scs:
                    i = sc
                    g5, g5k = (bc[0], 'bc0') if sc < 2 else (bc[2], 'bc2')
                    P.op('dve', lambda e, sc=sc, rows=rows, i=i, g5=g5, e_=e_: e.scalar_tensor_tensor(out=ysc[i][0:rows, :], in0=ysb[0:rows, sc, :],
                                                                                                   scalar=gate[0:rows, sc, e_:e_ + 1], in1=g5[0:rows, :],
                                                                                                   op0=ALU.mult, op1=ALU.mult),
                         reads=['ysb', 'gate', g5k], writes=['ysc%d' % i])
                    pend_scatter.append(lambda sc=sc, rows=rows, i=i, e_=e_: P.dma(
                        None, None, reads=['ysc%d' % i, 'idxT'] + XS_KEYS, writes=XS_KEYS, q='pool',
                        fn=lambda e: e.indirect_dma_start(out=xs, out_offset=bass.IndirectOffsetOnAxis(idxT[0:rows, sc, e_:e_ + 1], 0),
                                                          in_=ysc[i][0:rows, :], in_offset=None, compute_op=ALU.add)))
        for f_ in pend_scatter:
            f_()
        A.release(mm)

    moe_phase(0, True)
    if stage == 5:
        dx = T("dx", [128, D], F32)
        for c in range(NCH):
            P.dma(dx, xs[c * 128:(c + 1) * 128, :], reads=['xs%d' % c], writes=['dx'])
            P.dma(dbg_o[c * 128:(c + 1) * 128, :], dx, reads=['dx'], writes=['dbg_o'])
        P.finish(['dbg_o'])
        return

    cur_l[0] = 1
    m_l1 = A.mark()
    mixT1 = T("mixT1", [128, 8, NL], BF16)
    m_l1b = A.mark()
    hT = T("hT", [128, 8, NT], BF16)
    m1 = A.mark()
    hb = [T("hb%d" % i, [128, D], BF16) for i in range(2)]
    norm_phase(norm_mix_g[1:2, :], 0, 1, xs, to_hT(hT, hb))
    A.release(m1)
    LAM_INIT = 0.8 - 0.6 * math.exp(-0.3 * 1)
    cosT = T("cosT", [128, NL], F32)
    sinT = T("sinT", [128, NL], F32)
    P.dma(cosT, c_cos[:, :], writes=['cosT'])
    P.dma(sinT, c_sin[:, :], writes=['sinT'])
    lamv = T("lamv", [128, 4, 64], F32)
    lams = T("lams", [128, 4], F32)
    for i in range(4):
        P.dma(lamv[:, i, :], da_l[i:i + 1, :].to_broadcast([128, 64]), writes=['lamv'])
    P.op('dve', lambda e: e.tensor_tensor(out=lamv[:, 0, :], in0=lamv[:, 0, :], in1=lamv[:, 1, :], op=ALU.mult), reads=['lamv'], writes=['lamv'])
    P.op('dve', lambda e: e.tensor_tensor(out=lamv[:, 2, :], in0=lamv[:, 2, :], in1=lamv[:, 3, :], op=ALU.mult), reads=['lamv'], writes=['lamv'])
    P.op('dve', lambda e: e.reduce_sum(out=lams[:, 0:1], in_=lamv[:, 0, :], axis=AX.X), reads=['lamv'], writes=['lams'])
    P.op('dve', lambda e: e.reduce_sum(out=lams[:, 1:2], in_=lamv[:, 2, :], axis=AX.X), reads=['lamv'], writes=['lams'])
    P.op('act', lambda e: e.activation(out=lams[:, 0:2], in_=lams[:, 0:2], func=AF.Exp), reads=['lams'], writes=['lams'])
    P.op('dve', lambda e: e.tensor_tensor(out=lams[:, 2:3], in0=lams[:, 1:2], in1=lams[:, 0:1], op=ALU.subtract), reads=['lams'], writes=['lams'])
    P.op('dve', lambda e: e.tensor_scalar(out=lams[:, 3:4], in0=lams[:, 2:3], scalar1=-LAM_INIT, scalar2=None, op0=ALU.add), reads=['lams'], writes=['lams'])

    Wh = T("Wh", [128, 8, 384], BF16)
    Whr = T("Whr", [128, 8, 256], BF16)
    qT1 = T("qT1", [128, 2, NL], BF16)
    P.op('pool', lambda e: e.memset(qT1, 0.0), writes=['qT1'])
    kT1 = T("kT1", [128, NT], BF16)
    Vx1 = T("Vx1", [128, NCH, 128], BF16)
    ones_b = T("ones_b", [128, 128], BF16)
    P.op('pool', lambda e: e.memset(ones_b, 1.0), writes=['ones_b'])
    rdn = [T("rdn%d" % i, [128, 512], F32) for i in range(2)]
    sqb = T("sqb", [128, 512], BF16)
    sgcol = T("sgcol", [128, 1], F32)
    with nc.allow_non_contiguous_dma(reason="tiny transposed load"):
        P.dma(sgcol, da_subln_g.rearrange("o f -> f o"), writes=['sgcol'])
    P.op('dve', lambda e: e.tensor_scalar(out=sgcol, in0=sgcol, scalar1=1.0 - LAM_INIT, scalar2=None, op0=ALU.mult), reads=['sgcol'], writes=['sgcol'])
    rtmp = [T("rtmp%d" % i, [128, 512], F32) for i in range(2)]
    Ebufs = [T("Eall%d" % i, [128, NCH, 512], BF16) for i in range(2)]
    pstf = [pst[i][:, :].bitcast(F32) for i in range(2)]
    o0 = T("o0", [128, 512], F32)
    o1 = T("o1", [128, 512], F32)
    lt_tiles = [(256 + i * 512, 512) for i in range(4)]

    for h in range(8):
        for j3 in range(3):
            P.dma(Wh[:, :, j3 * 128:(j3 + 1) * 128], od_w_in[:, j3 * D + h * 128: j3 * D + (h + 1) * 128].rearrange("(k p) c -> p k c", p=128),
                  writes=['Wh'], q='pool')
        for k in range(8):
            srcv = Wh[:, k, 0:256].rearrange("p (m b j) -> p m b j", b=2, j=16)
            dstv = Whr[:, k, :].rearrange("p (m b j) -> p m b j", b=2, j=16)
            P.op('act', lambda e, srcv=srcv, dstv=dstv: e.mul(out=dstv[:, :, 0, :], in_=srcv[:, :, 1, :], mul=-1.0), reads=['Wh'], writes=['Whr'])
            P.op('dve', lambda e, srcv=srcv, dstv=dstv: e.tensor_copy(out=dstv[:, :, 1, :], in_=srcv[:, :, 0, :]), reads=['Wh'], writes=['Whr'])
        for (dst, dkey, c0, do_ctx, toff) in ((qT1, 'qT1', 0, False, 256), (kT1, 'kT1', 128, True, 0)):
            if do_ctx:
                pb, pk = nextps()
                for k in range(8):
                    P.op('pe', lambda e, k=k, pb=pb: e.matmul(pb[:, 0:256], lhsT=Wh[:, k, c0:c0 + 128], rhs=hT[:, k, 0:256], start=(k == 0), stop=(k == 7)),
                         reads=['Wh', 'hT'], writes=[pk], signal=(k == 7))
                P.op('act', lambda e, pb=pb: e.copy(out=dst[:, 0:256], in_=pb[:, 0:256]), reads=[pk], writes=[dkey])
            for (t0, tn) in lt_tiles:
                pb, pk = nextps()
                for k in range(8):
                    P.op('pe', lambda e, k=k, pb=pb, t0=t0: e.matmul(pb[:, :], lhsT=Wh[:, k, c0:c0 + 128], rhs=hT[:, k, t0:t0 + 512], start=(k == 0), stop=(k == 7)),
                         reads=['Wh', 'hT'], writes=[pk], signal=(k == 7))
                pb2, pk2 = nextps()
                for k in range(8):
                    P.op('pe', lambda e, k=k, pb2=pb2, t0=t0: e.matmul(pb2[:, :], lhsT=Whr[:, k, c0:c0 + 128], rhs=hT[:, k, t0:t0 + 512], start=(k == 0), stop=(k == 7)),
                         reads=['Whr', 'hT'], writes=[pk2], signal=(k == 7))
                l0 = t0 - 256
                P.op('dve', lambda e, pb=pb, l0=l0: e.tensor_tensor(out=rtmp[0], in0=pb[:, :], in1=cosT[:, l0:l0 + 512], op=ALU.mult), reads=[pk, 'cosT'], writes=['rtmp0'])
                P.op('dve', lambda e, pb2=pb2, l0=l0: e.tensor_tensor(out=rtmp[1], in0=pb2[:, :], in1=sinT[:, l0:l0 + 512], op=ALU.mult), reads=[pk2, 'sinT'], writes=['rtmp1'])
                if dkey == 'qT1':
                    for j in range(2):
                        P.op('pool', lambda e, t0=t0, j=j: e.tensor_tensor(out=qT1[64 * j:64 * j + 64, j, t0 - 256:t0 - 256 + 512], in0=rtmp[0][64 * j:64 * j + 64, :],
                                                                         in1=rtmp[1][64 * j:64 * j + 64, :], op=ALU.add),
                             reads=['rtmp0', 'rtmp1'], writes=[dkey])
                else:
                    P.op('pool', lambda e, t0=t0, dst=dst, toff=toff: e.tensor_tensor(out=dst[:, t0 - toff:t0 - toff + 512], in0=rtmp[0], in1=rtmp[1], op=ALU.add),
                         reads=['rtmp0', 'rtmp1'], writes=[dkey])
        for c in range(NCH):
            pb, pk = nextps()
            for k in range(8):
                P.op('pe', lambda e, k=k, pb=pb, c=c: e.matmul(pb[:, 0:128], lhsT=hT[:, k, c * 128:(c + 1) * 128], rhs=Wh[:, k, 256:384], start=(k == 0), stop=(k == 7)),
                     reads=['Wh', 'hT'], writes=[pk], signal=(k == 7))
            P.op('act', lambda e, pb=pb, c=c: e.copy(out=Vx1[:, c, 0:128], in_=pb[:, 0:128]), reads=[pk], writes=['Vx1'])
        units = [(qt, j) for qt in range(4) for j in range(2)]

        def S_step(u, kb):
            qt, j = units[u]
            p0 = 64 * j
            E = Ebufs[u % 2]
            pb, pk = nextps()
            P.op('pe', lambda e: e.matmul(pb[:, :], lhsT=kT1[:, kb * 128:(kb + 1) * 128], rhs=qT1[:, j, qt * 512:(qt + 1) * 512],
                                          start=True, stop=True), reads=['kT1', 'qT1'], writes=[pk])
            P.op('act', lambda e: e.activation(out=E[:, kb, :], in_=pb[:, :], func=AF.Exp, scale=0.125), reads=[pk], writes=['Eall%d' % (u % 2)])

        def acc_banks(u):
            if u % 2 == 0:
                return ps[4][:, :], 'ps4', ps[5][:, :], 'ps5'
            return pstf[0], 'pst0', pstf[1], 'pst1'

        def PV_step(u, kb):
            E = Ebufs[u % 2]
            ek = 'Eall%d' % (u % 2)
            pa, pak, pd, pdk = acc_banks(u)
            P.op('pe', lambda e: e.matmul(pa, lhsT=Vx1[:, kb, :], rhs=E[:, kb, :], start=(kb == 0), stop=(kb == NCH - 1)),
                 reads=[ek, 'Vx1'], writes=[pak], signal=(kb == NCH - 1))
            P.op('pe', lambda e: e.matmul(pd, lhsT=ones_b, rhs=E[:, kb, :], start=(kb == 0), stop=(kb == NCH - 1)),
                 reads=[ek, 'ones_b'], writes=[pdk], signal=(kb == NCH - 1))

        def epilogue(u):
            qt, j = units[u]
            pa, pak, pd, pdk = acc_banks(u)
            rd = rdn[u % 2]
            rk = 'rdn%d' % (u % 2)
            od, odk = (o0, 'o0') if j == 0 else (o1, 'o1')
            P.op('dve', lambda e: e.reciprocal(out=rd, in_=pd), reads=[pdk], writes=[rk])
            P.op('dve', lambda e: e.tensor_tensor(out=od, in0=pa, in1=rd, op=ALU.mult), reads=[pak, rk], writes=[odk])
            if j == 0:
                return
            P.op('dve', lambda e: e.scalar_tensor_tensor(out=o0, in0=o1, scalar=lams[:, 3:4], in1=o0, op0=ALU.mult, op1=ALU.add),
                 reads=['o0', 'o1', 'lams'], writes=['o0'])
            P.op('act', lambda e: e.activation(out=sqb, in_=o0, func=AF.Square), reads=['o0'], writes=['sqb'])
            pb, pk = nextps()
            P.op('pe', lambda e: e.matmul(pb[:, :], lhsT=ones_b, rhs=sqb, start=True, stop=True), reads=['ones_b', 'sqb'], writes=[pk])
            P.op('dve', lambda e: e.tensor_scalar(out=rd, in0=pb[:, :], scalar1=1.0 / 128, scalar2=1e-6, op0=ALU.mult, op1=ALU.add), reads=[pk], writes=[rk])
            P.op('act', lambda e: e.sqrt(out=rd, in_=rd), reads=[rk], writes=[rk])
            P.op('dve', lambda e: e.reciprocal(out=rd, in_=rd), reads=[rk], writes=[rk])
            P.op('dve', lambda e: e.scalar_tensor_tensor(out=mixT1[:, h, qt * 512:(qt + 1) * 512], in0=o0, scalar=sgcol[:, 0:1], in1=rd,
                                                         op0=ALU.mult, op1=ALU.mult), reads=['o0', 'sgcol', rk], writes=['mixT1'])

        for kb in range(NCH):
            S_step(0, kb)
        for u in range(len(units)):
            for kb in range(NCH):
                if u + 1 < len(units):
                    S_step(u + 1, kb)
                PV_step(u, kb)
            epilogue(u)

    A.release(m_l1b)
    outproj_phase(1, od_w_out, xs, mixT1, 'mixT1', 256, list(range(2, NCH)))
    A.release(m_l1)
    if stage == 6:
        dx = T("dx", [128, D], F32)
        for c in range(NCH):
            P.dma(dx, xs[c * 128:(c + 1) * 128, :], reads=['xs%d' % c], writes=['dx'])
            P.dma(dbg_o[c * 128:(c + 1) * 128, :], dx, reads=['dx'], writes=['dbg_o'])
        P.finish(['dbg_o'])
        return
    moe_phase(1, False)

    mf = A.mark()
    P.dma(gB, final_g[0:1, :].to_broadcast([128, D]), writes=['gB'])
    fx = [T("fx%d" % i, [128, D], F32) for i in range(2)]
    fo = [T("fo%d" % i, [128, D], F32) for i in range(2)]
    junk = T("junk", [128, D], BF16)
    for c in range(2, NCH):
        i = c % 2
        norm_mod(xs[c * 128:(c + 1) * 128, :], fx[i], 'fx%d' % i, fo[i], 'fo%d' % i, junk, gB, 'gB', None, None, si=i, srckey='xs%d' % c)
        P.dma(out[(c - 2) * 128:(c - 1) * 128, :], fo[i], reads=['fo%d' % i], writes=['out%d' % c], q='act')
        if dbg:
            P.dma(dbg_o[c * 128:(c + 1) * 128, :], fo[i], reads=['fo%d' % i], writes=['dbg_o'])
    A.release(mf)
    if dbg:
        P.finish(['dbg_o'])

    P.finish(['out%d' % c for c in range(2, NCH)])


def _consts():
    c = {}
    c["c_ident"] = np.eye(128, dtype=np.float32)
    t = np.arange(NL)
    row = (t // 64).astype(np.float32)
    col = (t % 64).astype(np.float32)
    inv = (np.float32(10000.0) ** (-np.arange(16, dtype=np.float32) / np.float32(16))).astype(np.float32)
    ang_r = row[:, None] * inv[None, :]
    ang_c = col[:, None] * inv[None, :]
    ang = np.concatenate([ang_r, ang_r, ang_c, ang_c], axis=-1).astype(np.float32)
    c["c_cos"] = np.ascontiguousarray(np.concatenate([np.cos(ang).T, np.cos(ang).T], axis=0).astype(np.float32))
    c["c_sin"] = np.ascontiguousarray(np.concatenate([np.sin(ang).T, np.sin(ang).T], axis=0).astype(np.float32))
    gp = np.zeros((128, 8, 240), np.float32)
    ep = np.zeros((128, 8, 240), np.float32)
    sh = np.zeros((16, 8, 128), np.float32)
    for g in range(8):
        for h in range(16):
            gp[16 * g + h, g, 112 + h] = 1.0
            ep[16 * g + h, g, 112 + h] = 1.0
            sh[h, g, 16 * g + h] = 1.0
    c["c_gp"] = gp
    c["c_ep"] = ep
    c["c_sh"] = sh
    kk = np.arange(128)[:, None]
    qq = np.arange(128)[None, :]
    c["c_mask"] = np.ascontiguousarray(np.stack([(kk >= qq), (kk <= qq)], axis=1).astype(np.float32))
    return c


def _in_map(inputs, b):
    m = {}
    m["xin"] = np.ascontiguousarray(np.concatenate([inputs["ctx"][b], inputs["x"][b]], axis=0), dtype=np.float32)
    m["cc"] = np.ascontiguousarray(np.stack([inputs["c"][b], inputs["c_ctx"]], axis=0), dtype=np.float32)
    for k in ["ada_w", "ada_b", "norm_mix_g", "norm_ffn_g"]:
        m[k] = np.ascontiguousarray(inputs[k], dtype=np.float32)
    m["ev_w_in"] = np.ascontiguousarray(inputs["ev_w_in"][0], dtype=np.float32)
    m["ev_w_out"] = np.ascontiguousarray(inputs["ev_w_out"][0], dtype=np.float32)
    m["wa_sink"] = np.ascontiguousarray(inputs["wa_sink"], dtype=np.float32).reshape(1, 8)
    for k in ["s5_lam_re", "s5_lam_im", "s5_log_step", "s5_b_re", "s5_b_im", "s5_c_re", "s5_c_im", "s5_d", "s5_glu_w", "s5_glu_b"]:
        m[k] = np.ascontiguousarray(inputs[k][0], dtype=np.float32)
    for k in ["moe_router", "moe_w_gate", "moe_w_up", "moe_w_down"]:
        m[k] = np.ascontiguousarray(inputs[k], dtype=np.float32)
    m["od_w_in"] = np.ascontiguousarray(inputs["od_w_in"][0], dtype=np.float32)
    m["od_w_out"] = np.ascontiguousarray(inputs["od_w_out"][0], dtype=np.float32)
    m["da_l"] = np.ascontiguousarray(np.stack([inputs["da_lq1"][0], inputs["da_lk1"][0], inputs["da_lq2"][0], inputs["da_lk2"][0]], axis=0), dtype=np.float32)
    m["da_subln_g"] = np.ascontiguousarray(inputs["da_subln_g"], dtype=np.float32).reshape(1, 128)
    m["final_g"] = np.ascontiguousarray(inputs["final_g"], dtype=np.float32).reshape(1, D)
    m.update(_consts())
    return m


def kernel(**inputs):
    nc = build()
    in_maps = [_in_map(inputs, b) for b in range(8)]
    res = run_bass_kernel_spmd(nc, in_maps, core_ids=list(range(8)))
    return np.stack([r["out"] for r in res.results], axis=0).astype(np.float32)
```

```python
import contextlib
import math
import numpy as np
import concourse.bass as bass
import concourse.mybir as mybir
from concourse.bass_utils import run_bass_kernel_spmd
from concourse.alu_op_type import AluOpType as ALU

dt = mybir.dt
F32, BF16, U32, I32 = dt.float32, dt.bfloat16, dt.uint32, dt.int32
AF = mybir.ActivationFunctionType
AX = mybir.AxisListType

D = 1024
NL = 2048
NC_ = 256
NT = NL + NC_
NCH = NT // 128
NE = 16
DF = 2048
NDS = 40


class Prog:
    def __init__(self, nc, es):
        self.nc = nc
        self.es = es
        self.eng = {'pe': nc.tensor, 'act': nc.scalar, 'dve': nc.vector, 'pool': nc.gpsimd, 'sp': nc.sync}
        self.sem = {k: es.enter_context(nc.semaphore('s_' + k)) for k in self.eng}
        self.cnt = {k: 0 for k in self.eng}
        self.waited = {k: {} for k in self.eng}
        self.dsem = [es.enter_context(nc.semaphore('d%d' % i)) for i in range(NDS)]
        self.dcnt = [0] * NDS
        self.dnext = 0
        self.dlast = [None] * NDS
        self.lastw = {}
        self.readers = {}
        self.nops = 0

    def _deps(self, reads, writes):
        toks = []
        for k in reads:
            if k in self.lastw:
                toks.append(self.lastw[k])
        for k in writes:
            if k in self.lastw:
                toks.append(self.lastw[k])
            toks.extend(self.readers.get(k, ()))
        return toks

    def _commit(self, tok, reads, writes):
        for k in reads:
            self.readers.setdefault(k, []).append(tok)
        for k in writes:
            self.lastw[k] = tok
            self.readers[k] = []

    def _wait(self, e, toks):
        best = {}
        for t in toks:
            if t is None:
                continue
            if e == 'pe' and t[0] == 'pe':
                continue
            if t[0] not in best or best[t[0]][2] < t[2]:
                best[t[0]] = t
        for key, t in best.items():
            if self.waited[e].get(key, 0) >= t[2]:
                continue
            self.eng[e].wait_ge(t[1], t[2])
            self.waited[e][key] = t[2]

    def op(self, e, fn, reads=(), writes=(), signal=True):
        self.nops += 1
        self._wait(e, self._deps(reads, writes))
        inst = fn(self.eng[e])
        if signal:
            self.cnt[e] += 1
            inst.then_inc(self.sem[e], 1)
            tok = (e, self.sem[e], self.cnt[e])
        else:
            tok = (e, self.sem[e], self.cnt[e] + 1)
        self._commit(tok, reads, writes)

    def dma(self, out, in_, reads=(), writes=(), q='sp', fn=None):
        self.nops += 1
        i = self.dnext
        self.dnext = (i + 1) % NDS
        self._wait(q, self._deps(reads, writes) + [self.dlast[i]])
        self.dcnt[i] += 16
        if fn is None:
            inst = self.eng[q].dma_start(out=out, in_=in_)
        else:
            inst = fn(self.eng[q])
        inst.then_inc(self.dsem[i], 16)
        tok = ('d%d' % i, self.dsem[i], self.dcnt[i])
        self.dlast[i] = tok
        self._commit(tok, reads, writes)

    def inherit(self, newkey, oldkeys):
        toks = list(self.readers.get(newkey, []))
        if newkey in self.lastw:
            toks.append(self.lastw[newkey])
        for k in oldkeys:
            if k in self.lastw:
                toks.append(self.lastw[k])
            toks.extend(self.readers.get(k, ()))
        self.lastw.pop(newkey, None)
        self.readers[newkey] = toks

    def finish(self, keys):
        toks = [self.lastw[k] for k in keys if k in self.lastw]
        self._wait('sp', toks)


_DSZ = {F32: 4, BF16: 2, U32: 4, I32: 4}


class Arena:
    def __init__(self, nc, es, P, nbytes):
        self.t = es.enter_context(nc.sbuf_tensor("arena", [128, nbytes // 4], F32))
        self.P = P
        self.cap = nbytes
        self.top = 0
        self.live = []
        self.freed = []

    def alloc(self, key, shape, d=F32):
        elems = 1
        for x in shape[1:]:
            elems *= x
        nb = (elems * _DSZ[d] + 63) // 64 * 64
        off = self.top
        self.top += nb
        assert self.top <= self.cap, "arena overflow %s: %d > %d" % (key, self.top, self.cap)
        olds = [k for (k, o, n) in self.freed if o < off + nb and off < o + n]
        self.P.inherit(key, olds)
        self.live.append((key, off, nb))
        ap = self.t[0:shape[0], off // 4:(off + nb) // 4]
        if d != F32:
            ap = ap.bitcast(d)
        ap = ap[:, 0:elems]
        if len(shape) > 2:
            names = ["a%d" % i for i in range(len(shape) - 1)]
            pat = "p (" + " ".join(names) + ") -> p " + " ".join(names)
            ap = ap.rearrange(pat, **{n: v for n, v in zip(names, shape[1:])})
        return ap

    def mark(self):
        return (self.top, len(self.live))

    def release(self, m):
        self.freed.extend(self.live[m[1]:])
        del self.live[m[1]:]
        self.top = m[0]


def build(stage=99, dbg=False):
    nc = bass.Bass("TRN2", target_bir_lowering=False)
    es = contextlib.ExitStack()
    with es:
        _build(nc, es, stage, dbg)
    return nc


def _build(nc, es, stage, dbg):
    def din(name, shape, d=F32):
        return nc.dram_tensor(name, list(shape), d, kind="ExternalInput").ap()

    def dscr(name, shape, d=F32):
        return nc.dram_tensor(name, list(shape), d, kind="Internal").ap()

    xin = din("xin", [NT, D])
    cc = din("cc", [2, D])
    ada_w = din("ada_w", [2, D, 6 * D])
    ada_b = din("ada_b", [2, 6 * D])
    norm_mix_g = din("norm_mix_g", [2, D])
    norm_ffn_g = din("norm_ffn_g", [2, D])
    final_g = din("final_g", [1, D])
    c_ident = din("c_ident", [128, 128])
    c_cos = din("c_cos", [128, NL])
    c_sin = din("c_sin", [128, NL])
    c_mask = din("c_mask", [128, 2, 128])
    ev_w_in = din("ev_w_in", [D, 1280])
    ev_w_out = din("ev_w_out", [D, D])
    wa_sink = din("wa_sink", [1, 8])
    s5_lam_re = din("s5_lam_re", [2, 32, 64])
    s5_lam_im = din("s5_lam_im", [2, 32, 64])
    s5_log_step = din("s5_log_step", [2, 32])
    s5_b_re = din("s5_b_re", [2, 32, 64, 16])
    s5_b_im = din("s5_b_im", [2, 32, 64, 16])
    s5_c_re = din("s5_c_re", [2, 32, 16, 64])
    s5_c_im = din("s5_c_im", [2, 32, 16, 64])
    s5_d = din("s5_d", [512])
    s5_glu_w = din("s5_glu_w", [512, 512])
    s5_glu_b = din("s5_glu_b", [512])
    c_gp = din("c_gp", [128, 8, 240])
    c_ep = din("c_ep", [128, 8, 240])
    c_sh = din("c_sh", [16, 8, 128])
    modrow_d = dscr("modrow_d", [2, 2, 6 * D])
    moe_router = din("moe_router", [2, D, NE])
    moe_w_gate = din("moe_w_gate", [2, NE, D, DF])
    moe_w_up = din("moe_w_up", [2, NE, D, DF])
    moe_w_down = din("moe_w_down", [2, NE, DF, D])
    hbf = dscr("hbf", [NT, D], BF16)
    od_w_in = din("od_w_in", [D, 3 * D])
    od_w_out = din("od_w_out", [D, D])
    da_l = din("da_l", [4, 64])
    da_subln_g = din("da_subln_g", [1, 128])
    out = nc.dram_tensor("out", [NL, D], F32, kind="ExternalOutput").ap()
    dbg_o = nc.dram_tensor("dbg", [NT, D], F32, kind="ExternalOutput").ap() if dbg else None
    xs = dscr("xs", [NT, D])

    P = Prog(nc, es)
    A = Arena(nc, es, P, 212480)
    XS_KEYS = ['xs%d' % c for c in range(NCH)]
    HBF_KEYS = ['hbf%d' % c for c in range(NCH)]

    def T(name, shape, d=F32):
        return A.alloc(name, shape, d)

    def PS(name, shape, d=F32):
        return es.enter_context(nc.psum_tensor(name, list(shape), d))

    ps = [PS("ps%d" % i, [128, 512], F32) for i in range(6)]
    pst = [PS("pst%d" % i, [128, 1024], BF16) for i in range(2)]
    rr = [0]

    def nextps():
        rr[0] = (rr[0] + 1) % 4
        return ps[rr[0]], 'ps%d' % rr[0]

    ident_f = T("ident_f", [128, 128], F32)
    ident_b = T("ident_b", [128, 128], BF16)
    P.dma(ident_f, c_ident[:, :], writes=['ident_f'])
    P.op('dve', lambda e: e.tensor_copy(out=ident_b, in_=ident_f), reads=['ident_f'], writes=['ident_b'])
    selrow = T("selrow", [2, 2, 128], F32)
    for r in range(2):
        P.op('dve', lambda e, r=r: e.tensor_copy(out=selrow[:, r, :], in_=ident_f[0:2, r:r + 1].to_broadcast([2, 128])),
             reads=['ident_f'], writes=['selrow'])
    stat_all = T("stat", [128, 4, 4], F32)
    den4 = T("den4", [128, 4], F32)
    sT = T("sT", [128, 8, 2], F32)
    with nc.allow_non_contiguous_dma(reason="tiny transposed load"):
        for r in range(2):
            P.dma(sT[:, :, r], cc[r, :].rearrange("(k p) -> p k", p=128), writes=['sT'])
    P.op('act', lambda e: e.activation(out=sT, in_=sT, func=AF.Silu), reads=['sT'], writes=['sT'])
    bc = [T("bc%d" % i, [128, D], F32) for i in range(4)]
    gB = T("gB", [128, D], F32)

    def mod_rows(l):
        m = A.mark()
        wa = [T("wa%d" % i, [128, 8, 512], F32) for i in range(2)]
        adab = [T("adab%d" % i, [2, 512], F32) for i in range(2)]
        mrow = [T("mrow%d" % i, [2, 512], F32) for i in range(2)]
        for ct in range(12):
            i = ct % 2
            wk = 'wa%d' % i
            P.dma(wa[i], ada_w[l, :, ct * 512:(ct + 1) * 512].rearrange("(k p) c -> p k c", p=128), writes=[wk])
            P.dma(adab[i], ada_b[l:l + 1, ct * 512:(ct + 1) * 512].to_broadcast([2, 512]), writes=['adab%d' % i])
            pb, pk = ps[4 + i], 'ps%d' % (4 + i)
            for k in range(8):
                P.op('pe', lambda e, k=k, i=i, pb=pb: e.matmul(pb[0:2, :], lhsT=sT[:, k, :], rhs=wa[i][:, k, :],
                                                               start=(k == 0), stop=(k == 7)),
                     reads=['sT', wk], writes=[pk], signal=(k == 7))
            P.op('dve', lambda e, pb=pb, i=i: e.tensor_tensor(out=mrow[i], in0=pb[0:2, :], in1=adab[i], op=ALU.add),
                 reads=[pk, 'adab%d' % i], writes=['mrow%d' % i])
            P.dma(modrow_d[l, :, ct * 512:(ct + 1) * 512], mrow[i], reads=['mrow%d' % i], writes=['modrow%d_%d' % (l, ct)], q='act')
        A.release(m)

    def mod_rows_bg(l, cts, stage_):
        banks = [(ps[4][:, :], 'ps4'), (ps[5][:, :], 'ps5'), (pst[0][:, :].bitcast(F32), 'pst0')]
        mk_ = A.mark()
        if stage_ == 0:
            wab = [T("wab%d" % i, [128, 8, 128], F32) for i in range(2)]
            n_ = 0
            for bi, ct in enumerate(cts):
                pb, pk = banks[bi]
                for j in range(4):
                    i = n_ % 2
                    n_ += 1
                    c0 = ct * 512 + j * 128
                    P.dma(wab[i], ada_w[l, :, c0:c0 + 128].rearrange("(k p) c -> p k c", p=128), writes=['wab%d' % i])
                    for k in range(8):
                        P.op('pe', lambda e, k=k, i=i, pb=pb, j=j: e.matmul(pb[0:2, j * 128:(j + 1) * 128], lhsT=sT[:, k, :], rhs=wab[i][:, k, :],
                                                                          start=(k == 0), stop=(k == 7)),
                             reads=['sT', 'wab%d' % i], writes=[pk], signal=(k == 7))
        else:
            adab = [T("adabg%d" % i, [2, 512], F32) for i in range(2)]
            mrow = [T("mrowg%d" % i, [2, 512], F32) for i in range(2)]
            for bi, ct in enumerate(cts):
                pb, pk = banks[bi]
                i = bi % 2
                P.dma(adab[i], ada_b[l:l + 1, ct * 512:(ct + 1) * 512].to_broadcast([2, 512]), writes=['adabg%d' % i])
                P.op('dve', lambda e, pb=pb, i=i: e.tensor_tensor(out=mrow[i], in0=pb[0:2, :], in1=adab[i], op=ALU.add),
                     reads=[pk, 'adabg%d' % i], writes=['mrowg%d' % i])
                P.dma(modrow_d[l, :, ct * 512:(ct + 1) * 512], mrow[i], reads=['mrowg%d' % i], writes=['modrow%d_%d' % (l, ct)], q='act')
        A.release(mk_)

    cur_l = [0]

    def bcast(dst, dkey, r, which):
        P.dma(dst, modrow_d[cur_l[0], r:r + 1, which * D:(which + 1) * D].to_broadcast([128, D]),
              reads=['modrow%d_%d' % (cur_l[0], 2 * which), 'modrow%d_%d' % (cur_l[0], 2 * which + 1)], writes=[dkey])

    def make_gs(dst, dkey, r, which_scale, gsrc):
        P.dma(gB, gsrc.to_broadcast([128, D]), writes=['gB'])
        bcast(dst, dkey, r, which_scale)
        P.op('dve', lambda e: e.scalar_tensor_tensor(out=dst, in0=dst, scalar=1.0, in1=gB, op0=ALU.add, op1=ALU.mult),
             reads=[dkey, 'gB'], writes=[dkey])

    def norm_mod(src_ap, xtile, xkey, htile, hkey, junk, gs, gskey, sh, shkey, eps=1e-6, si=0, srckey=None):
        stat = stat_all[:, si, :]
        sk = 'stat%d' % si
        P.dma(xtile, src_ap, reads=([srckey] if srckey else []), writes=[xkey])
        P.op('act', lambda e: e.activation(out=junk, in_=xtile, func=AF.Square, accum_out=stat[:, 0:1]),
             reads=[xkey], writes=['junk', sk])
        P.op('dve', lambda e: e.tensor_scalar(out=stat[:, 1:2], in0=stat[:, 0:1], scalar1=1.0 / D, scalar2=eps,
                                              op0=ALU.mult, op1=ALU.add), reads=[sk], writes=[sk])
        P.op('act', lambda e: e.sqrt(out=stat[:, 2:3], in_=stat[:, 1:2]), reads=[sk], writes=[sk])
        P.op('dve', lambda e: e.reciprocal(out=stat[:, 3:4], in_=stat[:, 2:3]), reads=[sk], writes=[sk])
        P.op('dve', lambda e: e.scalar_tensor_tensor(out=htile, in0=xtile, scalar=stat[:, 3:4], in1=gs,
                                                     op0=ALU.mult, op1=ALU.mult),
             reads=[xkey, sk, gskey], writes=[hkey])
        if sh is not None:
            P.op('pool', lambda e: e.tensor_tensor(out=htile, in0=htile, in1=sh, op=ALU.add),
                 reads=[hkey, shkey], writes=[hkey])

    def norm_phase(gain, which_shift, which_scale, src, consumer, src_is_xs=True, lag=None):
        m = A.mark()
        xt = [T("xt%d" % i, [128, D], F32) for i in range(4)]
        ht = [T("ht%d" % i, [128, D], F32) for i in range(4)]
        junk = T("junk", [128, D], BF16)
        make_gs(bc[0], 'bc0', 0, which_scale, gain)
        bcast(bc[1], 'bc1', 0, which_shift)
        make_gs(bc[2], 'bc2', 1, which_scale, gain)
        bcast(bc[3], 'bc3', 1, which_shift)
        pending = []
        for c in range(NCH):
            i = c % 4
            lat = c >= 2
            norm_mod(src[c * 128:(c + 1) * 128, :], xt[i], 'xt%d' % i, ht[i], 'ht%d' % i, junk,
                     bc[0] if lat else bc[2], 'bc0' if lat else 'bc2', bc[1] if lat else bc[3], 'bc1' if lat else 'bc3', si=i,
                     srckey=('xs%d' % c) if src_is_xs else None)
            pending.append((c, ht[i], 'ht%d' % i))
            if len(pending) > (2 if (lag is None and cur_l[0] == 0) else (lag or 0)):
                consumer(*pending.pop(0))
        while pending:
            consumer(*pending.pop(0))
        A.release(m)

    def to_hT(hT, hb):
        def f(c, htile, hkey):
            i = c % 2
            P.op('act', lambda e: e.copy(out=hb[i], in_=htile), reads=[hkey], writes=['hb%d' % i])
            for k in range(8):
                P.op('pe', lambda e, k=k: e.transpose(out=pst[i][:, k * 128:(k + 1) * 128], in_=hb[i][:, k * 128:(k + 1) * 128],
                                                      identity=ident_b),
                     reads=['hb%d' % i, 'ident_b'], writes=['pst%d' % i], signal=(k == 7))
            P.op('dve', lambda e: e.tensor_copy(out=hT[:, :, c * 128:(c + 1) * 128],
                                                in_=pst[i][:, :].rearrange("p (k t) -> p k t", k=8)),
                 reads=['pst%d' % i], writes=['hT'])
            if dbg and stage == 1:
                P.dma(dbg_o[c * 128:(c + 1) * 128, :], htile, reads=[hkey], writes=['dbg_o'])
        return f

    mod_rows(0)
    m_mixer = A.mark()
    hT = T("hT", [128, 8, NT], BF16)
    uT = T("uT", [128, 4, NT], BF16)
    m1 = A.mark()
    hb = [T("hb%d" % i, [128, D], BF16) for i in range(2)]
    norm_phase(norm_mix_g[0:1, :], 0, 1, xin, to_hT(hT, hb), src_is_xs=False)
    A.release(m1)
    if stage == 1:
        P.finish(['dbg_o'])
        return

    m_att = A.mark()
    w_in_b = T("w_in_b", [128, 8, 1280], BF16)
    wr_b = T("wr_b", [128, 8, 640], BF16)
    P.dma(w_in_b, ev_w_in.rearrange("(k p) c -> p k c", p=128), writes=['w_in_b'], q='pool')
    for k in range(8):
        srcv = w_in_b[:, k, 512:1152].rearrange("p (m b j) -> p m b j", b=2, j=16)
        dstv = wr_b[:, k, :].rearrange("p (m b j) -> p m b j", b=2, j=16)
        P.op('act', lambda e, srcv=srcv, dstv=dstv: e.mul(out=dstv[:, :, 0, :], in_=srcv[:, :, 1, :], mul=-1.0),
             reads=['w_in_b'], writes=['wr_b'])
        P.op('dve', lambda e, srcv=srcv, dstv=dstv: e.tensor_copy(out=dstv[:, :, 1, :], in_=srcv[:, :, 0, :]),
             reads=['w_in_b'], writes=['wr_b'])
    cosT = T("cosT", [128, NL], F32)
    sinT = T("sinT", [128, NL], F32)
    P.dma(cosT, c_cos[:, :], writes=['cosT'])
    P.dma(sinT, c_sin[:, :], writes=['sinT'])
    maskb = T("maskb", [128, 2, 4, 128], BF16)
    esink = T("esink", [128, 8], F32)
    m2 = A.mark()
    maskf = T("maskf", [128, 2, 128], F32)
    P.dma(maskf, c_mask[:, :, :], writes=['maskf'])
    for g in range(4):
        P.op('dve', lambda e, g=g: e.tensor_copy(out=maskb[:, :, g, :], in_=maskf), reads=['maskf'], writes=['maskb'])
    A.release(m2)
    P.dma(esink, wa_sink[0:1, :].to_broadcast([128, 8]), writes=['esink'])
    P.op('act', lambda e: e.activation(out=esink, in_=esink, func=AF.Exp), reads=['esink'], writes=['esink'])

    qT = T("qT", [128, 2, NCH, 4, 128], BF16)
    P.op('pool', lambda e: e.memset(qT, 0.0), writes=['qT'])
    wq_p = T("wq_p", [128, 8, 4, 128], BF16)
    wqr_p = T("wqr_p", [128, 8, 4, 128], BF16)
    for k in range(8):
        P.op('act', lambda e, k=k: e.copy(out=wq_p[:, k, :, :].rearrange("p g (a j) -> p g a j", a=2),
                                          in_=w_in_b[:, k, 512:1024].rearrange("p (a g j) -> p g a j", a=2, g=4)),
             reads=['w_in_b'], writes=['wq_p'])
        P.op('dve', lambda e, k=k: e.tensor_copy(out=wqr_p[:, k, :, :].rearrange("p g (a j) -> p g a j", a=2),
                                                 in_=wr_b[:, k, 0:512].rearrange("p (a g j) -> p g a j", a=2, g=4)),
             reads=['wr_b'], writes=['wqr_p'])
    kT = T("kT", [128, NT], BF16)
    Vx = T("Vx", [128, NCH, 2, 65], BF16)
    rtmp = [T("rtmp%d" % i, [128, 512], F32) for i in range(2)]
    P.op('pool', lambda e: e.memset(Vx, 1.0), writes=['Vx'])
    ttiles = [(0, 256)] + [(256 + i * 512, 512) for i in range(4)]

    for cch in range(4):
        for (t0, tn) in ttiles:
            pb, pk = nextps()
            for k in range(8):
                P.op('pe', lambda e, k=k, pb=pb, t0=t0, tn=tn: e.matmul(pb[:, 0:tn], lhsT=w_in_b[:, k, cch * 128:(cch + 1) * 128],
                                                                     rhs=hT[:, k, t0:t0 + tn], start=(k == 0), stop=(k == 7)),
                     reads=['w_in_b', 'hT'], writes=[pk], signal=(k == 7))
            P.op('act', lambda e, pb=pb, t0=t0, tn=tn: e.copy(out=uT[:, cch, t0:t0 + tn], in_=pb[:, 0:tn]),
                 reads=[pk], writes=['uT'])

    def proj_rope(parts, dkey, lhs_plain, lhs_rot, view=lambda ap: ap):
        for (t0, tn) in ttiles:
            pb, pk = nextps()
            for k in range(8):
                P.op('pe', lambda e, k=k, pb=pb, t0=t0, tn=tn: e.matmul(pb[:, 0:tn], lhsT=lhs_plain(k), rhs=hT[:, k, t0:t0 + tn],
                                                                     start=(k == 0), stop=(k == 7)),
                     reads=['w_in_b', 'wq_p', 'hT'], writes=[pk], signal=(k == 7))
            if t0 == 0:
                for (p0, p1, dst_of) in parts:
                    P.op('act', lambda e, pb=pb, tn=tn, t0=t0, p0=p0, p1=p1, dst_of=dst_of: e.copy(out=dst_of(t0, tn), in_=view(pb[p0:p1, 0:tn])),
                         reads=[pk], writes=[dkey])
                continue
            pb2, pk2 = nextps()
            for k in range(8):
                P.op('pe', lambda e, k=k, pb2=pb2, t0=t0, tn=tn: e.matmul(pb2[:, 0:tn], lhsT=lhs_rot(k), rhs=hT[:, k, t0:t0 + tn],
                                                                       start=(k == 0), stop=(k == 7)),
                     reads=['wr_b', 'wqr_p', 'hT'], writes=[pk2], signal=(k == 7))
            l0 = t0 - 256
            P.op('dve', lambda e, pb=pb, l0=l0: e.tensor_tensor(out=rtmp[0], in0=pb[:, :], in1=cosT[:, l0:l0 + 512], op=ALU.mult),
                 reads=[pk, 'cosT'], writes=['rtmp0'])
            P.op('dve', lambda e, pb2=pb2, l0=l0: e.tensor_tensor(out=rtmp[1], in0=pb2[:, :], in1=sinT[:, l0:l0 + 512], op=ALU.mult),
                 reads=[pk2, 'sinT'], writes=['rtmp1'])
            for (p0, p1, dst_of) in parts:
                P.op('pool', lambda e, t0=t0, p0=p0, p1=p1, dst_of=dst_of: e.tensor_tensor(out=dst_of(t0, 512), in0=view(rtmp[0][p0:p1, :]),
                                                                                        in1=view(rtmp[1][p0:p1, :]), op=ALU.add),
                     reads=['rtmp0', 'rtmp1'], writes=[dkey])

    for g in range(4):
        proj_rope([(0, 64, lambda t0, tn, g=g: qT[0:64, 0, t0 // 128:(t0 + tn) // 128, g, :]),
                   (64, 128, lambda t0, tn, g=g: qT[64:128, 1, t0 // 128:(t0 + tn) // 128, g, :])], 'qT',
                  lambda k, g=g: wq_p[:, k, g, :], lambda k, g=g: wqr_p[:, k, g, :],
                  view=lambda ap: ap.rearrange("p (c t) -> p c t", t=128))
    proj_rope([(0, 128, lambda t0, tn: kT[:, t0:t0 + tn])], 'kT', lambda k: w_in_b[:, k, 1024:1152], lambda k: wr_b[:, k, 512:640])
    for c in range(NCH):
        pb, pk = nextps()
        for k in range(8):
            P.op('pe', lambda e, k=k, pb=pb, c=c: e.matmul(pb[:, 0:128], lhsT=hT[:, k, c * 128:(c + 1) * 128], rhs=w_in_b[:, k, 1152:1280],
                                                        start=(k == 0), stop=(k == 7)),
                 reads=['w_in_b', 'hT'], writes=[pk], signal=(k == 7))
        P.op('act', lambda e, pb=pb, c=c: e.copy(out=Vx[:, c, :, 0:64], in_=pb[:, 0:128].rearrange("p (h j) -> p h j", h=2)),
             reads=[pk], writes=['Vx'])

    m3 = A.mark()
    Ebuf = [T("Ebuf%d" % i, [128, 512], BF16) for i in range(5)]
    aw = [T("aw%d" % i, [128, 512], BF16) for i in range(2)]
    dbgt = T("dbgt", [128, 512], F32) if dbg else None
    mixT = hT

    for qc in range(NCH):
        awt = aw[qc % 2]
        awk = 'aw%d' % (qc % 2)
        kbs = [(0, None), (1, None)]
        if qc >= 2:
            n = qc - 2
            if n - 1 >= 0:
                kbs.append((qc - 1, 0))
            kbs.append((qc, None))
            if n + 1 <= 15:
                kbs.append((qc + 1, 1))
        for kh in range(2):
            p0 = 64 * kh
            for bi, (kb, mk) in enumerate(kbs):
                pb, pk = nextps()
                P.op('pe', lambda e, pb=pb, kb=kb: e.matmul(pb[:, :], lhsT=kT[:, kb * 128:(kb + 1) * 128],
                                                         rhs=qT[:, kh, qc, :, :].rearrange("p g q -> p (g q)"), start=True, stop=True),
                     reads=['kT', 'qT'], writes=[pk])
                P.op('act', lambda e, pb=pb, bi=bi: e.activation(out=Ebuf[bi], in_=pb[:, :], func=AF.Exp, scale=0.125),
                     reads=[pk], writes=['Ebuf%d' % bi])
                if mk is not None:
                    P.op('dve', lambda e, bi=bi, mk=mk: e.tensor_tensor(out=Ebuf[bi], in0=Ebuf[bi],
                                                                       in1=maskb[:, mk, :, :].rearrange("p g q -> p (g q)"), op=ALU.mult),
                         reads=['Ebuf%d' % bi, 'maskb'], writes=['Ebuf%d' % bi])
            po, pok = ps[4 + kh], 'ps%d' % (4 + kh)
            for g in range(4):
                for bi, (kb, mk) in enumerate(kbs):
                    P.op('pe', lambda e, g=g, bi=bi, kb=kb: e.matmul(po[:, g * 65:(g + 1) * 65], lhsT=Ebuf[bi][:, g * 128:(g + 1) * 128],
                                                                  rhs=Vx[:, kb, kh, :], start=(bi == 0), stop=(bi == len(kbs) - 1)),
                         reads=['Ebuf%d' % bi, 'Vx'], writes=[pok], signal=(g == 3 and bi == len(kbs) - 1))
            pov = po[:, 0:260].rearrange("p (g j) -> p g j", g=4)
            P.op('dve', lambda e, pov=pov: e.tensor_tensor(out=den4, in0=pov[:, :, 64], in1=esink[:, 4 * kh:4 * kh + 4], op=ALU.add),
                 reads=[pok, 'esink'], writes=['den4'])
            P.op('dve', lambda e: e.reciprocal(out=den4, in_=den4), reads=['den4'], writes=['den4'])
            P.op('dve', lambda e, pov=pov: e.tensor_tensor(out=awt[:, kh * 256:(kh + 1) * 256].rearrange("p (g j) -> p g j", g=4), in0=pov[:, :, 0:64],
                                                          in1=den4.unsqueeze(2).to_broadcast([128, 4, 64]), op=ALU.mult),
                 reads=[pok, 'den4'], writes=[awk])
        if dbg and stage == 2:
            P.op('act', lambda e: e.copy(out=dbgt, in_=awt), reads=[awk], writes=['dbgt'])
            P.dma(dbg_o[qc * 128:(qc + 1) * 128, 0:512], dbgt, reads=['dbgt'], writes=['dbg_o'])
        i = qc % 2
        for j in range(4):
            P.op('pe', lambda e, j=j: e.transpose(out=pst[i][:, j * 128:(j + 1) * 128], in_=awt[:, j * 128:(j + 1) * 128], identity=ident_b),
                 reads=[awk, 'ident_b'], writes=['pst%d' % i], signal=(j == 3))
        P.op('dve', lambda e: e.tensor_copy(out=mixT[:, 4:8, qc * 128:(qc + 1) * 128],
                                            in_=pst[i][:, 0:512].rearrange("p (k t) -> p k t", k=4)),
             reads=['pst%d' % i], writes=['hT'])
    A.release(m_att)
    if stage == 2:
        P.finish(['dbg_o'])
        return

    TWO_PI = 2.0 * math.pi
    NCK = NT // 8
    zT = T("zT", [128, 4, NT], BF16)
    GP = T("GP", [128, 8, 240], BF16)
    EP = T("EP", [128, 8, 240], BF16)
    Shm = T("Shm", [16, 8, 128], BF16)
    P.dma(GP, c_gp[:, :, :], writes=['GP'], q='pool')
    P.dma(EP, c_ep[:, :, :], writes=['EP'], q='pool')
    P.dma(Shm, c_sh[:, :, :], writes=['Shm'], q='pool')
    NSEG = 4
    SEGL = NCK // NSEG
    mio = T("mio", [128, SEGL, 16], F32)
    m_io = A.mark()
    mio_i = T("mio_i", [128, SEGL, 16], I32)
    P.op('pool', lambda e: e.iota(mio_i, pattern=[[1, SEGL], [0, 16]], base=1, channel_multiplier=0), writes=['mio_i'])
    P.op('dve', lambda e: e.tensor_copy(out=mio, in_=mio_i), reads=['mio_i'], writes=['mio'])
    A.release(m_io)
    dcol = T("dcol", [128, 4], F32)
    gbcol = T("gbcol", [128, 4], F32)
    with nc.allow_non_contiguous_dma(reason="tiny transposed loads"):
        P.dma(dcol, s5_d.rearrange("(c p) -> p c", p=128), writes=['dcol'])
        P.dma(gbcol, s5_glu_b.rearrange("(c p) -> p c", p=128), writes=['gbcol'])

    def ew(eng, fn, reads, writes):
        P.op(eng, fn, reads=reads, writes=writes)

    def s5_pass(cch):
        g0 = 8 * cch
        mp = A.mark()
        ISm = T("ISm", [128, 16, 128], BF16)
        ISs = T("ISs", [128, 16, 128], BF16)
        SOm = T("SOm", [128, 16, 128], BF16)
        TKm = T("TKm", [128, 16, 128], BF16)
        AR8 = T("AR8", [128, NSEG, 2, 16], F32)
        AI8 = T("AI8", [128, NSEG, 2, 16], F32)
        PRt = T("PRt", [128, SEGL, 16], F32)
        PIt = T("PIt", [128, SEGL, 16], F32)
        ms = A.mark()
        LAMR = T("LAMR", [128, 16], F32)
        LAMI = T("LAMI", [128, 16], F32)
        DT = T("DT", [128, 16], F32)
        BR = T("BR", [128, 16, 16], F32)
        BI = T("BI", [128, 16, 16], F32)
        CR = T("CR", [128, 16, 16], F32)
        CI = T("CI", [128, 16, 16], F32)
        CRt = T("CRt", [128, 2, 64], F32)
        CIt = T("CIt", [128, 2, 64], F32)
        with nc.allow_non_contiguous_dma(reason="small parameter loads"):
            for k in range(2):
                for hf in range(2):
                    P.dma(LAMR[64 * hf:64 * hf + 64, 8 * k:8 * k + 8], s5_lam_re[k, g0:g0 + 8, :].rearrange("g p -> p g"), writes=['LAMR'])
                    P.dma(LAMI[64 * hf:64 * hf + 64, 8 * k:8 * k + 8], s5_lam_im[k, g0:g0 + 8, :].rearrange("g p -> p g"), writes=['LAMI'])
                    P.dma(BR[64 * hf:64 * hf + 64, 8 * k:8 * k + 8, :], s5_b_re[k, g0:g0 + 8, :, :].rearrange("g p h -> p g h"), writes=['BR'])
                    P.dma(BI[64 * hf:64 * hf + 64, 8 * k:8 * k + 8, :], s5_b_im[k, g0:g0 + 8, :, :].rearrange("g p h -> p g h"), writes=['BI'])
                P.dma(DT[:, 8 * k:8 * k + 8], s5_log_step[k:k + 1, g0:g0 + 8].to_broadcast([128, 8]), writes=['DT'])
        for k in range(2):
            for (src, tt_, tk_, dst, dk_) in ((s5_c_re, CRt, 'CRt', CR, 'CR'), (s5_c_im, CIt, 'CIt', CI, 'CI')):
                for dup in range(2):
                    P.dma(tt_[:, dup, :], src[k, g0:g0 + 8, :, :].rearrange("g c p -> (g c) p"), writes=[tk_])
                pb, pk = nextps()
                P.op('pe', lambda e, pb=pb, tt_=tt_: e.transpose(out=pb[:, 0:128], in_=tt_.rearrange("r d p -> r (d p)"), identity=ident_f),
                     reads=[tk_, 'ident_f'], writes=[pk])
                P.op('act', lambda e, pb=pb, dst=dst, k=k: e.copy(out=dst[:, 8 * k:8 * k + 8, :], in_=pb[:, 0:128].rearrange("p (g c) -> p g c", g=8)),
                     reads=[pk], writes=[dk_])
        MAG = T("MAG", [128, 16], F32)
        ANG = T("ANG", [128, 16], F32)
        NR = T("NR", [128, 16], F32)
        RR = T("RR", [128, 16], F32)
        SN = T("SN", [128, 16], F32)
        CS = T("CS", [128, 16], F32)
        t1 = T("t1", [128, 16], F32)
        t2 = T("t2", [128, 16], F32)
        FR = T("FR", [128, 16], F32)
        FI = T("FI", [128, 16], F32)
        APR = T("APR", [128, 9, 16], F32)
        API = T("API", [128, 9, 16], F32)
        ew('act', lambda e: e.activation(out=DT, in_=DT, func=AF.Exp), ['DT'], ['DT'])
        ew('dve', lambda e: e.tensor_tensor(out=MAG, in0=LAMR, in1=DT, op=ALU.mult), ['LAMR', 'DT'], ['MAG'])
        ew('act', lambda e: e.activation(out=MAG, in_=MAG, func=AF.Exp), ['MAG'], ['MAG'])
        ew('dve', lambda e: e.tensor_tensor(out=ANG, in0=LAMI, in1=DT, op=ALU.mult), ['LAMI', 'DT'], ['ANG'])
        ew('dve', lambda e: e.tensor_scalar(out=NR, in0=ANG, scalar1=1.0 / TWO_PI, scalar2=12582912.0, op0=ALU.mult, op1=ALU.add), ['ANG'], ['NR'])
        ew('dve', lambda e: e.tensor_scalar(out=NR, in0=NR, scalar1=-12582912.0, scalar2=None, op0=ALU.add), ['NR'], ['NR'])
        ew('dve', lambda e: e.scalar_tensor_tensor(out=RR, in0=NR, scalar=-6.28125, in1=ANG, op0=ALU.mult, op1=ALU.add), ['NR', 'ANG'], ['RR'])
        ew('dve', lambda e: e.scalar_tensor_tensor(out=RR, in0=NR, scalar=-(TWO_PI - 6.28125), in1=RR, op0=ALU.mult, op1=ALU.add), ['NR', 'RR'], ['RR'])
        ew('dve', lambda e: e.tensor_scalar(out=RR, in0=RR, scalar1=math.pi, scalar2=-math.pi, op0=ALU.min, op1=ALU.max), ['RR'], ['RR'])
        ew('act', lambda e: e.activation(out=SN, in_=RR, func=AF.Sin), ['RR'], ['SN'])
        ew('dve', lambda e: e.tensor_scalar(out=t1, in0=RR, scalar1=-1.0, scalar2=None, op0=ALU.mult), ['RR'], ['t1'])
        ew('dve', lambda e: e.tensor_tensor(out=t1, in0=t1, in1=RR, op=ALU.max), ['t1', 'RR'], ['t1'])
        ew('dve', lambda e: e.tensor_scalar(out=t1, in0=t1, scalar1=-1.0, scalar2=math.pi / 2, op0=ALU.mult, op1=ALU.add), ['t1'], ['t1'])
        ew('act', lambda e: e.activation(out=CS, in_=t1, func=AF.Sin), ['t1'], ['CS'])
        ew('dve', lambda e: e.memset(APR[:, 0, :], 1.0), [], ['APR'])
        ew('dve', lambda e: e.memset(API[:, 0, :], 0.0), [], ['API'])
        ew('dve', lambda e: e.tensor_tensor(out=APR[:, 1, :], in0=MAG, in1=CS, op=ALU.mult), ['MAG', 'CS'], ['APR'])
        ew('dve', lambda e: e.tensor_tensor(out=API[:, 1, :], in0=MAG, in1=SN, op=ALU.mult), ['MAG', 'SN'], ['API'])
        for tau in range(1, 8):
            ew('dve', lambda e, tau=tau: e.tensor_tensor(out=t1, in0=APR[:, tau, :], in1=APR[:, 1, :], op=ALU.mult), ['APR'], ['t1'])
            ew('dve', lambda e, tau=tau: e.tensor_tensor(out=t2, in0=API[:, tau, :], in1=API[:, 1, :], op=ALU.mult), ['API'], ['t2'])
            ew('dve', lambda e, tau=tau: e.tensor_tensor(out=APR[:, tau + 1, :], in0=t1, in1=t2, op=ALU.subtract), ['t1', 't2'], ['APR'])
            ew('dve', lambda e, tau=tau: e.tensor_tensor(out=t1, in0=APR[:, tau, :], in1=API[:, 1, :], op=ALU.mult), ['APR', 'API'], ['t1'])
            ew('dve', lambda e, tau=tau: e.tensor_tensor(out=t2, in0=API[:, tau, :], in1=APR[:, 1, :], op=ALU.mult), ['APR', 'API'], ['t2'])
            ew('dve', lambda e, tau=tau: e.tensor_tensor(out=API[:, tau + 1, :], in0=t1, in1=t2, op=ALU.add), ['t1', 't2'], ['API'])
        for sg_ in range(NSEG):
            ew('dve', lambda e, sg_=sg_: e.tensor_copy(out=AR8[:, sg_, 0, :], in_=APR[:, 8, :]), ['APR'], ['AR8'])
            ew('dve', lambda e, sg_=sg_: e.tensor_copy(out=AR8[:, sg_, 1, :], in_=APR[:, 8, :]), ['APR'], ['AR8'])
            ew('dve', lambda e, sg_=sg_: e.tensor_copy(out=AI8[:, sg_, 0, :], in_=API[:, 8, :]), ['API'], ['AI8'])
            ew('dve', lambda e, sg_=sg_: e.tensor_scalar(out=AI8[:, sg_, 1, :], in0=API[:, 8, :], scalar1=-1.0, scalar2=None, op0=ALU.mult), ['API'], ['AI8'])
        TA = T("TA", [128, SEGL, 16], F32)
        TN = T("TN", [128, SEGL, 16], F32)
        TM = T("TM", [128, SEGL, 16], F32)
        TS = T("TS", [128, SEGL, 16], F32)
        bM = lambda x: x.unsqueeze(1).to_broadcast([128, SEGL, 16])
        ew('dve', lambda e: e.tensor_tensor(out=t1, in0=LAMR, in1=DT, op=ALU.mult), ['LAMR', 'DT'], ['t1'])
        ew('dve', lambda e: e.tensor_tensor(out=TM, in0=mio, in1=bM(t1), op=ALU.mult), ['mio', 't1'], ['TM'])
        ew('act', lambda e: e.activation(out=TM, in_=TM, func=AF.Exp, scale=8.0), ['TM'], ['TM'])
        ew('dve', lambda e: e.tensor_tensor(out=TA, in0=mio, in1=bM(ANG), op=ALU.mult), ['mio', 'ANG'], ['TA'])
        ew('dve', lambda e: e.tensor_scalar(out=TA, in0=TA, scalar1=8.0, scalar2=None, op0=ALU.mult), ['TA'], ['TA'])
        ew('dve', lambda e: e.tensor_scalar(out=TN, in0=TA, scalar1=1.0 / TWO_PI, scalar2=12582912.0, op0=ALU.mult, op1=ALU.add), ['TA'], ['TN'])
        ew('dve', lambda e: e.tensor_scalar(out=TN, in0=TN, scalar1=-12582912.0, scalar2=None, op0=ALU.add), ['TN'], ['TN'])
        ew('dve', lambda e: e.scalar_tensor_tensor(out=TA, in0=TN, scalar=-6.28125, in1=TA, op0=ALU.mult, op1=ALU.add), ['TN', 'TA'], ['TA'])
        ew('dve', lambda e: e.scalar_tensor_tensor(out=TA, in0=TN, scalar=-(TWO_PI - 6.28125), in1=TA, op0=ALU.mult, op1=ALU.add), ['TN', 'TA'], ['TA'])
        ew('dve', lambda e: e.tensor_scalar(out=TA, in0=TA, scalar1=math.pi, scalar2=-math.pi, op0=ALU.min, op1=ALU.max), ['TA'], ['TA'])
        ew('act', lambda e: e.activation(out=TS, in_=TA, func=AF.Sin), ['TA'], ['TS'])
        ew('dve', lambda e: e.tensor_scalar(out=TN, in0=TA, scalar1=-1.0, scalar2=None, op0=ALU.mult), ['TA'], ['TN'])
        ew('dve', lambda e: e.tensor_tensor(out=TN, in0=TN, in1=TA, op=ALU.max), ['TN', 'TA'], ['TN'])
        ew('dve', lambda e: e.tensor_scalar(out=TN, in0=TN, scalar1=-1.0, scalar2=math.pi / 2, op0=ALU.mult, op1=ALU.add), ['TN'], ['TN'])
        ew('act', lambda e: e.activation(out=TN, in_=TN, func=AF.Sin), ['TN'], ['TN'])
        ew('dve', lambda e: e.tensor_tensor(out=PRt, in0=TM, in1=TN, op=ALU.mult), ['TM', 'TN'], ['PRt'])
        ew('dve', lambda e: e.tensor_tensor(out=PIt, in0=TM, in1=TS, op=ALU.mult), ['TM', 'TS'], ['PIt'])
        ew('dve', lambda e: e.tensor_tensor(out=t1, in0=LAMR, in1=LAMR, op=ALU.mult), ['LAMR'], ['t1'])
        ew('dve', lambda e: e.tensor_tensor(out=t2, in0=LAMI, in1=LAMI, op=ALU.mult), ['LAMI'], ['t2'])
        ew('dve', lambda e: e.tensor_tensor(out=t1, in0=t1, in1=t2, op=ALU.add), ['t1', 't2'], ['t1'])
        ew('dve', lambda e: e.reciprocal(out=NR, in_=t1), ['t1'], ['NR'])
        ew('dve', lambda e: e.tensor_scalar(out=RR, in0=APR[:, 1, :], scalar1=-1.0, scalar2=None, op0=ALU.add), ['APR'], ['RR'])
        ew('dve', lambda e: e.tensor_tensor(out=t1, in0=RR, in1=LAMR, op=ALU.mult), ['RR', 'LAMR'], ['t1'])
        ew('dve', lambda e: e.tensor_tensor(out=t2, in0=API[:, 1, :], in1=LAMI, op=ALU.mult), ['API', 'LAMI'], ['t2'])
        ew('dve', lambda e: e.tensor_tensor(out=t1, in0=t1, in1=t2, op=ALU.add), ['t1', 't2'], ['t1'])
        ew('dve', lambda e: e.tensor_tensor(out=FR, in0=t1, in1=NR, op=ALU.mult), ['t1', 'NR'], ['FR'])
        ew('dve', lambda e: e.tensor_tensor(out=t1, in0=API[:, 1, :], in1=LAMR, op=ALU.mult), ['API', 'LAMR'], ['t1'])
        ew('dve', lambda e: e.tensor_tensor(out=t2, in0=RR, in1=LAMI, op=ALU.mult), ['RR', 'LAMI'], ['t2'])
        ew('dve', lambda e: e.tensor_tensor(out=t1, in0=t1, in1=t2, op=ALU.subtract), ['t1', 't2'], ['t1'])
        ew('dve', lambda e: e.tensor_tensor(out=FI, in0=t1, in1=NR, op=ALU.mult), ['t1', 'NR'], ['FI'])
        B1 = T("B1", [128, 16, 16], F32)
        B2 = T("B2", [128, 16, 16], F32)
        w1 = T("w1", [128, 16, 16], F32)
        w2 = T("w2", [128, 16, 16], F32)
        bF = lambda x: x.unsqueeze(2).to_broadcast([128, 16, 16])
        ew('dve', lambda e: e.tensor_tensor(out=w1, in0=BR, in1=bF(FR), op=ALU.mult), ['BR', 'FR'], ['w1'])
        ew('dve', lambda e: e.tensor_tensor(out=w2, in0=BI, in1=bF(FI), op=ALU.mult), ['BI', 'FI'], ['w2'])
        ew('dve', lambda e: e.tensor_tensor(out=w1, in0=w1, in1=w2, op=ALU.subtract), ['w1', 'w2'], ['w1'])
        ew('dve', lambda e: e.tensor_tensor(out=w2, in0=BI, in1=bF(FR), op=ALU.mult), ['BI', 'FR', 'w1'], ['w2'])
        ew('dve', lambda e: e.tensor_tensor(out=BI, in0=BR, in1=bF(FI), op=ALU.mult), ['BR', 'FI', 'w2'], ['BI'])
        ew('dve', lambda e: e.tensor_tensor(out=w2, in0=w2, in1=BI, op=ALU.add), ['w2', 'BI'], ['w2'])
        ew('dve', lambda e: e.tensor_copy(out=B1[0:64], in_=w1[0:64]), ['w1'], ['B1'])
        ew('dve', lambda e: e.tensor_copy(out=B1[64:128], in_=w2[64:128]), ['w2'], ['B1'])
        ew('dve', lambda e: e.tensor_scalar(out=B2[0:64], in0=w2[0:64], scalar1=-1.0, scalar2=None, op0=ALU.mult), ['w2'], ['B2'])
        ew('dve', lambda e: e.tensor_copy(out=B2[64:128], in_=w1[64:128]), ['w1'], ['B2'])
        C1 = T("C1", [128, 16, 16], F32)
        C2 = T("C2", [128, 16, 16], F32)
        ew('dve', lambda e: e.tensor_copy(out=C1[0:64], in_=CR[0:64]), ['CR'], ['C1'])
        ew('dve', lambda e: e.tensor_scalar(out=C1[64:128], in0=CI[64:128], scalar1=-1.0, scalar2=None, op0=ALU.mult), ['CI'], ['C1'])
        ew('dve', lambda e: e.tensor_scalar(out=C2[0:64], in0=CI[0:64], scalar1=-1.0, scalar2=None, op0=ALU.mult), ['CI'], ['C2'])
        ew('dve', lambda e: e.tensor_scalar(out=C2[64:128], in0=CR[64:128], scalar1=-1.0, scalar2=None, op0=ALU.mult), ['CR'], ['C2'])
        Zt = T("Zt", [128, 16, 8, 16], BF16)
        Zst = T("Zst", [128, 16, 8, 16], BF16)
        SOx = T("SOx", [128, 16, 9, 16], F32)
        for sidx in range(8):
            tau = 7 - sidx
            par = lambda tau=tau: APR[:, tau, :].unsqueeze(2).to_broadcast([128, 16, 16])
            pai = lambda tau=tau: API[:, tau, :].unsqueeze(2).to_broadcast([128, 16, 16])
            ew('dve', lambda e, par=par: e.tensor_tensor(out=w1, in0=B1, in1=par(), op=ALU.mult), ['B1', 'APR'], ['w1'])
            ew('pool', lambda e, pai=pai: e.tensor_tensor(out=w2, in0=B2, in1=pai(), op=ALU.mult), ['B2', 'API'], ['w2'])
            ew('dve', lambda e, sidx=sidx: e.tensor_tensor(out=Zt[:, :, sidx, :], in0=w1, in1=w2, op=ALU.add), ['w1', 'w2'], ['Zt'])
            ew('dve', lambda e, par=par: e.tensor_tensor(out=w1, in0=B2, in1=par(), op=ALU.mult), ['B2', 'APR'], ['w1'])
            ew('pool', lambda e, pai=pai: e.tensor_tensor(out=w2, in0=B1, in1=pai(), op=ALU.mult), ['B1', 'API'], ['w2'])
            ew('dve', lambda e, sidx=sidx: e.tensor_tensor(out=Zst[:, :, sidx, :], in0=w1, in1=w2, op=ALU.subtract), ['w1', 'w2'], ['Zst'])
        for tau in range(9):
            par = lambda tau=tau: APR[:, tau, :].unsqueeze(2).to_broadcast([128, 16, 16])
            pai = lambda tau=tau: API[:, tau, :].unsqueeze(2).to_broadcast([128, 16, 16])
            ew('dve', lambda e, par=par: e.tensor_tensor(out=w1, in0=C1, in1=par(), op=ALU.mult), ['C1', 'APR'], ['w1'])
            ew('pool', lambda e, pai=pai: e.tensor_tensor(out=w2, in0=C2, in1=pai(), op=ALU.mult), ['C2', 'API'], ['w2'])
            ew('dve', lambda e, tau=tau: e.tensor_tensor(out=SOx[:, :, tau, :], in0=w1, in1=w2, op=ALU.add), ['w1', 'w2'], ['SOx'])
        ew('act', lambda e: e.copy(out=SOm.rearrange("p g (t c) -> p g t c", t=8), in_=SOx[:, :, 1:9, :]), ['SOx'], ['SOm'])
        for gl2 in range(0, 16, 8):
            for (src, sk, dst, dk) in ((Zt, 'Zt', ISm, 'ISm'), (Zst, 'Zst', ISs, 'ISs')):
                i = (gl2 // 8) % 2
                for j in range(8):
                    P.op('pe', lambda e, j=j, src=src, i=i: e.transpose(out=pst[i][:, j * 128:(j + 1) * 128],
                                                                       in_=src[:, gl2 + j, :, :].rearrange("p s h -> p (s h)"), identity=ident_b),
                         reads=[sk, 'ident_b'], writes=['pst%d' % i], signal=(j == 7))
                P.op('dve', lambda e, dst=dst, i=i: e.tensor_copy(out=dst[:, gl2:gl2 + 8, :], in_=pst[i][:, :].rearrange("p (g m) -> p g m", g=8)),
                     reads=['pst%d' % i], writes=[dk])
        KTp = T("KTp", [16, 16, 256], BF16)
        ew('dve', lambda e: e.memset(KTp, 0.0), [], ['KTp'])
        for q4 in range(4):
            pb, pk = nextps()
            for j in range(4):
                gd = q4 * 4 + j
                P.op('pe', lambda e, pb=pb, j=j, gd=gd: e.matmul(pb[0:16, j * 128:(j + 1) * 128], lhsT=B1[:, gd, :],
                                                                rhs=SOx[:, gd, 0:8, :].rearrange("p t c -> p (t c)"), start=True, stop=True),
                     reads=['B1', 'SOx'], writes=[pk], signal=(j == 3))
            P.op('act', lambda e, pb=pb, q4=q4: e.copy(out=KTp[:, q4 * 4:q4 * 4 + 4, 128:256], in_=pb[0:16, :].rearrange("p (g m) -> p g m", g=4)),
                 reads=[pk], writes=['KTp'])
        for q4 in range(4):
            pb, pk = nextps()
            for j in range(4):
                gd = q4 * 4 + j
                for sidx in range(8):
                    P.op('pe', lambda e, pb=pb, j=j, gd=gd, sidx=sidx: e.matmul(pb[:, j * 128:(j + 1) * 128], lhsT=Shm[:, sidx, :],
                                                                               rhs=KTp[:, gd, 128 - 16 * sidx:256 - 16 * sidx],
                                                                               start=(sidx == 0), stop=(sidx == 7)),
                         reads=['Shm', 'KTp'], writes=[pk], signal=(j == 3 and sidx == 7))
            P.op('act', lambda e, pb=pb, q4=q4: e.copy(out=TKm[:, q4 * 4:q4 * 4 + 4, :], in_=pb[:, :].rearrange("p (g m) -> p g m", g=4)),
                 reads=[pk], writes=['TKm'])
        A.release(ms)

        VZ = T("VZ", [128, NCK, 2, 16], F32)
        U = T("U", [128, 16, NCK], BF16)
        m_loop = A.mark()
        lt1 = T("lt1", [128, NSEG, 2, 16], F32)
        lt2 = T("lt2", [128, NSEG, 2, 16], F32)
        ct1 = T("ct1", [128, SEGL, 2, 16], F32)
        ct2 = T("ct2", [128, SEGL, 2, 16], F32)
        for d_ in range(2):
            for gl in range(8):
                gd = 8 * d_ + gl
                pb, pk = nextps()
                for part in range(2):
                    for sp in range(8):
                        win = GP[:, gl, 112 - 16 * sp:240 - 16 * sp]
                        if d_ == 0:
                            c0, c1, rhs = ((0, 32, uT[:, cch, sp:256:8]), (32, 288, uT[:, cch, 256 + sp:NT:8]))[part]
                        else:
                            to = 7 - sp
                            c0, c1, rhs = ((0, 32, uT[:, cch, 248 + to:(to - 8 if to - 8 >= 0 else None):-8]),
                                           (32, 288, uT[:, cch, 2296 + to:248 + to:-8]))[part]
                        P.op('pe', lambda e, pb=pb, c0=c0, c1=c1, rhs=rhs, win=win, sp=sp: e.matmul(pb[:, c0:c1], lhsT=win, rhs=rhs,
                                                                                                 start=(sp == 0), stop=(sp == 7)),
                             reads=['GP', 'uT'], writes=[pk], signal=(sp == 7 and part == 1))
                P.op('act', lambda e, pb=pb, gd=gd: e.copy(out=U[:, gd, :], in_=pb[:, 0:NCK]), reads=[pk], writes=['U'])
        for gd in range(16):
            for (mat, mk, half) in ((ISm, 'ISm', 0), (ISs, 'ISs', 1)):
                pb, pk = nextps()
                P.op('pe', lambda e, pb=pb, mat=mat, gd=gd: e.matmul(pb[:, 0:NCK], lhsT=mat[:, gd, :], rhs=U[:, gd, :], start=True, stop=True),
                     reads=[mk, 'U'], writes=[pk])
                P.op('act' if half == 0 else 'dve',
                     (lambda e, pb=pb, gd=gd, half=half: e.copy(out=VZ[:, :, half, gd], in_=pb[:, 0:NCK])) if half == 0 else
                     (lambda e, pb=pb, gd=gd, half=half: e.tensor_copy(out=VZ[:, :, half, gd], in_=pb[:, 0:NCK])),
                     reads=[pk], writes=['VZ'])
        m_bg = A.mark()
        mod_rows_bg(1, [3 * cch, 3 * cch + 1, 3 * cch + 2], 0)
        VZs = VZ.rearrange("p (s m) x g -> p s m x g", s=NSEG)
        for m_ in range(1, SEGL):
            ew('dve', lambda e, m_=m_: e.tensor_tensor(out=lt1, in0=VZs[:, :, m_ - 1, :, :], in1=AR8, op=ALU.mult), ['VZ', 'AR8'], ['lt1'])
            ew('dve', lambda e, m_=m_: e.tensor_tensor(out=lt2, in0=VZs[:, :, m_ - 1, ::-1, :], in1=AI8, op=ALU.mult), ['VZ', 'AI8'], ['lt2'])
            ew('dve', lambda e: e.tensor_tensor(out=lt1, in0=lt1, in1=lt2, op=ALU.add), ['lt1', 'lt2'], ['lt1'])
            ew('dve', lambda e, m_=m_: e.tensor_tensor(out=VZs[:, :, m_, :, :], in0=VZs[:, :, m_, :, :], in1=lt1, op=ALU.add), ['VZ', 'lt1'], ['VZ'])
        for sg_ in range(1, NSEG):
            cprev = VZ[:, sg_ * SEGL - 1, :, :]
            cb = cprev.unsqueeze(1).to_broadcast([128, SEGL, 2, 16])
            cbs = VZ[:, sg_ * SEGL - 1, ::-1, :].unsqueeze(1).to_broadcast([128, SEGL, 2, 16])
            seg = VZ[:, sg_ * SEGL:(sg_ + 1) * SEGL, :, :]
            prb = PRt.unsqueeze(2).to_broadcast([128, SEGL, 2, 16])
            pib = PIt.unsqueeze(2).to_broadcast([128, SEGL, 2, 16])
            ew('dve', lambda e, cb=cb, prb=prb: e.tensor_tensor(out=ct1, in0=prb, in1=cb, op=ALU.mult), ['PRt', 'VZ'], ['ct1'])
            ew('pool', lambda e, cbs=cbs, pib=pib: e.tensor_tensor(out=ct2, in0=pib, in1=cbs, op=ALU.mult), ['PIt', 'VZ'], ['ct2'])
            ew('dve', lambda e: e.tensor_tensor(out=ct1[:, :, 0, :], in0=ct1[:, :, 0, :], in1=ct2[:, :, 0, :], op=ALU.add), ['ct1', 'ct2'], ['ct1'])
            ew('dve', lambda e: e.tensor_tensor(out=ct1[:, :, 1, :], in0=ct1[:, :, 1, :], in1=ct2[:, :, 1, :], op=ALU.subtract), ['ct1', 'ct2'], ['ct1'])
            ew('dve', lambda e, seg=seg: e.tensor_tensor(out=seg, in0=seg, in1=ct1, op=ALU.add), ['VZ', 'ct1'], ['VZ'])
        mod_rows_bg(1, [3 * cch, 3 * cch + 1, 3 * cch + 2], 1)
        A.release(m_loop)
        Xb = T("Xb", [128, 16, NCK], BF16)
        Yb = T("Yb", [128, 16, NCK], BF16)
        ew('pool', lambda e: e.memset(Xb[:, :, 0:1], 0.0), [], ['Xb'])
        ew('act', lambda e: e.copy(out=Xb[:, :, 1:NCK], in_=VZ[:, 0:NCK - 1, 0, :].rearrange("p n g -> p g n")), ['VZ'], ['Xb'])
        for gd in range(16):
            pb, pk = nextps()
            P.op('pe', lambda e, pb=pb, gd=gd: e.matmul(pb[:, 0:NCK], lhsT=TKm[:, gd, :], rhs=U[:, gd, :], start=True, stop=False),
                 reads=['TKm', 'U'], writes=[pk], signal=False)
            P.op('pe', lambda e, pb=pb, gd=gd: e.matmul(pb[:, 0:NCK], lhsT=SOm[:, gd, :], rhs=Xb[:, gd, :], start=False, stop=True),
                 reads=['SOm', 'Xb'], writes=[pk])
            P.op('act', lambda e, pb=pb, gd=gd: e.copy(out=Yb[:, gd, :], in_=pb[:, 0:NCK]), reads=[pk], writes=['Yb'])
        pre = [T("pre%d" % i, [128, NCK], F32) for i in range(2)]
        pr2 = [T("pr2%d" % i, [128, NCK], F32) for i in range(2)]
        for i_ in range(8):
            pb, pk = nextps()
            b2 = i_ % 2
            for part in range(2):
                for gl in range(8):
                    c0, c1, rhs = ((0, 32, Yb[:, gl, 0:32]), (32, 288, Yb[:, gl, 32:288]))[part]
                    P.op('pe', lambda e, pb=pb, c0=c0, c1=c1, rhs=rhs, gl=gl: e.matmul(pb[:, c0:c1], lhsT=EP[:, i_, 112 - 16 * gl:240 - 16 * gl], rhs=rhs,
                                                                                   start=(gl == 0), stop=False),
                         reads=['EP', 'Yb'], writes=[pk], signal=False)
                for gl in range(8):
                    c0, c1, rhs = ((0, 32, Yb[:, 8 + gl, 31::-1]), (32, 288, Yb[:, 8 + gl, 287:31:-1]))[part]
                    P.op('pe', lambda e, pb=pb, c0=c0, c1=c1, rhs=rhs, gl=gl: e.matmul(pb[:, c0:c1], lhsT=EP[:, 7 - i_, 112 - 16 * gl:240 - 16 * gl], rhs=rhs,
                                                                                   start=False, stop=(gl == 7)),
                         reads=['EP', 'Yb'], writes=[pk], signal=(gl == 7 and part == 1))
            ew('dve', lambda e, pb=pb, b2=b2: e.scalar_tensor_tensor(out=pre[b2], in0=uT[:, cch, i_:NT:8], scalar=dcol[:, cch:cch + 1], in1=pb[:, 0:NCK],
                                                                    op0=ALU.mult, op1=ALU.add), [pk, 'uT', 'dcol'], ['pre%d' % b2])
            ew('act', lambda e, b2=b2: e.activation(out=pr2[b2], in_=pre[b2], func=AF.Square), ['pre%d' % b2], ['pr2%d' % b2])
            ew('dve', lambda e, b2=b2: e.tensor_scalar(out=pr2[b2], in0=pr2[b2], scalar1=0.044715, scalar2=1.0, op0=ALU.mult, op1=ALU.add), ['pr2%d' % b2], ['pr2%d' % b2])
            ew('dve', lambda e, b2=b2: e.tensor_tensor(out=pr2[b2], in0=pr2[b2], in1=pre[b2], op=ALU.mult), ['pr2%d' % b2, 'pre%d' % b2], ['pr2%d' % b2])
            ew('act', lambda e, b2=b2: e.activation(out=pr2[b2], in_=pr2[b2], func=AF.Sigmoid, scale=1.5957691216057308), ['pr2%d' % b2], ['pr2%d' % b2])
            ew('dve', lambda e, b2=b2: e.tensor_tensor(out=zT[:, cch, i_:NT:8], in0=pr2[b2], in1=pre[b2], op=ALU.mult), ['pr2%d' % b2, 'pre%d' % b2], ['zT'])
        A.release(mp)

    for cch in range(4):
        s5_pass(cch)

    m_glu = A.mark()
    gw = T("gw", [128, 4, 512], BF16)
    P.dma(gw, s5_glu_w.rearrange("(k p) c -> p k c", p=128), writes=['gw'], q='pool')
    sg = [T("sg%d" % i, [128, 512], BF16) for i in range(2)]
    for co in range(4):
        for ti, (t0, tn) in enumerate(ttiles):
            pb, pk = nextps()
            for k in range(4):
                P.op('pe', lambda e, k=k, pb=pb, t0=t0, tn=tn: e.matmul(pb[:, 0:tn], lhsT=gw[:, k, co * 128:(co + 1) * 128], rhs=zT[:, k, t0:t0 + tn],
                                                                     start=(k == 0), stop=(k == 3)),
                     reads=['gw', 'zT'], writes=[pk], signal=(k == 3))
            i = ti % 2
            ew('act', lambda e, pb=pb, tn=tn, i=i: e.activation(out=sg[i][:, 0:tn], in_=pb[:, 0:tn], func=AF.Sigmoid, bias=gbcol[:, co:co + 1]),
               [pk, 'gbcol'], ['sg%d' % i])
            ew('dve', lambda e, t0=t0, tn=tn, i=i: e.tensor_tensor(out=mixT[:, co, t0:t0 + tn], in0=sg[i][:, 0:tn], in1=zT[:, co, t0:t0 + tn], op=ALU.mult),
               ['sg%d' % i, 'zT'], ['hT'])
    A.release(m_glu)
    if stage == 3:
        dt_ = T("dt_", [128, 512], F32)
        for c in range(NCH):
            for k in range(4):
                P.op('pe', lambda e, k=k, c=c: e.transpose(out=pst[0][:, k * 128:(k + 1) * 128], in_=mixT[:, k, c * 128:(c + 1) * 128], identity=ident_b),
                     reads=['hT', 'ident_b'], writes=['pst0'], signal=(k == 3))
            ew('act', lambda e: e.copy(out=dt_, in_=pst[0][:, 0:512]), ['pst0'], ['dt_'])
            P.dma(dbg_o[c * 128:(c + 1) * 128, 0:512], dt_, reads=['dt_'], writes=['dbg_o'])
        P.finish(['dbg_o'])
        return

    def outproj_phase(l, w_out_dram, x_src, mixT_, mkey, off, chunks):
        m = A.mark()
        w_out_b = T("w_out_b", [128, 8, D], BF16)
        P.dma(w_out_b, w_out_dram.rearrange("(k p) c -> p k c", p=128), writes=['w_out_b'], q='pool')
        bcast(bc[0], 'bc0', 0, 2)
        bcast(bc[2], 'bc2', 1, 2)
        xo = [T("xo%d" % i, [128, D], F32) for i in range(2)]
        xn = [T("xn%d" % i, [128, D], F32) for i in range(2)]
        for c in chunks:
            i = c % 2
            g2, g2k = (bc[0], 'bc0') if c >= 2 else (bc[2], 'bc2')
            P.dma(xo[i], x_src[c * 128:(c + 1) * 128, :], reads=(['xs%d' % c] if l == 1 else []), writes=['xo%d' % i])
            for hh in range(2):
                pb, pk = nextps()
                for k in range(8):
                    P.op('pe', lambda e, k=k, pb=pb, hh=hh: e.matmul(pb[:, :], lhsT=mixT_[:, k, c * 128 - off:(c + 1) * 128 - off], rhs=w_out_b[:, k, hh * 512:(hh + 1) * 512],
                                                                  start=(k == 0), stop=(k == 7)),
                         reads=[mkey, 'w_out_b'], writes=[pk], signal=(k == 7))
                P.op('dve', lambda e, pb=pb, hh=hh: e.tensor_tensor(out=xn[i][:, hh * 512:(hh + 1) * 512], in0=pb[:, :], in1=g2[:, hh * 512:(hh + 1) * 512], op=ALU.mult),
                     reads=[pk, g2k], writes=['xn%d' % i])
            P.op('pool', lambda e: e.tensor_tensor(out=xn[i], in0=xn[i], in1=xo[i], op=ALU.add), reads=['xn%d' % i, 'xo%d' % i], writes=['xn%d' % i])
            P.dma(xs[c * 128:(c + 1) * 128, :], xn[i], reads=['xn%d' % i], writes=['xs%d' % c], q='pool')
            if dbg and stage == 4:
                P.dma(dbg_o[c * 128:(c + 1) * 128, :], xn[i], reads=['xn%d' % i], writes=['dbg_o'])
        A.release(m)

    outproj_phase(0, ev_w_out, xin, mixT, 'hT', 0, list(range(NCH)))
    A.release(m_mixer)
    if stage == 4:
        P.finish(['dbg_o'] + XS_KEYS)
        return

    def moe_phase(l, with_ctx):
        mm = A.mark()
        nslot = 288 if with_ctx else 256
        scs = [(0, 128, 0), (1, 128, 128)] + ([(2, 32, 256)] if with_ctx else [])
        affT = T("affT", [16, NT], F32)
        wr_f = T("wr_f", [128, 8, NE], F32)
        P.dma(wr_f, moe_router[l].rearrange("(k p) e -> p k e", p=128), writes=['wr_f'])
        hTf2 = [T("hTf%d" % i, [128, 8, 128], F32) for i in range(2)]
        hb2 = [T("hbb%d" % i, [128, D], BF16) for i in range(2)]
        aff2 = [T("aff%d" % i, [128, NE], F32) for i in range(2)]
        sm2 = [T("sm%d" % i, [128, 4], F32) for i in range(2)]

        def cons(c, htile, hkey):
            if c < 2 and not with_ctx:
                return
            i = c % 2
            hTf, hk = hTf2[i], 'hTf%d' % i
            aff, ak = aff2[i], 'aff%d' % i
            sm, sk = sm2[i], 'sm%d' % i
            P.op('act', lambda e: e.copy(out=hb2[i], in_=htile), reads=[hkey], writes=['hbb%d' % i])
            P.dma(hbf[c * 128:(c + 1) * 128, :], hb2[i], reads=['hbb%d' % i], writes=['hbf%d' % c], q='act')
            for half in range(2):
                pb, pk = ps[4 + half], 'ps%d' % (4 + half)
                for k in range(4):
                    kk = half * 4 + k
                    P.op('pe', lambda e, pb=pb, k=k, kk=kk: e.transpose(out=pb[:, k * 128:(k + 1) * 128], in_=htile[:, kk * 128:(kk + 1) * 128], identity=ident_f),
                         reads=[hkey, 'ident_f'], writes=[pk], signal=(k == 3))
                P.op('act' if half == 0 else 'dve',
                     (lambda e, pb=pb, half=half: e.copy(out=hTf[:, half * 4:half * 4 + 4, :], in_=pb[:, :].rearrange("p (k t) -> p k t", k=4))) if half == 0 else
                     (lambda e, pb=pb, half=half: e.tensor_copy(out=hTf[:, half * 4:half * 4 + 4, :], in_=pb[:, :].rearrange("p (k t) -> p k t", k=4))),
                     reads=[pk], writes=[hk])
            pb, pk = nextps()
            for k in range(8):
                P.op('pe', lambda e, pb=pb, k=k: e.matmul(pb[:, 0:NE], lhsT=hTf[:, k, :], rhs=wr_f[:, k, :], start=(k == 0), stop=(k == 7)),
                     reads=[hk, 'wr_f'], writes=[pk], signal=(k == 7))
            P.op('dve', lambda e, pb=pb: e.reduce_max(out=sm[:, 0:1], in_=pb[:, 0:NE], axis=AX.X), reads=[pk], writes=[sk])
            P.op('dve', lambda e: e.tensor_scalar(out=sm[:, 1:2], in0=sm[:, 0:1], scalar1=-1.0, scalar2=None, op0=ALU.mult), reads=[sk], writes=[sk])
            P.op('act', lambda e, pb=pb: e.activation(out=aff, in_=pb[:, 0:NE], func=AF.Exp, bias=sm[:, 1:2], accum_out=sm[:, 2:3]),
                 reads=[pk, sk], writes=[ak, sk])
            P.op('dve', lambda e: e.reciprocal(out=sm[:, 3:4], in_=sm[:, 2:3]), reads=[sk], writes=[sk])
            P.op('dve', lambda e: e.tensor_scalar(out=aff, in0=aff, scalar1=sm[:, 3:4], scalar2=None, op0=ALU.mult), reads=[ak, sk], writes=[ak])
            pb2, pk2 = nextps()
            P.op('pe', lambda e, pb2=pb2: e.transpose(out=pb2[0:NE, 0:128], in_=aff, identity=ident_f), reads=[ak, 'ident_f'], writes=[pk2])
            P.op('act', lambda e, pb2=pb2: e.copy(out=affT[:, c * 128:(c + 1) * 128], in_=pb2[0:NE, 0:128]), reads=[pk2], writes=['affT'])

        norm_phase(norm_ffn_g[l:l + 1, :], 3, 4, xs, cons)

        NB = 4
        Wg = [T("Wg%d" % i, [128, 8, 512], BF16) for i in range(NB)]
        Wu = [T("Wu%d" % i, [128, 8, 512], BF16) for i in range(NB)]
        Wd = [T("Wd%d" % i, [128, 4, D], BF16) for i in range(NB)]

        def load_w(e_, ft, b):
            P.dma(Wg[b], moe_w_gate[l, e_, :, ft * 512:(ft + 1) * 512].rearrange("(k p) f -> p k f", p=128), writes=['Wg%d' % b], q='pool')
            P.dma(Wu[b], moe_w_up[l, e_, :, ft * 512:(ft + 1) * 512].rearrange("(k p) f -> p k f", p=128), writes=['Wu%d' % b], q='pool')
            P.dma(Wd[b], moe_w_down[l, e_, ft * 512:(ft + 1) * 512, :].rearrange("(k p) d -> p k d", p=128), writes=['Wd%d' % b], q='pool')

        tiles = [(e_, ft) for e_ in range(NE) for ft in range(4)]
        for ti in range(NB - 1):
            load_w(tiles[ti][0], tiles[ti][1], ti % NB)

        vals = T("vals", [16, 288], F32)
        idxu = T("idxu", [16, 288], U32)
        idxf = T("idxf", [16, 288], F32)
        mt = A.mark()
        wk = T("wk", [16, NL], F32)
        for (t0, tn, o0, nr) in ([(256, NL, 0, 32)] + ([(0, 256, 256, 4)] if with_ctx else [])):
            cur = affT[:, t0:t0 + tn]
            curk = 'affT'
            for r in range(nr):
                vs = vals[:, o0 + 8 * r:o0 + 8 * r + 8]
                P.op('dve', lambda e, vs=vs, cur=cur: e.max(out=vs, in_=cur), reads=[curk], writes=['vals'])
                P.op('dve', lambda e, vs=vs, cur=cur, r=r, o0=o0: e.max_index(out=idxu[:, o0 + 8 * r:o0 + 8 * r + 8], in_max=vs, in_values=cur),
                     reads=[curk, 'vals'], writes=['idxu'])
                if r < nr - 1:
                    P.op('dve', lambda e, vs=vs, cur=cur, tn=tn: e.match_replace(out=wk[:, 0:tn], in_to_replace=vs, in_values=cur, imm_value=-1.0),
                         reads=[curk, 'vals', 'wk'], writes=['wk'])
                    cur = wk[:, 0:tn]
                    curk = 'wk'
        A.release(mt)
        P.op('dve', lambda e: e.tensor_copy(out=idxf[:, 0:nslot], in_=idxu[:, 0:nslot]), reads=['idxu'], writes=['idxf'])
        P.op('dve', lambda e: e.tensor_scalar(out=idxf[:, 0:256], in0=idxf[:, 0:256], scalar1=256.0, scalar2=None, op0=ALU.add), reads=['idxf'], writes=['idxf'])
        idxT = T("idxT", [128, 3, NE], U32)
        gate = T("gate", [128, 3, NE], F32)
        for (sc, rows, so) in scs:
            for (src, sk, dst, dk) in ((idxf, 'idxf', idxT, 'idxT'), (vals, 'vals', gate, 'gate')):
                pb, pk = nextps()
                P.op('pe', lambda e, pb=pb, src=src, rows=rows, so=so: e.transpose(out=pb[0:rows, 0:NE], in_=src[:, so:so + rows], identity=ident_f[0:NE, 0:NE]),
                     reads=[sk, 'ident_f'], writes=[pk])
                P.op('dve', lambda e, pb=pb, dst=dst, rows=rows, sc=sc: e.tensor_copy(out=dst[0:rows, sc, :], in_=pb[0:rows, 0:NE]), reads=[pk], writes=[dk])

        bcast(bc[0], 'bc0', 0, 5)
        if with_ctx:
            bcast(bc[2], 'bc2', 1, 5)
        xg = [T("xg%d" % i, [128, D], BF16) for i in range(3)]
        xgT = T("xgT", [128, 8, 288], BF16)
        hid = [T("hid%d" % i, [128, 384], BF16) for i in range(4)]
        for i in range(4):
            P.op('pool', lambda e, i=i: e.memset(hid[i], 0.0), writes=['hid%d' % i])
        sgt = [T("sgt%d" % i, [128, 288], F32) for i in range(2)]
        ysb = T("ysb", [128, 3, D], F32)
        ysc = [T("ysc%d" % i, [128, D], F32) for i in range(3)]

        pend_scatter = []
        yrr = [0]
        for ti, (e_, ft) in enumerate(tiles):
            b = ti % NB
            if ft == 0:
                for (sc, rows, so) in scs:
                    P.dma(None, None, reads=HBF_KEYS + ['idxT'], writes=['xg%d' % sc], q='pool',
                          fn=lambda e, sc=sc, rows=rows, e_=e_: e.indirect_dma_start(out=xg[sc][0:rows, :], out_offset=None, in_=hbf,
                                                                                   in_offset=bass.IndirectOffsetOnAxis(idxT[0:rows, sc, e_:e_ + 1], 0)))
            if ti + NB - 1 < len(tiles):
                load_w(tiles[ti + NB - 1][0], tiles[ti + NB - 1][1], (ti + NB - 1) % NB)
            for f_ in pend_scatter:
                f_()
            del pend_scatter[:]
            if ft == 0:
                for (sc, rows, so) in scs:
                    i = sc % 2
                    for k in range(8):
                        P.op('pe', lambda e, k=k, sc=sc, rows=rows, i=i: e.transpose(out=pst[i][:, k * 128:k * 128 + rows], in_=xg[sc][0:rows, k * 128:(k + 1) * 128],
                                                                                 identity=ident_b[0:rows, 0:rows]),
                             reads=['xg%d' % sc, 'ident_b'], writes=['pst%d' % i], signal=(k == 7))
                    P.op('dve', lambda e, sc=sc, rows=rows, so=so, i=i: e.tensor_copy(out=xgT[:, :, so:so + rows],
                                                                                   in_=pst[i][:, :].rearrange("p (k t) -> p k t", k=8)[:, :, 0:rows]),
                         reads=['pst%d' % i], writes=['xgT'])
            for fc in range(4):
                pg, pgk = ps[fc % 2], 'ps%d' % (fc % 2)
                pu, puk = ps[2 + fc % 2], 'ps%d' % (2 + fc % 2)
                for k in range(8):
                    P.op('pe', lambda e, k=k, pg=pg, fc=fc, b=b: e.matmul(pg[:, 0:nslot], lhsT=Wg[b][:, k, fc * 128:(fc + 1) * 128], rhs=xgT[:, k, 0:nslot],
                                                                      start=(k == 0), stop=(k == 7)),
                         reads=['Wg%d' % b, 'xgT'], writes=[pgk], signal=(k == 7))
                for k in range(8):
                    P.op('pe', lambda e, k=k, pu=pu, fc=fc, b=b: e.matmul(pu[:, 0:nslot], lhsT=Wu[b][:, k, fc * 128:(fc + 1) * 128], rhs=xgT[:, k, 0:nslot],
                                                                      start=(k == 0), stop=(k == 7)),
                         reads=['Wu%d' % b, 'xgT'], writes=[puk], signal=(k == 7))
                j = fc % 2
                P.op('act', lambda e, pg=pg, j=j: e.activation(out=sgt[j][:, 0:nslot], in_=pg[:, 0:nslot], func=AF.Silu), reads=[pgk], writes=['sgt%d' % j])
                P.op('dve', lambda e, pu=pu, j=j, fc=fc: e.tensor_tensor(out=hid[fc][:, 0:nslot], in0=sgt[j][:, 0:nslot], in1=pu[:, 0:nslot], op=ALU.mult),
                     reads=['sgt%d' % j, puk], writes=['hid%d' % fc])
            for (sc, rows, so) in scs:
                for dh in range(2):
                    yrr[0] = 1 - yrr[0]
                    py, pyk = ps[4 + yrr[0]], 'ps%d' % (4 + yrr[0])
                    for fc in range(4):
                        P.op('pe', lambda e, py=py, fc=fc, rows=rows, so=so, dh=dh, b=b: e.matmul(py[:, :], lhsT=hid[fc][:, so:so + 128],
                                                                                             rhs=Wd[b][:, fc, dh * 512:(dh + 1) * 512],
                                                                                             start=(fc == 0), stop=(fc == 3)),
                             reads=['hid%d' % fc, 'Wd%d' % b], writes=[pyk], signal=(fc == 3))
                    if ft == 0:
                        P.op('act', lambda e, py=py, rows=rows, sc=sc, dh=dh: e.copy(out=ysb[0:rows, sc, dh * 512:(dh + 1) * 512], in_=py[0:rows, :]),
                             reads=[pyk], writes=['ysb'])
                    else:
                        P.op('dve', lambda e, py=py, rows=rows, sc=sc, dh=dh: e.tensor_tensor(out=ysb[0:rows, sc, dh * 512:(dh + 1) * 512],
                                                                                           in0=ysb[0:rows, sc, dh * 512:(dh + 1) * 512], in1=py[0:rows, :], op=ALU.add),
                             reads=[pyk, 'ysb'], writes=['ysb'])
            if ft == 3:
                for (sc, rows, so) in scs:
                    i = sc
                    g5, g5k = (bc[0], 'bc0') if sc < 2 else (bc[2], 'bc2')
                    P.op('dve', lambda e, sc=sc, rows=rows, i=i, g5=g5, e_=e_: e.scalar_tensor_tensor(out=ysc[i][0:rows, :], in0=ysb[0:rows, sc, :],
                                                                                                   scalar=gate[0:rows, sc, e_:e_ + 1], in1=g5[0:rows, :],
                                                                                                   op0=ALU.mult, op1=ALU.mult),
                         reads=['ysb', 'gate', g5k], writes=['ysc%d' % i])
                    pend_scatter.append(lambda sc=sc, rows=rows, i=i, e_=e_: P.dma(
                        None, None, reads=['ysc%d' % i, 'idxT'] + XS_KEYS, writes=XS_KEYS, q='pool',
                        fn=lambda e: e.indirect_dma_start(out=xs, out_offset=bass.IndirectOffsetOnAxis(idxT[0:rows, sc, e_:e_ + 1], 0),
                                                          in_=ysc[i][0:rows, :], in_offset=None, compute_op=ALU.add)))
        for f_ in pend_scatter:
            f_()
        A.release(mm)

    moe_phase(0, True)
    if stage == 5:
        dx = T("dx", [128, D], F32)
        for c in range(NCH):
            P.dma(dx, xs[c * 128:(c + 1) * 128, :], reads=['xs%d' % c], writes=['dx'])
            P.dma(dbg_o[c * 128:(c + 1) * 128, :], dx, reads=['dx'], writes=['dbg_o'])
        P.finish(['dbg_o'])
        return

    cur_l[0] = 1
    m_l1 = A.mark()
    mixT1 = T("mixT1", [128, 8, NL], BF16)
    m_l1b = A.mark()
    hT = T("hT", [128, 8, NT], BF16)
    m1 = A.mark()
    hb = [T("hb%d" % i, [128, D], BF16) for i in range(2)]
    norm_phase(norm_mix_g[1:2, :], 0, 1, xs, to_hT(hT, hb), lag=2)
    A.release(m1)
    LAM_INIT = 0.8 - 0.6 * math.exp(-0.3 * 1)
    cosT = T("cosT", [128, NL], F32)
    sinT = T("sinT", [128, NL], F32)
    P.dma(cosT, c_cos[:, :], writes=['cosT'])
    P.dma(sinT, c_sin[:, :], writes=['sinT'])
    lamv = T("lamv", [128, 4, 64], F32)
    lams = T("lams", [128, 4], F32)
    for i in range(4):
        P.dma(lamv[:, i, :], da_l[i:i + 1, :].to_broadcast([128, 64]), writes=['lamv'])
    P.op('dve', lambda e: e.tensor_tensor(out=lamv[:, 0, :], in0=lamv[:, 0, :], in1=lamv[:, 1, :], op=ALU.mult), reads=['lamv'], writes=['lamv'])
    P.op('dve', lambda e: e.tensor_tensor(out=lamv[:, 2, :], in0=lamv[:, 2, :], in1=lamv[:, 3, :], op=ALU.mult), reads=['lamv'], writes=['lamv'])
    P.op('dve', lambda e: e.reduce_sum(out=lams[:, 0:1], in_=lamv[:, 0, :], axis=AX.X), reads=['lamv'], writes=['lams'])
    P.op('dve', lambda e: e.reduce_sum(out=lams[:, 1:2], in_=lamv[:, 2, :], axis=AX.X), reads=['lamv'], writes=['lams'])
    P.op('act', lambda e: e.activation(out=lams[:, 0:2], in_=lams[:, 0:2], func=AF.Exp), reads=['lams'], writes=['lams'])
    P.op('dve', lambda e: e.tensor_tensor(out=lams[:, 2:3], in0=lams[:, 1:2], in1=lams[:, 0:1], op=ALU.subtract), reads=['lams'], writes=['lams'])
    P.op('dve', lambda e: e.tensor_scalar(out=lams[:, 3:4], in0=lams[:, 2:3], scalar1=-LAM_INIT, scalar2=None, op0=ALU.add), reads=['lams'], writes=['lams'])

    Wh = T("Wh", [128, 8, 384], BF16)
    Whr = T("Whr", [128, 8, 256], BF16)
    qT1 = T("qT1", [128, 2, NL], BF16)
    P.op('pool', lambda e: e.memset(qT1, 0.0), writes=['qT1'])
    kT1 = T("kT1", [128, NT], BF16)
    Vx1 = T("Vx1", [128, NCH, 128], BF16)
    ones_b = T("ones_b", [128, 128], BF16)
    P.op('pool', lambda e: e.memset(ones_b, 1.0), writes=['ones_b'])
    rdn = [T("rdn%d" % i, [128, 512], F32) for i in range(2)]
    sqb = T("sqb", [128, 512], BF16)
    sgcol = T("sgcol", [128, 1], F32)
    with nc.allow_non_contiguous_dma(reason="tiny transposed load"):
        P.dma(sgcol, da_subln_g.rearrange("o f -> f o"), writes=['sgcol'])
    P.op('dve', lambda e: e.tensor_scalar(out=sgcol, in0=sgcol, scalar1=1.0 - LAM_INIT, scalar2=None, op0=ALU.mult), reads=['sgcol'], writes=['sgcol'])
    rtmp = [T("rtmp%d" % i, [128, 512], F32) for i in range(2)]
    Ebufs = [T("Eall%d" % i, [128, NCH, 512], BF16) for i in range(2)]
    pstf = [pst[i][:, :].bitcast(F32) for i in range(2)]
    o0 = T("o0", [128, 512], F32)
    o1 = T("o1", [128, 512], F32)
    lt_tiles = [(256 + i * 512, 512) for i in range(4)]

    for h in range(8):
        for j3 in range(3):
            P.dma(Wh[:, :, j3 * 128:(j3 + 1) * 128], od_w_in[:, j3 * D + h * 128: j3 * D + (h + 1) * 128].rearrange("(k p) c -> p k c", p=128),
                  writes=['Wh'], q='pool')
        for k in range(8):
            srcv = Wh[:, k, 0:256].rearrange("p (m b j) -> p m b j", b=2, j=16)
            dstv = Whr[:, k, :].rearrange("p (m b j) -> p m b j", b=2, j=16)
            P.op('act', lambda e, srcv=srcv, dstv=dstv: e.mul(out=dstv[:, :, 0, :], in_=srcv[:, :, 1, :], mul=-1.0), reads=['Wh'], writes=['Whr'])
            P.op('dve', lambda e, srcv=srcv, dstv=dstv: e.tensor_copy(out=dstv[:, :, 1, :], in_=srcv[:, :, 0, :]), reads=['Wh'], writes=['Whr'])
        for (dst, dkey, c0, do_ctx, toff) in ((qT1, 'qT1', 0, False, 256), (kT1, 'kT1', 128, True, 0)):
            if do_ctx:
                pb, pk = nextps()
                for k in range(8):
                    P.op('pe', lambda e, k=k, pb=pb: e.matmul(pb[:, 0:256], lhsT=Wh[:, k, c0:c0 + 128], rhs=hT[:, k, 0:256], start=(k == 0), stop=(k == 7)),
                         reads=['Wh', 'hT'], writes=[pk], signal=(k == 7))
                P.op('act', lambda e, pb=pb: e.copy(out=dst[:, 0:256], in_=pb[:, 0:256]), reads=[pk], writes=[dkey])
            for (t0, tn) in lt_tiles:
                pb, pk = nextps()
                for k in range(8):
                    P.op('pe', lambda e, k=k, pb=pb, t0=t0: e.matmul(pb[:, :], lhsT=Wh[:, k, c0:c0 + 128], rhs=hT[:, k, t0:t0 + 512], start=(k == 0), stop=(k == 7)),
                         reads=['Wh', 'hT'], writes=[pk], signal=(k == 7))
                pb2, pk2 = nextps()
                for k in range(8):
                    P.op('pe', lambda e, k=k, pb2=pb2, t0=t0: e.matmul(pb2[:, :], lhsT=Whr[:, k, c0:c0 + 128], rhs=hT[:, k, t0:t0 + 512], start=(k == 0), stop=(k == 7)),
                         reads=['Whr', 'hT'], writes=[pk2], signal=(k == 7))
                l0 = t0 - 256
                P.op('dve', lambda e, pb=pb, l0=l0: e.tensor_tensor(out=rtmp[0], in0=pb[:, :], in1=cosT[:, l0:l0 + 512], op=ALU.mult), reads=[pk, 'cosT'], writes=['rtmp0'])
                P.op('dve', lambda e, pb2=pb2, l0=l0: e.tensor_tensor(out=rtmp[1], in0=pb2[:, :], in1=sinT[:, l0:l0 + 512], op=ALU.mult), reads=[pk2, 'sinT'], writes=['rtmp1'])
                if dkey == 'qT1':
                    for j in range(2):
                        P.op('pool', lambda e, t0=t0, j=j: e.tensor_tensor(out=qT1[64 * j:64 * j + 64, j, t0 - 256:t0 - 256 + 512], in0=rtmp[0][64 * j:64 * j + 64, :],
                                                                         in1=rtmp[1][64 * j:64 * j + 64, :], op=ALU.add),
                             reads=['rtmp0', 'rtmp1'], writes=[dkey])
                else:
                    P.op('pool', lambda e, t0=t0, dst=dst, toff=toff: e.tensor_tensor(out=dst[:, t0 - toff:t0 - toff + 512], in0=rtmp[0], in1=rtmp[1], op=ALU.add),
                         reads=['rtmp0', 'rtmp1'], writes=[dkey])
        for c in range(NCH):
            pb, pk = nextps()
            for k in range(8):
                P.op('pe', lambda e, k=k, pb=pb, c=c: e.matmul(pb[:, 0:128], lhsT=hT[:, k, c * 128:(c + 1) * 128], rhs=Wh[:, k, 256:384], start=(k == 0), stop=(k == 7)),
                     reads=['Wh', 'hT'], writes=[pk], signal=(k == 7))
            P.op('act', lambda e, pb=pb, c=c: e.copy(out=Vx1[:, c, 0:128], in_=pb[:, 0:128]), reads=[pk], writes=['Vx1'])
        units = [(qt, j) for qt in range(4) for j in range(2)]

        def S_step(u, kb):
            qt, j = units[u]
            p0 = 64 * j
            E = Ebufs[u % 2]
            pb, pk = nextps()
            P.op('pe', lambda e: e.matmul(pb[:, :], lhsT=kT1[:, kb * 128:(kb + 1) * 128], rhs=qT1[:, j, qt * 512:(qt + 1) * 512],
                                          start=True, stop=True), reads=['kT1', 'qT1'], writes=[pk])
            P.op('act', lambda e: e.activation(out=E[:, kb, :], in_=pb[:, :], func=AF.Exp, scale=0.125), reads=[pk], writes=['Eall%d' % (u % 2)])

        def acc_banks(u):
            if u % 2 == 0:
                return ps[4][:, :], 'ps4', ps[5][:, :], 'ps5'
            return pstf[0], 'pst0', pstf[1], 'pst1'

        def PV_step(u, kb):
            E = Ebufs[u % 2]
            ek = 'Eall%d' % (u % 2)
            pa, pak, pd, pdk = acc_banks(u)
            P.op('pe', lambda e: e.matmul(pa, lhsT=Vx1[:, kb, :], rhs=E[:, kb, :], start=(kb == 0), stop=(kb == NCH - 1)),
                 reads=[ek, 'Vx1'], writes=[pak], signal=(kb == NCH - 1))
            P.op('pe', lambda e: e.matmul(pd, lhsT=ones_b, rhs=E[:, kb, :], start=(kb == 0), stop=(kb == NCH - 1)),
                 reads=[ek, 'ones_b'], writes=[pdk], signal=(kb == NCH - 1))

        def epilogue(u):
            qt, j = units[u]
            pa, pak, pd, pdk = acc_banks(u)
            rd = rdn[u % 2]
            rk = 'rdn%d' % (u % 2)
            od, odk = (o0, 'o0') if j == 0 else (o1, 'o1')
            P.op('dve', lambda e: e.reciprocal(out=rd, in_=pd), reads=[pdk], writes=[rk])
            P.op('dve', lambda e: e.tensor_tensor(out=od, in0=pa, in1=rd, op=ALU.mult), reads=[pak, rk], writes=[odk])
            if j == 0:
                return
            P.op('dve', lambda e: e.scalar_tensor_tensor(out=o0, in0=o1, scalar=lams[:, 3:4], in1=o0, op0=ALU.mult, op1=ALU.add),
                 reads=['o0', 'o1', 'lams'], writes=['o0'])
            P.op('act', lambda e: e.activation(out=sqb, in_=o0, func=AF.Square), reads=['o0'], writes=['sqb'])
            pb, pk = nextps()
            P.op('pe', lambda e: e.matmul(pb[:, :], lhsT=ones_b, rhs=sqb, start=True, stop=True), reads=['ones_b', 'sqb'], writes=[pk])
            P.op('dve', lambda e: e.tensor_scalar(out=rd, in0=pb[:, :], scalar1=1.0 / 128, scalar2=1e-6, op0=ALU.mult, op1=ALU.add), reads=[pk], writes=[rk])
            P.op('act', lambda e: e.sqrt(out=rd, in_=rd), reads=[rk], writes=[rk])
            P.op('dve', lambda e: e.reciprocal(out=rd, in_=rd), reads=[rk], writes=[rk])
            P.op('dve', lambda e: e.scalar_tensor_tensor(out=mixT1[:, h, qt * 512:(qt + 1) * 512], in0=o0, scalar=sgcol[:, 0:1], in1=rd,
                                                         op0=ALU.mult, op1=ALU.mult), reads=['o0', 'sgcol', rk], writes=['mixT1'])

        for kb in range(NCH):
            S_step(0, kb)
        for u in range(len(units)):
            for kb in range(NCH):
                if u + 1 < len(units):
                    S_step(u + 1, kb)
                PV_step(u, kb)
            epilogue(u)

    A.release(m_l1b)
    outproj_phase(1, od_w_out, xs, mixT1, 'mixT1', 256, list(range(2, NCH)))
    A.release(m_l1)
    if stage == 6:
        dx = T("dx", [128, D], F32)
        for c in range(NCH):
            P.dma(dx, xs[c * 128:(c + 1) * 128, :], reads=['xs%d' % c], writes=['dx'])
            P.dma(dbg_o[c * 128:(c + 1) * 128, :], dx, reads=['dx'], writes=['dbg_o'])
        P.finish(['dbg_o'])
        return
    moe_phase(1, False)

    mf = A.mark()
    P.dma(gB, final_g[0:1, :].to_broadcast([128, D]), writes=['gB'])
    fx = [T("fx%d" % i, [128, D], F32) for i in range(2)]
    fo = [T("fo%d" % i, [128, D], F32) for i in range(2)]
    junk = T("junk", [128, D], BF16)
    for c in range(2, NCH):
        i = c % 2
        norm_mod(xs[c * 128:(c + 1) * 128, :], fx[i], 'fx%d' % i, fo[i], 'fo%d' % i, junk, gB, 'gB', None, None, si=i, srckey='xs%d' % c)
        P.dma(out[(c - 2) * 128:(c - 1) * 128, :], fo[i], reads=['fo%d' % i], writes=['out%d' % c], q='act')
        if dbg:
            P.dma(dbg_o[c * 128:(c + 1) * 128, :], fo[i], reads=['fo%d' % i], writes=['dbg_o'])
    A.release(mf)
    if dbg:
        P.finish(['dbg_o'])

    P.finish(['out%d' % c for c in range(2, NCH)])


def _consts():
    c = {}
    c["c_ident"] = np.eye(128, dtype=np.float32)
    t = np.arange(NL)
    row = (t // 64).astype(np.float32)
    col = (t % 64).astype(np.float32)
    inv = (np.float32(10000.0) ** (-np.arange(16, dtype=np.float32) / np.float32(16))).astype(np.float32)
    ang_r = row[:, None] * inv[None, :]
    ang_c = col[:, None] * inv[None, :]
    ang = np.concatenate([ang_r, ang_r, ang_c, ang_c], axis=-1).astype(np.float32)
    c["c_cos"] = np.ascontiguousarray(np.concatenate([np.cos(ang).T, np.cos(ang).T], axis=0).astype(np.float32))
    c["c_sin"] = np.ascontiguousarray(np.concatenate([np.sin(ang).T, np.sin(ang).T], axis=0).astype(np.float32))
    gp = np.zeros((128, 8, 240), np.float32)
    ep = np.zeros((128, 8, 240), np.float32)
    sh = np.zeros((16, 8, 128), np.float32)
    for g in range(8):
        for h in range(16):
            gp[16 * g + h, g, 112 + h] = 1.0
            ep[16 * g + h, g, 112 + h] = 1.0
            sh[h, g, 16 * g + h] = 1.0
    c["c_gp"] = gp
    c["c_ep"] = ep
    c["c_sh"] = sh
    kk = np.arange(128)[:, None]
    qq = np.arange(128)[None, :]
    c["c_mask"] = np.ascontiguousarray(np.stack([(kk >= qq), (kk <= qq)], axis=1).astype(np.float32))
    return c


def _in_map(inputs, b):
    m = {}
    m["xin"] = np.ascontiguousarray(np.concatenate([inputs["ctx"][b], inputs["x"][b]], axis=0), dtype=np.float32)
    m["cc"] = np.ascontiguousarray(np.stack([inputs["c"][b], inputs["c_ctx"]], axis=0), dtype=np.float32)
    for k in ["ada_w", "ada_b", "norm_mix_g", "norm_ffn_g"]:
        m[k] = np.ascontiguousarray(inputs[k], dtype=np.float32)
    m["ev_w_in"] = np.ascontiguousarray(inputs["ev_w_in"][0], dtype=np.float32)
    m["ev_w_out"] = np.ascontiguousarray(inputs["ev_w_out"][0], dtype=np.float32)
    m["wa_sink"] = np.ascontiguousarray(inputs["wa_sink"], dtype=np.float32).reshape(1, 8)
    for k in ["s5_lam_re", "s5_lam_im", "s5_log_step", "s5_b_re", "s5_b_im", "s5_c_re", "s5_c_im", "s5_d", "s5_glu_w", "s5_glu_b"]:
        m[k] = np.ascontiguousarray(inputs[k][0], dtype=np.float32)
    for k in ["moe_router", "moe_w_gate", "moe_w_up", "moe_w_down"]:
        m[k] = np.ascontiguousarray(inputs[k], dtype=np.float32)
    m["od_w_in"] = np.ascontiguousarray(inputs["od_w_in"][0], dtype=np.float32)
    m["od_w_out"] = np.ascontiguousarray(inputs["od_w_out"][0], dtype=np.float32)
    m["da_l"] = np.ascontiguousarray(np.stack([inputs["da_lq1"][0], inputs["da_lk1"][0], inputs["da_lq2"][0], inputs["da_lk2"][0]], axis=0), dtype=np.float32)
    m["da_subln_g"] = np.ascontiguousarray(inputs["da_subln_g"], dtype=np.float32).reshape(1, 128)
    m["final_g"] = np.ascontiguousarray(inputs["final_g"], dtype=np.float32).reshape(1, D)
    m.update(_consts())
    return m


def kernel(**inputs):
    nc = build()
    in_maps = [_in_map(inputs, b) for b in range(8)]
    res = run_bass_kernel_spmd(nc, in_maps, core_ids=list(range(8)))
    return np.stack([r["out"] for r in res.results], axis=0).astype(np.float32)
```

```python
import contextlib
import math
import numpy as np
import concourse.bass as bass
import concourse.mybir as mybir
from concourse.bass_utils import run_bass_kernel_spmd
from concourse.alu_op_type import AluOpType as ALU

dt = mybir.dt
F32, BF16, U32, I32 = dt.float32, dt.bfloat16, dt.uint32, dt.int32
AF = mybir.ActivationFunctionType
AX = mybir.AxisListType

D = 1024
NL = 2048
NC_ = 256
NT = NL + NC_
NCH = NT // 128
NE = 16
DF = 2048
NDS = 40


class Prog:
    def __init__(self, nc, es):
        self.nc = nc
        self.es = es
        self.eng = {'pe': nc.tensor, 'act': nc.scalar, 'dve': nc.vector, 'pool': nc.gpsimd, 'sp': nc.sync}
        self.sem = {k: es.enter_context(nc.semaphore('s_' + k)) for k in self.eng}
        self.cnt = {k: 0 for k in self.eng}
        self.waited = {k: {} for k in self.eng}
        self.dsem = [es.enter_context(nc.semaphore('d%d' % i)) for i in range(NDS)]
        self.dcnt = [0] * NDS
        self.dnext = 0
        self.dlast = [None] * NDS
        self.lastw = {}
        self.readers = {}
        self.nops = 0

    def _deps(self, reads, writes):
        toks = []
        for k in reads:
            if k in self.lastw:
                toks.append(self.lastw[k])
        for k in writes:
            if k in self.lastw:
                toks.append(self.lastw[k])
            toks.extend(self.readers.get(k, ()))
        return toks

    def _commit(self, tok, reads, writes):
        for k in reads:
            self.readers.setdefault(k, []).append(tok)
        for k in writes:
            self.lastw[k] = tok
            self.readers[k] = []

    def _wait(self, e, toks):
        best = {}
        for t in toks:
            if t is None:
                continue
            if e == 'pe' and t[0] == 'pe':
                continue
            if t[0] not in best or best[t[0]][2] < t[2]:
                best[t[0]] = t
        for key, t in best.items():
            if self.waited[e].get(key, 0) >= t[2]:
                continue
            self.eng[e].wait_ge(t[1], t[2])
            self.waited[e][key] = t[2]

    def op(self, e, fn, reads=(), writes=(), signal=True):
        self.nops += 1
        self._wait(e, self._deps(reads, writes))
        inst = fn(self.eng[e])
        if signal:
            self.cnt[e] += 1
            inst.then_inc(self.sem[e], 1)
            tok = (e, self.sem[e], self.cnt[e])
        else:
            tok = (e, self.sem[e], self.cnt[e] + 1)
        self._commit(tok, reads, writes)

    def dma(self, out, in_, reads=(), writes=(), q='sp', fn=None):
        self.nops += 1
        i = self.dnext
        self.dnext = (i + 1) % NDS
        self._wait(q, self._deps(reads, writes) + [self.dlast[i]])
        self.dcnt[i] += 16
        if fn is None:
            inst = self.eng[q].dma_start(out=out, in_=in_)
        else:
            inst = fn(self.eng[q])
        inst.then_inc(self.dsem[i], 16)
        tok = ('d%d' % i, self.dsem[i], self.dcnt[i])
        self.dlast[i] = tok
        self._commit(tok, reads, writes)

    def inherit(self, newkey, oldkeys):
        toks = list(self.readers.get(newkey, []))
        if newkey in self.lastw:
            toks.append(self.lastw[newkey])
        for k in oldkeys:
            if k in self.lastw:
                toks.append(self.lastw[k])
            toks.extend(self.readers.get(k, ()))
        self.lastw.pop(newkey, None)
        self.readers[newkey] = toks

    def finish(self, keys):
        toks = [self.lastw[k] for k in keys if k in self.lastw]
        self._wait('sp', toks)


_DSZ = {F32: 4, BF16: 2, U32: 4, I32: 4}


class Arena:
    def __init__(self, nc, es, P, nbytes):
        self.t = es.enter_context(nc.sbuf_tensor("arena", [128, nbytes // 4], F32))
        self.P = P
        self.cap = nbytes
        self.top = 0
        self.live = []
        self.freed = []

    def alloc(self, key, shape, d=F32):
        elems = 1
        for x in shape[1:]:
            elems *= x
        nb = (elems * _DSZ[d] + 63) // 64 * 64
        off = self.top
        self.top += nb
        assert self.top <= self.cap, "arena overflow %s: %d > %d" % (key, self.top, self.cap)
        olds = [k for (k, o, n) in self.freed if o < off + nb and off < o + n]
        self.P.inherit(key, olds)
        self.live.append((key, off, nb))
        ap = self.t[0:shape[0], off // 4:(off + nb) // 4]
        if d != F32:
            ap = ap.bitcast(d)
        ap = ap[:, 0:elems]
        if len(shape) > 2:
            names = ["a%d" % i for i in range(len(shape) - 1)]
            pat = "p (" + " ".join(names) + ") -> p " + " ".join(names)
            ap = ap.rearrange(pat, **{n: v for n, v in zip(names, shape[1:])})
        return ap

    def mark(self):
        return (self.top, len(self.live))

    def release(self, m):
        self.freed.extend(self.live[m[1]:])
        del self.live[m[1]:]
        self.top = m[0]


def build(stage=99, dbg=False):
    nc = bass.Bass("TRN2", target_bir_lowering=False)
    es = contextlib.ExitStack()
    with es:
        _build(nc, es, stage, dbg)
    return nc


def _build(nc, es, stage, dbg):
    def din(name, shape, d=F32):
        return nc.dram_tensor(name, list(shape), d, kind="ExternalInput").ap()

    def dscr(name, shape, d=F32):
        return nc.dram_tensor(name, list(shape), d, kind="Internal").ap()

    xin = din("xin", [NT, D])
    cc = din("cc", [2, D])
    ada_w = din("ada_w", [2, D, 6 * D])
    ada_b = din("ada_b", [2, 6 * D])
    norm_mix_g = din("norm_mix_g", [2, D])
    norm_ffn_g = din("norm_ffn_g", [2, D])
    final_g = din("final_g", [1, D])
    c_ident = din("c_ident", [128, 128])
    c_cos = din("c_cos", [128, NL])
    c_sin = din("c_sin", [128, NL])
    c_mask = din("c_mask", [128, 2, 128])
    ev_w_in = din("ev_w_in", [D, 1280])
    ev_w_out = din("ev_w_out", [D, D])
    wa_sink = din("wa_sink", [1, 8])
    s5_lam_re = din("s5_lam_re", [2, 32, 64])
    s5_lam_im = din("s5_lam_im", [2, 32, 64])
    s5_log_step = din("s5_log_step", [2, 32])
    s5_b_re = din("s5_b_re", [2, 32, 64, 16])
    s5_b_im = din("s5_b_im", [2, 32, 64, 16])
    s5_c_re = din("s5_c_re", [2, 32, 16, 64])
    s5_c_im = din("s5_c_im", [2, 32, 16, 64])
    s5_d = din("s5_d", [512])
    s5_glu_w = din("s5_glu_w", [512, 512])
    s5_glu_b = din("s5_glu_b", [512])
    c_gp = din("c_gp", [128, 8, 240])
    c_ep = din("c_ep", [128, 8, 240])
    c_sh = din("c_sh", [16, 8, 128])
    modrow_d = dscr("modrow_d", [2, 2, 6 * D])
    moe_router = din("moe_router", [2, D, NE])
    moe_w_gate = din("moe_w_gate", [2, NE, D, DF])
    moe_w_up = din("moe_w_up", [2, NE, D, DF])
    moe_w_down = din("moe_w_down", [2, NE, DF, D])
    hbf = dscr("hbf", [NT, D], BF16)
    od_w_in = din("od_w_in", [D, 3 * D])
    od_w_out = din("od_w_out", [D, D])
    da_l = din("da_l", [4, 64])
    da_subln_g = din("da_subln_g", [1, 128])
    out = nc.dram_tensor("out", [NL, D], F32, kind="ExternalOutput").ap()
    dbg_o = nc.dram_tensor("dbg", [NT, D], F32, kind="ExternalOutput").ap() if dbg else None
    xs = dscr("xs", [NT, D])

    P = Prog(nc, es)
    A = Arena(nc, es, P, 212480)
    XS_KEYS = ['xs%d' % c for c in range(NCH)]
    HBF_KEYS = ['hbf%d' % c for c in range(NCH)]

    def T(name, shape, d=F32):
        return A.alloc(name, shape, d)

    def PS(name, shape, d=F32):
        return es.enter_context(nc.psum_tensor(name, list(shape), d))

    ps = [PS("ps%d" % i, [128, 512], F32) for i in range(6)]
    pst = [PS("pst%d" % i, [128, 1024], BF16) for i in range(2)]
    rr = [0]

    def nextps():
        rr[0] = (rr[0] + 1) % 4
        return ps[rr[0]], 'ps%d' % rr[0]

    ident_f = T("ident_f", [128, 128], F32)
    ident_b = T("ident_b", [128, 128], BF16)
    P.dma(ident_f, c_ident[:, :], writes=['ident_f'])
    P.op('dve', lambda e: e.tensor_copy(out=ident_b, in_=ident_f), reads=['ident_f'], writes=['ident_b'])
    selrow = T("selrow", [2, 2, 128], F32)
    for r in range(2):
        P.op('dve', lambda e, r=r: e.tensor_copy(out=selrow[:, r, :], in_=ident_f[0:2, r:r + 1].to_broadcast([2, 128])),
             reads=['ident_f'], writes=['selrow'])
    stat_all = T("stat", [128, 4, 4], F32)
    den4 = T("den4", [128, 4], F32)
    sT = T("sT", [128, 8, 2], F32)
    with nc.allow_non_contiguous_dma(reason="tiny transposed load"):
        for r in range(2):
            P.dma(sT[:, :, r], cc[r, :].rearrange("(k p) -> p k", p=128), writes=['sT'])
    P.op('act', lambda e: e.activation(out=sT, in_=sT, func=AF.Silu), reads=['sT'], writes=['sT'])
    bc = [T("bc%d" % i, [128, D], F32) for i in range(4)]
    gB = T("gB", [128, D], F32)

    def mod_rows(l):
        m = A.mark()
        wa = [T("wa%d" % i, [128, 8, 512], F32) for i in range(2)]
        adab = [T("adab%d" % i, [2, 512], F32) for i in range(2)]
        mrow = [T("mrow%d" % i, [2, 512], F32) for i in range(2)]
        for ct in range(12):
            i = ct % 2
            wk = 'wa%d' % i
            P.dma(wa[i], ada_w[l, :, ct * 512:(ct + 1) * 512].rearrange("(k p) c -> p k c", p=128), writes=[wk])
            P.dma(adab[i], ada_b[l:l + 1, ct * 512:(ct + 1) * 512].to_broadcast([2, 512]), writes=['adab%d' % i])
            pb, pk = ps[4 + i], 'ps%d' % (4 + i)
            for k in range(8):
                P.op('pe', lambda e, k=k, i=i, pb=pb: e.matmul(pb[0:2, :], lhsT=sT[:, k, :], rhs=wa[i][:, k, :],
                                                               start=(k == 0), stop=(k == 7)),
                     reads=['sT', wk], writes=[pk], signal=(k == 7))
            P.op('dve', lambda e, pb=pb, i=i: e.tensor_tensor(out=mrow[i], in0=pb[0:2, :], in1=adab[i], op=ALU.add),
                 reads=[pk, 'adab%d' % i], writes=['mrow%d' % i])
            P.dma(modrow_d[l, :, ct * 512:(ct + 1) * 512], mrow[i], reads=['mrow%d' % i], writes=['modrow%d_%d' % (l, ct)], q='act')
        A.release(m)

    def mod_rows_bg(l, cts, stage_):
        banks = [(ps[4][:, :], 'ps4'), (ps[5][:, :], 'ps5'), (pst[0][:, :].bitcast(F32), 'pst0')]
        mk_ = A.mark()
        if stage_ == 0:
            wab = [T("wab%d" % i, [128, 8, 128], F32) for i in range(2)]
            n_ = 0
            for bi, ct in enumerate(cts):
                pb, pk = banks[bi]
                for j in range(4):
                    i = n_ % 2
                    n_ += 1
                    c0 = ct * 512 + j * 128
                    P.dma(wab[i], ada_w[l, :, c0:c0 + 128].rearrange("(k p) c -> p k c", p=128), writes=['wab%d' % i])
                    for k in range(8):
                        P.op('pe', lambda e, k=k, i=i, pb=pb, j=j: e.matmul(pb[0:2, j * 128:(j + 1) * 128], lhsT=sT[:, k, :], rhs=wab[i][:, k, :],
                                                                          start=(k == 0), stop=(k == 7)),
                             reads=['sT', 'wab%d' % i], writes=[pk], signal=(k == 7))
        else:
            adab = [T("adabg%d" % i, [2, 512], F32) for i in range(2)]
            mrow = [T("mrowg%d" % i, [2, 512], F32) for i in range(2)]
            for bi, ct in enumerate(cts):
                pb, pk = banks[bi]
                i = bi % 2
                P.dma(adab[i], ada_b[l:l + 1, ct * 512:(ct + 1) * 512].to_broadcast([2, 512]), writes=['adabg%d' % i])
                P.op('dve', lambda e, pb=pb, i=i: e.tensor_tensor(out=mrow[i], in0=pb[0:2, :], in1=adab[i], op=ALU.add),
                     reads=[pk, 'adabg%d' % i], writes=['mrowg%d' % i])
                P.dma(modrow_d[l, :, ct * 512:(ct + 1) * 512], mrow[i], reads=['mrowg%d' % i], writes=['modrow%d_%d' % (l, ct)], q='act')
        A.release(mk_)

    cur_l = [0]

    def bcast(dst, dkey, r, which):
        P.dma(dst, modrow_d[cur_l[0], r:r + 1, which * D:(which + 1) * D].to_broadcast([128, D]),
              reads=['modrow%d_%d' % (cur_l[0], 2 * which), 'modrow%d_%d' % (cur_l[0], 2 * which + 1)], writes=[dkey])

    def make_gs(dst, dkey, r, which_scale, gsrc):
        P.dma(gB, gsrc.to_broadcast([128, D]), writes=['gB'])
        bcast(dst, dkey, r, which_scale)
        P.op('dve', lambda e: e.scalar_tensor_tensor(out=dst, in0=dst, scalar=1.0, in1=gB, op0=ALU.add, op1=ALU.mult),
             reads=[dkey, 'gB'], writes=[dkey])

    def norm_mod(src_ap, xtile, xkey, htile, hkey, junk, gs, gskey, sh, shkey, eps=1e-6, si=0, srckey=None):
        stat = stat_all[:, si, :]
        sk = 'stat%d' % si
        P.dma(xtile, src_ap, reads=([srckey] if srckey else []), writes=[xkey])
        P.op('act', lambda e: e.activation(out=junk, in_=xtile, func=AF.Square, accum_out=stat[:, 0:1]),
             reads=[xkey], writes=['junk', sk])
        P.op('dve', lambda e: e.tensor_scalar(out=stat[:, 1:2], in0=stat[:, 0:1], scalar1=1.0 / D, scalar2=eps,
                                              op0=ALU.mult, op1=ALU.add), reads=[sk], writes=[sk])
        P.op('act', lambda e: e.sqrt(out=stat[:, 2:3], in_=stat[:, 1:2]), reads=[sk], writes=[sk])
        P.op('dve', lambda e: e.reciprocal(out=stat[:, 3:4], in_=stat[:, 2:3]), reads=[sk], writes=[sk])
        P.op('dve', lambda e: e.scalar_tensor_tensor(out=htile, in0=xtile, scalar=stat[:, 3:4], in1=gs,
                                                     op0=ALU.mult, op1=ALU.mult),
             reads=[xkey, sk, gskey], writes=[hkey])
        if sh is not None:
            P.op('pool', lambda e: e.tensor_tensor(out=htile, in0=htile, in1=sh, op=ALU.add),
                 reads=[hkey, shkey], writes=[hkey])

    def norm_phase(gain, which_shift, which_scale, src, consumer, src_is_xs=True, lag=None):
        m = A.mark()
        xt = [T("xt%d" % i, [128, D], F32) for i in range(4)]
        ht = [T("ht%d" % i, [128, D], F32) for i in range(4)]
        junk = T("junk", [128, D], BF16)
        make_gs(bc[0], 'bc0', 0, which_scale, gain)
        bcast(bc[1], 'bc1', 0, which_shift)
        make_gs(bc[2], 'bc2', 1, which_scale, gain)
        bcast(bc[3], 'bc3', 1, which_shift)
        pending = []
        for c in range(NCH):
            i = c % 4
            lat = c >= 2
            norm_mod(src[c * 128:(c + 1) * 128, :], xt[i], 'xt%d' % i, ht[i], 'ht%d' % i, junk,
                     bc[0] if lat else bc[2], 'bc0' if lat else 'bc2', bc[1] if lat else bc[3], 'bc1' if lat else 'bc3', si=i,
                     srckey=('xs%d' % c) if src_is_xs else None)
            pending.append((c, ht[i], 'ht%d' % i))
            if len(pending) > (3 if (lag is None and cur_l[0] == 0) else (lag or 0)):
                consumer(*pending.pop(0))
        while pending:
            consumer(*pending.pop(0))
        A.release(m)

    def to_hT(hT, hb):
        def f(c, htile, hkey):
            i = c % 2
            P.op('act', lambda e: e.copy(out=hb[i], in_=htile), reads=[hkey], writes=['hb%d' % i])
            for k in range(8):
                P.op('pe', lambda e, k=k: e.transpose(out=pst[i][:, k * 128:(k + 1) * 128], in_=hb[i][:, k * 128:(k + 1) * 128],
                                                      identity=ident_b),
                     reads=['hb%d' % i, 'ident_b'], writes=['pst%d' % i], signal=(k == 7))
            P.op('dve', lambda e: e.tensor_copy(out=hT[:, :, c * 128:(c + 1) * 128],
                                                in_=pst[i][:, :].rearrange("p (k t) -> p k t", k=8)),
                 reads=['pst%d' % i], writes=['hT'])
            if dbg and stage == 1:
                P.dma(dbg_o[c * 128:(c + 1) * 128, :], htile, reads=[hkey], writes=['dbg_o'])
        return f

    mod_rows(0)
    m_mixer = A.mark()
    hT = T("hT", [128, 8, NT], BF16)
    uT = T("uT", [128, 4, NT], BF16)
    m1 = A.mark()
    hb = [T("hb%d" % i, [128, D], BF16) for i in range(2)]
    norm_phase(norm_mix_g[0:1, :], 0, 1, xin, to_hT(hT, hb), src_is_xs=False)
    A.release(m1)
    if stage == 1:
        P.finish(['dbg_o'])
        return

    m_att = A.mark()
    w_in_b = T("w_in_b", [128, 8, 1280], BF16)
    wr_b = T("wr_b", [128, 8, 640], BF16)
    P.dma(w_in_b, ev_w_in.rearrange("(k p) c -> p k c", p=128), writes=['w_in_b'], q='pool')
    for k in range(8):
        srcv = w_in_b[:, k, 512:1152].rearrange("p (m b j) -> p m b j", b=2, j=16)
        dstv = wr_b[:, k, :].rearrange("p (m b j) -> p m b j", b=2, j=16)
        P.op('act', lambda e, srcv=srcv, dstv=dstv: e.mul(out=dstv[:, :, 0, :], in_=srcv[:, :, 1, :], mul=-1.0),
             reads=['w_in_b'], writes=['wr_b'])
        P.op('dve', lambda e, srcv=srcv, dstv=dstv: e.tensor_copy(out=dstv[:, :, 1, :], in_=srcv[:, :, 0, :]),
             reads=['w_in_b'], writes=['wr_b'])
    cosT = T("cosT", [128, NL], F32)
    sinT = T("sinT", [128, NL], F32)
    P.dma(cosT, c_cos[:, :], writes=['cosT'])
    P.dma(sinT, c_sin[:, :], writes=['sinT'])
    maskb = T("maskb", [128, 2, 4, 128], BF16)
    esink = T("esink", [128, 8], F32)
    m2 = A.mark()
    maskf = T("maskf", [128, 2, 128], F32)
    P.dma(maskf, c_mask[:, :, :], writes=['maskf'])
    for g in range(4):
        P.op('dve', lambda e, g=g: e.tensor_copy(out=maskb[:, :, g, :], in_=maskf), reads=['maskf'], writes=['maskb'])
    A.release(m2)
    P.dma(esink, wa_sink[0:1, :].to_broadcast([128, 8]), writes=['esink'])
    P.op('act', lambda e: e.activation(out=esink, in_=esink, func=AF.Exp), reads=['esink'], writes=['esink'])

    qT = T("qT", [128, 2, NCH, 4, 128], BF16)
    P.op('pool', lambda e: e.memset(qT, 0.0), writes=['qT'])
    wq_p = T("wq_p", [128, 8, 4, 128], BF16)
    wqr_p = T("wqr_p", [128, 8, 4, 128], BF16)
    for k in range(8):
        P.op('act', lambda e, k=k: e.copy(out=wq_p[:, k, :, :].rearrange("p g (a j) -> p g a j", a=2),
                                          in_=w_in_b[:, k, 512:1024].rearrange("p (a g j) -> p g a j", a=2, g=4)),
             reads=['w_in_b'], writes=['wq_p'])
        P.op('dve', lambda e, k=k: e.tensor_copy(out=wqr_p[:, k, :, :].rearrange("p g (a j) -> p g a j", a=2),
                                                 in_=wr_b[:, k, 0:512].rearrange("p (a g j) -> p g a j", a=2, g=4)),
             reads=['wr_b'], writes=['wqr_p'])
    kT = T("kT", [128, NT], BF16)
    Vx = T("Vx", [128, NCH, 2, 65], BF16)
    rtmp = [T("rtmp%d" % i, [128, 512], F32) for i in range(2)]
    P.op('pool', lambda e: e.memset(Vx, 1.0), writes=['Vx'])
    ttiles = [(0, 256)] + [(256 + i * 512, 512) for i in range(4)]

    for cch in range(4):
        for (t0, tn) in ttiles:
            pb, pk = nextps()
            for k in range(8):
                P.op('pe', lambda e, k=k, pb=pb, t0=t0, tn=tn: e.matmul(pb[:, 0:tn], lhsT=w_in_b[:, k, cch * 128:(cch + 1) * 128],
                                                                     rhs=hT[:, k, t0:t0 + tn], start=(k == 0), stop=(k == 7)),
                     reads=['w_in_b', 'hT'], writes=[pk], signal=(k == 7))
            P.op('act', lambda e, pb=pb, t0=t0, tn=tn: e.copy(out=uT[:, cch, t0:t0 + tn], in_=pb[:, 0:tn]),
                 reads=[pk], writes=['uT'])

    def proj_rope(parts, dkey, lhs_plain, lhs_rot, view=lambda ap: ap):
        for (t0, tn) in ttiles:
            pb, pk = nextps()
            for k in range(8):
                P.op('pe', lambda e, k=k, pb=pb, t0=t0, tn=tn: e.matmul(pb[:, 0:tn], lhsT=lhs_plain(k), rhs=hT[:, k, t0:t0 + tn],
                                                                     start=(k == 0), stop=(k == 7)),
                     reads=['w_in_b', 'wq_p', 'hT'], writes=[pk], signal=(k == 7))
            if t0 == 0:
                for (p0, p1, dst_of) in parts:
                    P.op('act', lambda e, pb=pb, tn=tn, t0=t0, p0=p0, p1=p1, dst_of=dst_of: e.copy(out=dst_of(t0, tn), in_=view(pb[p0:p1, 0:tn])),
                         reads=[pk], writes=[dkey])
                continue
            pb2, pk2 = nextps()
            for k in range(8):
                P.op('pe', lambda e, k=k, pb2=pb2, t0=t0, tn=tn: e.matmul(pb2[:, 0:tn], lhsT=lhs_rot(k), rhs=hT[:, k, t0:t0 + tn],
                                                                       start=(k == 0), stop=(k == 7)),
                     reads=['wr_b', 'wqr_p', 'hT'], writes=[pk2], signal=(k == 7))
            l0 = t0 - 256
            P.op('dve', lambda e, pb=pb, l0=l0: e.tensor_tensor(out=rtmp[0], in0=pb[:, :], in1=cosT[:, l0:l0 + 512], op=ALU.mult),
                 reads=[pk, 'cosT'], writes=['rtmp0'])
            P.op('dve', lambda e, pb2=pb2, l0=l0: e.tensor_tensor(out=rtmp[1], in0=pb2[:, :], in1=sinT[:, l0:l0 + 512], op=ALU.mult),
                 reads=[pk2, 'sinT'], writes=['rtmp1'])
            for (p0, p1, dst_of) in parts:
                P.op('pool', lambda e, t0=t0, p0=p0, p1=p1, dst_of=dst_of: e.tensor_tensor(out=dst_of(t0, 512), in0=view(rtmp[0][p0:p1, :]),
                                                                                        in1=view(rtmp[1][p0:p1, :]), op=ALU.add),
                     reads=['rtmp0', 'rtmp1'], writes=[dkey])

    for g in range(4):
        proj_rope([(0, 64, lambda t0, tn, g=g: qT[0:64, 0, t0 // 128:(t0 + tn) // 128, g, :]),
                   (64, 128, lambda t0, tn, g=g: qT[64:128, 1, t0 // 128:(t0 + tn) // 128, g, :])], 'qT',
                  lambda k, g=g: wq_p[:, k, g, :], lambda k, g=g: wqr_p[:, k, g, :],
                  view=lambda ap: ap.rearrange("p (c t) -> p c t", t=128))
    proj_rope([(0, 128, lambda t0, tn: kT[:, t0:t0 + tn])], 'kT', lambda k: w_in_b[:, k, 1024:1152], lambda k: wr_b[:, k, 512:640])
    for c in range(NCH):
        pb, pk = nextps()
        for k in range(8):
            P.op('pe', lambda e, k=k, pb=pb, c=c: e.matmul(pb[:, 0:128], lhsT=hT[:, k, c * 128:(c + 1) * 128], rhs=w_in_b[:, k, 1152:1280],
                                                        start=(k == 0), stop=(k == 7)),
                 reads=['w_in_b', 'hT'], writes=[pk], signal=(k == 7))
        P.op('act', lambda e, pb=pb, c=c: e.copy(out=Vx[:, c, :, 0:64], in_=pb[:, 0:128].rearrange("p (h j) -> p h j", h=2)),
             reads=[pk], writes=['Vx'])

    m3 = A.mark()
    Ebuf = [T("Ebuf%d" % i, [128, 512], BF16) for i in range(5)]
    aw = [T("aw%d" % i, [128, 512], BF16) for i in range(2)]
    dbgt = T("dbgt", [128, 512], F32) if dbg else None
    mixT = hT

    for qc in range(NCH):
        awt = aw[qc % 2]
        awk = 'aw%d' % (qc % 2)
        kbs = [(0, None), (1, None)]
        if qc >= 2:
            n = qc - 2
            if n - 1 >= 0:
                kbs.append((qc - 1, 0))
            kbs.append((qc, None))
            if n + 1 <= 15:
                kbs.append((qc + 1, 1))
        for kh in range(2):
            p0 = 64 * kh
            for bi, (kb, mk) in enumerate(kbs):
                pb, pk = nextps()
                P.op('pe', lambda e, pb=pb, kb=kb: e.matmul(pb[:, :], lhsT=kT[:, kb * 128:(kb + 1) * 128],
                                                         rhs=qT[:, kh, qc, :, :].rearrange("p g q -> p (g q)"), start=True, stop=True),
                     reads=['kT', 'qT'], writes=[pk])
                P.op('act', lambda e, pb=pb, bi=bi: e.activation(out=Ebuf[bi], in_=pb[:, :], func=AF.Exp, scale=0.125),
                     reads=[pk], writes=['Ebuf%d' % bi])
                if mk is not None:
                    P.op('dve', lambda e, bi=bi, mk=mk: e.tensor_tensor(out=Ebuf[bi], in0=Ebuf[bi],
                                                                       in1=maskb[:, mk, :, :].rearrange("p g q -> p (g q)"), op=ALU.mult),
                         reads=['Ebuf%d' % bi, 'maskb'], writes=['Ebuf%d' % bi])
            po, pok = ps[4 + kh], 'ps%d' % (4 + kh)
            for g in range(4):
                for bi, (kb, mk) in enumerate(kbs):
                    P.op('pe', lambda e, g=g, bi=bi, kb=kb: e.matmul(po[:, g * 65:(g + 1) * 65], lhsT=Ebuf[bi][:, g * 128:(g + 1) * 128],
                                                                  rhs=Vx[:, kb, kh, :], start=(bi == 0), stop=(bi == len(kbs) - 1)),
                         reads=['Ebuf%d' % bi, 'Vx'], writes=[pok], signal=(g == 3 and bi == len(kbs) - 1))
            pov = po[:, 0:260].rearrange("p (g j) -> p g j", g=4)
            P.op('dve', lambda e, pov=pov: e.tensor_tensor(out=den4, in0=pov[:, :, 64], in1=esink[:, 4 * kh:4 * kh + 4], op=ALU.add),
                 reads=[pok, 'esink'], writes=['den4'])
            P.op('dve', lambda e: e.reciprocal(out=den4, in_=den4), reads=['den4'], writes=['den4'])
            P.op('dve', lambda e, pov=pov: e.tensor_tensor(out=awt[:, kh * 256:(kh + 1) * 256].rearrange("p (g j) -> p g j", g=4), in0=pov[:, :, 0:64],
                                                          in1=den4.unsqueeze(2).to_broadcast([128, 4, 64]), op=ALU.mult),
                 reads=[pok, 'den4'], writes=[awk])
        if dbg and stage == 2:
            P.op('act', lambda e: e.copy(out=dbgt, in_=awt), reads=[awk], writes=['dbgt'])
            P.dma(dbg_o[qc * 128:(qc + 1) * 128, 0:512], dbgt, reads=['dbgt'], writes=['dbg_o'])
        i = qc % 2
        for j in range(4):
            P.op('pe', lambda e, j=j: e.transpose(out=pst[i][:, j * 128:(j + 1) * 128], in_=awt[:, j * 128:(j + 1) * 128], identity=ident_b),
                 reads=[awk, 'ident_b'], writes=['pst%d' % i], signal=(j == 3))
        P.op('dve', lambda e: e.tensor_copy(out=mixT[:, 4:8, qc * 128:(qc + 1) * 128],
                                            in_=pst[i][:, 0:512].rearrange("p (k t) -> p k t", k=4)),
             reads=['pst%d' % i], writes=['hT'])
    A.release(m_att)
    if stage == 2:
        P.finish(['dbg_o'])
        return

    TWO_PI = 2.0 * math.pi
    NCK = NT // 8
    zT = T("zT", [128, 4, NT], BF16)
    GP = T("GP", [128, 8, 240], BF16)
    EP = T("EP", [128, 8, 240], BF16)
    Shm = T("Shm", [16, 8, 128], BF16)
    P.dma(GP, c_gp[:, :, :], writes=['GP'], q='pool')
    P.dma(EP, c_ep[:, :, :], writes=['EP'], q='pool')
    P.dma(Shm, c_sh[:, :, :], writes=['Shm'], q='pool')
    NSEG = 4
    SEGL = NCK // NSEG
    mio = T("mio", [128, SEGL, 16], F32)
    m_io = A.mark()
    mio_i = T("mio_i", [128, SEGL, 16], I32)
    P.op('pool', lambda e: e.iota(mio_i, pattern=[[1, SEGL], [0, 16]], base=1, channel_multiplier=0), writes=['mio_i'])
    P.op('dve', lambda e: e.tensor_copy(out=mio, in_=mio_i), reads=['mio_i'], writes=['mio'])
    A.release(m_io)
    dcol = T("dcol", [128, 4], F32)
    gbcol = T("gbcol", [128, 4], F32)
    with nc.allow_non_contiguous_dma(reason="tiny transposed loads"):
        P.dma(dcol, s5_d.rearrange("(c p) -> p c", p=128), writes=['dcol'])
        P.dma(gbcol, s5_glu_b.rearrange("(c p) -> p c", p=128), writes=['gbcol'])

    def ew(eng, fn, reads, writes):
        P.op(eng, fn, reads=reads, writes=writes)

    def s5_pass(cch):
        g0 = 8 * cch
        mp = A.mark()
        ISm = T("ISm", [128, 16, 128], BF16)
        ISs = T("ISs", [128, 16, 128], BF16)
        SOm = T("SOm", [128, 16, 128], BF16)
        TKm = T("TKm", [128, 16, 128], BF16)
        AR8 = T("AR8", [128, NSEG, 2, 16], F32)
        AI8 = T("AI8", [128, NSEG, 2, 16], F32)
        PRt = T("PRt", [128, SEGL, 16], F32)
        PIt = T("PIt", [128, SEGL, 16], F32)
        ms = A.mark()
        LAMR = T("LAMR", [128, 16], F32)
        LAMI = T("LAMI", [128, 16], F32)
        DT = T("DT", [128, 16], F32)
        BR = T("BR", [128, 16, 16], F32)
        BI = T("BI", [128, 16, 16], F32)
        CR = T("CR", [128, 16, 16], F32)
        CI = T("CI", [128, 16, 16], F32)
        CRt = T("CRt", [128, 2, 64], F32)
        CIt = T("CIt", [128, 2, 64], F32)
        with nc.allow_non_contiguous_dma(reason="small parameter loads"):
            for k in range(2):
                for hf in range(2):
                    P.dma(LAMR[64 * hf:64 * hf + 64, 8 * k:8 * k + 8], s5_lam_re[k, g0:g0 + 8, :].rearrange("g p -> p g"), writes=['LAMR'])
                    P.dma(LAMI[64 * hf:64 * hf + 64, 8 * k:8 * k + 8], s5_lam_im[k, g0:g0 + 8, :].rearrange("g p -> p g"), writes=['LAMI'])
                    P.dma(BR[64 * hf:64 * hf + 64, 8 * k:8 * k + 8, :], s5_b_re[k, g0:g0 + 8, :, :].rearrange("g p h -> p g h"), writes=['BR'])
                    P.dma(BI[64 * hf:64 * hf + 64, 8 * k:8 * k + 8, :], s5_b_im[k, g0:g0 + 8, :, :].rearrange("g p h -> p g h"), writes=['BI'])
                P.dma(DT[:, 8 * k:8 * k + 8], s5_log_step[k:k + 1, g0:g0 + 8].to_broadcast([128, 8]), writes=['DT'])
        for k in range(2):
            for (src, tt_, tk_, dst, dk_) in ((s5_c_re, CRt, 'CRt', CR, 'CR'), (s5_c_im, CIt, 'CIt', CI, 'CI')):
                for dup in range(2):
                    P.dma(tt_[:, dup, :], src[k, g0:g0 + 8, :, :].rearrange("g c p -> (g c) p"), writes=[tk_])
                pb, pk = nextps()
                P.op('pe', lambda e, pb=pb, tt_=tt_: e.transpose(out=pb[:, 0:128], in_=tt_.rearrange("r d p -> r (d p)"), identity=ident_f),
                     reads=[tk_, 'ident_f'], writes=[pk])
                P.op('act', lambda e, pb=pb, dst=dst, k=k: e.copy(out=dst[:, 8 * k:8 * k + 8, :], in_=pb[:, 0:128].rearrange("p (g c) -> p g c", g=8)),
                     reads=[pk], writes=[dk_])
        MAG = T("MAG", [128, 16], F32)
        ANG = T("ANG", [128, 16], F32)
        NR = T("NR", [128, 16], F32)
        RR = T("RR", [128, 16], F32)
        SN = T("SN", [128, 16], F32)
        CS = T("CS", [128, 16], F32)
        t1 = T("t1", [128, 16], F32)
        t2 = T("t2", [128, 16], F32)
        FR = T("FR", [128, 16], F32)
        FI = T("FI", [128, 16], F32)
        APR = T("APR", [128, 9, 16], F32)
        API = T("API", [128, 9, 16], F32)
        ew('act', lambda e: e.activation(out=DT, in_=DT, func=AF.Exp), ['DT'], ['DT'])
        ew('dve', lambda e: e.tensor_tensor(out=MAG, in0=LAMR, in1=DT, op=ALU.mult), ['LAMR', 'DT'], ['MAG'])
        ew('act', lambda e: e.activation(out=MAG, in_=MAG, func=AF.Exp), ['MAG'], ['MAG'])
        ew('dve', lambda e: e.tensor_tensor(out=ANG, in0=LAMI, in1=DT, op=ALU.mult), ['LAMI', 'DT'], ['ANG'])
        ew('dve', lambda e: e.tensor_scalar(out=NR, in0=ANG, scalar1=1.0 / TWO_PI, scalar2=12582912.0, op0=ALU.mult, op1=ALU.add), ['ANG'], ['NR'])
        ew('dve', lambda e: e.tensor_scalar(out=NR, in0=NR, scalar1=-12582912.0, scalar2=None, op0=ALU.add), ['NR'], ['NR'])
        ew('dve', lambda e: e.scalar_tensor_tensor(out=RR, in0=NR, scalar=-6.28125, in1=ANG, op0=ALU.mult, op1=ALU.add), ['NR', 'ANG'], ['RR'])
        ew('dve', lambda e: e.scalar_tensor_tensor(out=RR, in0=NR, scalar=-(TWO_PI - 6.28125), in1=RR, op0=ALU.mult, op1=ALU.add), ['NR', 'RR'], ['RR'])
        ew('dve', lambda e: e.tensor_scalar(out=RR, in0=RR, scalar1=math.pi, scalar2=-math.pi, op0=ALU.min, op1=ALU.max), ['RR'], ['RR'])
        ew('act', lambda e: e.activation(out=SN, in_=RR, func=AF.Sin), ['RR'], ['SN'])
        ew('dve', lambda e: e.tensor_scalar(out=t1, in0=RR, scalar1=-1.0, scalar2=None, op0=ALU.mult), ['RR'], ['t1'])
        ew('dve', lambda e: e.tensor_tensor(out=t1, in0=t1, in1=RR, op=ALU.max), ['t1', 'RR'], ['t1'])
        ew('dve', lambda e: e.tensor_scalar(out=t1, in0=t1, scalar1=-1.0, scalar2=math.pi / 2, op0=ALU.mult, op1=ALU.add), ['t1'], ['t1'])
        ew('act', lambda e: e.activation(out=CS, in_=t1, func=AF.Sin), ['t1'], ['CS'])
        ew('dve', lambda e: e.memset(APR[:, 0, :], 1.0), [], ['APR'])
        ew('dve', lambda e: e.memset(API[:, 0, :], 0.0), [], ['API'])
        ew('dve', lambda e: e.tensor_tensor(out=APR[:, 1, :], in0=MAG, in1=CS, op=ALU.mult), ['MAG', 'CS'], ['APR'])
        ew('dve', lambda e: e.tensor_tensor(out=API[:, 1, :], in0=MAG, in1=SN, op=ALU.mult), ['MAG', 'SN'], ['API'])
        for tau in range(1, 8):
            ew('dve', lambda e, tau=tau: e.tensor_tensor(out=t1, in0=APR[:, tau, :], in1=APR[:, 1, :], op=ALU.mult), ['APR'], ['t1'])
            ew('dve', lambda e, tau=tau: e.tensor_tensor(out=t2, in0=API[:, tau, :], in1=API[:, 1, :], op=ALU.mult), ['API'], ['t2'])
            ew('dve', lambda e, tau=tau: e.tensor_tensor(out=APR[:, tau + 1, :], in0=t1, in1=t2, op=ALU.subtract), ['t1', 't2'], ['APR'])
            ew('dve', lambda e, tau=tau: e.tensor_tensor(out=t1, in0=APR[:, tau, :], in1=API[:, 1, :], op=ALU.mult), ['APR', 'API'], ['t1'])
            ew('dve', lambda e, tau=tau: e.tensor_tensor(out=t2, in0=API[:, tau, :], in1=APR[:, 1, :], op=ALU.mult), ['APR', 'API'], ['t2'])
            ew('dve', lambda e, tau=tau: e.tensor_tensor(out=API[:, tau + 1, :], in0=t1, in1=t2, op=ALU.add), ['t1', 't2'], ['API'])
        for sg_ in range(NSEG):
            ew('dve', lambda e, sg_=sg_: e.tensor_copy(out=AR8[:, sg_, 0, :], in_=APR[:, 8, :]), ['APR'], ['AR8'])
            ew('dve', lambda e, sg_=sg_: e.tensor_copy(out=AR8[:, sg_, 1, :], in_=APR[:, 8, :]), ['APR'], ['AR8'])
            ew('dve', lambda e, sg_=sg_: e.tensor_copy(out=AI8[:, sg_, 0, :], in_=API[:, 8, :]), ['API'], ['AI8'])
            ew('dve', lambda e, sg_=sg_: e.tensor_scalar(out=AI8[:, sg_, 1, :], in0=API[:, 8, :], scalar1=-1.0, scalar2=None, op0=ALU.mult), ['API'], ['AI8'])
        TA = T("TA", [128, SEGL, 16], F32)
        TN = T("TN", [128, SEGL, 16], F32)
        TM = T("TM", [128, SEGL, 16], F32)
        TS = T("TS", [128, SEGL, 16], F32)
        bM = lambda x: x.unsqueeze(1).to_broadcast([128, SEGL, 16])
        ew('dve', lambda e: e.tensor_tensor(out=t1, in0=LAMR, in1=DT, op=ALU.mult), ['LAMR', 'DT'], ['t1'])
        ew('dve', lambda e: e.tensor_tensor(out=TM, in0=mio, in1=bM(t1), op=ALU.mult), ['mio', 't1'], ['TM'])
        ew('act', lambda e: e.activation(out=TM, in_=TM, func=AF.Exp, scale=8.0), ['TM'], ['TM'])
        ew('dve', lambda e: e.tensor_tensor(out=TA, in0=mio, in1=bM(ANG), op=ALU.mult), ['mio', 'ANG'], ['TA'])
        ew('dve', lambda e: e.tensor_scalar(out=TA, in0=TA, scalar1=8.0, scalar2=None, op0=ALU.mult), ['TA'], ['TA'])
        ew('dve', lambda e: e.tensor_scalar(out=TN, in0=TA, scalar1=1.0 / TWO_PI, scalar2=12582912.0, op0=ALU.mult, op1=ALU.add), ['TA'], ['TN'])
        ew('dve', lambda e: e.tensor_scalar(out=TN, in0=TN, scalar1=-12582912.0, scalar2=None, op0=ALU.add), ['TN'], ['TN'])
        ew('dve', lambda e: e.scalar_tensor_tensor(out=TA, in0=TN, scalar=-6.28125, in1=TA, op0=ALU.mult, op1=ALU.add), ['TN', 'TA'], ['TA'])
        ew('dve', lambda e: e.scalar_tensor_tensor(out=TA, in0=TN, scalar=-(TWO_PI - 6.28125), in1=TA, op0=ALU.mult, op1=ALU.add), ['TN', 'TA'], ['TA'])
        ew('dve', lambda e: e.tensor_scalar(out=TA, in0=TA, scalar1=math.pi, scalar2=-math.pi, op0=ALU.min, op1=ALU.max), ['TA'], ['TA'])
        ew('act', lambda e: e.activation(out=TS, in_=TA, func=AF.Sin), ['TA'], ['TS'])
        ew('dve', lambda e: e.tensor_scalar(out=TN, in0=TA, scalar1=-1.0, scalar2=None, op0=ALU.mult), ['TA'], ['TN'])
        ew('dve', lambda e: e.tensor_tensor(out=TN, in0=TN, in1=TA, op=ALU.max), ['TN', 'TA'], ['TN'])
        ew('dve', lambda e: e.tensor_scalar(out=TN, in0=TN, scalar1=-1.0, scalar2=math.pi / 2, op0=ALU.mult, op1=ALU.add), ['TN'], ['TN'])
        ew('act', lambda e: e.activation(out=TN, in_=TN, func=AF.Sin), ['TN'], ['TN'])
        ew('dve', lambda e: e.tensor_tensor(out=PRt, in0=TM, in1=TN, op=ALU.mult), ['TM', 'TN'], ['PRt'])
        ew('dve', lambda e: e.tensor_tensor(out=PIt, in0=TM, in1=TS, op=ALU.mult), ['TM', 'TS'], ['PIt'])
        ew('dve', lambda e: e.tensor_tensor(out=t1, in0=LAMR, in1=LAMR, op=ALU.mult), ['LAMR'], ['t1'])
        ew('dve', lambda e: e.tensor_tensor(out=t2, in0=LAMI, in1=LAMI, op=ALU.mult), ['LAMI'], ['t2'])
        ew('dve', lambda e: e.tensor_tensor(out=t1, in0=t1, in1=t2, op=ALU.add), ['t1', 't2'], ['t1'])
        ew('dve', lambda e: e.reciprocal(out=NR, in_=t1), ['t1'], ['NR'])
        ew('dve', lambda e: e.tensor_scalar(out=RR, in0=APR[:, 1, :], scalar1=-1.0, scalar2=None, op0=ALU.add), ['APR'], ['RR'])
        ew('dve', lambda e: e.tensor_tensor(out=t1, in0=RR, in1=LAMR, op=ALU.mult), ['RR', 'LAMR'], ['t1'])
        ew('dve', lambda e: e.tensor_tensor(out=t2, in0=API[:, 1, :], in1=LAMI, op=ALU.mult), ['API', 'LAMI'], ['t2'])
        ew('dve', lambda e: e.tensor_tensor(out=t1, in0=t1, in1=t2, op=ALU.add), ['t1', 't2'], ['t1'])
        ew('dve', lambda e: e.tensor_tensor(out=FR, in0=t1, in1=NR, op=ALU.mult), ['t1', 'NR'], ['FR'])
        ew('dve', lambda e: e.tensor_tensor(out=t1, in0=API[:, 1, :], in1=LAMR, op=ALU.mult), ['API', 'LAMR'], ['t1'])
        ew('dve', lambda e: e.tensor_tensor(out=t2, in0=RR, in1=LAMI, op=ALU.mult), ['RR', 'LAMI'], ['t2'])
        ew('dve', lambda e: e.tensor_tensor(out=t1, in0=t1, in1=t2, op=ALU.subtract), ['t1', 't2'], ['t1'])
        ew('dve', lambda e: e.tensor_tensor(out=FI, in0=t1, in1=NR, op=ALU.mult), ['t1', 'NR'], ['FI'])
        B1 = T("B1", [128, 16, 16], F32)
        B2 = T("B2", [128, 16, 16], F32)
        w1 = T("w1", [128, 16, 16], F32)
        w2 = T("w2", [128, 16, 16], F32)
        bF = lambda x: x.unsqueeze(2).to_broadcast([128, 16, 16])
        ew('dve', lambda e: e.tensor_tensor(out=w1, in0=BR, in1=bF(FR), op=ALU.mult), ['BR', 'FR'], ['w1'])
        ew('dve', lambda e: e.tensor_tensor(out=w2, in0=BI, in1=bF(FI), op=ALU.mult), ['BI', 'FI'], ['w2'])
        ew('dve', lambda e: e.tensor_tensor(out=w1, in0=w1, in1=w2, op=ALU.subtract), ['w1', 'w2'], ['w1'])
        ew('dve', lambda e: e.tensor_tensor(out=w2, in0=BI, in1=bF(FR), op=ALU.mult), ['BI', 'FR', 'w1'], ['w2'])
        ew('dve', lambda e: e.tensor_tensor(out=BI, in0=BR, in1=bF(FI), op=ALU.mult), ['BR', 'FI', 'w2'], ['BI'])
        ew('dve', lambda e: e.tensor_tensor(out=w2, in0=w2, in1=BI, op=ALU.add), ['w2', 'BI'], ['w2'])
        ew('dve', lambda e: e.tensor_copy(out=B1[0:64], in_=w1[0:64]), ['w1'], ['B1'])
        ew('dve', lambda e: e.tensor_copy(out=B1[64:128], in_=w2[64:128]), ['w2'], ['B1'])
        ew('dve', lambda e: e.tensor_scalar(out=B2[0:64], in0=w2[0:64], scalar1=-1.0, scalar2=None, op0=ALU.mult), ['w2'], ['B2'])
        ew('dve', lambda e: e.tensor_copy(out=B2[64:128], in_=w1[64:128]), ['w1'], ['B2'])
        C1 = T("C1", [128, 16, 16], F32)
        C2 = T("C2", [128, 16, 16], F32)
        ew('dve', lambda e: e.tensor_copy(out=C1[0:64], in_=CR[0:64]), ['CR'], ['C1'])
        ew('dve', lambda e: e.tensor_scalar(out=C1[64:128], in0=CI[64:128], scalar1=-1.0, scalar2=None, op0=ALU.mult), ['CI'], ['C1'])
        ew('dve', lambda e: e.tensor_scalar(out=C2[0:64], in0=CI[0:64], scalar1=-1.0, scalar2=None, op0=ALU.mult), ['CI'], ['C2'])
        ew('dve', lambda e: e.tensor_scalar(out=C2[64:128], in0=CR[64:128], scalar1=-1.0, scalar2=None, op0=ALU.mult), ['CR'], ['C2'])
        Zt = T("Zt", [128, 16, 8, 16], BF16)
        Zst = T("Zst", [128, 16, 8, 16], BF16)
        SOx = T("SOx", [128, 16, 9, 16], F32)
        for sidx in range(8):
            tau = 7 - sidx
            par = lambda tau=tau: APR[:, tau, :].unsqueeze(2).to_broadcast([128, 16, 16])
            pai = lambda tau=tau: API[:, tau, :].unsqueeze(2).to_broadcast([128, 16, 16])
            ew('dve', lambda e, par=par: e.tensor_tensor(out=w1, in0=B1, in1=par(), op=ALU.mult), ['B1', 'APR'], ['w1'])
            ew('pool', lambda e, pai=pai: e.tensor_tensor(out=w2, in0=B2, in1=pai(), op=ALU.mult), ['B2', 'API'], ['w2'])
            ew('dve', lambda e, sidx=sidx: e.tensor_tensor(out=Zt[:, :, sidx, :], in0=w1, in1=w2, op=ALU.add), ['w1', 'w2'], ['Zt'])
            ew('dve', lambda e, par=par: e.tensor_tensor(out=w1, in0=B2, in1=par(), op=ALU.mult), ['B2', 'APR'], ['w1'])
            ew('pool', lambda e, pai=pai: e.tensor_tensor(out=w2, in0=B1, in1=pai(), op=ALU.mult), ['B1', 'API'], ['w2'])
            ew('dve', lambda e, sidx=sidx: e.tensor_tensor(out=Zst[:, :, sidx, :], in0=w1, in1=w2, op=ALU.subtract), ['w1', 'w2'], ['Zst'])
        for tau in range(9):
            par = lambda tau=tau: APR[:, tau, :].unsqueeze(2).to_broadcast([128, 16, 16])
            pai = lambda tau=tau: API[:, tau, :].unsqueeze(2).to_broadcast([128, 16, 16])
            ew('dve', lambda e, par=par: e.tensor_tensor(out=w1, in0=C1, in1=par(), op=ALU.mult), ['C1', 'APR'], ['w1'])
            ew('pool', lambda e, pai=pai: e.tensor_tensor(out=w2, in0=C2, in1=pai(), op=ALU.mult), ['C2', 'API'], ['w2'])
            ew('dve', lambda e, tau=tau: e.tensor_tensor(out=SOx[:, :, tau, :], in0=w1, in1=w2, op=ALU.add), ['w1', 'w2'], ['SOx'])
        ew('act', lambda e: e.copy(out=SOm.rearrange("p g (t c) -> p g t c", t=8), in_=SOx[:, :, 1:9, :]), ['SOx'], ['SOm'])
        for gl2 in range(0, 16, 8):
            for (src, sk, dst, dk) in ((Zt, 'Zt', ISm, 'ISm'), (Zst, 'Zst', ISs, 'ISs')):
                i = (gl2 // 8) % 2
                for j in range(8):
                    P.op('pe', lambda e, j=j, src=src, i=i: e.transpose(out=pst[i][:, j * 128:(j + 1) * 128],
                                                                       in_=src[:, gl2 + j, :, :].rearrange("p s h -> p (s h)"), identity=ident_b),
                         reads=[sk, 'ident_b'], writes=['pst%d' % i], signal=(j == 7))
                P.op('dve', lambda e, dst=dst, i=i: e.tensor_copy(out=dst[:, gl2:gl2 + 8, :], in_=pst[i][:, :].rearrange("p (g m) -> p g m", g=8)),
                     reads=['pst%d' % i], writes=[dk])
        KTp = T("KTp", [16, 16, 256], BF16)
        ew('dve', lambda e: e.memset(KTp, 0.0), [], ['KTp'])
        for q4 in range(4):
            pb, pk = nextps()
            for j in range(4):
                gd = q4 * 4 + j
                P.op('pe', lambda e, pb=pb, j=j, gd=gd: e.matmul(pb[0:16, j * 128:(j + 1) * 128], lhsT=B1[:, gd, :],
                                                                rhs=SOx[:, gd, 0:8, :].rearrange("p t c -> p (t c)"), start=True, stop=True),
                     reads=['B1', 'SOx'], writes=[pk], signal=(j == 3))
            P.op('act', lambda e, pb=pb, q4=q4: e.copy(out=KTp[:, q4 * 4:q4 * 4 + 4, 128:256], in_=pb[0:16, :].rearrange("p (g m) -> p g m", g=4)),
                 reads=[pk], writes=['KTp'])
        for q4 in range(4):
            pb, pk = nextps()
            for j in range(4):
                gd = q4 * 4 + j
                for sidx in range(8):
                    P.op('pe', lambda e, pb=pb, j=j, gd=gd, sidx=sidx: e.matmul(pb[:, j * 128:(j + 1) * 128], lhsT=Shm[:, sidx, :],
                                                                               rhs=KTp[:, gd, 128 - 16 * sidx:256 - 16 * sidx],
                                                                               start=(sidx == 0), stop=(sidx == 7)),
                         reads=['Shm', 'KTp'], writes=[pk], signal=(j == 3 and sidx == 7))
            P.op('act', lambda e, pb=pb, q4=q4: e.copy(out=TKm[:, q4 * 4:q4 * 4 + 4, :], in_=pb[:, :].rearrange("p (g m) -> p g m", g=4)),
                 reads=[pk], writes=['TKm'])
        A.release(ms)

        VZ = T("VZ", [128, NCK, 2, 16], F32)
        U = T("U", [128, 16, NCK], BF16)
        m_loop = A.mark()
        lt1 = T("lt1", [128, NSEG, 2, 16], F32)
        lt2 = T("lt2", [128, NSEG, 2, 16], F32)
        ct1 = T("ct1", [128, SEGL, 2, 16], F32)
        ct2 = T("ct2", [128, SEGL, 2, 16], F32)
        for d_ in range(2):
            for gl in range(8):
                gd = 8 * d_ + gl
                pb, pk = nextps()
                for part in range(2):
                    for sp in range(8):
                        win = GP[:, gl, 112 - 16 * sp:240 - 16 * sp]
                        if d_ == 0:
                            c0, c1, rhs = ((0, 32, uT[:, cch, sp:256:8]), (32, 288, uT[:, cch, 256 + sp:NT:8]))[part]
                        else:
                            to = 7 - sp
                            c0, c1, rhs = ((0, 32, uT[:, cch, 248 + to:(to - 8 if to - 8 >= 0 else None):-8]),
                                           (32, 288, uT[:, cch, 2296 + to:248 + to:-8]))[part]
                        P.op('pe', lambda e, pb=pb, c0=c0, c1=c1, rhs=rhs, win=win, sp=sp: e.matmul(pb[:, c0:c1], lhsT=win, rhs=rhs,
                                                                                                 start=(sp == 0), stop=(sp == 7)),
                             reads=['GP', 'uT'], writes=[pk], signal=(sp == 7 and part == 1))
                P.op('act', lambda e, pb=pb, gd=gd: e.copy(out=U[:, gd, :], in_=pb[:, 0:NCK]), reads=[pk], writes=['U'])
        for gd in range(16):
            for (mat, mk, half) in ((ISm, 'ISm', 0), (ISs, 'ISs', 1)):
                pb, pk = nextps()
                P.op('pe', lambda e, pb=pb, mat=mat, gd=gd: e.matmul(pb[:, 0:NCK], lhsT=mat[:, gd, :], rhs=U[:, gd, :], start=True, stop=True),
                     reads=[mk, 'U'], writes=[pk])
                P.op('act' if half == 0 else 'dve',
                     (lambda e, pb=pb, gd=gd, half=half: e.copy(out=VZ[:, :, half, gd], in_=pb[:, 0:NCK])) if half == 0 else
                     (lambda e, pb=pb, gd=gd, half=half: e.tensor_copy(out=VZ[:, :, half, gd], in_=pb[:, 0:NCK])),
                     reads=[pk], writes=['VZ'])
        m_bg = A.mark()
        mod_rows_bg(1, [3 * cch, 3 * cch + 1, 3 * cch + 2], 0)
        VZs = VZ.rearrange("p (s m) x g -> p s m x g", s=NSEG)
        for m_ in range(1, SEGL):
            ew('dve', lambda e, m_=m_: e.tensor_tensor(out=lt1, in0=VZs[:, :, m_ - 1, :, :], in1=AR8, op=ALU.mult), ['VZ', 'AR8'], ['lt1'])
            ew('dve', lambda e, m_=m_: e.tensor_tensor(out=lt2, in0=VZs[:, :, m_ - 1, ::-1, :], in1=AI8, op=ALU.mult), ['VZ', 'AI8'], ['lt2'])
            ew('dve', lambda e: e.tensor_tensor(out=lt1, in0=lt1, in1=lt2, op=ALU.add), ['lt1', 'lt2'], ['lt1'])
            ew('dve', lambda e, m_=m_: e.tensor_tensor(out=VZs[:, :, m_, :, :], in0=VZs[:, :, m_, :, :], in1=lt1, op=ALU.add), ['VZ', 'lt1'], ['VZ'])
        for sg_ in range(1, NSEG):
            cprev = VZ[:, sg_ * SEGL - 1, :, :]
            cb = cprev.unsqueeze(1).to_broadcast([128, SEGL, 2, 16])
            cbs = VZ[:, sg_ * SEGL - 1, ::-1, :].unsqueeze(1).to_broadcast([128, SEGL, 2, 16])
            seg = VZ[:, sg_ * SEGL:(sg_ + 1) * SEGL, :, :]
            prb = PRt.unsqueeze(2).to_broadcast([128, SEGL, 2, 16])
            pib = PIt.unsqueeze(2).to_broadcast([128, SEGL, 2, 16])
            ew('dve', lambda e, cb=cb, prb=prb: e.tensor_tensor(out=ct1, in0=prb, in1=cb, op=ALU.mult), ['PRt', 'VZ'], ['ct1'])
            ew('pool', lambda e, cbs=cbs, pib=pib: e.tensor_tensor(out=ct2, in0=pib, in1=cbs, op=ALU.mult), ['PIt', 'VZ'], ['ct2'])
            ew('dve', lambda e: e.tensor_tensor(out=ct1[:, :, 0, :], in0=ct1[:, :, 0, :], in1=ct2[:, :, 0, :], op=ALU.add), ['ct1', 'ct2'], ['ct1'])
            ew('dve', lambda e: e.tensor_tensor(out=ct1[:, :, 1, :], in0=ct1[:, :, 1, :], in1=ct2[:, :, 1, :], op=ALU.subtract), ['ct1', 'ct2'], ['ct1'])
            ew('dve', lambda e, seg=seg: e.tensor_tensor(out=seg, in0=seg, in1=ct1, op=ALU.add), ['VZ', 'ct1'], ['VZ'])
        mod_rows_bg(1, [3 * cch, 3 * cch + 1, 3 * cch + 2], 1)
        A.release(m_loop)
        Xb = T("Xb", [128, 16, NCK], BF16)
        Yb = T("Yb", [128, 16, NCK], BF16)
        ew('pool', lambda e: e.memset(Xb[:, :, 0:1], 0.0), [], ['Xb'])
        ew('act', lambda e: e.copy(out=Xb[:, :, 1:NCK], in_=VZ[:, 0:NCK - 1, 0, :].rearrange("p n g -> p g n")), ['VZ'], ['Xb'])
        for gd in range(16):
            pb, pk = nextps()
            P.op('pe', lambda e, pb=pb, gd=gd: e.matmul(pb[:, 0:NCK], lhsT=TKm[:, gd, :], rhs=U[:, gd, :], start=True, stop=False),
                 reads=['TKm', 'U'], writes=[pk], signal=False)
            P.op('pe', lambda e, pb=pb, gd=gd: e.matmul(pb[:, 0:NCK], lhsT=SOm[:, gd, :], rhs=Xb[:, gd, :], start=False, stop=True),
                 reads=['SOm', 'Xb'], writes=[pk])
            P.op('act', lambda e, pb=pb, gd=gd: e.copy(out=Yb[:, gd, :], in_=pb[:, 0:NCK]), reads=[pk], writes=['Yb'])
        pre = [T("pre%d" % i, [128, NCK], F32) for i in range(2)]
        pr2 = [T("pr2%d" % i, [128, NCK], F32) for i in range(2)]
        for i_ in range(8):
            pb, pk = nextps()
            b2 = i_ % 2
            for part in range(2):
                for gl in range(8):
                    c0, c1, rhs = ((0, 32, Yb[:, gl, 0:32]), (32, 288, Yb[:, gl, 32:288]))[part]
                    P.op('pe', lambda e, pb=pb, c0=c0, c1=c1, rhs=rhs, gl=gl: e.matmul(pb[:, c0:c1], lhsT=EP[:, i_, 112 - 16 * gl:240 - 16 * gl], rhs=rhs,
                                                                                   start=(gl == 0), stop=False),
                         reads=['EP', 'Yb'], writes=[pk], signal=False)
                for gl in range(8):
                    c0, c1, rhs = ((0, 32, Yb[:, 8 + gl, 31::-1]), (32, 288, Yb[:, 8 + gl, 287:31:-1]))[part]
                    P.op('pe', lambda e, pb=pb, c0=c0, c1=c1, rhs=rhs, gl=gl: e.matmul(pb[:, c0:c1], lhsT=EP[:, 7 - i_, 112 - 16 * gl:240 - 16 * gl], rhs=rhs,
                                                                                   start=False, stop=(gl == 7)),
                         reads=['EP', 'Yb'], writes=[pk], signal=(gl == 7 and part == 1))
            ew('dve', lambda e, pb=pb, b2=b2: e.scalar_tensor_tensor(out=pre[b2], in0=uT[:, cch, i_:NT:8], scalar=dcol[:, cch:cch + 1], in1=pb[:, 0:NCK],
                                                                    op0=ALU.mult, op1=ALU.add), [pk, 'uT', 'dcol'], ['pre%d' % b2])
            ew('act', lambda e, b2=b2: e.activation(out=pr2[b2], in_=pre[b2], func=AF.Square), ['pre%d' % b2], ['pr2%d' % b2])
            ew('dve', lambda e, b2=b2: e.tensor_scalar(out=pr2[b2], in0=pr2[b2], scalar1=0.044715, scalar2=1.0, op0=ALU.mult, op1=ALU.add), ['pr2%d' % b2], ['pr2%d' % b2])
            ew('dve', lambda e, b2=b2: e.tensor_tensor(out=pr2[b2], in0=pr2[b2], in1=pre[b2], op=ALU.mult), ['pr2%d' % b2, 'pre%d' % b2], ['pr2%d' % b2])
            ew('act', lambda e, b2=b2: e.activation(out=pr2[b2], in_=pr2[b2], func=AF.Sigmoid, scale=1.5957691216057308), ['pr2%d' % b2], ['pr2%d' % b2])
            ew('dve', lambda e, b2=b2: e.tensor_tensor(out=zT[:, cch, i_:NT:8], in0=pr2[b2], in1=pre[b2], op=ALU.mult), ['pr2%d' % b2, 'pre%d' % b2], ['zT'])
        A.release(mp)

    for cch in range(4):
        s5_pass(cch)

    m_glu = A.mark()
    gw = T("gw", [128, 4, 512], BF16)
    P.dma(gw, s5_glu_w.rearrange("(k p) c -> p k c", p=128), writes=['gw'], q='pool')
    sg = [T("sg%d" % i, [128, 512], BF16) for i in range(2)]
    for co in range(4):
        for ti, (t0, tn) in enumerate(ttiles):
            pb, pk = nextps()
            for k in range(4):
                P.op('pe', lambda e, k=k, pb=pb, t0=t0, tn=tn: e.matmul(pb[:, 0:tn], lhsT=gw[:, k, co * 128:(co + 1) * 128], rhs=zT[:, k, t0:t0 + tn],
                                                                     start=(k == 0), stop=(k == 3)),
                     reads=['gw', 'zT'], writes=[pk], signal=(k == 3))
            i = ti % 2
            ew('act', lambda e, pb=pb, tn=tn, i=i: e.activation(out=sg[i][:, 0:tn], in_=pb[:, 0:tn], func=AF.Sigmoid, bias=gbcol[:, co:co + 1]),
               [pk, 'gbcol'], ['sg%d' % i])
            ew('dve', lambda e, t0=t0, tn=tn, i=i: e.tensor_tensor(out=mixT[:, co, t0:t0 + tn], in0=sg[i][:, 0:tn], in1=zT[:, co, t0:t0 + tn], op=ALU.mult),
               ['sg%d' % i, 'zT'], ['hT'])
    A.release(m_glu)
    if stage == 3:
        dt_ = T("dt_", [128, 512], F32)
        for c in range(NCH):
            for k in range(4):
                P.op('pe', lambda e, k=k, c=c: e.transpose(out=pst[0][:, k * 128:(k + 1) * 128], in_=mixT[:, k, c * 128:(c + 1) * 128], identity=ident_b),
                     reads=['hT', 'ident_b'], writes=['pst0'], signal=(k == 3))
            ew('act', lambda e: e.copy(out=dt_, in_=pst[0][:, 0:512]), ['pst0'], ['dt_'])
            P.dma(dbg_o[c * 128:(c + 1) * 128, 0:512], dt_, reads=['dt_'], writes=['dbg_o'])
        P.finish(['dbg_o'])
        return

    def outproj_phase(l, w_out_dram, x_src, mixT_, mkey, off, chunks):
        m = A.mark()
        w_out_b = T("w_out_b", [128, 8, D], BF16)
        P.dma(w_out_b, w_out_dram.rearrange("(k p) c -> p k c", p=128), writes=['w_out_b'], q='pool')
        bcast(bc[0], 'bc0', 0, 2)
        bcast(bc[2], 'bc2', 1, 2)
        xo = [T("xo%d" % i, [128, D], F32) for i in range(2)]
        xn = [T("xn%d" % i, [128, D], F32) for i in range(2)]
        for c in chunks:
            i = c % 2
            g2, g2k = (bc[0], 'bc0') if c >= 2 else (bc[2], 'bc2')
            P.dma(xo[i], x_src[c * 128:(c + 1) * 128, :], reads=(['xs%d' % c] if l == 1 else []), writes=['xo%d' % i])
            for hh in range(2):
                pb, pk = nextps()
                for k in range(8):
                    P.op('pe', lambda e, k=k, pb=pb, hh=hh: e.matmul(pb[:, :], lhsT=mixT_[:, k, c * 128 - off:(c + 1) * 128 - off], rhs=w_out_b[:, k, hh * 512:(hh + 1) * 512],
                                                                  start=(k == 0), stop=(k == 7)),
                         reads=[mkey, 'w_out_b'], writes=[pk], signal=(k == 7))
                P.op('dve', lambda e, pb=pb, hh=hh: e.tensor_tensor(out=xn[i][:, hh * 512:(hh + 1) * 512], in0=pb[:, :], in1=g2[:, hh * 512:(hh + 1) * 512], op=ALU.mult),
                     reads=[pk, g2k], writes=['xn%d' % i])
            P.op('pool', lambda e: e.tensor_tensor(out=xn[i], in0=xn[i], in1=xo[i], op=ALU.add), reads=['xn%d' % i, 'xo%d' % i], writes=['xn%d' % i])
            P.dma(xs[c * 128:(c + 1) * 128, :], xn[i], reads=['xn%d' % i], writes=['xs%d' % c], q='pool')
            if dbg and stage == 4:
                P.dma(dbg_o[c * 128:(c + 1) * 128, :], xn[i], reads=['xn%d' % i], writes=['dbg_o'])
        A.release(m)

    outproj_phase(0, ev_w_out, xin, mixT, 'hT', 0, list(range(NCH)))
    A.release(m_mixer)
    if stage == 4:
        P.finish(['dbg_o'] + XS_KEYS)
        return

    def moe_phase(l, with_ctx):
        mm = A.mark()
        nslot = 288 if with_ctx else 256
        scs = [(0, 128, 0), (1, 128, 128)] + ([(2, 32, 256)] if with_ctx else [])
        affT = T("affT", [16, NT], F32)
        wr_f = T("wr_f", [128, 8, NE], F32)
        P.dma(wr_f, moe_router[l].rearrange("(k p) e -> p k e", p=128), writes=['wr_f'])
        hTf2 = [T("hTf%d" % i, [128, 8, 128], F32) for i in range(2)]
        hb2 = [T("hbb%d" % i, [128, D], BF16) for i in range(2)]
        aff2 = [T("aff%d" % i, [128, NE], F32) for i in range(2)]
        sm2 = [T("sm%d" % i, [128, 4], F32) for i in range(2)]

        def cons(c, htile, hkey):
            if c < 2 and not with_ctx:
                return
            i = c % 2
            hTf, hk = hTf2[i], 'hTf%d' % i
            aff, ak = aff2[i], 'aff%d' % i
            sm, sk = sm2[i], 'sm%d' % i
            P.op('act', lambda e: e.copy(out=hb2[i], in_=htile), reads=[hkey], writes=['hbb%d' % i])
            P.dma(hbf[c * 128:(c + 1) * 128, :], hb2[i], reads=['hbb%d' % i], writes=['hbf%d' % c], q='act')
            for half in range(2):
                pb, pk = ps[4 + half], 'ps%d' % (4 + half)
                for k in range(4):
                    kk = half * 4 + k
                    P.op('pe', lambda e, pb=pb, k=k, kk=kk: e.transpose(out=pb[:, k * 128:(k + 1) * 128], in_=htile[:, kk * 128:(kk + 1) * 128], identity=ident_f),
                         reads=[hkey, 'ident_f'], writes=[pk], signal=(k == 3))
                P.op('act' if half == 0 else 'dve',
                     (lambda e, pb=pb, half=half: e.copy(out=hTf[:, half * 4:half * 4 + 4, :], in_=pb[:, :].rearrange("p (k t) -> p k t", k=4))) if half == 0 else
                     (lambda e, pb=pb, half=half: e.tensor_copy(out=hTf[:, half * 4:half * 4 + 4, :], in_=pb[:, :].rearrange("p (k t) -> p k t", k=4))),
                     reads=[pk], writes=[hk])
            pb, pk = nextps()
            for k in range(8):
                P.op('pe', lambda e, pb=pb, k=k: e.matmul(pb[:, 0:NE], lhsT=hTf[:, k, :], rhs=wr_f[:, k, :], start=(k == 0), stop=(k == 7)),
                     reads=[hk, 'wr_f'], writes=[pk], signal=(k == 7))
            P.op('dve', lambda e, pb=pb: e.reduce_max(out=sm[:, 0:1], in_=pb[:, 0:NE], axis=AX.X), reads=[pk], writes=[sk])
            P.op('dve', lambda e: e.tensor_scalar(out=sm[:, 1:2], in0=sm[:, 0:1], scalar1=-1.0, scalar2=None, op0=ALU.mult), reads=[sk], writes=[sk])
            P.op('act', lambda e, pb=pb: e.activation(out=aff, in_=pb[:, 0:NE], func=AF.Exp, bias=sm[:, 1:2], accum_out=sm[:, 2:3]),
                 reads=[pk, sk], writes=[ak, sk])
            P.op('dve', lambda e: e.reciprocal(out=sm[:, 3:4], in_=sm[:, 2:3]), reads=[sk], writes=[sk])
            P.op('dve', lambda e: e.tensor_scalar(out=aff, in0=aff, scalar1=sm[:, 3:4], scalar2=None, op0=ALU.mult), reads=[ak, sk], writes=[ak])
            pb2, pk2 = nextps()
            P.op('pe', lambda e, pb2=pb2: e.transpose(out=pb2[0:NE, 0:128], in_=aff, identity=ident_f), reads=[ak, 'ident_f'], writes=[pk2])
            P.op('act', lambda e, pb2=pb2: e.copy(out=affT[:, c * 128:(c + 1) * 128], in_=pb2[0:NE, 0:128]), reads=[pk2], writes=['affT'])

        norm_phase(norm_ffn_g[l:l + 1, :], 3, 4, xs, cons)

        NB = 4
        Wg = [T("Wg%d" % i, [128, 8, 512], BF16) for i in range(NB)]
        Wu = [T("Wu%d" % i, [128, 8, 512], BF16) for i in range(NB)]
        Wd = [T("Wd%d" % i, [128, 4, D], BF16) for i in range(NB)]

        def load_w(e_, ft, b):
            P.dma(Wg[b], moe_w_gate[l, e_, :, ft * 512:(ft + 1) * 512].rearrange("(k p) f -> p k f", p=128), writes=['Wg%d' % b], q='pool')
            P.dma(Wu[b], moe_w_up[l, e_, :, ft * 512:(ft + 1) * 512].rearrange("(k p) f -> p k f", p=128), writes=['Wu%d' % b], q='pool')
            P.dma(Wd[b], moe_w_down[l, e_, ft * 512:(ft + 1) * 512, :].rearrange("(k p) d -> p k d", p=128), writes=['Wd%d' % b], q='pool')

        tiles = [(e_, ft) for e_ in range(NE) for ft in range(4)]
        for ti in range(NB - 1):
            load_w(tiles[ti][0], tiles[ti][1], ti % NB)

        vals = T("vals", [16, 288], F32)
        idxu = T("idxu", [16, 288], U32)
        idxf = T("idxf", [16, 288], F32)
        mt = A.mark()
        wk = T("wk", [16, NL], F32)
        for (t0, tn, o0, nr) in ([(256, NL, 0, 32)] + ([(0, 256, 256, 4)] if with_ctx else [])):
            cur = affT[:, t0:t0 + tn]
            curk = 'affT'
            for r in range(nr):
                vs = vals[:, o0 + 8 * r:o0 + 8 * r + 8]
                P.op('dve', lambda e, vs=vs, cur=cur: e.max(out=vs, in_=cur), reads=[curk], writes=['vals'])
                P.op('dve', lambda e, vs=vs, cur=cur, r=r, o0=o0: e.max_index(out=idxu[:, o0 + 8 * r:o0 + 8 * r + 8], in_max=vs, in_values=cur),
                     reads=[curk, 'vals'], writes=['idxu'])
                if r < nr - 1:
                    P.op('dve', lambda e, vs=vs, cur=cur, tn=tn: e.match_replace(out=wk[:, 0:tn], in_to_replace=vs, in_values=cur, imm_value=-1.0),
                         reads=[curk, 'vals', 'wk'], writes=['wk'])
                    cur = wk[:, 0:tn]
                    curk = 'wk'
        A.release(mt)
        P.op('dve', lambda e: e.tensor_copy(out=idxf[:, 0:nslot], in_=idxu[:, 0:nslot]), reads=['idxu'], writes=['idxf'])
        P.op('dve', lambda e: e.tensor_scalar(out=idxf[:, 0:256], in0=idxf[:, 0:256], scalar1=256.0, scalar2=None, op0=ALU.add), reads=['idxf'], writes=['idxf'])
        idxT = T("idxT", [128, 3, NE], U32)
        gate = T("gate", [128, 3, NE], F32)
        for (sc, rows, so) in scs:
            for (src, sk, dst, dk) in ((idxf, 'idxf', idxT, 'idxT'), (vals, 'vals', gate, 'gate')):
                pb, pk = nextps()
                P.op('pe', lambda e, pb=pb, src=src, rows=rows, so=so: e.transpose(out=pb[0:rows, 0:NE], in_=src[:, so:so + rows], identity=ident_f[0:NE, 0:NE]),
                     reads=[sk, 'ident_f'], writes=[pk])
                P.op('dve', lambda e, pb=pb, dst=dst, rows=rows, sc=sc: e.tensor_copy(out=dst[0:rows, sc, :], in_=pb[0:rows, 0:NE]), reads=[pk], writes=[dk])

        bcast(bc[0], 'bc0', 0, 5)
        if with_ctx:
            bcast(bc[2], 'bc2', 1, 5)
        xg = [T("xg%d" % i, [128, D], BF16) for i in range(3)]
        xgT = T("xgT", [128, 8, 288], BF16)
        hid = [T("hid%d" % i, [128, 384], BF16) for i in range(4)]
        for i in range(4):
            P.op('pool', lambda e, i=i: e.memset(hid[i], 0.0), writes=['hid%d' % i])
        sgt = [T("sgt%d" % i, [128, 288], F32) for i in range(2)]
        ysb = T("ysb", [128, 3, D], F32)
        ysc = [T("ysc%d" % i, [128, D], F32) for i in range(3)]

        pend_scatter = []
        yrr = [0]
        for ti, (e_, ft) in enumerate(tiles):
            b = ti % NB
            if ft == 0:
                for (sc, rows, so) in scs:
                    P.dma(None, None, reads=HBF_KEYS + ['idxT'], writes=['xg%d' % sc], q='pool',
                          fn=lambda e, sc=sc, rows=rows, e_=e_: e.indirect_dma_start(out=xg[sc][0:rows, :], out_offset=None, in_=hbf,
                                                                                   in_offset=bass.IndirectOffsetOnAxis(idxT[0:rows, sc, e_:e_ + 1], 0)))
            if ti + NB - 1 < len(tiles):
                load_w(tiles[ti + NB - 1][0], tiles[ti + NB - 1][1], (ti + NB - 1) % NB)
            for f_ in pend_scatter:
                f_()
            del pend_scatter[:]
            if ft == 0:
                for (sc, rows, so) in scs:
                    i = sc % 2
                    for k in range(8):
                        P.op('pe', lambda e, k=k, sc=sc, rows=rows, i=i: e.transpose(out=pst[i][:, k * 128:k * 128 + rows], in_=xg[sc][0:rows, k * 128:(k + 1) * 128],
                                                                                 identity=ident_b[0:rows, 0:rows]),
                             reads=['xg%d' % sc, 'ident_b'], writes=['pst%d' % i], signal=(k == 7))
                    P.op('dve', lambda e, sc=sc, rows=rows, so=so, i=i: e.tensor_copy(out=xgT[:, :, so:so + rows],
                                                                                   in_=pst[i][:, :].rearrange("p (k t) -> p k t", k=8)[:, :, 0:rows]),
                         reads=['pst%d' % i], writes=['xgT'])
            for fc in range(4):
                pg, pgk = ps[fc % 2], 'ps%d' % (fc % 2)
                pu, puk = ps[2 + fc % 2], 'ps%d' % (2 + fc % 2)
                for k in range(8):
                    P.op('pe', lambda e, k=k, pg=pg, fc=fc, b=b: e.matmul(pg[:, 0:nslot], lhsT=Wg[b][:, k, fc * 128:(fc + 1) * 128], rhs=xgT[:, k, 0:nslot],
                                                                      start=(k == 0), stop=(k == 7)),
                         reads=['Wg%d' % b, 'xgT'], writes=[pgk], signal=(k == 7))
                for k in range(8):
                    P.op('pe', lambda e, k=k, pu=pu, fc=fc, b=b: e.matmul(pu[:, 0:nslot], lhsT=Wu[b][:, k, fc * 128:(fc + 1) * 128], rhs=xgT[:, k, 0:nslot],
                                                                      start=(k == 0), stop=(k == 7)),
                         reads=['Wu%d' % b, 'xgT'], writes=[puk], signal=(k == 7))
                j = fc % 2
                P.op('act', lambda e, pg=pg, j=j: e.activation(out=sgt[j][:, 0:nslot], in_=pg[:, 0:nslot], func=AF.Silu), reads=[pgk], writes=['sgt%d' % j])
                P.op('dve', lambda e, pu=pu, j=j, fc=fc: e.tensor_tensor(out=hid[fc][:, 0:nslot], in0=sgt[j][:, 0:nslot], in1=pu[:, 0:nslot], op=ALU.mult),
                     reads=['sgt%d' % j, puk], writes=['hid%d' % fc])
            for (sc, rows, so) in scs:
                for dh in range(2):
                    yrr[0] = 1 - yrr[0]
                    py, pyk = ps[4 + yrr[0]], 'ps%d' % (4 + yrr[0])
                    for fc in range(4):
                        P.op('pe', lambda e, py=py, fc=fc, rows=rows, so=so, dh=dh, b=b: e.matmul(py[:, :], lhsT=hid[fc][:, so:so + 128],
                                                                                             rhs=Wd[b][:, fc, dh * 512:(dh + 1) * 512],
                                                                                             start=(fc == 0), stop=(fc == 3)),
                             reads=['hid%d' % fc, 'Wd%d' % b], writes=[pyk], signal=(fc == 3))
                    if ft == 0:
                        P.op('act', lambda e, py=py, rows=rows, sc=sc, dh=dh: e.copy(out=ysb[0:rows, sc, dh * 512:(dh + 1) * 512], in_=py[0:rows, :]),
                             reads=[pyk], writes=['ysb'])
                    else:
                        P.op('dve', lambda e, py=py, rows=rows, sc=sc, dh=dh: e.tensor_tensor(out=ysb[0:rows, sc, dh * 512:(dh + 1) * 512],
                                                                                           in0=ysb[0:rows, sc, dh * 512:(dh + 1) * 512], in1=py[0:rows, :], op=ALU.add),
                             reads=[pyk, 'ysb'], writes=['ysb'])
            if ft == 3:
                for (sc, rows, so) in scs:
                    i = sc
                    g5, g5k = (bc[0], 'bc0') if sc < 2 else (bc[2], 'bc2')
                    P.op('dve', lambda e, sc=sc, rows=rows, i=i, g5=g5, e_=e_: e.scalar_tensor_tensor(out=ysc[i][0:rows, :], in0=ysb[0:rows, sc, :],
                                                                                                   scalar=gate[0:rows, sc, e_:e_ + 1], in1=g5[0:rows, :],
                                                                                                   op0=ALU.mult, op1=ALU.mult),
                         reads=['ysb', 'gate', g5k], writes=['ysc%d' % i])
                    pend_scatter.append(lambda sc=sc, rows=rows, i=i, e_=e_: P.dma(
                        None, None, reads=['ysc%d' % i, 'idxT'] + XS_KEYS, writes=XS_KEYS, q='pool',
                        fn=lambda e: e.indirect_dma_start(out=xs, out_offset=bass.IndirectOffsetOnAxis(idxT[0:rows, sc, e_:e_ + 1], 0),
                                                          in_=ysc[i][0:rows, :], in_offset=None, compute_op=ALU.add)))
        for f_ in pend_scatter:
            f_()
        A.release(mm)

    moe_phase(0, True)
    if stage == 5:
        dx = T("dx", [128, D], F32)
        for c in range(NCH):
            P.dma(dx, xs[c * 128:(c + 1) * 128, :], reads=['xs%d' % c], writes=['dx'])
            P.dma(dbg_o[c * 128:(c + 1) * 128, :], dx, reads=['dx'], writes=['dbg_o'])
        P.finish(['dbg_o'])
        return

    cur_l[0] = 1
    m_l1 = A.mark()
    mixT1 = T("mixT1", [128, 8, NL], BF16)
    m_l1b = A.mark()
    hT = T("hT", [128, 8, NT], BF16)
    m1 = A.mark()
    hb = [T("hb%d" % i, [128, D], BF16) for i in range(2)]
    norm_phase(norm_mix_g[1:2, :], 0, 1, xs, to_hT(hT, hb), lag=3)
    A.release(m1)
    LAM_INIT = 0.8 - 0.6 * math.exp(-0.3 * 1)
    cosT = T("cosT", [128, NL], F32)
    sinT = T("sinT", [128, NL], F32)
    P.dma(cosT, c_cos[:, :], writes=['cosT'])
    P.dma(sinT, c_sin[:, :], writes=['sinT'])
    lamv = T("lamv", [128, 4, 64], F32)
    lams = T("lams", [128, 4], F32)
    for i in range(4):
        P.dma(lamv[:, i, :], da_l[i:i + 1, :].to_broadcast([128, 64]), writes=['lamv'])
    P.op('dve', lambda e: e.tensor_tensor(out=lamv[:, 0, :], in0=lamv[:, 0, :], in1=lamv[:, 1, :], op=ALU.mult), reads=['lamv'], writes=['lamv'])
    P.op('dve', lambda e: e.tensor_tensor(out=lamv[:, 2, :], in0=lamv[:, 2, :], in1=lamv[:, 3, :], op=ALU.mult), reads=['lamv'], writes=['lamv'])
    P.op('dve', lambda e: e.reduce_sum(out=lams[:, 0:1], in_=lamv[:, 0, :], axis=AX.X), reads=['lamv'], writes=['lams'])
    P.op('dve', lambda e: e.reduce_sum(out=lams[:, 1:2], in_=lamv[:, 2, :], axis=AX.X), reads=['lamv'], writes=['lams'])
    P.op('act', lambda e: e.activation(out=lams[:, 0:2], in_=lams[:, 0:2], func=AF.Exp), reads=['lams'], writes=['lams'])
    P.op('dve', lambda e: e.tensor_tensor(out=lams[:, 2:3], in0=lams[:, 1:2], in1=lams[:, 0:1], op=ALU.subtract), reads=['lams'], writes=['lams'])
    P.op('dve', lambda e: e.tensor_scalar(out=lams[:, 3:4], in0=lams[:, 2:3], scalar1=-LAM_INIT, scalar2=None, op0=ALU.add), reads=['lams'], writes=['lams'])

    Wh = T("Wh", [128, 8, 384], BF16)
    Whr = T("Whr", [128, 8, 256], BF16)
    qT1 = T("qT1", [128, 2, NL], BF16)
    P.op('pool', lambda e: e.memset(qT1, 0.0), writes=['qT1'])
    kT1 = T("kT1", [128, NT], BF16)
    Vx1 = T("Vx1", [128, NCH, 128], BF16)
    ones_b = T("ones_b", [128, 128], BF16)
    P.op('pool', lambda e: e.memset(ones_b, 1.0), writes=['ones_b'])
    rdn = [T("rdn%d" % i, [128, 512], F32) for i in range(2)]
    sqb = T("sqb", [128, 512], BF16)
    sgcol = T("sgcol", [128, 1], F32)
    with nc.allow_non_contiguous_dma(reason="tiny transposed load"):
        P.dma(sgcol, da_subln_g.rearrange("o f -> f o"), writes=['sgcol'])
    P.op('dve', lambda e: e.tensor_scalar(out=sgcol, in0=sgcol, scalar1=1.0 - LAM_INIT, scalar2=None, op0=ALU.mult), reads=['sgcol'], writes=['sgcol'])
    rtmp = [T("rtmp%d" % i, [128, 512], F32) for i in range(2)]
    Ebufs = [T("Eall%d" % i, [128, NCH, 512], BF16) for i in range(2)]
    pstf = [pst[i][:, :].bitcast(F32) for i in range(2)]
    o0 = T("o0", [128, 512], F32)
    o1 = T("o1", [128, 512], F32)
    lt_tiles = [(256 + i * 512, 512) for i in range(4)]

    for h in range(8):
        for j3 in range(3):
            P.dma(Wh[:, :, j3 * 128:(j3 + 1) * 128], od_w_in[:, j3 * D + h * 128: j3 * D + (h + 1) * 128].rearrange("(k p) c -> p k c", p=128),
                  writes=['Wh'], q='pool')
        for k in range(8):
            srcv = Wh[:, k, 0:256].rearrange("p (m b j) -> p m b j", b=2, j=16)
            dstv = Whr[:, k, :].rearrange("p (m b j) -> p m b j", b=2, j=16)
            P.op('act', lambda e, srcv=srcv, dstv=dstv: e.mul(out=dstv[:, :, 0, :], in_=srcv[:, :, 1, :], mul=-1.0), reads=['Wh'], writes=['Whr'])
            P.op('dve', lambda e, srcv=srcv, dstv=dstv: e.tensor_copy(out=dstv[:, :, 1, :], in_=srcv[:, :, 0, :]), reads=['Wh'], writes=['Whr'])
        for (dst, dkey, c0, do_ctx, toff) in ((qT1, 'qT1', 0, False, 256), (kT1, 'kT1', 128, True, 0)):
            if do_ctx:
                pb, pk = nextps()
                for k in range(8):
                    P.op('pe', lambda e, k=k, pb=pb: e.matmul(pb[:, 0:256], lhsT=Wh[:, k, c0:c0 + 128], rhs=hT[:, k, 0:256], start=(k == 0), stop=(k == 7)),
                         reads=['Wh', 'hT'], writes=[pk], signal=(k == 7))
                P.op('act', lambda e, pb=pb: e.copy(out=dst[:, 0:256], in_=pb[:, 0:256]), reads=[pk], writes=[dkey])
            for (t0, tn) in lt_tiles:
                pb, pk = nextps()
                for k in range(8):
                    P.op('pe', lambda e, k=k, pb=pb, t0=t0: e.matmul(pb[:, :], lhsT=Wh[:, k, c0:c0 + 128], rhs=hT[:, k, t0:t0 + 512], start=(k == 0), stop=(k == 7)),
                         reads=['Wh', 'hT'], writes=[pk], signal=(k == 7))
                pb2, pk2 = nextps()
                for k in range(8):
                    P.op('pe', lambda e, k=k, pb2=pb2, t0=t0: e.matmul(pb2[:, :], lhsT=Whr[:, k, c0:c0 + 128], rhs=hT[:, k, t0:t0 + 512], start=(k == 0), stop=(k == 7)),
                         reads=['Whr', 'hT'], writes=[pk2], signal=(k == 7))
                l0 = t0 - 256
                P.op('dve', lambda e, pb=pb, l0=l0: e.tensor_tensor(out=rtmp[0], in0=pb[:, :], in1=cosT[:, l0:l0 + 512], op=ALU.mult), reads=[pk, 'cosT'], writes=['rtmp0'])
                P.op('dve', lambda e, pb2=pb2, l0=l0: e.tensor_tensor(out=rtmp[1], in0=pb2[:, :], in1=sinT[:, l0:l0 + 512], op=ALU.mult), reads=[pk2, 'sinT'], writes=['rtmp1'])
                if dkey == 'qT1':
                    for j in range(2):
                        P.op('pool', lambda e, t0=t0, j=j: e.tensor_tensor(out=qT1[64 * j:64 * j + 64, j, t0 - 256:t0 - 256 + 512], in0=rtmp[0][64 * j:64 * j + 64, :],
                                                                         in1=rtmp[1][64 * j:64 * j + 64, :], op=ALU.add),
                             reads=['rtmp0', 'rtmp1'], writes=[dkey])
                else:
                    P.op('pool', lambda e, t0=t0, dst=dst, toff=toff: e.tensor_tensor(out=dst[:, t0 - toff:t0 - toff + 512], in0=rtmp[0], in1=rtmp[1], op=ALU.add),
                         reads=['rtmp0', 'rtmp1'], writes=[dkey])
        for c in range(NCH):
            pb, pk = nextps()
            for k in range(8):
                P.op('pe', lambda e, k=k, pb=pb, c=c: e.matmul(pb[:, 0:128], lhsT=hT[:, k, c * 128:(c + 1) * 128], rhs=Wh[:, k, 256:384], start=(k == 0), stop=(k == 7)),
                     reads=['Wh', 'hT'], writes=[pk], signal=(k == 7))
            P.op('act', lambda e, pb=pb, c=c: e.copy(out=Vx1[:, c, 0:128], in_=pb[:, 0:128]), reads=[pk], writes=['Vx1'])
        units = [(qt, j) for qt in range(4) for j in range(2)]

        def S_step(u, kb):
            qt, j = units[u]
            p0 = 64 * j
            E = Ebufs[u % 2]
            pb, pk = nextps()
            P.op('pe', lambda e: e.matmul(pb[:, :], lhsT=kT1[:, kb * 128:(kb + 1) * 128], rhs=qT1[:, j, qt * 512:(qt + 1) * 512],
                                          start=True, stop=True), reads=['kT1', 'qT1'], writes=[pk])
            P.op('act', lambda e: e.activation(out=E[:, kb, :], in_=pb[:, :], func=AF.Exp, scale=0.125), reads=[pk], writes=['Eall%d' % (u % 2)])

        def acc_banks(u):
            if u % 2 == 0:
                return ps[4][:, :], 'ps4', ps[5][:, :], 'ps5'
            return pstf[0], 'pst0', pstf[1], 'pst1'

        def PV_step(u, kb):
            E = Ebufs[u % 2]
            ek = 'Eall%d' % (u % 2)
            pa, pak, pd, pdk = acc_banks(u)
            P.op('pe', lambda e: e.matmul(pa, lhsT=Vx1[:, kb, :], rhs=E[:, kb, :], start=(kb == 0), stop=(kb == NCH - 1)),
                 reads=[ek, 'Vx1'], writes=[pak], signal=(kb == NCH - 1))
            P.op('pe', lambda e: e.matmul(pd, lhsT=ones_b, rhs=E[:, kb, :], start=(kb == 0), stop=(kb == NCH - 1)),
                 reads=[ek, 'ones_b'], writes=[pdk], signal=(kb == NCH - 1))

        def epilogue(u):
            qt, j = units[u]
            pa, pak, pd, pdk = acc_banks(u)
            rd = rdn[u % 2]
            rk = 'rdn%d' % (u % 2)
            od, odk = (o0, 'o0') if j == 0 else (o1, 'o1')
            P.op('dve', lambda e: e.reciprocal(out=rd, in_=pd), reads=[pdk], writes=[rk])
            P.op('dve', lambda e: e.tensor_tensor(out=od, in0=pa, in1=rd, op=ALU.mult), reads=[pak, rk], writes=[odk])
            if j == 0:
                return
            P.op('dve', lambda e: e.scalar_tensor_tensor(out=o0, in0=o1, scalar=lams[:, 3:4], in1=o0, op0=ALU.mult, op1=ALU.add),
                 reads=['o0', 'o1', 'lams'], writes=['o0'])
            P.op('act', lambda e: e.activation(out=sqb, in_=o0, func=AF.Square), reads=['o0'], writes=['sqb'])
            pb, pk = nextps()
            P.op('pe', lambda e: e.matmul(pb[:, :], lhsT=ones_b, rhs=sqb, start=True, stop=True), reads=['ones_b', 'sqb'], writes=[pk])
            P.op('dve', lambda e: e.tensor_scalar(out=rd, in0=pb[:, :], scalar1=1.0 / 128, scalar2=1e-6, op0=ALU.mult, op1=ALU.add), reads=[pk], writes=[rk])
            P.op('act', lambda e: e.sqrt(out=rd, in_=rd), reads=[rk], writes=[rk])
            P.op('dve', lambda e: e.reciprocal(out=rd, in_=rd), reads=[rk], writes=[rk])
            P.op('dve', lambda e: e.scalar_tensor_tensor(out=mixT1[:, h, qt * 512:(qt + 1) * 512], in0=o0, scalar=sgcol[:, 0:1], in1=rd,
                                                         op0=ALU.mult, op1=ALU.mult), reads=['o0', 'sgcol', rk], writes=['mixT1'])

        for kb in range(NCH):
            S_step(0, kb)
        for u in range(len(units)):
            for kb in range(NCH):
                if u + 1 < len(units):
                    S_step(u + 1, kb)
                PV_step(u, kb)
            epilogue(u)

    A.release(m_l1b)
    outproj_phase(1, od_w_out, xs, mixT1, 'mixT1', 256, list(range(2, NCH)))
    A.release(m_l1)
    if stage == 6:
        dx = T("dx", [128, D], F32)
        for c in range(NCH):
            P.dma(dx, xs[c * 128:(c + 1) * 128, :], reads=['xs%d' % c], writes=['dx'])
            P.dma(dbg_o[c * 128:(c + 1) * 128, :], dx, reads=['dx'], writes=['dbg_o'])
        P.finish(['dbg_o'])
        return
    moe_phase(1, False)

    mf = A.mark()
    P.dma(gB, final_g[0:1, :].to_broadcast([128, D]), writes=['gB'])
    fx = [T("fx%d" % i, [128, D], F32) for i in range(2)]
    fo = [T("fo%d" % i, [128, D], F32) for i in range(2)]
    junk = T("junk", [128, D], BF16)
    for c in range(2, NCH):
        i = c % 2
        norm_mod(xs[c * 128:(c + 1) * 128, :], fx[i], 'fx%d' % i, fo[i], 'fo%d' % i, junk, gB, 'gB', None, None, si=i, srckey='xs%d' % c)
        P.dma(out[(c - 2) * 128:(c - 1) * 128, :], fo[i], reads=['fo%d' % i], writes=['out%d' % c], q='act')
        if dbg:
            P.dma(dbg_o[c * 128:(c + 1) * 128, :], fo[i], reads=['fo%d' % i], writes=['dbg_o'])
    A.release(mf)
    if dbg:
        P.finish(['dbg_o'])

    P.finish(['out%d' % c for c in range(2, NCH)])


def _consts():
    c = {}
    c["c_ident"] = np.eye(128, dtype=np.float32)
    t = np.arange(NL)
    row = (t // 64).astype(np.float32)
    col = (t % 64).astype(np.float32)
    inv = (np.float32(10000.0) ** (-np.arange(16, dtype=np.float32) / np.float32(16))).astype(np.float32)
    ang_r = row[:, None] * inv[None, :]
    ang_c = col[:, None] * inv[None, :]
    ang = np.concatenate([ang_r, ang_r, ang_c, ang_c], axis=-1).astype(np.float32)
    c["c_cos"] = np.ascontiguousarray(np.concatenate([np.cos(ang).T, np.cos(ang).T], axis=0).astype(np.float32))
    c["c_sin"] = np.ascontiguousarray(np.concatenate([np.sin(ang).T, np.sin(ang).T], axis=0).astype(np.float32))
    gp = np.zeros((128, 8, 240), np.float32)
    ep = np.zeros((128, 8, 240), np.float32)
    sh = np.zeros((16, 8, 128), np.float32)
    for g in range(8):
        for h in range(16):
            gp[16 * g + h, g, 112 + h] = 1.0
            ep[16 * g + h, g, 112 + h] = 1.0
            sh[h, g, 16 * g + h] = 1.0
    c["c_gp"] = gp
    c["c_ep"] = ep
    c["c_sh"] = sh
    kk = np.arange(128)[:, None]
    qq = np.arange(128)[None, :]
    c["c_mask"] = np.ascontiguousarray(np.stack([(kk >= qq), (kk <= qq)], axis=1).astype(np.float32))
    return c


def _in_map(inputs, b):
    m = {}
    m["xin"] = np.ascontiguousarray(np.concatenate([inputs["ctx"][b], inputs["x"][b]], axis=0), dtype=np.float32)
    m["cc"] = np.ascontiguousarray(np.stack([inputs["c"][b], inputs["c_ctx"]], axis=0), dtype=np.float32)
    for k in ["ada_w", "ada_b", "norm_mix_g", "norm_ffn_g"]:
        m[k] = np.ascontiguousarray(inputs[k], dtype=np.float32)
    m["ev_w_in"] = np.ascontiguousarray(inputs["ev_w_in"][0], dtype=np.float32)
    m["ev_w_out"] = np.ascontiguousarray(inputs["ev_w_out"][0], dtype=np.float32)
    m["wa_sink"] = np.ascontiguousarray(inputs["wa_sink"], dtype=np.float32).reshape(1, 8)
    for k in ["s5_lam_re", "s5_lam_im", "s5_log_step", "s5_b_re", "s5_b_im", "s5_c_re", "s5_c_im", "s5_d", "s5_glu_w", "s5_glu_b"]:
        m[k] = np.ascontiguousarray(inputs[k][0], dtype=np.float32)
    for k in ["moe_router", "moe_w_gate", "moe_w_up", "moe_w_down"]:
        m[k] = np.ascontiguousarray(inputs[k], dtype=np.float32)
    m["od_w_in"] = np.ascontiguousarray(inputs["od_w_in"][0], dtype=np.float32)
    m["od_w_out"] = np.ascontiguousarray(inputs["od_w_out"][0], dtype=np.float32)
    m["da_l"] = np.ascontiguousarray(np.stack([inputs["da_lq1"][0], inputs["da_lk1"][0], inputs["da_lq2"][0], inputs["da_lk2"][0]], axis=0), dtype=np.float32)
    m["da_subln_g"] = np.ascontiguousarray(inputs["da_subln_g"], dtype=np.float32).reshape(1, 128)
    m["final_g"] = np.ascontiguousarray(inputs["final_g"], dtype=np.float32).reshape(1, D)
    m.update(_consts())
    return m


def kernel(**inputs):
    nc = build()
    in_maps = [_in_map(inputs, b) for b in range(8)]
    res = run_bass_kernel_spmd(nc, in_maps, core_ids=list(range(8)))
    return np.stack([r["out"] for r in res.results], axis=0).astype(np.float32)
```
